# Optimizing a Trainium2 kernel written in Bass

```python
import math, functools
import jax, jax.numpy as jnp
from jax import lax
import numpy as np

D_MODEL = 1024
BATCH = 8
SEQ = 8192
DEPTH = 1

GRID_W = 64
CTX_LEN = 256
D_FOURIER = 512
F_GROUPS = 4
F_GROUP_DIM = D_FOURIER // F_GROUPS
D_RNN = 1024
RNN_HEADS = 8
RNN_BLOCK = D_RNN // RNN_HEADS
CONV_W = 4
CONV_LEFT = 2
LRU_C = 8.0
COL_R0 = D_FOURIER
COL_G0 = COL_R0 + D_RNN
COL_M0 = COL_G0 + D_RNN
D_IN = COL_M0 + 2 * D_MODEL
N_EXPERTS = 32
TOP_K = 4
D_EXPERT = 1024
SWIGLU_ALPHA = 1.702
SWIGLU_LIMIT = 7.0
MOE_BLOCK = 256
EPS = 1e-6

kernel_name = "hybrid_fnet_rglru_moe_dit_block"


def rms_norm(x, g):
    xf = x.astype(jnp.float32)
    y = xf * lax.rsqrt(jnp.mean(xf * xf, axis=-1, keepdims=True) + EPS)
    return (y * g.astype(jnp.float32)).astype(x.dtype)


def ada_modulation(cond, w_ada, b_ada):
    m = jax.nn.silu(cond) @ w_ada + b_ada
    return jnp.split(m, 6, axis=-1)


def centred_conv(u, w, b):
    L = u.shape[-2]
    pad = [(0, 0)] * (u.ndim - 2) + [(CONV_LEFT, CONV_W - 1 - CONV_LEFT), (0, 0)]
    up = jnp.pad(u, pad)
    out = b + up[..., 0:L, :] * w[0]
    for k in range(1, CONV_W):
        out = out + up[..., k:k + L, :] * w[k]
    return out


def fourier_mix(u):
    b_, L, _ = u.shape
    ug = u.astype(jnp.float32).reshape(b_, L, F_GROUPS, F_GROUP_DIM)
    y = jnp.fft.fftn(ug, axes=(1, 3), norm="ortho").real
    return y.reshape(b_, L, D_FOURIER).astype(u.dtype)


def rglru_coeffs(xc, wa, ba, wx, bx, lam):
    b_, L, _ = xc.shape
    xh = xc.reshape(b_, L, RNN_HEADS, RNN_BLOCK)
    r = jax.nn.sigmoid(jnp.einsum('blhi,hij->blhj', xh, wa.astype(jnp.float32)).reshape(b_, L, D_RNN) + ba.astype(jnp.float32))
    i = jax.nn.sigmoid(jnp.einsum('blhi,hij->blhj', xh, wx.astype(jnp.float32)).reshape(b_, L, D_RNN) + bx.astype(jnp.float32))
    log_a = -LRU_C * r * jax.nn.softplus(-lam.astype(jnp.float32))
    a = jnp.exp(log_a)
    bt = jnp.sqrt(-jnp.expm1(2.0 * log_a)) * (i * xc)
    return a, bt


def _combine(e1, e2):
    a1, b1 = e1
    a2, b2 = e2
    return a1 * a2, a2 * b1 + b2


def linear_scan(a, bt, h0, reverse):
    idx = -1 if reverse else 0
    bt = bt.at[:, idx].add(a[:, idx] * h0)
    _, h = lax.associative_scan(_combine, (a, bt), axis=1, reverse=reverse)
    return h


def bidir_rglru(xc, wa, ba, wx, bx, lam, h0s):
    y = None
    finals = []
    for d, rev in enumerate((False, True)):
        a, bt = rglru_coeffs(xc, wa[d], ba[d], wx[d], bx[d], lam[d])
        h = linear_scan(a, bt, h0s[d], rev)
        y = h if y is None else y + h
        finals.append(h[:, 0] if rev else h[:, -1])
    return y, finals


def merge_branches(z, y_rnn, w_f, w_r, w_o):
    f_out = fourier_mix(z[..., :COL_R0]) @ w_f
    r_out = (y_rnn.astype(z.dtype) * jax.nn.gelu(z[..., COL_G0:COL_M0])) @ w_r
    g_f = jax.nn.sigmoid(z[..., COL_M0:COL_M0 + D_MODEL])
    g_r = jax.nn.sigmoid(z[..., COL_M0 + D_MODEL:])
    return (g_f * f_out + g_r * r_out) @ w_o


def moe_ffn(h, w_router, b_router, w_gu, b_gu, w_down, b_down):
    shp = h.shape
    d = shp[-1]
    t = h.reshape(-1, d)
    T = t.shape[0]
    logits = (t @ w_router + b_router).astype(jnp.float32)
    top_v, top_i = lax.top_k(logits, TOP_K)
    wts = jax.nn.softmax(top_v, axis=-1)
    A = T * TOP_K
    flat_e = top_i.reshape(-1).astype(jnp.int32)
    order = jnp.argsort(flat_e, stable=True).astype(jnp.int32)
    sorted_e = flat_e[order]
    counts = jnp.bincount(flat_e, length=N_EXPERTS).astype(jnp.int32)
    padded = (counts + MOE_BLOCK - 1) // MOE_BLOCK * MOE_BLOCK
    start_sorted = jnp.cumsum(counts) - counts
    ends_pad = jnp.cumsum(padded)
    start_pad = ends_pad - padded
    rank = jnp.arange(A, dtype=jnp.int32) - start_sorted[sorted_e]
    dest = start_pad[sorted_e] + rank
    n_blocks = (A + N_EXPERTS * (MOE_BLOCK - 1) + MOE_BLOCK - 1) // MOE_BLOCK
    P = n_blocks * MOE_BLOCK
    slot_tok = jnp.full((P,), T, jnp.int32).at[dest].set(order // TOP_K)
    slot_w = jnp.zeros((P,), jnp.float32).at[dest].set(wts.reshape(-1)[order])
    block_start = jnp.arange(n_blocks, dtype=jnp.int32) * MOE_BLOCK
    block_expert = jnp.minimum(jnp.searchsorted(ends_pad, block_start, side='right'), N_EXPERTS - 1).astype(jnp.int32)
    t_pad = jnp.concatenate([t, jnp.zeros((1, d), t.dtype)], axis=0)

    def expert_block(args):
        idx, wgt, e = args
        xb = t_pad[idx]
        gu = xb @ w_gu[e] + b_gu[e]
        gate = jnp.minimum(gu[:, :D_EXPERT], SWIGLU_LIMIT)
        up = jnp.clip(gu[:, D_EXPERT:], -SWIGLU_LIMIT, SWIGLU_LIMIT)
        act = (up + 1.0) * gate * jax.nn.sigmoid(SWIGLU_ALPHA * gate)
        yb = act @ w_down[e] + b_down[e]
        return yb * wgt[:, None].astype(yb.dtype)

    ys = lax.map(expert_block, (slot_tok.reshape(n_blocks, MOE_BLOCK),
                                slot_w.reshape(n_blocks, MOE_BLOCK), block_expert))
    y = jnp.zeros((T + 1, d), ys.dtype).at[slot_tok].add(ys.reshape(P, d))[:T]
    return y.reshape(shp).astype(h.dtype)


def setup_inputs(seed: int = 0) -> dict:
    key = jax.random.key(seed)
    ks = jax.random.split(key, 32)
    f32 = jnp.float32
    nrm = lambda k, s, sc: jax.random.normal(k, s, f32) * sc
    u = jax.random.uniform(ks[16], (DEPTH, 2, D_RNN), f32, 0.9, 0.999)
    a0 = u ** (1.0 / LRU_C)
    lru_lambda = jnp.log(a0) - jnp.log1p(-a0)
    return {
        "x": nrm(ks[0], (BATCH, SEQ, D_MODEL), 1.0),
        "c": nrm(ks[1], (BATCH, D_MODEL), 1.0),
        "ctx": nrm(ks[2], (BATCH, CTX_LEN, D_MODEL), 1.0),
        "c_ctx": nrm(ks[3], (D_MODEL,), 1.0),
        "w_ada": nrm(ks[4], (DEPTH, D_MODEL, 6 * D_MODEL), 0.5 * D_MODEL ** -0.5),
        "b_ada": nrm(ks[5], (DEPTH, 6 * D_MODEL), 0.01),
        "norm1": 1.0 + nrm(ks[6], (DEPTH, D_MODEL), 0.02),
        "w_in": nrm(ks[7], (DEPTH, D_MODEL, D_IN), D_MODEL ** -0.5),
        "conv_w": nrm(ks[8], (DEPTH, CONV_W, D_RNN), CONV_W ** -0.5),
        "conv_b": nrm(ks[9], (DEPTH, D_RNN), 0.01),
        "gate_a_w": nrm(ks[10], (DEPTH, 2, RNN_HEADS, RNN_BLOCK, RNN_BLOCK), RNN_BLOCK ** -0.5),
        "gate_a_b": nrm(ks[11], (DEPTH, 2, D_RNN), 0.01),
        "gate_x_w": nrm(ks[12], (DEPTH, 2, RNN_HEADS, RNN_BLOCK, RNN_BLOCK), RNN_BLOCK ** -0.5),
        "gate_x_b": nrm(ks[13], (DEPTH, 2, D_RNN), 0.01),
        "lru_lambda": lru_lambda,
        "w_fourier": nrm(ks[14], (DEPTH, D_FOURIER, D_MODEL), D_FOURIER ** -0.5),
        "w_rnn": nrm(ks[15], (DEPTH, D_RNN, D_MODEL), D_RNN ** -0.5),
        "w_out": nrm(ks[17], (DEPTH, D_MODEL, D_MODEL), D_MODEL ** -0.5),
        "norm2": 1.0 + nrm(ks[18], (DEPTH, D_MODEL), 0.02),
        "w_router": nrm(ks[19], (DEPTH, D_MODEL, N_EXPERTS), D_MODEL ** -0.5),
        "b_router": nrm(ks[20], (DEPTH, N_EXPERTS), 0.01),
        "w_gu": nrm(ks[21], (DEPTH, N_EXPERTS, D_MODEL, 2 * D_EXPERT), D_MODEL ** -0.5),
        "b_gu": nrm(ks[22], (DEPTH, N_EXPERTS, 2 * D_EXPERT), 0.01),
        "w_down": nrm(ks[23], (DEPTH, N_EXPERTS, D_EXPERT, D_MODEL), D_EXPERT ** -0.5),
        "b_down": nrm(ks[24], (DEPTH, N_EXPERTS, D_MODEL), 0.01),
        "norm_f": 1.0 + nrm(ks[25], (D_MODEL,), 0.02),
    }


def reference(x, c, ctx, c_ctx, w_ada, b_ada, norm1, w_in, conv_w, conv_b, gate_a_w, gate_a_b,
              gate_x_w, gate_x_b, lru_lambda, w_fourier, w_rnn, w_out, norm2, w_router, b_router,
              w_gu, b_gu, w_down, b_down, norm_f):
    bsz, seq, _ = x.shape
    rows = seq // GRID_W
    for l in range(DEPTH):
        last = l == DEPTH - 1
        sh1, sc1, g1, sh2, sc2, g2 = [m[:, None, :] for m in ada_modulation(c, w_ada[l], b_ada[l])]
        csh1, csc1, cg1, csh2, csc2, cg2 = ada_modulation(c_ctx, w_ada[l], b_ada[l])
        lru_p = (gate_a_w[l], gate_a_b[l], gate_x_w[l], gate_x_b[l], lru_lambda[l])

        hc = rms_norm(ctx, norm1[l]) * (1.0 + csc1) + csh1
        uc = centred_conv(hc @ w_in[l][:, COL_R0:COL_G0], conv_w[l], conv_b[l]).astype(jnp.float32)
        h_zero = jnp.zeros((bsz, D_RNN), jnp.float32)
        yc, h_ctx_final = bidir_rglru(uc, *lru_p, [h_zero, h_zero])

        hx = rms_norm(x, norm1[l]) * (1.0 + sc1) + sh1
        zx = hx @ w_in[l]
        ux = centred_conv(zx[..., COL_R0:COL_G0].reshape(bsz, rows, GRID_W, D_RNN),
                          conv_w[l], conv_b[l]).reshape(bsz, seq, D_RNN).astype(jnp.float32)
        yx, _ = bidir_rglru(ux, *lru_p, h_ctx_final)
        x = x + g1 * merge_branches(zx, yx, w_fourier[l], w_rnn[l], w_out[l])
        if not last:
            zc = hc @ w_in[l]
            ctx = ctx + cg1 * merge_branches(zc, yc, w_fourier[l], w_rnn[l], w_out[l])

        moe_p = (w_router[l], b_router[l], w_gu[l], b_gu[l], w_down[l], b_down[l])
        x = x + g2 * moe_ffn(rms_norm(x, norm2[l]) * (1.0 + sc2) + sh2, *moe_p)
        if not last:
            ctx = ctx + cg2 * moe_ffn(rms_norm(ctx, norm2[l]) * (1.0 + csc2) + csh2, *moe_p)
    return rms_norm(x, norm_f)
```

```python
import contextlib
import math
import os
import numpy as np
import concourse.bass as bass
import concourse.mybir as mybir
from concourse.bass_utils import run_bass_kernel_spmd

F32 = mybir.dt.float32
BF16 = mybir.dt.bfloat16
I32 = mybir.dt.int32
U32 = mybir.dt.uint32
ALU = mybir.AluOpType
AF = mybir.ActivationFunctionType
AX = mybir.AxisListType

SELF_SYNC = True
NDSEM = 6

D = 1024
S = 8192
CTX = 256
NE = 32
BLK = 512
NBLK = 96
PSLOTS = NBLK * BLK
EPS = 1e-6


class Prog:
    ENGS = ("pe", "act", "dve", "pool", "sp")

    def __init__(self, nc):
        self.nc = nc
        self.ops = []

    def add(self, eng, fn, reads=(), writes=(), dma=False):
        self.ops.append(dict(eng=eng, fn=fn, reads=tuple(reads), writes=tuple(writes), dma=dma, bar=False))

    def pe(self, fn, reads=(), writes=()):
        self.add("pe", fn, reads, writes)

    def act(self, fn, reads=(), writes=()):
        self.add("act", fn, reads, writes)

    def dve(self, fn, reads=(), writes=()):
        self.add("dve", fn, reads, writes)

    def pool(self, fn, reads=(), writes=()):
        self.add("pool", fn, reads, writes)

    def dma(self, q, fn, reads=(), writes=()):
        self.add(q, fn, reads, writes, dma=True)

    def barrier(self):
        for e in self.ENGS:
            self.ops.append(dict(eng=e, fn=None, reads=(), writes=(), dma=False, bar=True))

    def emit(self):
        nc = self.nc
        ops = self.ops
        cnt = {e: 0 for e in self.ENGS}
        dcnt = {e: 0 for e in self.ENGS}
        latest = {}
        for op in ops:
            e = op["eng"]
            if op["bar"]:
                op["barvals"] = dict(latest)
                continue
            if op["dma"]:
                i = dcnt[e]
                dcnt[e] += 1
                op["sem"] = ("d", e, i % NDSEM)
                op["val"] = 16 * (i // NDSEM + 1)
            else:
                cnt[e] += 1
                op["sem"] = ("c", e)
                op["val"] = cnt[e]
            latest[op["sem"]] = op["val"]
        last_w = {}
        readers = {}
        for op in ops:
            if op["bar"]:
                continue
            deps = []
            for k in op["reads"]:
                if k in last_w:
                    deps.append(last_w[k])
            for k in op["writes"]:
                if k in last_w:
                    deps.append(last_w[k])
                deps.extend(readers.get(k, ()))
            op["deps"] = deps
            for k in op["writes"]:
                last_w[k] = op
                readers[k] = []
            for k in op["reads"]:
                if k not in op["writes"]:
                    readers.setdefault(k, []).append(op)
        known = {e: {} for e in self.ENGS}
        for op in ops:
            e = op["eng"]
            need = {}
            if op["bar"]:
                need = dict(op["barvals"])
                need.pop(("c", e), None)
            else:
                for d in op["deps"]:
                    if d is op:
                        continue
                    if (not d["dma"]) and d["eng"] == e and (e == "pe" or not SELF_SYNC):
                        continue
                    s, v = d["sem"], d["val"]
                    if need.get(s, 0) < v:
                        need[s] = v
                if op["dma"] and op["val"] > 16:
                    s = op["sem"]
                    if need.get(s, 0) < op["val"] - 16:
                        need[s] = op["val"] - 16
            w = []
            for s, v in need.items():
                if known[e].get(s, 0) < v:
                    known[e][s] = v
                    w.append((s, v))
            op["waits"] = w
        final = latest
        semkeys = sorted(final.keys(), key=str)
        with contextlib.ExitStack() as st:
            sems = {}
            for k in semkeys:
                sems[k] = st.enter_context(nc.semaphore("s_" + "_".join(map(str, k))))
            block = st.enter_context(nc.Block())

            def run(engname):
                def body(eng):
                    for op in ops:
                        if op["eng"] != engname:
                            continue
                        for s, v in op["waits"]:
                            eng.wait_ge(sems[s], v)
                        if op["bar"]:
                            continue
                        ins = op["fn"](eng)
                        ins.then_inc(sems[op["sem"]], 16 if op["dma"] else 1)
                    for k, v in final.items():
                        if k[0] == "d" and k[1] == engname:
                            eng.wait_ge(sems[k], v)
                return body

            block.sync(run("sp"))
            block.scalar(run("act"))
            block.vector(run("dve"))
            block.gpsimd(run("pool"))
            block.tensor(run("pe"))


class Arena:
    def __init__(self, base_ap, nbytes):
        self.base = base_ap
        self.nbytes = nbytes
        self.off = 0
        self.marks = []

    def alloc(self, shape, dt=F32):
        esz = {F32: 4, BF16: 2, I32: 4, U32: 4}[dt]
        npart = shape[0]
        fshape = list(shape[1:])
        n = 1
        for s_ in fshape:
            n *= s_
        nb = (n * esz + 31) // 32 * 32
        assert self.off + nb <= self.nbytes, ("SBUF arena overflow", self.off, nb, self.nbytes)
        a = self.base[0:npart, self.off // 4:(self.off + nb) // 4]
        self.off += nb
        if dt != F32:
            a = a.bitcast(dt)
        a = a[:, 0:n]
        if len(fshape) == 2:
            a = a.rearrange("p (a b) -> p a b", a=fshape[0])
        elif len(fshape) == 3:
            a = a.rearrange("p (a b c) -> p a b c", a=fshape[0], b=fshape[1])
        return a

    def mark(self):
        self.marks.append(self.off)

    def release(self):
        self.off = self.marks.pop()


def dft_tables():
    n = np.arange(128)
    ang = 2 * np.pi * np.outer(n, n) / 128.0
    c128 = np.cos(ang)
    s128 = np.sin(ang)
    k1 = np.arange(128)[:, None]
    n2 = np.arange(64)[None, :]
    tw = 2 * np.pi * k1 * n2 / 8192.0
    twc = np.cos(tw)
    tws = np.sin(tw)
    a64 = 2 * np.pi * np.outer(np.arange(64), np.arange(64)) / 64.0
    c2 = np.cos(a64)
    s2 = np.sin(a64)
    t3 = np.zeros((128, 128))
    t3[0:64, 0:64] = c2
    t3[64:128, 0:64] = -s2
    t3[0:64, 64:128] = s2
    t3[64:128, 64:128] = c2
    t3 = t3 / 1024.0
    f = np.float32
    return dict(k_c128=c128.astype(f), k_s128=s128.astype(f), k_twc=twc.astype(f), k_tws=tws.astype(f), k_t3=t3.astype(f))


def build_program(stop_after=None, debug=False):
    nc = bass.Bass("TRN2", target_bir_lowering=False)

    def din(name, shape, dt=F32):
        return nc.dram_tensor(name, list(shape), dt, kind="ExternalInput").ap()

    DBGSET = set(os.environ.get("KDBG_OUT", "").split(",")) if debug else set()

    def dscr(name, shape, dt=F32):
        if name in DBGSET:
            return nc.dram_tensor(name, list(shape), dt, kind="ExternalOutput").ap()
        return nc.dram_tensor(name, list(shape), dt).ap()

    x = din("x", [S, D]); c = din("c", [1, D]); ctx = din("ctx", [CTX, D]); c_ctx = din("c_ctx", [1, D])
    w_ada = din("w_ada", [D, 6 * D]); b_ada = din("b_ada", [1, 6 * D]); norm1 = din("norm1", [1, D])
    w_in = din("w_in", [D, 4608]); conv_w = din("conv_w", [4, D]); conv_b = din("conv_b", [1, D])
    gate_a_w = din("gate_a_w", [2, 8, 128, 128]); gate_a_b = din("gate_a_b", [2, D])
    gate_x_w = din("gate_x_w", [2, 8, 128, 128]); gate_x_b = din("gate_x_b", [2, D])
    lru_lambda = din("lru_lambda", [2, D])
    w_fourier = din("w_fourier", [512, D]); w_rnn = din("w_rnn", [D, D]); w_out = din("w_out", [D, D])
    norm2 = din("norm2", [1, D]); w_router = din("w_router", [D, NE]); b_router = din("b_router", [1, NE])
    w_gu = din("w_gu", [NE, D, 2 * D]); b_gu = din("b_gu", [NE, 2 * D])
    w_down = din("w_down", [NE, D, D]); b_down = din("b_down", [NE, D]); norm_f = din("norm_f", [1, D])
    k_c128 = din("k_c128", [128, 128]); k_s128 = din("k_s128", [128, 128])
    k_twc = din("k_twc", [128, 64]); k_tws = din("k_tws", [128, 64]); k_t3 = din("k_t3", [128, 128])
    y = nc.dram_tensor("y", [S, D], F32, kind="ExternalOutput").ap()
    dbg = nc.dram_tensor("dbg", [128, 2048], F32, kind="ExternalOutput").ap() if debug else None

    u_d = dscr("u_d", [S, 512], BF16)
    xc_d = dscr("xc_d", [D, S], F32)
    gg_d = dscr("gg_d", [D, S], BF16)
    gfr_d = dscr("gfr_d", [2 * D, S], BF16)
    q_d = dscr("q_d", [4, 2, 64, 128, 128], BF16)
    rt_d = dscr("rt_d", [D, S], BF16)
    yg_d = dscr("yg_d", [D, S], BF16)
    x1_d = dscr("x1_d", [S, D], F32)
    h2_d = dscr("h2_d", [S, D], BF16)
    xg_d = dscr("xg_d", [PSLOTS, D], BF16)
    ys_d = dscr("ys_d", [PSLOTS, D], F32)

    st = contextlib.ExitStack()
    SBN = 206848
    sball = st.enter_context(nc.sbuf_tensor("sball", [128, SBN // 4], F32))
    AR = Arena(sball[:], SBN)
    banks = [st.enter_context(nc.psum_tensor("psb%d" % i, [128, 512], F32)) for i in range(8)]
    P = Prog(nc)
    psn = [0]

    def next_ps():
        i = psn[0] % 8
        psn[0] += 1
        return banks[i][:], "ps%d" % i

    uid = [0]

    def K(name):
        uid[0] += 1
        return "%s#%d" % (name, uid[0])

    def finish():
        with nc.allow_low_precision(reason="bf16 matmul operands, fp32 accumulation"):
            P.emit()
        st.close()
        return nc

    LQ = "sp"
    SQ = "pool"

    ident_f = AR.alloc([128, 128]); ident_b = AR.alloc([128, 128], BF16)
    ones_b = AR.alloc([128, 512], BF16); ones_f = AR.alloc([128, 128])
    ltri_b = AR.alloc([128, 128], BF16)
    g2_bc = AR.alloc([128, D]); nf_bc = AR.alloc([128, D])
    dest_i = AR.alloc([128, 64, 4], I32); wk = AR.alloc([128, 64, 4])
    eb_i = AR.alloc([128, NBLK], I32); widx = AR.alloc([128, NBLK, 8], I32)
    AR.mark()
    g1_bc = AR.alloc([128, D]); gm2_bc = AR.alloc([128, D]); sh2_bc = AR.alloc([128, D])
    gm1T = AR.alloc([128, 8]); sh1T = AR.alloc([128, 8]); gmcT = AR.alloc([128, 8]); shcT = AR.alloc([128, 8])
    cwT = AR.alloc([128, 8, 4]); cbT = AR.alloc([128, 8])
    nbaT = AR.alloc([128, 16]); nbxT = AR.alloc([128, 16]); coefT = AR.alloc([128, 16])
    h0T = AR.alloc([128, 16])
    Lg = AR.alloc([128, 64, NE])

    P.pool(lambda e: e.memset(ident_f, 0.0), writes=["ident_f"])
    P.pool(lambda e: e.affine_select(out=ident_f, in_=ident_f, pattern=[[-1, 128]], compare_op=ALU.not_equal, fill=1.0, base=0, channel_multiplier=1), reads=["ident_f"], writes=["ident_f"])
    P.dve(lambda e: e.tensor_copy(ident_b, ident_f), reads=["ident_f"], writes=["ident_b"])
    P.pool(lambda e: e.memset(ones_b, 1.0), writes=["ones_b"])
    P.pool(lambda e: e.memset(ones_f, 1.0), writes=["ones_f"])
    P.pool(lambda e: e.memset(ltri_b, 1.0), writes=["ltri_b"])
    P.pool(lambda e: e.affine_select(out=ltri_b, in_=ltri_b, pattern=[[1, 128]], compare_op=ALU.is_gt, fill=0.0, base=0, channel_multiplier=-1), reads=["ltri_b"], writes=["ltri_b"])

    AR.mark()
    cT = AR.alloc([128, 16])
    crep = AR.alloc([128, 16, 128])
    mb = AR.alloc([128, 6 * D])
    mcb = AR.alloc([128, 2 * D])
    wa_buf = [AR.alloc([128, 8, 512]) for _ in range(2)]
    n1_bc = AR.alloc([128, D]); n2_bc = AR.alloc([128, D])
    P.dma(LQ, lambda e: e.dma_start(out=cT[:, 0:8], in_=c.rearrange("o (k p) -> p (o k)", p=128), allow_slow_non_contiguous=True), writes=["cT"])
    P.dma(LQ, lambda e: e.dma_start(out=cT[:, 8:16], in_=c_ctx.rearrange("o (k p) -> p (o k)", p=128), allow_slow_non_contiguous=True), reads=["cT"], writes=["cT"])
    P.dma(LQ, lambda e: e.dma_start(out=mb, in_=b_ada.partition_broadcast(128)), writes=["mb"])
    P.dma(LQ, lambda e: e.dma_start(out=n1_bc, in_=norm1.partition_broadcast(128)), writes=["n1_bc"])
    P.dma(LQ, lambda e: e.dma_start(out=n2_bc, in_=norm2.partition_broadcast(128)), writes=["n2_bc"])
    P.dma(LQ, lambda e: e.dma_start(out=nf_bc, in_=norm_f.partition_broadcast(128)), writes=["nf_bc"])
    ctmp = AR.alloc([128, 16])
    P.act(lambda e: e.activation(out=ctmp, in_=cT, func=AF.Exp, scale=-1.0), reads=["cT"], writes=["ctmp"])
    P.dve(lambda e: e.tensor_scalar_add(ctmp, ctmp, 1.0), reads=["ctmp"], writes=["ctmp"])
    P.dve(lambda e: e.reciprocal(ctmp, ctmp), reads=["ctmp"], writes=["ctmp"])
    P.dve(lambda e: e.tensor_tensor(cT, cT, ctmp, ALU.mult), reads=["ctmp", "cT"], writes=["cT"])
    for j in range(16):
        P.dve(lambda e, j=j: e.tensor_copy(crep[:, j, :], cT[:, j:j + 1].to_broadcast([128, 128])), reads=["cT"], writes=["crep"])
    w_ada_v = w_ada.rearrange("(k p) n -> p k n", p=128)
    for ch in range(12):
        bi = ch % 2
        P.dma(LQ, lambda e, ch=ch, bi=bi: e.dma_start(out=wa_buf[bi], in_=w_ada_v[:, :, ch * 512:(ch + 1) * 512]), writes=["wa%d" % bi])
        ps, pk = next_ps()

        def f(e, ps=ps, bi=bi):
            for k in range(8):
                ins = e.matmul(ps, crep[:, k, :], wa_buf[bi][:, k, :], start=(k == 0), stop=(k == 7))
            return ins
        P.pe(f, reads=["crep", "wa%d" % bi], writes=[pk])
        P.dve(lambda e, ps=ps, ch=ch: e.tensor_tensor(mb[:, ch * 512:(ch + 1) * 512], mb[:, ch * 512:(ch + 1) * 512], ps, ALU.add), reads=[pk, "mb"], writes=["mb"])
        if ch < 4:
            ps2, pk2 = next_ps()

            def f2(e, ps2=ps2, bi=bi):
                for k in range(8):
                    ins = e.matmul(ps2, crep[:, 8 + k, :], wa_buf[bi][:, k, :], start=(k == 0), stop=(k == 7))
                return ins
            P.pe(f2, reads=["crep", "wa%d" % bi], writes=[pk2])
            P.act(lambda e, ps2=ps2, ch=ch: e.copy(mcb[:, ch * 512:(ch + 1) * 512], ps2), reads=[pk2], writes=["mcb"])
    bada2 = AR.alloc([128, 2 * D])
    P.dma(LQ, lambda e: e.dma_start(out=bada2, in_=b_ada[:, 0:2 * D].partition_broadcast(128)), writes=["bada2"])
    P.dve(lambda e: e.tensor_tensor(mcb, mcb, bada2, ALU.add), reads=["mcb", "bada2"], writes=["mcb"])
    gm1_bc = AR.alloc([128, D]); gmc_bc = AR.alloc([128, D])
    P.dve(lambda e: e.scalar_tensor_tensor(gm1_bc, mb[:, D:2 * D], 1.0, n1_bc, ALU.add, ALU.mult), reads=["mb", "n1_bc"], writes=["gm1_bc"])
    P.dve(lambda e: e.scalar_tensor_tensor(gmc_bc, mcb[:, D:2 * D], 1.0, n1_bc, ALU.add, ALU.mult), reads=["mcb", "n1_bc"], writes=["gmc_bc"])
    P.dve(lambda e: e.scalar_tensor_tensor(gm2_bc, mb[:, 4 * D:5 * D], 1.0, n2_bc, ALU.add, ALU.mult), reads=["mb", "n2_bc"], writes=["gm2_bc"])
    P.act(lambda e: e.copy(g1_bc, mb[:, 2 * D:3 * D]), reads=["mb"], writes=["g1_bc"])
    P.act(lambda e: e.copy(sh2_bc, mb[:, 3 * D:4 * D]), reads=["mb"], writes=["sh2_bc"])
    P.act(lambda e: e.copy(g2_bc, mb[:, 5 * D:6 * D]), reads=["mb"], writes=["g2_bc"])
    for (src, sk, dst, dk) in ((gm1_bc, "gm1_bc", gm1T, "gm1T"), (mb, "mb", sh1T, "sh1T"), (gmc_bc, "gmc_bc", gmcT, "gmcT"), (mcb, "mcb", shcT, "shcT")):
        for kc in range(8):
            ps, pk = next_ps()
            P.pe(lambda e, ps=ps, src=src, kc=kc: e.transpose(ps[:, 0:128], src[:, kc * 128:(kc + 1) * 128], ident_f), reads=[sk, "ident_f"], writes=[pk])
            P.dve(lambda e, ps=ps, dst=dst, kc=kc: e.tensor_copy(dst[:, kc:kc + 1], ps[:, 0:1]), reads=[pk], writes=[dk])
    for kk_ in range(4):
        P.dma(LQ, lambda e, kk_=kk_: e.dma_start(out=cwT[:, :, kk_], in_=conv_w[kk_:kk_ + 1, :].rearrange("o (h p) -> p (o h)", p=128), allow_slow_non_contiguous=True), reads=["cwT"], writes=["cwT"])
    P.dma(LQ, lambda e: e.dma_start(out=cbT, in_=conv_b.rearrange("o (h p) -> p (o h)", p=128), allow_slow_non_contiguous=True), writes=["cbT"])
    P.dma(LQ, lambda e: e.dma_start(out=nbaT, in_=gate_a_b.rearrange("d (h p) -> p (d h)", p=128), allow_slow_non_contiguous=True), writes=["nbaT"])
    P.dma(LQ, lambda e: e.dma_start(out=nbxT, in_=gate_x_b.rearrange("d (h p) -> p (d h)", p=128), allow_slow_non_contiguous=True), writes=["nbxT"])
    P.dma(LQ, lambda e: e.dma_start(out=coefT, in_=lru_lambda.rearrange("d (h p) -> p (d h)", p=128), allow_slow_non_contiguous=True), writes=["coefT"])
    P.dve(lambda e: e.tensor_scalar_mul(nbaT, nbaT, -1.0), reads=["nbaT"], writes=["nbaT"])
    P.dve(lambda e: e.tensor_scalar_mul(nbxT, nbxT, -1.0), reads=["nbxT"], writes=["nbxT"])
    P.act(lambda e: e.activation(out=coefT, in_=coefT, func=AF.Exp, scale=-1.0), reads=["coefT"], writes=["coefT"])
    P.act(lambda e: e.activation(out=coefT, in_=coefT, func=AF.Ln, bias=1.0), reads=["coefT"], writes=["coefT"])
    P.dve(lambda e: e.tensor_scalar_mul(coefT, coefT, -8.0), reads=["coefT"], writes=["coefT"])
    P.barrier()
    AR.release()
    if stop_after == "A":
        return finish()

    AR.mark()
    gw_b = AR.alloc([128, 32, 128], BF16)
    AR.mark()
    win_b = AR.alloc([128, 8, 4608], BF16)
    AR.mark()
    stg = [AR.alloc([128, 8, 512]) for _ in range(2)]
    w_in_v = w_in.rearrange("(k p) n -> p k n", p=128)
    for ch in range(9):
        bi = ch % 2
        P.dma(LQ, lambda e, ch=ch, bi=bi: e.dma_start(out=stg[bi], in_=w_in_v[:, :, ch * 512:(ch + 1) * 512]), writes=["stg%d" % bi])
        if ch % 2 == 0:
            P.act(lambda e, ch=ch, bi=bi: e.copy(win_b[:, :, ch * 512:(ch + 1) * 512], stg[bi]), reads=["stg%d" % bi], writes=["win_b"])
        else:
            P.dve(lambda e, ch=ch, bi=bi: e.tensor_copy(win_b[:, :, ch * 512:(ch + 1) * 512], stg[bi]), reads=["stg%d" % bi], writes=["win_b"])
    for gi, gwd in enumerate((gate_a_w, gate_x_w)):
        bi = gi % 2
        P.dma(LQ, lambda e, gwd=gwd, bi=bi: e.dma_start(out=stg[bi][:, 0:4, :].rearrange("p a (b c) -> p (a b) c", c=128), in_=gwd.rearrange("d h i j -> i (d h) j")), writes=["stg%d" % bi])
        P.dve(lambda e, gi=gi, bi=bi: e.tensor_copy(gw_b[:, gi * 16:(gi + 1) * 16, :], stg[bi][:, 0:4, :].rearrange("p a (b c) -> p (a b) c", c=128)), reads=["stg%d" % bi], writes=["gw_b"])
    P.barrier()
    AR.release()
    if stop_after == "W":
        return finish()

    def rms_rows(xt, nsub, ssq, rstd, junk, kx, kpre):
        for s_ in range(nsub):
            P.act(lambda e, s_=s_: e.activation(out=junk, in_=xt[:, s_, :], func=AF.Square, accum_out=ssq[:, s_:s_ + 1]), reads=[kx], writes=[kpre + "junk", kpre + "ssq"])
        P.dve(lambda e: e.tensor_scalar(rstd[:, 0:nsub], ssq[:, 0:nsub], 1.0 / D, EPS, ALU.mult, ALU.add), reads=[kpre + "ssq"], writes=[kpre + "rstd"])
        P.act(lambda e: e.activation(out=rstd[:, 0:nsub], in_=rstd[:, 0:nsub], func=AF.Ln), reads=[kpre + "rstd"], writes=[kpre + "rstd"])
        P.act(lambda e: e.activation(out=rstd[:, 0:nsub], in_=rstd[:, 0:nsub], func=AF.Exp, scale=-0.5), reads=[kpre + "rstd"], writes=[kpre + "rstd"])

    def norm_transpose(xt, nsub, rstd, xs_b, hT, gT, sT, kx, kpre, khT):
        for s_ in range(nsub):
            P.act(lambda e, s_=s_: e.activation(out=xs_b[:, s_, :], in_=xt[:, s_, :], func=AF.Copy, scale=rstd[:, s_:s_ + 1]), reads=[kx, kpre + "rstd"], writes=[kpre + "xs"])
        for kc in range(8):
            ps, pk = next_ps()
            psb = ps.bitcast(BF16)

            def f(e, psb=psb, kc=kc):
                for s_ in range(nsub):
                    ins = e.transpose(psb[:, s_ * 128:(s_ + 1) * 128], xs_b[:, s_, kc * 128:(kc + 1) * 128], ident_b)
                return ins
            P.pe(f, reads=[kpre + "xs", "ident_b"], writes=[pk])
            n = nsub * 128
            if kc % 2 == 0:
                P.act(lambda e, psb=psb, kc=kc, n=n: e.activation(out=hT[:, kc, 0:n], in_=psb[:, 0:n], func=AF.Identity, scale=gT[:, kc:kc + 1], bias=sT[:, kc:kc + 1]), reads=[pk], writes=[khT])
            else:
                P.dve(lambda e, psb=psb, kc=kc, n=n: e.tensor_scalar(hT[:, kc, 0:n], psb[:, 0:n], gT[:, kc:kc + 1], sT[:, kc:kc + 1], ALU.mult, ALU.add), reads=[pk], writes=[khT])

    def conv_from_psum(ps, out_t, h, ntok, rowlen, kps, kout):
        nr = ntok // rowlen
        P.dve(lambda e: e.tensor_scalar(out_t[:, 0:ntok], ps[:, 0:ntok], cwT[:, h, 2:3], cbT[:, h:h + 1], ALU.mult, ALU.add), reads=[kps, "cwT", "cbT"], writes=[kout])
        o3 = out_t[:, 0:ntok].rearrange("p (r t) -> p r t", t=rowlen)
        z3 = ps[:, 0:ntok].rearrange("p (r t) -> p r t", t=rowlen)
        for (kk, sh) in ((0, -2), (1, -1), (3, 1)):
            if sh < 0:
                oo = o3[:, :, -sh:rowlen]; zz = z3[:, :, 0:rowlen + sh]
            else:
                oo = o3[:, :, 0:rowlen - sh]; zz = z3[:, :, sh:rowlen]
            P.dve(lambda e, oo=oo, zz=zz, kk=kk: e.scalar_tensor_tensor(oo, zz, cwT[:, h, kk:kk + 1], oo, ALU.mult, ALU.add), reads=[kps, kout], writes=[kout])

    def rnn_chunk(xc_f, xc_b, d, h, n, bufs, kxc, kpre):
        ia = 0 * 16 + d * 8 + h
        ix = 1 * 16 + d * 8 + h
        dh = d * 8 + h
        e1, a_, e2, s_, b_ = bufs["e1"], bufs["a"], bufs["e2"], bufs["s"], bufs["b"]
        nch = (n + 511) // 512
        psr = []
        for g_, wi in ((0, ia), (1, ix)):
            lst = []
            for j in range(nch):
                ps, pk = next_ps()
                w_ = min(512, n - j * 512)
                P.pe(lambda e, ps=ps, wi=wi, j=j, w_=w_: e.matmul(ps[:, 0:w_], gw_b[:, wi, :], xc_b[:, j * 512:j * 512 + w_], start=True, stop=True), reads=[kxc + "b", "gw_b"], writes=[pk])
                lst.append((ps, pk, j, w_))
            psr.append(lst)
        for (ps, pk, j, w_) in psr[0]:
            P.act(lambda e, ps=ps, j=j, w_=w_: e.activation(out=e1[:, j * 512:j * 512 + w_], in_=ps[:, 0:w_], func=AF.Exp, scale=-1.0, bias=nbaT[:, dh:dh + 1]), reads=[pk, "nbaT"], writes=[kpre + "e1"])
        for (ps, pk, j, w_) in psr[1]:
            P.act(lambda e, ps=ps, j=j, w_=w_: e.activation(out=e2[:, j * 512:j * 512 + w_], in_=ps[:, 0:w_], func=AF.Exp, scale=-1.0, bias=nbxT[:, dh:dh + 1]), reads=[pk, "nbxT"], writes=[kpre + "e2"])
        P.dve(lambda e: e.tensor_scalar_add(e1[:, 0:n], e1[:, 0:n], 1.0), reads=[kpre + "e1"], writes=[kpre + "e1"])
        P.dve(lambda e: e.reciprocal(e1[:, 0:n], e1[:, 0:n]), reads=[kpre + "e1"], writes=[kpre + "e1"])
        P.act(lambda e: e.activation(out=a_[:, 0:n], in_=e1[:, 0:n], func=AF.Exp, scale=coefT[:, dh:dh + 1]), reads=[kpre + "e1", "coefT"], writes=[kpre + "a"])
        P.dve(lambda e: e.tensor_scalar_add(e2[:, 0:n], e2[:, 0:n], 1.0), reads=[kpre + "e2"], writes=[kpre + "e2"])
        P.dve(lambda e: e.reciprocal(e2[:, 0:n], e2[:, 0:n]), reads=[kpre + "e2"], writes=[kpre + "e2"])
        P.dve(lambda e: e.tensor_tensor(s_[:, 0:n], a_[:, 0:n], a_[:, 0:n], ALU.mult), reads=[kpre + "a"], writes=[kpre + "s"])
        P.act(lambda e: e.activation(out=s_[:, 0:n], in_=s_[:, 0:n], func=AF.Ln, scale=-1.0, bias=1.0), reads=[kpre + "s"], writes=[kpre + "s"])
        P.act(lambda e: e.activation(out=s_[:, 0:n], in_=s_[:, 0:n], func=AF.Exp, scale=0.5), reads=[kpre + "s"], writes=[kpre + "s"])
        P.dve(lambda e: e.tensor_tensor(b_[:, 0:n], e2[:, 0:n], xc_f, ALU.mult), reads=[kpre + "e2", kxc], writes=[kpre + "b"])
        P.dve(lambda e: e.tensor_tensor(b_[:, 0:n], b_[:, 0:n], s_[:, 0:n], ALU.mult), reads=[kpre + "b", kpre + "s"], writes=[kpre + "b"])

    AR.mark()
    cx = AR.alloc([128, 2, D]); cjunk = AR.alloc([128, D]); cssq = AR.alloc([128, 4]); crstd = AR.alloc([128, 4])
    cxs = AR.alloc([128, 2, D], BF16); hcT = AR.alloc([128, 8, CTX], BF16)
    xcc = AR.alloc([128, 8, CTX]); xccb = AR.alloc([128, 8, CTX], BF16)
    cb_ = dict(e1=AR.alloc([128, CTX]), a=AR.alloc([128, CTX]), e2=AR.alloc([128, CTX]), s=AR.alloc([128, CTX]), b=AR.alloc([128, CTX]))
    chh = AR.alloc([128, CTX])
    P.dma(LQ, lambda e: e.dma_start(out=cx, in_=ctx.rearrange("(s p) d -> p s d", p=128)), writes=["cx"])
    rms_rows(cx, 2, cssq, crstd, cjunk, "cx", "c_")
    norm_transpose(cx, 2, crstd, cxs, hcT, gmcT, shcT, "cx", "c_", "hcT")
    for h in range(8):
        ps, pk = next_ps()

        def f(e, ps=ps, h=h):
            for k in range(8):
                ins = e.matmul(ps[:, 0:CTX], win_b[:, k, 512 + h * 128:512 + (h + 1) * 128], hcT[:, k, :], start=(k == 0), stop=(k == 7))
            return ins
        P.pe(f, reads=["win_b", "hcT"], writes=[pk])
        conv_from_psum(ps, xcc[:, h, :], h, CTX, CTX, pk, "xcc%d" % h)
        P.act(lambda e, h=h: e.copy(xccb[:, h, :], xcc[:, h, :]), reads=["xcc%d" % h], writes=["xcc%db" % h])
        for d in range(2):
            rnn_chunk(xcc[:, h, :], xccb[:, h, :], d, h, CTX, cb_, "xcc%d" % h, "c_")
            if d == 0:
                P.dve(lambda e: e.tensor_tensor_scan(chh, cb_["a"], cb_["b"], 0.0, ALU.mult, ALU.add), reads=["c_a", "c_b"], writes=["chh"])
                P.dve(lambda e, h=h: e.tensor_copy(h0T[:, h:h + 1], chh[:, CTX - 1:CTX]), reads=["chh"], writes=["h0T"])
            else:
                P.dve(lambda e: e.tensor_tensor_scan(chh[:, ::-1], cb_["a"][:, ::-1], cb_["b"][:, ::-1], 0.0, ALU.mult, ALU.add), reads=["c_a", "c_b"], writes=["chh"])
                P.dve(lambda e, h=h: e.tensor_copy(h0T[:, 8 + h:9 + h], chh[:, 0:1]), reads=["chh"], writes=["h0T"])
    P.barrier()
    AR.release()
    if stop_after == "C":
        return finish()

    AR.mark()
    NT = S // 512
    xt = [AR.alloc([128, 4, D]) for _ in range(2)]
    djunk = AR.alloc([128, D], BF16); dssq = AR.alloc([128, 4]); drstd = AR.alloc([128, 4])
    dxs = AR.alloc([128, 4, D], BF16)
    hxT = AR.alloc([128, 8, 512], BF16)
    u_t = AR.alloc([128, 4, 512], BF16)
    xc_t = [AR.alloc([128, 512]) for _ in range(2)]
    gg_t = AR.alloc([128, 8, 512], BF16)
    gfr_t = AR.alloc([128, 8, 512], BF16)
    tA = [AR.alloc([128, 512]) for _ in range(2)]
    tB = [AR.alloc([128, 512]) for _ in range(2)]
    x_v = x.rearrange("(t s p) d -> t p s d", p=128, s=4)
    u_v = u_d.rearrange("(t s p) n -> t p s n", p=128, s=4)
    xc_v = xc_d.rearrange("(h p) t -> p h t", p=128)
    gg_v = gg_d.rearrange("(h p) t -> p h t", p=128)
    gfr_v = gfr_d.rearrange("(h p) t -> p h t", p=128)
    P.dma(LQ, lambda e: e.dma_start(out=xt[0], in_=x_v[0]), writes=["xt0"])
    for t in range(NT):
        bi = t % 2
        if t + 1 < NT:
            P.dma(LQ, lambda e, t=t: e.dma_start(out=xt[(t + 1) % 2], in_=x_v[t + 1]), writes=["xt%d" % ((t + 1) % 2)])
        rms_rows(xt[bi], 4, dssq, drstd, djunk, "xt%d" % bi, "d_")
        norm_transpose(xt[bi], 4, drstd, dxs, hxT, gm1T, sh1T, "xt%d" % bi, "d_", "hxT")
        for s_ in range(4):
            ps, pk = next_ps()

            def f(e, ps=ps, s_=s_):
                for k in range(8):
                    ins = e.matmul(ps, hxT[:, k, s_ * 128:(s_ + 1) * 128], win_b[:, k, 0:512], start=(k == 0), stop=(k == 7))
                return ins
            P.pe(f, reads=["hxT", "win_b"], writes=[pk])
            P.act(lambda e, ps=ps, s_=s_: e.copy(u_t[:, s_, :], ps), reads=[pk], writes=["u_t"])
        P.dma(SQ, lambda e, t=t: e.dma_start(out=u_v[t], in_=u_t), reads=["u_t"], writes=["u_d"])
        for cc in range(4, 36):
            ps, pk = next_ps()

            def f(e, ps=ps, cc=cc):
                for k in range(8):
                    ins = e.matmul(ps, win_b[:, k, cc * 128:(cc + 1) * 128], hxT[:, k, :], start=(k == 0), stop=(k == 7))
                return ins
            P.pe(f, reads=["hxT", "win_b"], writes=[pk])
            if cc < 12:
                h = cc - 4
                ob = xc_t[h % 2]; ok = "xc_t%d" % (h % 2)
                conv_from_psum(ps, ob, h, 512, 64, pk, ok)
                P.dma(SQ, lambda e, ob=ob, h=h, t=t: e.dma_start(out=xc_v[:, h, t * 512:(t + 1) * 512], in_=ob), reads=[ok], writes=["xc_d"])
            elif cc < 20:
                h = cc - 12
                a_ = tA[h % 2]; b_ = tB[h % 2]; ka = "tA%d" % (h % 2); kb = "tB%d" % (h % 2)
                P.act(lambda e, ps=ps, a_=a_: e.activation(out=a_, in_=ps, func=AF.Square), reads=[pk], writes=[ka])
                P.dve(lambda e, a_=a_: e.tensor_scalar(a_, a_, 0.044715, 1.0, ALU.mult, ALU.add), reads=[ka], writes=[ka])
                P.dve(lambda e, ps=ps, a_=a_: e.tensor_tensor(a_, a_, ps, ALU.mult), reads=[ka, pk], writes=[ka])
                P.act(lambda e, a_=a_, b_=b_: e.activation(out=b_, in_=a_, func=AF.Exp, scale=-1.5957691216057308), reads=[ka], writes=[kb])
                P.dve(lambda e, b_=b_: e.tensor_scalar_add(b_, b_, 1.0), reads=[kb], writes=[kb])
                P.dve(lambda e, b_=b_: e.reciprocal(b_, b_), reads=[kb], writes=[kb])
                P.dve(lambda e, ps=ps, b_=b_, h=h: e.tensor_tensor(gg_t[:, h, :], b_, ps, ALU.mult), reads=[kb, pk], writes=["gg_t"])
            else:
                h = cc - 20
                b_ = tB[h % 2]; kb = "tB%d" % (h % 2)
                P.act(lambda e, ps=ps, b_=b_: e.activation(out=b_, in_=ps, func=AF.Exp, scale=-1.0), reads=[pk], writes=[kb])
                P.dve(lambda e, b_=b_, h=h: e.tensor_scalar_add(b_, b_, 1.0), reads=[kb], writes=[kb])
                P.dve(lambda e, b_=b_, h=h: e.reciprocal(gfr_t[:, h % 8, :], b_), reads=[kb], writes=["gfr_t"])
                if h % 8 == 7:
                    P.dma(SQ, lambda e, t=t, h=h: e.dma_start(out=gfr_v[:, (h // 8) * 8:(h // 8) * 8 + 8, t * 512:(t + 1) * 512], in_=gfr_t), reads=["gfr_t"], writes=["gfr_d"])
        P.dma(SQ, lambda e, t=t: e.dma_start(out=gg_v[:, :, t * 512:(t + 1) * 512], in_=gg_t), reads=["gg_t"], writes=["gg_d"])
    P.barrier()
    AR.release()
    AR.release()
    if stop_after == "D":
        return finish()

    AR.mark()
    xcf = AR.alloc([128, S]); xcb = AR.alloc([128, S], BF16); hf = AR.alloc([128, S])
    CH = 1024
    NCH = S // CH
    rb = [dict(e1=AR.alloc([128, CH]), a=AR.alloc([128, CH]), e2=AR.alloc([128, CH]), s=AR.alloc([128, CH]), b=AR.alloc([128, CH])) for _ in range(2)]
    hb = [AR.alloc([128, CH]) for _ in range(2)]
    ggc = [AR.alloc([128, CH], BF16) for _ in range(2)]
    ygc = [AR.alloc([128, CH], BF16) for _ in range(2)]
    yg_v = yg_d.rearrange("(h p) t -> p h t", p=128)
    for h in range(8):
        P.dma(LQ, lambda e, h=h: e.dma_start(out=xcf, in_=xc_v[:, h, :]), reads=["xc_d"], writes=["xcf"])
        P.act(lambda e: e.copy(xcb[:, 0:S // 2], xcf[:, 0:S // 2]), reads=["xcf"], writes=["xcfb"])
        P.dve(lambda e: e.tensor_copy(xcb[:, S // 2:S], xcf[:, S // 2:S]), reads=["xcf", "xcfb"], writes=["xcfb"])
        it = 0
        for d in range(2):
            order = list(range(NCH)) if d == 0 else list(range(NCH - 1, -1, -1))
            prev = None
            for ci in order:
                bi = it % 2
                it += 1
                sl = slice(ci * CH, (ci + 1) * CH)
                kp = "r%d_" % bi
                rnn_chunk(xcf[:, sl], xcb[:, sl], d, h, CH, rb[bi], "xcf", kp)
                dh = d * 8 + h
                if d == 0:
                    init = h0T[:, dh:dh + 1] if prev is None else hf[:, ci * CH - 1:ci * CH]
                    P.dve(lambda e, bi=bi, sl=sl, init=init: e.tensor_tensor_scan(hf[:, sl], rb[bi]["a"], rb[bi]["b"], init, ALU.mult, ALU.add), reads=[kp + "a", kp + "b", "hf", "h0T"], writes=["hf"])
                else:
                    if prev is None:
                        init = h0T[:, dh:dh + 1]; kinit = "h0T"
                    else:
                        init = hb[prev][:, 0:1]; kinit = "hb%d" % prev
                    P.dma(LQ, lambda e, bi=bi, sl=sl, h=h: e.dma_start(out=ggc[bi], in_=gg_v[:, h, sl]), reads=["gg_d"], writes=["ggc%d" % bi])
                    P.dve(lambda e, bi=bi, init=init: e.tensor_tensor_scan(hb[bi][:, ::-1], rb[bi]["a"][:, ::-1], rb[bi]["b"][:, ::-1], init, ALU.mult, ALU.add), reads=[kp + "a", kp + "b", kinit], writes=["hb%d" % bi])
                    P.dve(lambda e, bi=bi, sl=sl: e.tensor_tensor(rb[bi]["s"], hb[bi], hf[:, sl], ALU.add), reads=["hb%d" % bi, "hf", kp + "s"], writes=[kp + "s"])
                    P.dve(lambda e, bi=bi: e.tensor_tensor(ygc[bi], rb[bi]["s"], ggc[bi], ALU.mult), reads=[kp + "s", "ggc%d" % bi], writes=["ygc%d" % bi])
                    P.dma(SQ, lambda e, bi=bi, sl=sl, h=h: e.dma_start(out=yg_v[:, h, sl], in_=ygc[bi]), reads=["ygc%d" % bi], writes=["yg_d"])
                    prev = bi
                if d == 0:
                    prev = bi
    P.barrier()
    AR.release()
    AR.release()
    if stop_after == "E":
        return finish()

    AR.mark()
    c1b = AR.alloc([128, 128], BF16); s1b = AR.alloc([128, 128], BF16); t3b = AR.alloc([128, 128], BF16)
    twc = AR.alloc([128, 64]); tws = AR.alloc([128, 64])
    ftmp = AR.alloc([128, 128])
    for (src, dst, kk) in ((k_c128, c1b, "c1b"), (k_s128, s1b, "s1b"), (k_t3, t3b, "t3b")):
        P.dma(LQ, lambda e, src=src: e.dma_start(out=ftmp, in_=src), writes=["ftmp"])
        P.dve(lambda e, dst=dst: e.tensor_copy(dst, ftmp), reads=["ftmp"], writes=[kk])
    P.dma(LQ, lambda e: e.dma_start(out=twc, in_=k_twc), writes=["twc"])
    P.dma(LQ, lambda e: e.dma_start(out=tws, in_=k_tws), writes=["tws"])
    AR.mark()
    U = AR.alloc([128, 64, 512], BF16)
    qt = [AR.alloc([128, 2, 512], BF16) for _ in range(2)]
    f1 = [AR.alloc([128, 512]) for _ in range(2)]
    f2 = [AR.alloc([128, 512]) for _ in range(2)]
    u_pv = u_d.rearrange("(p n) c -> p n c", n=64)
    for uq in range(4):
        P.dma(LQ, lambda e, uq=uq: e.dma_start(out=U[:, uq * 16:(uq + 1) * 16, :], in_=u_pv[:, uq * 16:(uq + 1) * 16, :]), reads=["u_d"], writes=["U"])
    FDBG = int(os.environ.get("FDBG", "0"))
    for n2 in range(64 if FDBG != 2 else 0):
        bi = n2 % 2
        psr, kr = next_ps()
        psi, ki = next_ps()
        P.pe(lambda e, psr=psr, n2=n2: e.matmul(psr, c1b, U[:, n2, :], start=True, stop=True), reads=["U", "c1b"], writes=[kr])
        P.pe(lambda e, psi=psi, n2=n2: e.matmul(psi, s1b, U[:, n2, :], start=True, stop=True), reads=["U", "s1b"], writes=[ki])
        P.dve(lambda e, psi=psi, n2=n2, bi=bi: e.tensor_scalar_mul(f1[bi], psi, tws[:, n2:n2 + 1]), reads=[ki, "tws"], writes=["f1%d" % bi])
        P.dve(lambda e, psr=psr, n2=n2, bi=bi: e.tensor_scalar_mul(f2[bi], psr, tws[:, n2:n2 + 1]), reads=[kr, "tws"], writes=["f2%d" % bi])
        P.dve(lambda e, psr=psr, n2=n2, bi=bi: e.scalar_tensor_tensor(qt[bi][:, 0, :], psr, twc[:, n2:n2 + 1], f1[bi], ALU.mult, ALU.subtract), reads=[kr, "twc", "f1%d" % bi], writes=["qt%d" % bi])
        P.dve(lambda e, psi=psi, n2=n2, bi=bi: e.scalar_tensor_tensor(qt[bi][:, 1, :], psi, twc[:, n2:n2 + 1], f2[bi], ALU.mult, ALU.add), reads=[ki, "twc", "f2%d" % bi, "qt%d" % bi], writes=["qt%d" % bi])
        for r in range(2 if FDBG != 1 else 0):
            P.dma(SQ, lambda e, n2=n2, bi=bi, r=r: e.dma_start(out=q_d[:, r, n2, :, :].rearrange("g k c -> k g c"), in_=qt[bi][:, r, :].rearrange("k (g c) -> k g c", g=4)), reads=["qt%d" % bi], writes=["q_d"])
    P.barrier()
    AR.release()
    if stop_after == "F1":
        return finish()
    AR.mark()
    Qg = [AR.alloc([128, 128, 128], BF16) for _ in range(2)]
    RT = [AR.alloc([128, 2, S], BF16) for _ in range(2)]
    rt_v = rt_d.rearrange("(g r j) t -> g j r t", r=2, j=128)
    for g in range(4):
        bi = g % 2
        for r in range(2):
            P.dma(LQ, lambda e, g=g, r=r, bi=bi: e.dma_start(out=Qg[bi][r * 64:(r + 1) * 64, :, :], in_=q_d[g, r]), reads=["q_d"], writes=["Qg%d" % bi])
        for k0 in range(0, 128, 4):
            ps, pk = next_ps()

            def f(e, ps=ps, k0=k0, bi=bi):
                for kk in range(4):
                    ins = e.matmul(ps[:, kk * 128:(kk + 1) * 128], Qg[bi][:, k0 + kk, :], t3b, start=True, stop=True)
                return ins
            P.pe(f, reads=["Qg%d" % bi, "t3b"], writes=[pk])
            psv = ps.rearrange("j (k r n) -> j r k n", k=4, r=2)
            for r in range(2):
                ov = RT[bi][:, r, :].rearrange("j (n k) -> j k n", k=128)[:, k0:k0 + 4, :]
                if r == 0:
                    P.act(lambda e, ov=ov, psv=psv, r=r: e.copy(ov, psv[:, r, :, :]), reads=[pk], writes=["RT%d" % bi])
                else:
                    P.dve(lambda e, ov=ov, psv=psv, r=r: e.tensor_copy(ov, psv[:, r, :, :]), reads=[pk], writes=["RT%d" % bi])
        P.dma(SQ, lambda e, g=g, bi=bi: e.dma_start(out=rt_v[g], in_=RT[bi]), reads=["RT%d" % bi], writes=["rt_d"])
    P.barrier()
    AR.release()
    AR.release()
    if stop_after == "F":
        return finish()

    AR.mark()
    wfp = AR.alloc([128, 8, D], BF16)
    wr_b = AR.alloc([128, 8, D], BF16)
    wo_b = AR.alloc([128, 8, D], BF16)
    wrt_f = AR.alloc([128, 8, NE])
    brt = AR.alloc([1, NE])
    AR.mark()
    gstg = AR.alloc([128, 8, D])
    cdb = AR.alloc([128, 128], BF16); sdb = AR.alloc([128, 128], BF16)
    wf_b = AR.alloc([128, 4, D], BF16)
    P.dma(LQ, lambda e: e.dma_start(out=gstg[:, 0, 0:128], in_=k_c128), writes=["gstg"])
    P.dve(lambda e: e.tensor_copy(cdb, gstg[:, 0, 0:128]), reads=["gstg"], writes=["cdb"])
    P.dma(LQ, lambda e: e.dma_start(out=gstg[:, 0, 0:128], in_=k_s128), reads=["gstg"], writes=["gstg"])
    P.dve(lambda e: e.tensor_scalar_mul(sdb, gstg[:, 0, 0:128], -1.0), reads=["gstg"], writes=["sdb"])
    P.dma(LQ, lambda e: e.dma_start(out=gstg[:, 0:4, :], in_=w_fourier.rearrange("(g m) n -> m g n", m=128)), reads=["gstg"], writes=["gstg"])
    P.dve(lambda e: e.tensor_copy(wf_b, gstg[:, 0:4, :]), reads=["gstg"], writes=["wf_b"])
    for g in range(4):
        for ri, mat, mk in ((0, cdb, "cdb"), (1, sdb, "sdb")):
            for half in range(2):
                ps, pk = next_ps()
                P.pe(lambda e, ps=ps, mat=mat, g=g, half=half: e.matmul(ps, mat, wf_b[:, g, half * 512:(half + 1) * 512], start=True, stop=True), reads=[mk, "wf_b"], writes=[pk])
                P.act(lambda e, ps=ps, g=g, ri=ri, half=half: e.copy(wfp[:, g * 2 + ri, half * 512:(half + 1) * 512], ps), reads=[pk], writes=["wfp"])
    P.dma(LQ, lambda e: e.dma_start(out=gstg, in_=w_rnn.rearrange("(k p) n -> p k n", p=128)), reads=["gstg"], writes=["gstg"])
    P.dve(lambda e: e.tensor_copy(wr_b, gstg), reads=["gstg"], writes=["wr_b"])
    P.dma(LQ, lambda e: e.dma_start(out=gstg, in_=w_out.rearrange("(k p) n -> p k n", p=128)), reads=["gstg"], writes=["gstg"])
    for k in range(8):
        P.dve(lambda e, k=k: e.tensor_tensor(wo_b[:, k, :], gstg[:, k, :], g1_bc, ALU.mult), reads=["gstg", "g1_bc"], writes=["wo_b"])
    P.dma(LQ, lambda e: e.dma_start(out=wrt_f, in_=w_router.rearrange("(k p) n -> p k n", p=128)), writes=["wrt_f"])
    P.dma(LQ, lambda e: e.dma_start(out=brt, in_=b_router), writes=["brt"])
    P.barrier()
    AR.release()
    rtt = [AR.alloc([128, 8, 512], BF16) for _ in range(2)]
    ygt = [AR.alloc([128, 8, 512], BF16) for _ in range(2)]
    gft = [AR.alloc([128, 16, 512], BF16)] * 2
    xg_ = [AR.alloc([128, 4, D]) for _ in range(2)]
    mT = AR.alloc([128, 8, 512], BF16)
    g1t = [AR.alloc([128, 512]) for _ in range(2)]
    g2t = [AR.alloc([128, 512]) for _ in range(2)]
    h2f = AR.alloc([128, D])
    h2b = AR.alloc([128, 4, D], BF16)
    h2T = AR.alloc([128, 8, 128])
    gjunk = AR.alloc([128, D], BF16); gssq = AR.alloc([128, 4]); grstd = AR.alloc([128, 4])
    rt_tv = rt_d.rearrange("(c j) t -> j c t", j=128)
    x1_v = x1_d.rearrange("(t s p) d -> t p s d", p=128, s=4)
    h2_v = h2_d.rearrange("(t s p) d -> t p s d", p=128, s=4)

    def g_load(t):
        bi = t % 2
        sl = slice(t * 512, (t + 1) * 512)
        P.dma(LQ, lambda e: e.dma_start(out=rtt[bi], in_=rt_tv[:, :, sl]), reads=["rt_d"], writes=["rtt%d" % bi])
        P.dma(LQ, lambda e: e.dma_start(out=ygt[bi], in_=yg_v[:, :, sl]), reads=["yg_d"], writes=["ygt%d" % bi])
        P.dma(LQ, lambda e: e.dma_start(out=xg_[bi], in_=x_v[t]), writes=["xg_%d" % bi])
    def gft_load(t):
        P.dma(LQ, lambda e: e.dma_start(out=gft[0], in_=gfr_v[:, :, t * 512:(t + 1) * 512]), reads=["gfr_d"], writes=["gft0"])
    g_load(0)
    gft_load(0)
    for t in range(NT):
        bi = t % 2
        if t + 1 < NT:
            g_load(t + 1)
        x1t = xg_[bi]
        kx1 = "xg_%d" % bi
        for n in range(8):
            psF, kF = next_ps()
            psR, kR = next_ps()

            def fF(e, psF=psF, n=n, bi=bi):
                for k in range(8):
                    ins = e.matmul(psF, wfp[:, k, n * 128:(n + 1) * 128], rtt[bi][:, k, :], start=(k == 0), stop=(k == 7))
                return ins

            def fR(e, psR=psR, n=n, bi=bi):
                for k in range(8):
                    ins = e.matmul(psR, wr_b[:, k, n * 128:(n + 1) * 128], ygt[bi][:, k, :], start=(k == 0), stop=(k == 7))
                return ins
            P.pe(fF, reads=["wfp", "rtt%d" % bi], writes=[kF])
            P.pe(fR, reads=["wr_b", "ygt%d" % bi], writes=[kR])
            a_ = g1t[n % 2]; b_ = g2t[n % 2]; ka = "g1t%d" % (n % 2); kb = "g2t%d" % (n % 2)
            P.dve(lambda e, psF=psF, a_=a_, n=n, bi=bi: e.tensor_tensor(a_, psF, gft[bi][:, n, :], ALU.mult), reads=[kF, "gft0"], writes=[ka])
            P.dve(lambda e, psR=psR, b_=b_, n=n, bi=bi: e.tensor_tensor(b_, psR, gft[bi][:, 8 + n, :], ALU.mult), reads=[kR, "gft0"], writes=[kb])
            P.dve(lambda e, a_=a_, b_=b_, n=n: e.tensor_tensor(mT[:, n, :], a_, b_, ALU.add), reads=[ka, kb], writes=["mT"])
        if t + 1 < NT:
            gft_load(t + 1)
        for s_ in range(4):
            for half in range(2):
                ps, pk = next_ps()

                def fO(e, ps=ps, s_=s_, half=half):
                    for k in range(8):
                        ins = e.matmul(ps, mT[:, k, s_ * 128:(s_ + 1) * 128], wo_b[:, k, half * 512:(half + 1) * 512], start=(k == 0), stop=(k == 7))
                    return ins
                P.pe(fO, reads=["mT", "wo_b"], writes=[pk])
                P.dve(lambda e, ps=ps, s_=s_, half=half, bi=bi: e.tensor_tensor(xg_[bi][:, s_, half * 512:(half + 1) * 512], ps, xg_[bi][:, s_, half * 512:(half + 1) * 512], ALU.add), reads=[pk, kx1], writes=[kx1])
        P.dma(SQ, lambda e, t=t, x1t=x1t: e.dma_start(out=x1_v[t], in_=x1t), reads=[kx1], writes=["x1_d"])
        rms_rows(x1t, 4, gssq, grstd, gjunk, kx1, "g_")
        for s_ in range(4):
            ti = t * 4 + s_
            P.dve(lambda e, s_=s_, x1t=x1t: e.scalar_tensor_tensor(h2f, x1t[:, s_, :], grstd[:, s_:s_ + 1], gm2_bc, ALU.mult, ALU.mult), reads=[kx1, "g_rstd", "gm2_bc"], writes=["h2f"])
            P.dve(lambda e: e.tensor_tensor(h2f, h2f, sh2_bc, ALU.add), reads=["h2f", "sh2_bc"], writes=["h2f"])
            P.act(lambda e, s_=s_: e.copy(h2b[:, s_, :], h2f), reads=["h2f"], writes=["h2b"])
            for q in range(2):
                ps, pk = next_ps()

                def fT(e, ps=ps, q=q):
                    for kk in range(4):
                        kc = q * 4 + kk
                        ins = e.transpose(ps[:, kk * 128:(kk + 1) * 128], h2f[:, kc * 128:(kc + 1) * 128], ident_f)
                    return ins
                P.pe(fT, reads=["h2f", "ident_f"], writes=[pk])
                if q == 0:
                    P.act(lambda e, ps=ps, q=q: e.copy(h2T[:, q * 4:(q + 1) * 4, :], ps.rearrange("p (a b) -> p a b", a=4)), reads=[pk], writes=["h2T"])
                else:
                    P.dve(lambda e, ps=ps, q=q: e.tensor_copy(h2T[:, q * 4:(q + 1) * 4, :], ps.rearrange("p (a b) -> p a b", a=4)), reads=[pk], writes=["h2T"])
            ps, pk = next_ps()

            def fL(e, ps=ps):
                for k in range(8):
                    e.matmul(ps[:, 0:NE], h2T[:, k, :], wrt_f[:, k, :], start=(k == 0), stop=False)
                return e.matmul(ps[:, 0:NE], ones_f[0:1, :], brt[0:1, :], start=False, stop=True)
            P.pe(fL, reads=["h2T", "wrt_f", "brt", "ones_f"], writes=[pk])
            P.act(lambda e, ps=ps, ti=ti: e.copy(Lg[:, ti, :], ps[:, 0:NE]), reads=[pk], writes=["Lg"])
        P.dma(SQ, lambda e, t=t: e.dma_start(out=h2_v[t], in_=h2b), reads=["h2b"], writes=["h2_d"])
    P.barrier()
    AR.release()
    if stop_after == "G":
        return finish()

    AR.mark()
    NTI = 64
    m8 = AR.alloc([128, NTI, 8]); i8 = AR.alloc([128, NTI, 8], U32); i8f = AR.alloc([128, NTI, 8])
    iota_e = AR.alloc([128, NE]); iota_i = AR.alloc([128, NE], I32)
    oh = [AR.alloc([128, NTI, NE]) for _ in range(4)]
    msk = AR.alloc([128, NTI, NE]); ex = AR.alloc([128, NTI, NE]); den = AR.alloc([128, NTI]); nmx = AR.alloc([128, NTI])
    cntp = AR.alloc([128, NE]); cntp_b = AR.alloc([128, NE], BF16)
    base = AR.alloc([128, NE]); tot = AR.alloc([128, NE]); pad = AR.alloc([128, NE]); ends = AR.alloc([128, NE]); starts = AR.alloc([128, NE])
    pref = AR.alloc([128, NTI, NE]); dst = AR.alloc([128, NTI, NE]); tmp3 = AR.alloc([128, NTI, NE])
    dk = AR.alloc([128, NTI, 4]); ones_e = AR.alloc([128, NTI])
    bthr = AR.alloc([128, NBLK]); bthr_i = AR.alloc([128, NBLK], I32); cmp = AR.alloc([128, NBLK, NE]); ebf = AR.alloc([128, NBLK])
    P.pool(lambda e: e.iota(iota_i, pattern=[[1, NE]], base=0, channel_multiplier=0), writes=["iota_i"])
    P.dve(lambda e: e.tensor_copy(iota_e, iota_i), reads=["iota_i"], writes=["iota_e"])
    P.pool(lambda e: e.iota(bthr_i, pattern=[[BLK, NBLK]], base=0, channel_multiplier=0), writes=["bthr_i"])
    P.dve(lambda e: e.tensor_copy(bthr, bthr_i), reads=["bthr_i"], writes=["bthr"])
    P.pool(lambda e: e.memset(ones_e, 1.0), writes=["ones_e"])
    for ti in range(NTI):
        P.dve(lambda e, ti=ti: e.max(m8[:, ti, :], Lg[:, ti, :]), reads=["Lg"], writes=["m8"])
        P.dve(lambda e, ti=ti: e.max_index(i8[:, ti, :], m8[:, ti, :], Lg[:, ti, :]), reads=["Lg", "m8"], writes=["i8"])
    P.dve(lambda e: e.tensor_copy(i8f, i8), reads=["i8"], writes=["i8f"])
    for k in range(4):
        P.dve(lambda e, k=k: e.tensor_tensor(oh[k], iota_e.unsqueeze(1).to_broadcast([128, NTI, NE]), i8f[:, :, k:k + 1].to_broadcast([128, NTI, NE]), ALU.is_equal), reads=["iota_e", "i8f"], writes=["oh%d" % k])
    P.dve(lambda e: e.tensor_tensor(msk, oh[0], oh[1], ALU.add), reads=["oh0", "oh1"], writes=["msk"])
    P.dve(lambda e: e.tensor_tensor(msk, msk, oh[2], ALU.add), reads=["msk", "oh2"], writes=["msk"])
    P.dve(lambda e: e.tensor_tensor(msk, msk, oh[3], ALU.add), reads=["msk", "oh3"], writes=["msk"])
    P.dve(lambda e: e.tensor_tensor(ex, Lg, m8[:, :, 0:1].to_broadcast([128, NTI, NE]), ALU.subtract), reads=["Lg", "m8"], writes=["ex"])
    P.act(lambda e: e.activation(out=ex, in_=ex, func=AF.Exp), reads=["ex"], writes=["ex"])
    P.dve(lambda e: e.tensor_tensor(ex, ex, msk, ALU.mult), reads=["ex", "msk"], writes=["ex"])
    P.dve(lambda e: e.tensor_reduce(den, ex, AX.X, ALU.add), reads=["ex"], writes=["den"])
    P.dve(lambda e: e.reciprocal(den, den), reads=["den"], writes=["den"])
    P.dve(lambda e: e.tensor_tensor(ex, ex, den.unsqueeze(2).to_broadcast([128, NTI, NE]), ALU.mult), reads=["ex", "den"], writes=["ex"])
    P.dve(lambda e: e.tensor_reduce(cntp, msk.rearrange("p t e -> p e t"), AX.X, ALU.add), reads=["msk"], writes=["cntp"])
    P.dve(lambda e: e.tensor_copy(cntp_b, cntp), reads=["cntp"], writes=["cntp_b"])
    psb_, kb_ = next_ps()
    P.pe(lambda e: e.matmul(psb_[:, 0:NE], ltri_b, cntp_b, start=True, stop=True), reads=["ltri_b", "cntp_b"], writes=[kb_])
    P.dve(lambda e: e.tensor_copy(base, psb_[:, 0:NE]), reads=[kb_], writes=["base"])
    pst_, kt_ = next_ps()
    P.pe(lambda e: e.matmul(pst_[:, 0:NE], ones_b[:, 0:128], cntp_b, start=True, stop=True), reads=["ones_b", "cntp_b"], writes=[kt_])
    P.dve(lambda e: e.tensor_copy(tot, pst_[:, 0:NE]), reads=[kt_], writes=["tot"])
    P.dve(lambda e: e.tensor_scalar(pad, tot, float(BLK - 1), 1.0 / BLK, ALU.add, ALU.mult), reads=["tot"], writes=["pad"])
    P.dve(lambda e: e.tensor_scalar_add(pad, pad, -0.4990234375), reads=["pad"], writes=["pad"])
    P.dve(lambda e: e.tensor_scalar_add(pad, pad, 8388608.0), reads=["pad"], writes=["pad"])
    P.dve(lambda e: e.tensor_scalar_add(pad, pad, -8388608.0), reads=["pad"], writes=["pad"])
    P.dve(lambda e: e.tensor_scalar_mul(pad, pad, float(BLK)), reads=["pad"], writes=["pad"])
    P.dve(lambda e: e.tensor_tensor_scan(ends, ones_e[:, 0:NE], pad, 0.0, ALU.mult, ALU.add), reads=["pad", "ones_e"], writes=["ends"])
    P.dve(lambda e: e.tensor_tensor(starts, ends, pad, ALU.subtract), reads=["ends", "pad"], writes=["starts"])
    P.dve(lambda e: e.tensor_tensor(base, base, starts, ALU.add), reads=["base", "starts"], writes=["base"])
    for ee in range(NE):
        P.dve(lambda e, ee=ee: e.tensor_tensor_scan(pref[:, :, ee], ones_e, msk[:, :, ee], 0.0, ALU.mult, ALU.add), reads=["msk", "ones_e"], writes=["pref"])
    P.dve(lambda e: e.tensor_tensor(pref, pref, msk, ALU.subtract), reads=["pref", "msk"], writes=["pref"])
    P.dve(lambda e: e.tensor_tensor(dst, pref, base.unsqueeze(1).to_broadcast([128, NTI, NE]), ALU.add), reads=["pref", "base"], writes=["dst"])
    for k in range(4):
        P.dve(lambda e, k=k: e.tensor_tensor(tmp3, oh[k], dst, ALU.mult), reads=["oh%d" % k, "dst"], writes=["tmp3"])
        P.dve(lambda e, k=k: e.tensor_reduce(dk[:, :, k], tmp3, AX.X, ALU.add), reads=["tmp3"], writes=["dk"])
        P.dve(lambda e, k=k: e.tensor_tensor(tmp3, oh[k], ex, ALU.mult), reads=["oh%d" % k, "ex", "tmp3"], writes=["tmp3"])
        P.dve(lambda e, k=k: e.tensor_reduce(wk[:, :, k], tmp3, AX.X, ALU.add), reads=["tmp3"], writes=["wk"])
    P.dve(lambda e: e.tensor_copy(dest_i, dk), reads=["dk"], writes=["dest_i"])
    P.dve(lambda e: e.tensor_tensor(cmp, ends.unsqueeze(1).to_broadcast([128, NBLK, NE]), bthr.unsqueeze(2).to_broadcast([128, NBLK, NE]), ALU.is_le), reads=["ends", "bthr"], writes=["cmp"])
    P.dve(lambda e: e.tensor_reduce(ebf, cmp, AX.X, ALU.add), reads=["cmp"], writes=["ebf"])
    P.dve(lambda e: e.tensor_scalar_min(ebf, ebf, float(NE - 1)), reads=["ebf"], writes=["ebf"])
    P.dve(lambda e: e.tensor_copy(eb_i, ebf), reads=["ebf"], writes=["eb_i"])
    pidx_i = AR.alloc([128, 8], I32); pidx = AR.alloc([128, 8]); widx_f = AR.alloc([128, NBLK, 8])
    P.pool(lambda e: e.iota(pidx_i, pattern=[[128, 8]], base=0, channel_multiplier=1), writes=["pidx_i"])
    P.dve(lambda e: e.tensor_copy(pidx, pidx_i), reads=["pidx_i"], writes=["pidx"])
    P.dve(lambda e: e.tensor_scalar_mul(ebf, ebf, 1024.0), reads=["ebf", "eb_i"], writes=["ebf"])
    P.dve(lambda e: e.tensor_tensor(widx_f, ebf.unsqueeze(2).to_broadcast([128, NBLK, 8]), pidx.unsqueeze(1).to_broadcast([128, NBLK, 8]), ALU.add), reads=["ebf", "pidx"], writes=["widx_f"])
    P.dve(lambda e: e.tensor_copy(widx, widx_f), reads=["widx_f"], writes=["widx"])
    AR.mark()
    hrow = [AR.alloc([128, D], BF16) for _ in range(2)]
    h2_r = h2_d.rearrange("(t p) d -> t p d", p=128)
    for ti in range(NTI):
        bi = ti % 2
        P.dma(LQ, lambda e, ti=ti, bi=bi: e.dma_start(out=hrow[bi], in_=h2_r[ti]), reads=["h2_d"], writes=["hrow%d" % bi])
        for k in range(4):
            P.dma("pool", lambda e, ti=ti, bi=bi, k=k: e.indirect_dma_start(out=xg_d, out_offset=bass.IndirectOffsetOnAxis(ap=dest_i[:, ti, k:k + 1], axis=0), in_=hrow[bi], in_offset=None), reads=["hrow%d" % bi, "dest_i"], writes=["xg_d"])
    P.barrier()
    AR.release()
    AR.release()
    if stop_after == "H":
        return finish()

    AR.release()
    AR.mark()
    stgp = [AR.alloc([128, 2048]) for _ in range(2)]
    stga = [AR.alloc([128, 2048]) for _ in range(2)]
    wgu_b = [AR.alloc([128, 8, 2 * D], BF16) for _ in range(2)]
    wdn_b = AR.alloc([128, 8, D], BF16)
    bgb = [AR.alloc([1, 3 * D], BF16) for _ in range(2)]
    xrows = AR.alloc([128, 4, D], BF16)
    xT = [AR.alloc([128, 8, BLK], BF16) for _ in range(2)]
    aT = AR.alloc([128, 8, BLK], BF16)
    eg = AR.alloc([128, BLK]); es = AR.alloc([128, BLK]); eu = AR.alloc([128, BLK])
    ysb = [AR.alloc([128, D]) for _ in range(2)]
    xg_v = xg_d.rearrange("(b s p) d -> b p s d", p=128, s=4)
    ys_v = ys_d.rearrange("(b s p) d -> b s p d", p=128, s=4)
    wgu_rows = w_gu.rearrange("e k n -> (e k) n")
    wdn_rows = w_down.rearrange("e k n -> (e k) n")
    rp = [0]
    ra = [0]

    def load_wgu(b):
        pb = b % 2
        for kc in range(8):
            si = rp[0] % 2
            rp[0] += 1
            P.dma("pool", lambda e, b=b, kc=kc, si=si: e.indirect_dma_start(out=stgp[si], out_offset=None, in_=wgu_rows, in_offset=bass.IndirectOffsetOnAxis(ap=widx[:, b, kc:kc + 1], axis=0)), reads=["widx"], writes=["stgp%d" % si])
            P.pool(lambda e, pb=pb, kc=kc, si=si: e.tensor_copy(wgu_b[pb][:, kc, :], stgp[si]), reads=["stgp%d" % si], writes=["wgu%d_%d" % (pb, kc)])

    def act_stage(b, src_rows, idx_ap, ncol, dst_ap, dkey, p0=128):
        si = ra[0] % 2
        ra[0] += 1
        P.dma("pool", lambda e: e.indirect_dma_start(out=stga[si][:, 0:ncol], out_offset=None, in_=src_rows, in_offset=bass.IndirectOffsetOnAxis(ap=idx_ap, axis=0)), reads=["widx", "eb_i"], writes=["stga%d" % si])
        P.act(lambda e: e.copy(dst_ap, stga[si][0:p0, 0:ncol]), reads=["stga%d" % si], writes=[dkey])

    load_wgu(0)
    for b in range(NBLK):
        pb = b % 2
        P.dma(LQ, lambda e, b=b: e.dma_start(out=xrows, in_=xg_v[b]), reads=["xg_d"], writes=["xrows"])
        act_stage(b, b_gu, eb_i[:, b:b + 1], 2048, bgb[pb][0:1, 0:2048], "bgb%d" % pb, p0=1)
        act_stage(b, b_down, eb_i[:, b:b + 1], 1024, bgb[pb][0:1, 2048:3072], "bgb%d" % pb, p0=1)
        for kc in range(8):
            act_stage(b, wdn_rows, widx[:, b, kc:kc + 1], 1024, wdn_b[:, kc, :], "wdn_%d" % kc)
        if b + 1 < NBLK:
            load_wgu(b + 1)
        for kc in range(8):
            ps, pk = next_ps()
            psb = ps.bitcast(BF16)

            def fx(e, psb=psb, kc=kc):
                for s_ in range(4):
                    ins = e.transpose(psb[:, s_ * 128:(s_ + 1) * 128], xrows[:, s_, kc * 128:(kc + 1) * 128], ident_b)
                return ins
            P.pe(fx, reads=["xrows", "ident_b"], writes=[pk])
            if kc % 2 == 0:
                P.act(lambda e, psb=psb, kc=kc, pb=pb: e.copy(xT[pb][:, kc, :], psb[:, 0:BLK]), reads=[pk], writes=["xT%d" % pb])
            else:
                P.dve(lambda e, psb=psb, kc=kc, pb=pb: e.tensor_copy(xT[pb][:, kc, :], psb[:, 0:BLK]), reads=[pk], writes=["xT%d" % pb])
        wkeys = ["wgu%d_%d" % (pb, kc) for kc in range(8)]
        for cc in range(8):
            psg, kg = next_ps()
            psu, ku = next_ps()

            def fg(e, psg=psg, cc=cc, pb=pb):
                for k in range(8):
                    e.matmul(psg, wgu_b[pb][:, k, cc * 128:(cc + 1) * 128], xT[pb][:, k, :], start=(k == 0), stop=False)
                return e.matmul(psg, bgb[pb][0:1, cc * 128:(cc + 1) * 128], ones_b[0:1, 0:BLK], start=False, stop=True)

            def fu(e, psu=psu, cc=cc, pb=pb):
                for k in range(8):
                    e.matmul(psu, wgu_b[pb][:, k, D + cc * 128:D + (cc + 1) * 128], xT[pb][:, k, :], start=(k == 0), stop=False)
                return e.matmul(psu, bgb[pb][0:1, D + cc * 128:D + (cc + 1) * 128], ones_b[0:1, 0:BLK], start=False, stop=True)
            P.pe(fg, reads=wkeys + ["xT%d" % pb, "bgb%d" % pb, "ones_b"], writes=[kg])
            P.pe(fu, reads=wkeys + ["xT%d" % pb, "bgb%d" % pb, "ones_b"], writes=[ku])
            P.dve(lambda e, psg=psg: e.tensor_scalar_min(eg, psg, 7.0), reads=[kg], writes=["eg"])
            P.act(lambda e: e.activation(out=es, in_=eg, func=AF.Exp, scale=-1.702), reads=["eg"], writes=["es"])
            P.dve(lambda e, psu=psu: e.tensor_scalar(eu, psu, 7.0, -7.0, ALU.min, ALU.max), reads=[ku], writes=["eu"])
            P.dve(lambda e: e.tensor_scalar_add(es, es, 1.0), reads=["es"], writes=["es"])
            P.dve(lambda e: e.reciprocal(es, es), reads=["es"], writes=["es"])
            P.dve(lambda e: e.tensor_tensor(eg, eg, es, ALU.mult), reads=["eg", "es"], writes=["eg"])
            P.dve(lambda e, cc=cc: e.scalar_tensor_tensor(aT[:, cc, :], eu, 1.0, eg, ALU.add, ALU.mult), reads=["eu", "eg"], writes=["aT"])
        dkeys = ["wdn_%d" % kc for kc in range(8)]
        for s_ in range(4):
            yb = ysb[s_ % 2]; ky = "ysb%d" % (s_ % 2)
            for half in range(2):
                ps, pk = next_ps()

                def fd(e, ps=ps, s_=s_, half=half, pb=pb):
                    for k in range(8):
                        e.matmul(ps, aT[:, k, s_ * 128:(s_ + 1) * 128], wdn_b[:, k, half * 512:(half + 1) * 512], start=(k == 0), stop=False)
                    c0 = 2 * D + half * 512
                    return e.matmul(ps, ones_b[0:1, 0:128], bgb[pb][0:1, c0:c0 + 512], start=False, stop=True)
                P.pe(fd, reads=["aT", "bgb%d" % pb, "ones_b"] + dkeys, writes=[pk])
                if half == 0:
                    P.act(lambda e, ps=ps, yb=yb: e.copy(yb[:, 0:512], ps), reads=[pk], writes=[ky])
                else:
                    P.dve(lambda e, ps=ps, yb=yb: e.tensor_copy(yb[:, 512:1024], ps), reads=[pk], writes=[ky])
            P.dma(LQ, lambda e, b=b, s_=s_, yb=yb: e.dma_start(out=ys_v[b, s_], in_=yb), reads=[ky], writes=["ys_d"])
    P.barrier()
    AR.release()
    if stop_after == "I":
        return finish()

    AR.mark()
    yk = [[AR.alloc([128, D]) for _ in range(4)] for _ in range(2)]
    x1r = [AR.alloc([128, D]) for _ in range(2)]
    acc = AR.alloc([128, D]); outt = [AR.alloc([128, D]) for _ in range(2)]
    jjunk = AR.alloc([128, D]); jssq = AR.alloc([128, 64]); jrstd = AR.alloc([128, 64])
    x1_r = x1_d.rearrange("(t p) d -> t p d", p=128)
    y_r = y.rearrange("(t p) d -> t p d", p=128)

    def j_load(ti):
        bi = ti % 2
        for k in range(4):
            P.dma("pool", lambda e, k=k: e.indirect_dma_start(out=yk[bi][k], out_offset=None, in_=ys_d, in_offset=bass.IndirectOffsetOnAxis(ap=dest_i[:, ti, k:k + 1], axis=0)), reads=["ys_d", "dest_i"], writes=["yk%d_%d" % (bi, k)])
        P.dma(LQ, lambda e: e.dma_start(out=x1r[bi], in_=x1_r[ti]), reads=["x1_d"], writes=["x1r%d" % bi])
    j_load(0)
    for ti in range(NTI):
        bi = ti % 2
        if ti + 1 < NTI:
            j_load(ti + 1)
        P.dve(lambda e, bi=bi, ti=ti: e.tensor_scalar_mul(acc, yk[bi][0], wk[:, ti, 0:1]), reads=["yk%d_0" % bi, "wk"], writes=["acc"])
        for k in range(1, 4):
            P.dve(lambda e, bi=bi, ti=ti, k=k: e.scalar_tensor_tensor(acc, yk[bi][k], wk[:, ti, k:k + 1], acc, ALU.mult, ALU.add), reads=["yk%d_%d" % (bi, k), "wk", "acc"], writes=["acc"])
        P.pool(lambda e: e.tensor_tensor(acc, acc, g2_bc, ALU.mult), reads=["acc", "g2_bc"], writes=["acc"])
        P.pool(lambda e, bi=bi: e.tensor_tensor(acc, acc, x1r[bi], ALU.add), reads=["acc", "x1r%d" % bi], writes=["acc"])
        P.act(lambda e, ti=ti: e.activation(out=jjunk, in_=acc, func=AF.Square, accum_out=jssq[:, ti:ti + 1]), reads=["acc"], writes=["jjunk", "jssq"])
        P.dve(lambda e, ti=ti: e.tensor_scalar(jrstd[:, ti:ti + 1], jssq[:, ti:ti + 1], 1.0 / D, EPS, ALU.mult, ALU.add), reads=["jssq"], writes=["jrstd"])
        P.act(lambda e, ti=ti: e.activation(out=jrstd[:, ti:ti + 1], in_=jrstd[:, ti:ti + 1], func=AF.Ln), reads=["jrstd"], writes=["jrstd"])
        P.act(lambda e, ti=ti: e.activation(out=jrstd[:, ti:ti + 1], in_=jrstd[:, ti:ti + 1], func=AF.Exp, scale=-0.5), reads=["jrstd"], writes=["jrstd"])
        P.dve(lambda e, bi=bi, ti=ti: e.scalar_tensor_tensor(outt[bi], acc, jrstd[:, ti:ti + 1], nf_bc, ALU.mult, ALU.mult), reads=["acc", "jrstd", "nf_bc"], writes=["outt%d" % bi])
        P.dma(LQ, lambda e, bi=bi, ti=ti: e.dma_start(out=y_r[ti], in_=outt[bi]), reads=["outt%d" % bi], writes=["y"])
    AR.release()
    return finish()


_CACHE = {}


def kernel(**inputs):
    n = 8
    if "nc" not in _CACHE:
        _CACHE["nc"] = build_program()
    nc = _CACHE["nc"]
    tabs = dft_tables()
    f = np.float32

    def a(v):
        return np.ascontiguousarray(np.asarray(v, dtype=f))
    shared = dict(
        c_ctx=a(inputs["c_ctx"]).reshape(1, D), w_ada=a(inputs["w_ada"][0]), b_ada=a(inputs["b_ada"][0]).reshape(1, -1),
        norm1=a(inputs["norm1"][0]).reshape(1, D), w_in=a(inputs["w_in"][0]), conv_w=a(inputs["conv_w"][0]),
        conv_b=a(inputs["conv_b"][0]).reshape(1, D), gate_a_w=a(inputs["gate_a_w"][0]), gate_a_b=a(inputs["gate_a_b"][0]),
        gate_x_w=a(inputs["gate_x_w"][0]), gate_x_b=a(inputs["gate_x_b"][0]), lru_lambda=a(inputs["lru_lambda"][0]),
        w_fourier=a(inputs["w_fourier"][0]), w_rnn=a(inputs["w_rnn"][0]), w_out=a(inputs["w_out"][0]),
        norm2=a(inputs["norm2"][0]).reshape(1, D), w_router=a(inputs["w_router"][0]), b_router=a(inputs["b_router"][0]).reshape(1, NE),
        w_gu=a(inputs["w_gu"][0]), b_gu=a(inputs["b_gu"][0]), w_down=a(inputs["w_down"][0]), b_down=a(inputs["b_down"][0]),
        norm_f=a(inputs["norm_f"]).reshape(1, D), **tabs)
    xs = a(inputs["x"]); cs = a(inputs["c"]); cx = a(inputs["ctx"])
    in_maps = []
    for i in range(n):
        m = dict(shared)
        m["x"] = xs[i]; m["c"] = cs[i].reshape(1, D); m["ctx"] = cx[i]
        in_maps.append(m)
    ncore = int(os.environ.get("KDBG_NCORE", "8"))
    if ncore != 8:
        res = run_bass_kernel_spmd(nc, in_maps[:ncore], core_ids=list(range(ncore)))
        return np.stack([np.asarray(r["y"], dtype=f) for r in res.results], axis=0)
    res = run_bass_kernel_spmd(nc, in_maps, core_ids=list(range(n)))
    return np.stack([np.asarray(r["y"], dtype=f) for r in res.results], axis=0)
```

```python
import contextlib
import math
import os
import numpy as np
import concourse.bass as bass
import concourse.mybir as mybir
from concourse.bass_utils import run_bass_kernel_spmd

F32 = mybir.dt.float32
BF16 = mybir.dt.bfloat16
I32 = mybir.dt.int32
U32 = mybir.dt.uint32
ALU = mybir.AluOpType
AF = mybir.ActivationFunctionType
AX = mybir.AxisListType

SELF_SYNC = True
NDSEM = 6

D = 1024
S = 8192
CTX = 256
NE = 32
BLK = 512
NBLK = 96
PSLOTS = NBLK * BLK
EPS = 1e-6


class Prog:
    ENGS = ("pe", "act", "dve", "pool", "sp")

    def __init__(self, nc):
        self.nc = nc
        self.ops = []

    def add(self, eng, fn, reads=(), writes=(), dma=False):
        self.ops.append(dict(eng=eng, fn=fn, reads=tuple(reads), writes=tuple(writes), dma=dma, bar=False))

    def pe(self, fn, reads=(), writes=()):
        self.add("pe", fn, reads, writes)

    def act(self, fn, reads=(), writes=()):
        self.add("act", fn, reads, writes)

    def dve(self, fn, reads=(), writes=()):
        self.add("dve", fn, reads, writes)

    def pool(self, fn, reads=(), writes=()):
        self.add("pool", fn, reads, writes)

    def dma(self, q, fn, reads=(), writes=()):
        self.add(q, fn, reads, writes, dma=True)

    def barrier(self):
        for e in self.ENGS:
            self.ops.append(dict(eng=e, fn=None, reads=(), writes=(), dma=False, bar=True))

    def emit(self):
        nc = self.nc
        ops = self.ops
        cnt = {e: 0 for e in self.ENGS}
        dcnt = {e: 0 for e in self.ENGS}
        latest = {}
        for op in ops:
            e = op["eng"]
            if op["bar"]:
                op["barvals"] = dict(latest)
                continue
            if op["dma"]:
                i = dcnt[e]
                dcnt[e] += 1
                op["sem"] = ("d", e, i % NDSEM)
                op["val"] = 16 * (i // NDSEM + 1)
            else:
                cnt[e] += 1
                op["sem"] = ("c", e)
                op["val"] = cnt[e]
            latest[op["sem"]] = op["val"]
        last_w = {}
        readers = {}
        for op in ops:
            if op["bar"]:
                continue
            deps = []
            for k in op["reads"]:
                if k in last_w:
                    deps.append(last_w[k])
            for k in op["writes"]:
                if k in last_w:
                    deps.append(last_w[k])
                deps.extend(readers.get(k, ()))
            op["deps"] = deps
            for k in op["writes"]:
                last_w[k] = op
                readers[k] = []
            for k in op["reads"]:
                if k not in op["writes"]:
                    readers.setdefault(k, []).append(op)
        known = {e: {} for e in self.ENGS}
        for op in ops:
            e = op["eng"]
            need = {}
            if op["bar"]:
                need = dict(op["barvals"])
                need.pop(("c", e), None)
            else:
                for d in op["deps"]:
                    if d is op:
                        continue
                    if (not d["dma"]) and d["eng"] == e and (e == "pe" or not SELF_SYNC):
                        continue
                    s, v = d["sem"], d["val"]
                    if need.get(s, 0) < v:
                        need[s] = v
                if op["dma"] and op["val"] > 16:
                    s = op["sem"]
                    if need.get(s, 0) < op["val"] - 16:
                        need[s] = op["val"] - 16
            w = []
            for s, v in need.items():
                if known[e].get(s, 0) < v:
                    known[e][s] = v
                    w.append((s, v))
            op["waits"] = w
        final = latest
        semkeys = sorted(final.keys(), key=str)
        with contextlib.ExitStack() as st:
            sems = {}
            for k in semkeys:
                sems[k] = st.enter_context(nc.semaphore("s_" + "_".join(map(str, k))))
            block = st.enter_context(nc.Block())

            def run(engname):
                def body(eng):
                    for op in ops:
                        if op["eng"] != engname:
                            continue
                        for s, v in op["waits"]:
                            eng.wait_ge(sems[s], v)
                        if op["bar"]:
                            continue
                        ins = op["fn"](eng)
                        ins.then_inc(sems[op["sem"]], 16 if op["dma"] else 1)
                    for k, v in final.items():
                        if k[0] == "d" and k[1] == engname:
                            eng.wait_ge(sems[k], v)
                return body

            block.sync(run("sp"))
            block.scalar(run("act"))
            block.vector(run("dve"))
            block.gpsimd(run("pool"))
            block.tensor(run("pe"))


class Arena:
    def __init__(self, base_ap, nbytes):
        self.base = base_ap
        self.nbytes = nbytes
        self.off = 0
        self.marks = []

    def alloc(self, shape, dt=F32):
        esz = {F32: 4, BF16: 2, I32: 4, U32: 4}[dt]
        npart = shape[0]
        fshape = list(shape[1:])
        n = 1
        for s_ in fshape:
            n *= s_
        nb = (n * esz + 31) // 32 * 32
        assert self.off + nb <= self.nbytes, ("SBUF arena overflow", self.off, nb, self.nbytes)
        a = self.base[0:npart, self.off // 4:(self.off + nb) // 4]
        self.off += nb
        if dt != F32:
            a = a.bitcast(dt)
        a = a[:, 0:n]
        if len(fshape) == 2:
            a = a.rearrange("p (a b) -> p a b", a=fshape[0])
        elif len(fshape) == 3:
            a = a.rearrange("p (a b c) -> p a b c", a=fshape[0], b=fshape[1])
        return a

    def mark(self):
        self.marks.append(self.off)

    def release(self):
        self.off = self.marks.pop()


def dft_tables():
    n = np.arange(128)
    ang = 2 * np.pi * np.outer(n, n) / 128.0
    c128 = np.cos(ang)
    s128 = np.sin(ang)
    k1 = np.arange(128)[:, None]
    n2 = np.arange(64)[None, :]
    tw = 2 * np.pi * k1 * n2 / 8192.0
    twc = np.cos(tw)
    tws = np.sin(tw)
    a64 = 2 * np.pi * np.outer(np.arange(64), np.arange(64)) / 64.0
    c2 = np.cos(a64)
    s2 = np.sin(a64)
    t3 = np.zeros((128, 128))
    t3[0:64, 0:64] = c2
    t3[64:128, 0:64] = -s2
    t3[0:64, 64:128] = s2
    t3[64:128, 64:128] = c2
    t3 = t3 / 1024.0
    f = np.float32
    return dict(k_c128=c128.astype(f), k_s128=s128.astype(f), k_twc=twc.astype(f), k_tws=tws.astype(f), k_t3=t3.astype(f))


def build_program(stop_after=None, debug=False):
    nc = bass.Bass("TRN2", target_bir_lowering=False)

    def din(name, shape, dt=F32):
        return nc.dram_tensor(name, list(shape), dt, kind="ExternalInput").ap()

    DBGSET = set(os.environ.get("KDBG_OUT", "").split(",")) if debug else set()

    def dscr(name, shape, dt=F32):
        if name in DBGSET:
            return nc.dram_tensor(name, list(shape), dt, kind="ExternalOutput").ap()
        return nc.dram_tensor(name, list(shape), dt).ap()

    x = din("x", [S, D]); c = din("c", [1, D]); ctx = din("ctx", [CTX, D]); c_ctx = din("c_ctx", [1, D])
    w_ada = din("w_ada", [D, 6 * D]); b_ada = din("b_ada", [1, 6 * D]); norm1 = din("norm1", [1, D])
    w_in = din("w_in", [D, 4608]); conv_w = din("conv_w", [4, D]); conv_b = din("conv_b", [1, D])
    gate_a_w = din("gate_a_w", [2, 8, 128, 128]); gate_a_b = din("gate_a_b", [2, D])
    gate_x_w = din("gate_x_w", [2, 8, 128, 128]); gate_x_b = din("gate_x_b", [2, D])
    lru_lambda = din("lru_lambda", [2, D])
    w_fourier = din("w_fourier", [512, D]); w_rnn = din("w_rnn", [D, D]); w_out = din("w_out", [D, D])
    norm2 = din("norm2", [1, D]); w_router = din("w_router", [D, NE]); b_router = din("b_router", [1, NE])
    w_gu = din("w_gu", [NE, D, 2 * D]); b_gu = din("b_gu", [NE, 2 * D])
    w_down = din("w_down", [NE, D, D]); b_down = din("b_down", [NE, D]); norm_f = din("norm_f", [1, D])
    k_c128 = din("k_c128", [128, 128]); k_s128 = din("k_s128", [128, 128])
    k_twc = din("k_twc", [128, 64]); k_tws = din("k_tws", [128, 64]); k_t3 = din("k_t3", [128, 128])
    y = nc.dram_tensor("y", [S, D], F32, kind="ExternalOutput").ap()
    dbg = nc.dram_tensor("dbg", [128, 2048], F32, kind="ExternalOutput").ap() if debug else None

    u_d = dscr("u_d", [S, 512], BF16)
    xc_d = dscr("xc_d", [D, S], F32)
    gg_d = dscr("gg_d", [D, S], BF16)
    gfr_d = dscr("gfr_d", [2 * D, S], BF16)
    q_d = dscr("q_d", [4, 2, 64, 128, 128], BF16)
    rt_d = dscr("rt_d", [D, S], BF16)
    yg_d = dscr("yg_d", [D, S], BF16)
    x1_d = dscr("x1_d", [S, D], F32)
    h2_d = dscr("h2_d", [S, D], BF16)
    xg_d = dscr("xg_d", [PSLOTS, D], BF16)
    ys_d = dscr("ys_d", [PSLOTS, D], F32)

    st = contextlib.ExitStack()
    SBN = 206848
    sball = st.enter_context(nc.sbuf_tensor("sball", [128, SBN // 4], F32))
    AR = Arena(sball[:], SBN)
    banks = [st.enter_context(nc.psum_tensor("psb%d" % i, [128, 512], F32)) for i in range(8)]
    P = Prog(nc)
    psn = [0]

    def next_ps():
        i = psn[0] % 8
        psn[0] += 1
        return banks[i][:], "ps%d" % i

    uid = [0]

    def K(name):
        uid[0] += 1
        return "%s#%d" % (name, uid[0])

    def finish():
        with nc.allow_low_precision(reason="bf16 matmul operands, fp32 accumulation"):
            P.emit()
        st.close()
        return nc

    LQ = "sp"
    SQ = "pool"

    ident_f = AR.alloc([128, 128]); ident_b = AR.alloc([128, 128], BF16)
    ones_b = AR.alloc([128, 512], BF16); ones_f = AR.alloc([128, 128])
    ltri_b = AR.alloc([128, 128], BF16)
    g2_bc = AR.alloc([128, D]); nf_bc = AR.alloc([128, D])
    dest_i = AR.alloc([128, 64, 4], I32); wk = AR.alloc([128, 64, 4])
    eb_i = AR.alloc([128, NBLK], I32); widx = AR.alloc([128, NBLK, 8], I32)
    AR.mark()
    g1_bc = AR.alloc([128, D]); gm2_bc = AR.alloc([128, D]); sh2_bc = AR.alloc([128, D])
    gm1T = AR.alloc([128, 8]); sh1T = AR.alloc([128, 8]); gmcT = AR.alloc([128, 8]); shcT = AR.alloc([128, 8])
    cwT = AR.alloc([128, 8, 4]); cbT = AR.alloc([128, 8])
    nbaT = AR.alloc([128, 16]); nbxT = AR.alloc([128, 16]); coefT = AR.alloc([128, 16])
    h0T = AR.alloc([128, 16])
    Lg = AR.alloc([128, 64, NE])

    P.pool(lambda e: e.memset(ident_f, 0.0), writes=["ident_f"])
    P.pool(lambda e: e.affine_select(out=ident_f, in_=ident_f, pattern=[[-1, 128]], compare_op=ALU.not_equal, fill=1.0, base=0, channel_multiplier=1), reads=["ident_f"], writes=["ident_f"])
    P.dve(lambda e: e.tensor_copy(ident_b, ident_f), reads=["ident_f"], writes=["ident_b"])
    P.pool(lambda e: e.memset(ones_b, 1.0), writes=["ones_b"])
    P.pool(lambda e: e.memset(ones_f, 1.0), writes=["ones_f"])
    P.pool(lambda e: e.memset(ltri_b, 1.0), writes=["ltri_b"])
    P.pool(lambda e: e.affine_select(out=ltri_b, in_=ltri_b, pattern=[[1, 128]], compare_op=ALU.is_gt, fill=0.0, base=0, channel_multiplier=-1), reads=["ltri_b"], writes=["ltri_b"])

    AR.mark()
    cT = AR.alloc([128, 16])
    crep = AR.alloc([128, 16, 128])
    mb = AR.alloc([128, 6 * D])
    mcb = AR.alloc([128, 2 * D])
    wa_buf = [AR.alloc([128, 8, 512]) for _ in range(2)]
    n1_bc = AR.alloc([128, D]); n2_bc = AR.alloc([128, D])
    P.dma(LQ, lambda e: e.dma_start(out=cT[:, 0:8], in_=c.rearrange("o (k p) -> p (o k)", p=128), allow_slow_non_contiguous=True), writes=["cT"])
    P.dma(LQ, lambda e: e.dma_start(out=cT[:, 8:16], in_=c_ctx.rearrange("o (k p) -> p (o k)", p=128), allow_slow_non_contiguous=True), reads=["cT"], writes=["cT"])
    P.dma(LQ, lambda e: e.dma_start(out=mb, in_=b_ada.partition_broadcast(128)), writes=["mb"])
    P.dma(LQ, lambda e: e.dma_start(out=n1_bc, in_=norm1.partition_broadcast(128)), writes=["n1_bc"])
    P.dma(LQ, lambda e: e.dma_start(out=n2_bc, in_=norm2.partition_broadcast(128)), writes=["n2_bc"])
    P.dma(LQ, lambda e: e.dma_start(out=nf_bc, in_=norm_f.partition_broadcast(128)), writes=["nf_bc"])
    ctmp = AR.alloc([128, 16])
    P.act(lambda e: e.activation(out=ctmp, in_=cT, func=AF.Exp, scale=-1.0), reads=["cT"], writes=["ctmp"])
    P.dve(lambda e: e.tensor_scalar_add(ctmp, ctmp, 1.0), reads=["ctmp"], writes=["ctmp"])
    P.dve(lambda e: e.reciprocal(ctmp, ctmp), reads=["ctmp"], writes=["ctmp"])
    P.dve(lambda e: e.tensor_tensor(cT, cT, ctmp, ALU.mult), reads=["ctmp", "cT"], writes=["cT"])
    for j in range(16):
        P.dve(lambda e, j=j: e.tensor_copy(crep[:, j, :], cT[:, j:j + 1].to_broadcast([128, 128])), reads=["cT"], writes=["crep"])
    w_ada_v = w_ada.rearrange("(k p) n -> p k n", p=128)
    for ch in range(12):
        bi = ch % 2
        P.dma(LQ, lambda e, ch=ch, bi=bi: e.dma_start(out=wa_buf[bi], in_=w_ada_v[:, :, ch * 512:(ch + 1) * 512]), writes=["wa%d" % bi])
        ps, pk = next_ps()

        def f(e, ps=ps, bi=bi):
            for k in range(8):
                ins = e.matmul(ps, crep[:, k, :], wa_buf[bi][:, k, :], start=(k == 0), stop=(k == 7))
            return ins
        P.pe(f, reads=["crep", "wa%d" % bi], writes=[pk])
        P.dve(lambda e, ps=ps, ch=ch: e.tensor_tensor(mb[:, ch * 512:(ch + 1) * 512], mb[:, ch * 512:(ch + 1) * 512], ps, ALU.add), reads=[pk, "mb"], writes=["mb"])
        if ch < 4:
            ps2, pk2 = next_ps()

            def f2(e, ps2=ps2, bi=bi):
                for k in range(8):
                    ins = e.matmul(ps2, crep[:, 8 + k, :], wa_buf[bi][:, k, :], start=(k == 0), stop=(k == 7))
                return ins
            P.pe(f2, reads=["crep", "wa%d" % bi], writes=[pk2])
            P.act(lambda e, ps2=ps2, ch=ch: e.copy(mcb[:, ch * 512:(ch + 1) * 512], ps2), reads=[pk2], writes=["mcb"])
    bada2 = AR.alloc([128, 2 * D])
    P.dma(LQ, lambda e: e.dma_start(out=bada2, in_=b_ada[:, 0:2 * D].partition_broadcast(128)), writes=["bada2"])
    P.dve(lambda e: e.tensor_tensor(mcb, mcb, bada2, ALU.add), reads=["mcb", "bada2"], writes=["mcb"])
    gm1_bc = AR.alloc([128, D]); gmc_bc = AR.alloc([128, D])
    P.dve(lambda e: e.scalar_tensor_tensor(gm1_bc, mb[:, D:2 * D], 1.0, n1_bc, ALU.add, ALU.mult), reads=["mb", "n1_bc"], writes=["gm1_bc"])
    P.dve(lambda e: e.scalar_tensor_tensor(gmc_bc, mcb[:, D:2 * D], 1.0, n1_bc, ALU.add, ALU.mult), reads=["mcb", "n1_bc"], writes=["gmc_bc"])
    P.dve(lambda e: e.scalar_tensor_tensor(gm2_bc, mb[:, 4 * D:5 * D], 1.0, n2_bc, ALU.add, ALU.mult), reads=["mb", "n2_bc"], writes=["gm2_bc"])
    P.act(lambda e: e.copy(g1_bc, mb[:, 2 * D:3 * D]), reads=["mb"], writes=["g1_bc"])
    P.act(lambda e: e.copy(sh2_bc, mb[:, 3 * D:4 * D]), reads=["mb"], writes=["sh2_bc"])
    P.act(lambda e: e.copy(g2_bc, mb[:, 5 * D:6 * D]), reads=["mb"], writes=["g2_bc"])
    for (src, sk, dst, dk) in ((gm1_bc, "gm1_bc", gm1T, "gm1T"), (mb, "mb", sh1T, "sh1T"), (gmc_bc, "gmc_bc", gmcT, "gmcT"), (mcb, "mcb", shcT, "shcT")):
        for kc in range(8):
            ps, pk = next_ps()
            P.pe(lambda e, ps=ps, src=src, kc=kc: e.transpose(ps[:, 0:128], src[:, kc * 128:(kc + 1) * 128], ident_f), reads=[sk, "ident_f"], writes=[pk])
            P.dve(lambda e, ps=ps, dst=dst, kc=kc: e.tensor_copy(dst[:, kc:kc + 1], ps[:, 0:1]), reads=[pk], writes=[dk])
    for kk_ in range(4):
        P.dma(LQ, lambda e, kk_=kk_: e.dma_start(out=cwT[:, :, kk_], in_=conv_w[kk_:kk_ + 1, :].rearrange("o (h p) -> p (o h)", p=128), allow_slow_non_contiguous=True), reads=["cwT"], writes=["cwT"])
    P.dma(LQ, lambda e: e.dma_start(out=cbT, in_=conv_b.rearrange("o (h p) -> p (o h)", p=128), allow_slow_non_contiguous=True), writes=["cbT"])
    P.dma(LQ, lambda e: e.dma_start(out=nbaT, in_=gate_a_b.rearrange("d (h p) -> p (d h)", p=128), allow_slow_non_contiguous=True), writes=["nbaT"])
    P.dma(LQ, lambda e: e.dma_start(out=nbxT, in_=gate_x_b.rearrange("d (h p) -> p (d h)", p=128), allow_slow_non_contiguous=True), writes=["nbxT"])
    P.dma(LQ, lambda e: e.dma_start(out=coefT, in_=lru_lambda.rearrange("d (h p) -> p (d h)", p=128), allow_slow_non_contiguous=True), writes=["coefT"])
    P.dve(lambda e: e.tensor_scalar_mul(nbaT, nbaT, -1.0), reads=["nbaT"], writes=["nbaT"])
    P.dve(lambda e: e.tensor_scalar_mul(nbxT, nbxT, -1.0), reads=["nbxT"], writes=["nbxT"])
    P.act(lambda e: e.activation(out=coefT, in_=coefT, func=AF.Exp, scale=-1.0), reads=["coefT"], writes=["coefT"])
    P.act(lambda e: e.activation(out=coefT, in_=coefT, func=AF.Ln, bias=1.0), reads=["coefT"], writes=["coefT"])
    P.dve(lambda e: e.tensor_scalar_mul(coefT, coefT, -8.0), reads=["coefT"], writes=["coefT"])
    P.barrier()
    AR.release()
    if stop_after == "A":
        return finish()

    AR.mark()
    gw_b = AR.alloc([128, 32, 128], BF16)
    AR.mark()
    win_b = AR.alloc([128, 8, 4608], BF16)
    AR.mark()
    stg = [AR.alloc([128, 8, 512]) for _ in range(2)]
    w_in_v = w_in.rearrange("(k p) n -> p k n", p=128)
    for ch in range(9):
        bi = ch % 2
        P.dma(LQ, lambda e, ch=ch, bi=bi: e.dma_start(out=stg[bi], in_=w_in_v[:, :, ch * 512:(ch + 1) * 512]), writes=["stg%d" % bi])
        if ch % 2 == 0:
            P.act(lambda e, ch=ch, bi=bi: e.copy(win_b[:, :, ch * 512:(ch + 1) * 512], stg[bi]), reads=["stg%d" % bi], writes=["win_b"])
        else:
            P.dve(lambda e, ch=ch, bi=bi: e.tensor_copy(win_b[:, :, ch * 512:(ch + 1) * 512], stg[bi]), reads=["stg%d" % bi], writes=["win_b"])
    for gi, gwd in enumerate((gate_a_w, gate_x_w)):
        bi = gi % 2
        P.dma(LQ, lambda e, gwd=gwd, bi=bi: e.dma_start(out=stg[bi][:, 0:4, :].rearrange("p a (b c) -> p (a b) c", c=128), in_=gwd.rearrange("d h i j -> i (d h) j")), writes=["stg%d" % bi])
        P.dve(lambda e, gi=gi, bi=bi: e.tensor_copy(gw_b[:, gi * 16:(gi + 1) * 16, :], stg[bi][:, 0:4, :].rearrange("p a (b c) -> p (a b) c", c=128)), reads=["stg%d" % bi], writes=["gw_b"])
    P.barrier()
    AR.release()
    if stop_after == "W":
        return finish()

    def rms_rows(xt, nsub, ssq, rstd, junk, kx, kpre):
        for s_ in range(nsub):
            P.act(lambda e, s_=s_: e.activation(out=junk, in_=xt[:, s_, :], func=AF.Square, accum_out=ssq[:, s_:s_ + 1]), reads=[kx], writes=[kpre + "junk", kpre + "ssq"])
        P.dve(lambda e: e.tensor_scalar(rstd[:, 0:nsub], ssq[:, 0:nsub], 1.0 / D, EPS, ALU.mult, ALU.add), reads=[kpre + "ssq"], writes=[kpre + "rstd"])
        P.act(lambda e: e.activation(out=rstd[:, 0:nsub], in_=rstd[:, 0:nsub], func=AF.Ln), reads=[kpre + "rstd"], writes=[kpre + "rstd"])
        P.act(lambda e: e.activation(out=rstd[:, 0:nsub], in_=rstd[:, 0:nsub], func=AF.Exp, scale=-0.5), reads=[kpre + "rstd"], writes=[kpre + "rstd"])

    def norm_transpose(xt, nsub, rstd, xs_b, hT, gT, sT, kx, kpre, khT):
        for s_ in range(nsub):
            P.act(lambda e, s_=s_: e.activation(out=xs_b[:, s_, :], in_=xt[:, s_, :], func=AF.Copy, scale=rstd[:, s_:s_ + 1]), reads=[kx, kpre + "rstd"], writes=[kpre + "xs"])
        for kc in range(8):
            ps, pk = next_ps()
            psb = ps.bitcast(BF16)

            def f(e, psb=psb, kc=kc):
                for s_ in range(nsub):
                    ins = e.transpose(psb[:, s_ * 128:(s_ + 1) * 128], xs_b[:, s_, kc * 128:(kc + 1) * 128], ident_b)
                return ins
            P.pe(f, reads=[kpre + "xs", "ident_b"], writes=[pk])
            n = nsub * 128
            if kc % 2 == 0:
                P.act(lambda e, psb=psb, kc=kc, n=n: e.activation(out=hT[:, kc, 0:n], in_=psb[:, 0:n], func=AF.Identity, scale=gT[:, kc:kc + 1], bias=sT[:, kc:kc + 1]), reads=[pk], writes=[khT])
            else:
                P.dve(lambda e, psb=psb, kc=kc, n=n: e.tensor_scalar(hT[:, kc, 0:n], psb[:, 0:n], gT[:, kc:kc + 1], sT[:, kc:kc + 1], ALU.mult, ALU.add), reads=[pk], writes=[khT])

    def conv_from_psum(ps, out_t, h, ntok, rowlen, kps, kout):
        nr = ntok // rowlen
        P.dve(lambda e: e.tensor_scalar(out_t[:, 0:ntok], ps[:, 0:ntok], cwT[:, h, 2:3], cbT[:, h:h + 1], ALU.mult, ALU.add), reads=[kps, "cwT", "cbT"], writes=[kout])
        o3 = out_t[:, 0:ntok].rearrange("p (r t) -> p r t", t=rowlen)
        z3 = ps[:, 0:ntok].rearrange("p (r t) -> p r t", t=rowlen)
        for (kk, sh) in ((0, -2), (1, -1), (3, 1)):
            if sh < 0:
                oo = o3[:, :, -sh:rowlen]; zz = z3[:, :, 0:rowlen + sh]
            else:
                oo = o3[:, :, 0:rowlen - sh]; zz = z3[:, :, sh:rowlen]
            P.dve(lambda e, oo=oo, zz=zz, kk=kk: e.scalar_tensor_tensor(oo, zz, cwT[:, h, kk:kk + 1], oo, ALU.mult, ALU.add), reads=[kps, kout], writes=[kout])

    def rnn_chunk(xc_f, xc_b, d, h, n, bufs, kxc, kpre):
        ia = 0 * 16 + d * 8 + h
        ix = 1 * 16 + d * 8 + h
        dh = d * 8 + h
        e1, a_, e2, s_, b_ = bufs["e1"], bufs["a"], bufs["e2"], bufs["s"], bufs["b"]
        nch = (n + 511) // 512
        psr = []
        for g_, wi in ((0, ia), (1, ix)):
            lst = []
            for j in range(nch):
                ps, pk = next_ps()
                w_ = min(512, n - j * 512)
                P.pe(lambda e, ps=ps, wi=wi, j=j, w_=w_: e.matmul(ps[:, 0:w_], gw_b[:, wi, :], xc_b[:, j * 512:j * 512 + w_], start=True, stop=True), reads=[kxc + "b", "gw_b"], writes=[pk])
                lst.append((ps, pk, j, w_))
            psr.append(lst)
        for (ps, pk, j, w_) in psr[0]:
            P.act(lambda e, ps=ps, j=j, w_=w_: e.activation(out=e1[:, j * 512:j * 512 + w_], in_=ps[:, 0:w_], func=AF.Exp, scale=-1.0, bias=nbaT[:, dh:dh + 1]), reads=[pk, "nbaT"], writes=[kpre + "e1"])
        for (ps, pk, j, w_) in psr[1]:
            P.act(lambda e, ps=ps, j=j, w_=w_: e.activation(out=e2[:, j * 512:j * 512 + w_], in_=ps[:, 0:w_], func=AF.Exp, scale=-1.0, bias=nbxT[:, dh:dh + 1]), reads=[pk, "nbxT"], writes=[kpre + "e2"])
        P.dve(lambda e: e.tensor_scalar_add(e1[:, 0:n], e1[:, 0:n], 1.0), reads=[kpre + "e1"], writes=[kpre + "e1"])
        P.dve(lambda e: e.reciprocal(e1[:, 0:n], e1[:, 0:n]), reads=[kpre + "e1"], writes=[kpre + "e1"])
        P.act(lambda e: e.activation(out=a_[:, 0:n], in_=e1[:, 0:n], func=AF.Exp, scale=coefT[:, dh:dh + 1]), reads=[kpre + "e1", "coefT"], writes=[kpre + "a"])
        P.dve(lambda e: e.tensor_scalar_add(e2[:, 0:n], e2[:, 0:n], 1.0), reads=[kpre + "e2"], writes=[kpre + "e2"])
        P.dve(lambda e: e.reciprocal(e2[:, 0:n], e2[:, 0:n]), reads=[kpre + "e2"], writes=[kpre + "e2"])
        P.dve(lambda e: e.tensor_tensor(s_[:, 0:n], a_[:, 0:n], a_[:, 0:n], ALU.mult), reads=[kpre + "a"], writes=[kpre + "s"])
        P.act(lambda e: e.activation(out=s_[:, 0:n], in_=s_[:, 0:n], func=AF.Ln, scale=-1.0, bias=1.0), reads=[kpre + "s"], writes=[kpre + "s"])
        P.act(lambda e: e.activation(out=s_[:, 0:n], in_=s_[:, 0:n], func=AF.Exp, scale=0.5), reads=[kpre + "s"], writes=[kpre + "s"])
        P.dve(lambda e: e.tensor_tensor(b_[:, 0:n], e2[:, 0:n], xc_f, ALU.mult), reads=[kpre + "e2", kxc], writes=[kpre + "b"])
        P.dve(lambda e: e.tensor_tensor(b_[:, 0:n], b_[:, 0:n], s_[:, 0:n], ALU.mult), reads=[kpre + "b", kpre + "s"], writes=[kpre + "b"])

    AR.mark()
    cx = AR.alloc([128, 2, D]); cjunk = AR.alloc([128, D]); cssq = AR.alloc([128, 4]); crstd = AR.alloc([128, 4])
    cxs = AR.alloc([128, 2, D], BF16); hcT = AR.alloc([128, 8, CTX], BF16)
    xcc = AR.alloc([128, 8, CTX]); xccb = AR.alloc([128, 8, CTX], BF16)
    cb_ = dict(e1=AR.alloc([128, CTX]), a=AR.alloc([128, CTX]), e2=AR.alloc([128, CTX]), s=AR.alloc([128, CTX]), b=AR.alloc([128, CTX]))
    chh = AR.alloc([128, CTX])
    P.dma(LQ, lambda e: e.dma_start(out=cx, in_=ctx.rearrange("(s p) d -> p s d", p=128)), writes=["cx"])
    rms_rows(cx, 2, cssq, crstd, cjunk, "cx", "c_")
    norm_transpose(cx, 2, crstd, cxs, hcT, gmcT, shcT, "cx", "c_", "hcT")
    for h in range(8):
        ps, pk = next_ps()

        def f(e, ps=ps, h=h):
            for k in range(8):
                ins = e.matmul(ps[:, 0:CTX], win_b[:, k, 512 + h * 128:512 + (h + 1) * 128], hcT[:, k, :], start=(k == 0), stop=(k == 7))
            return ins
        P.pe(f, reads=["win_b", "hcT"], writes=[pk])
        conv_from_psum(ps, xcc[:, h, :], h, CTX, CTX, pk, "xcc%d" % h)
        P.act(lambda e, h=h: e.copy(xccb[:, h, :], xcc[:, h, :]), reads=["xcc%d" % h], writes=["xcc%db" % h])
        for d in range(2):
            rnn_chunk(xcc[:, h, :], xccb[:, h, :], d, h, CTX, cb_, "xcc%d" % h, "c_")
            if d == 0:
                P.dve(lambda e: e.tensor_tensor_scan(chh, cb_["a"], cb_["b"], 0.0, ALU.mult, ALU.add), reads=["c_a", "c_b"], writes=["chh"])
                P.dve(lambda e, h=h: e.tensor_copy(h0T[:, h:h + 1], chh[:, CTX - 1:CTX]), reads=["chh"], writes=["h0T"])
            else:
                P.dve(lambda e: e.tensor_tensor_scan(chh[:, ::-1], cb_["a"][:, ::-1], cb_["b"][:, ::-1], 0.0, ALU.mult, ALU.add), reads=["c_a", "c_b"], writes=["chh"])
                P.dve(lambda e, h=h: e.tensor_copy(h0T[:, 8 + h:9 + h], chh[:, 0:1]), reads=["chh"], writes=["h0T"])
    P.barrier()
    AR.release()
    if stop_after == "C":
        return finish()

    AR.mark()
    NT = S // 512
    xt = [AR.alloc([128, 4, D]) for _ in range(2)]
    djunk = AR.alloc([128, D], BF16); dssq = AR.alloc([128, 4]); drstd = AR.alloc([128, 4])
    dxs = AR.alloc([128, 4, D], BF16)
    hxT = AR.alloc([128, 8, 512], BF16)
    u_t = AR.alloc([128, 4, 512], BF16)
    xc_t = [AR.alloc([128, 512]) for _ in range(2)]
    gg_t = AR.alloc([128, 8, 512], BF16)
    gfr_t = AR.alloc([128, 8, 512], BF16)
    tA = [AR.alloc([128, 512]) for _ in range(2)]
    tB = [AR.alloc([128, 512]) for _ in range(2)]
    x_v = x.rearrange("(t s p) d -> t p s d", p=128, s=4)
    u_v = u_d.rearrange("(t s p) n -> t p s n", p=128, s=4)
    xc_v = xc_d.rearrange("(h p) t -> p h t", p=128)
    gg_v = gg_d.rearrange("(h p) t -> p h t", p=128)
    gfr_v = gfr_d.rearrange("(h p) t -> p h t", p=128)
    P.dma(LQ, lambda e: e.dma_start(out=xt[0], in_=x_v[0]), writes=["xt0"])
    for t in range(NT):
        bi = t % 2
        if t + 1 < NT:
            P.dma(LQ, lambda e, t=t: e.dma_start(out=xt[(t + 1) % 2], in_=x_v[t + 1]), writes=["xt%d" % ((t + 1) % 2)])
        rms_rows(xt[bi], 4, dssq, drstd, djunk, "xt%d" % bi, "d_")
        norm_transpose(xt[bi], 4, drstd, dxs, hxT, gm1T, sh1T, "xt%d" % bi, "d_", "hxT")
        for s_ in range(4):
            ps, pk = next_ps()

            def f(e, ps=ps, s_=s_):
                for k in range(8):
                    ins = e.matmul(ps, hxT[:, k, s_ * 128:(s_ + 1) * 128], win_b[:, k, 0:512], start=(k == 0), stop=(k == 7))
                return ins
            P.pe(f, reads=["hxT", "win_b"], writes=[pk])
            P.act(lambda e, ps=ps, s_=s_: e.copy(u_t[:, s_, :], ps), reads=[pk], writes=["u_t"])
        P.dma(SQ, lambda e, t=t: e.dma_start(out=u_v[t], in_=u_t), reads=["u_t"], writes=["u_d"])
        for cc in range(4, 36):
            ps, pk = next_ps()

            def f(e, ps=ps, cc=cc):
                for k in range(8):
                    ins = e.matmul(ps, win_b[:, k, cc * 128:(cc + 1) * 128], hxT[:, k, :], start=(k == 0), stop=(k == 7))
                return ins
            P.pe(f, reads=["hxT", "win_b"], writes=[pk])
            if cc < 12:
                h = cc - 4
                ob = xc_t[h % 2]; ok = "xc_t%d" % (h % 2)
                conv_from_psum(ps, ob, h, 512, 64, pk, ok)
                P.dma(SQ, lambda e, ob=ob, h=h, t=t: e.dma_start(out=xc_v[:, h, t * 512:(t + 1) * 512], in_=ob), reads=[ok], writes=["xc_d"])
            elif cc < 20:
                h = cc - 12
                a_ = tA[h % 2]; b_ = tB[h % 2]; ka = "tA%d" % (h % 2); kb = "tB%d" % (h % 2)
                P.act(lambda e, ps=ps, a_=a_: e.activation(out=a_, in_=ps, func=AF.Square), reads=[pk], writes=[ka])
                P.dve(lambda e, a_=a_: e.tensor_scalar(a_, a_, 0.044715, 1.0, ALU.mult, ALU.add), reads=[ka], writes=[ka])
                P.dve(lambda e, ps=ps, a_=a_: e.tensor_tensor(a_, a_, ps, ALU.mult), reads=[ka, pk], writes=[ka])
                P.act(lambda e, a_=a_, b_=b_: e.activation(out=b_, in_=a_, func=AF.Exp, scale=-1.5957691216057308), reads=[ka], writes=[kb])
                P.dve(lambda e, b_=b_: e.tensor_scalar_add(b_, b_, 1.0), reads=[kb], writes=[kb])
                P.dve(lambda e, b_=b_: e.reciprocal(b_, b_), reads=[kb], writes=[kb])
                P.dve(lambda e, ps=ps, b_=b_, h=h: e.tensor_tensor(gg_t[:, h, :], b_, ps, ALU.mult), reads=[kb, pk], writes=["gg_t"])
            else:
                h = cc - 20
                b_ = tB[h % 2]; kb = "tB%d" % (h % 2)
                P.act(lambda e, ps=ps, b_=b_: e.activation(out=b_, in_=ps, func=AF.Exp, scale=-1.0), reads=[pk], writes=[kb])
                P.dve(lambda e, b_=b_, h=h: e.tensor_scalar_add(b_, b_, 1.0), reads=[kb], writes=[kb])
                P.dve(lambda e, b_=b_, h=h: e.reciprocal(gfr_t[:, h % 8, :], b_), reads=[kb], writes=["gfr_t"])
                if h % 8 == 7:
                    P.dma(SQ, lambda e, t=t, h=h: e.dma_start(out=gfr_v[:, (h // 8) * 8:(h // 8) * 8 + 8, t * 512:(t + 1) * 512], in_=gfr_t), reads=["gfr_t"], writes=["gfr_d"])
        P.dma(SQ, lambda e, t=t: e.dma_start(out=gg_v[:, :, t * 512:(t + 1) * 512], in_=gg_t), reads=["gg_t"], writes=["gg_d"])
    P.barrier()
    AR.release()
    AR.release()
    if stop_after == "D":
        return finish()

    AR.mark()
    xcf = AR.alloc([128, S]); xcb = AR.alloc([128, S], BF16); hf = AR.alloc([128, S])
    CH = 1024
    NCH = S // CH
    rb = [dict(e1=AR.alloc([128, CH]), a=AR.alloc([128, CH]), e2=AR.alloc([128, CH]), s=AR.alloc([128, CH]), b=AR.alloc([128, CH])) for _ in range(2)]
    hb = [AR.alloc([128, CH]) for _ in range(2)]
    ggc = [AR.alloc([128, CH], BF16) for _ in range(2)]
    ygc = [AR.alloc([128, CH], BF16) for _ in range(2)]
    yg_v = yg_d.rearrange("(h p) t -> p h t", p=128)
    for h in range(8):
        P.dma(LQ, lambda e, h=h: e.dma_start(out=xcf, in_=xc_v[:, h, :]), reads=["xc_d"], writes=["xcf"])
        P.act(lambda e: e.copy(xcb[:, 0:S // 2], xcf[:, 0:S // 2]), reads=["xcf"], writes=["xcfb"])
        P.dve(lambda e: e.tensor_copy(xcb[:, S // 2:S], xcf[:, S // 2:S]), reads=["xcf", "xcfb"], writes=["xcfb"])
        it = 0
        for d in range(2):
            order = list(range(NCH)) if d == 0 else list(range(NCH - 1, -1, -1))
            prev = None
            for ci in order:
                bi = it % 2
                it += 1
                sl = slice(ci * CH, (ci + 1) * CH)
                kp = "r%d_" % bi
                rnn_chunk(xcf[:, sl], xcb[:, sl], d, h, CH, rb[bi], "xcf", kp)
                dh = d * 8 + h
                if d == 0:
                    init = h0T[:, dh:dh + 1] if prev is None else hf[:, ci * CH - 1:ci * CH]
                    P.dve(lambda e, bi=bi, sl=sl, init=init: e.tensor_tensor_scan(hf[:, sl], rb[bi]["a"], rb[bi]["b"], init, ALU.mult, ALU.add), reads=[kp + "a", kp + "b", "hf", "h0T"], writes=["hf"])
                else:
                    if prev is None:
                        init = h0T[:, dh:dh + 1]; kinit = "h0T"
                    else:
                        init = hb[prev][:, 0:1]; kinit = "hb%d" % prev
                    P.dma(LQ, lambda e, bi=bi, sl=sl, h=h: e.dma_start(out=ggc[bi], in_=gg_v[:, h, sl]), reads=["gg_d"], writes=["ggc%d" % bi])
                    P.dve(lambda e, bi=bi, init=init: e.tensor_tensor_scan(hb[bi][:, ::-1], rb[bi]["a"][:, ::-1], rb[bi]["b"][:, ::-1], init, ALU.mult, ALU.add), reads=[kp + "a", kp + "b", kinit], writes=["hb%d" % bi])
                    P.dve(lambda e, bi=bi, sl=sl: e.tensor_tensor(rb[bi]["s"], hb[bi], hf[:, sl], ALU.add), reads=["hb%d" % bi, "hf", kp + "s"], writes=[kp + "s"])
                    P.dve(lambda e, bi=bi: e.tensor_tensor(ygc[bi], rb[bi]["s"], ggc[bi], ALU.mult), reads=[kp + "s", "ggc%d" % bi], writes=["ygc%d" % bi])
                    P.dma(SQ, lambda e, bi=bi, sl=sl, h=h: e.dma_start(out=yg_v[:, h, sl], in_=ygc[bi]), reads=["ygc%d" % bi], writes=["yg_d"])
                    prev = bi
                if d == 0:
                    prev = bi
    P.barrier()
    AR.release()
    AR.release()
    if stop_after == "E":
        return finish()

    AR.mark()
    c1b = AR.alloc([128, 128], BF16); s1b = AR.alloc([128, 128], BF16); t3b = AR.alloc([128, 128], BF16)
    twc = AR.alloc([128, 64]); tws = AR.alloc([128, 64])
    ftmp = AR.alloc([128, 128])
    for (src, dst, kk) in ((k_c128, c1b, "c1b"), (k_s128, s1b, "s1b"), (k_t3, t3b, "t3b")):
        P.dma(LQ, lambda e, src=src: e.dma_start(out=ftmp, in_=src), writes=["ftmp"])
        P.dve(lambda e, dst=dst: e.tensor_copy(dst, ftmp), reads=["ftmp"], writes=[kk])
    P.dma(LQ, lambda e: e.dma_start(out=twc, in_=k_twc), writes=["twc"])
    P.dma(LQ, lambda e: e.dma_start(out=tws, in_=k_tws), writes=["tws"])
    AR.mark()
    U = AR.alloc([128, 64, 512], BF16)
    qt = [AR.alloc([128, 2, 512], BF16) for _ in range(2)]
    f1 = [AR.alloc([128, 512]) for _ in range(2)]
    f2 = [AR.alloc([128, 512]) for _ in range(2)]
    u_pv = u_d.rearrange("(p n) c -> p n c", n=64)
    for uq in range(4):
        P.dma(LQ, lambda e, uq=uq: e.dma_start(out=U[:, uq * 16:(uq + 1) * 16, :], in_=u_pv[:, uq * 16:(uq + 1) * 16, :]), reads=["u_d"], writes=["U"])
    FDBG = int(os.environ.get("FDBG", "0"))
    for n2 in range(64 if FDBG != 2 else 0):
        bi = n2 % 2
        psr, kr = next_ps()
        psi, ki = next_ps()
        P.pe(lambda e, psr=psr, n2=n2: e.matmul(psr, c1b, U[:, n2, :], start=True, stop=True), reads=["U", "c1b"], writes=[kr])
        P.pe(lambda e, psi=psi, n2=n2: e.matmul(psi, s1b, U[:, n2, :], start=True, stop=True), reads=["U", "s1b"], writes=[ki])
        P.dve(lambda e, psi=psi, n2=n2, bi=bi: e.tensor_scalar_mul(f1[bi], psi, tws[:, n2:n2 + 1]), reads=[ki, "tws"], writes=["f1%d" % bi])
        P.dve(lambda e, psr=psr, n2=n2, bi=bi: e.tensor_scalar_mul(f2[bi], psr, tws[:, n2:n2 + 1]), reads=[kr, "tws"], writes=["f2%d" % bi])
        P.dve(lambda e, psr=psr, n2=n2, bi=bi: e.scalar_tensor_tensor(qt[bi][:, 0, :], psr, twc[:, n2:n2 + 1], f1[bi], ALU.mult, ALU.subtract), reads=[kr, "twc", "f1%d" % bi], writes=["qt%d" % bi])
        P.dve(lambda e, psi=psi, n2=n2, bi=bi: e.scalar_tensor_tensor(qt[bi][:, 1, :], psi, twc[:, n2:n2 + 1], f2[bi], ALU.mult, ALU.add), reads=[ki, "twc", "f2%d" % bi, "qt%d" % bi], writes=["qt%d" % bi])
        for r in range(2 if FDBG != 1 else 0):
            P.dma(SQ, lambda e, n2=n2, bi=bi, r=r: e.dma_start(out=q_d[:, r, n2, :, :].rearrange("g k c -> k g c"), in_=qt[bi][:, r, :].rearrange("k (g c) -> k g c", g=4)), reads=["qt%d" % bi], writes=["q_d"])
    P.barrier()
    AR.release()
    if stop_after == "F1":
        return finish()
    AR.mark()
    Qg = [AR.alloc([128, 128, 128], BF16) for _ in range(2)]
    RT = [AR.alloc([128, 2, S], BF16) for _ in range(2)]
    rt_v = rt_d.rearrange("(g r j) t -> g j r t", r=2, j=128)
    for g in range(4):
        bi = g % 2
        for r in range(2):
            P.dma(LQ, lambda e, g=g, r=r, bi=bi: e.dma_start(out=Qg[bi][r * 64:(r + 1) * 64, :, :], in_=q_d[g, r]), reads=["q_d"], writes=["Qg%d" % bi])
        for k0 in range(0, 128, 4):
            ps, pk = next_ps()

            def f(e, ps=ps, k0=k0, bi=bi):
                for kk in range(4):
                    ins = e.matmul(ps[:, kk * 128:(kk + 1) * 128], Qg[bi][:, k0 + kk, :], t3b, start=True, stop=True)
                return ins
            P.pe(f, reads=["Qg%d" % bi, "t3b"], writes=[pk])
            psv = ps.rearrange("j (k r n) -> j r k n", k=4, r=2)
            for r in range(2):
                ov = RT[bi][:, r, :].rearrange("j (n k) -> j k n", k=128)[:, k0:k0 + 4, :]
                if r == 0:
                    P.act(lambda e, ov=ov, psv=psv, r=r: e.copy(ov, psv[:, r, :, :]), reads=[pk], writes=["RT%d" % bi])
                else:
                    P.dve(lambda e, ov=ov, psv=psv, r=r: e.tensor_copy(ov, psv[:, r, :, :]), reads=[pk], writes=["RT%d" % bi])
        P.dma(SQ, lambda e, g=g, bi=bi: e.dma_start(out=rt_v[g], in_=RT[bi]), reads=["RT%d" % bi], writes=["rt_d"])
    P.barrier()
    AR.release()
    AR.release()
    if stop_after == "F":
        return finish()

    AR.mark()
    wfp = AR.alloc([128, 8, D], BF16)
    wr_b = AR.alloc([128, 8, D], BF16)
    wo_b = AR.alloc([128, 8, D], BF16)
    wrt_f = AR.alloc([128, 8, NE])
    brt = AR.alloc([1, NE])
    AR.mark()
    gstg = AR.alloc([128, 8, D])
    cdb = AR.alloc([128, 128], BF16); sdb = AR.alloc([128, 128], BF16)
    wf_b = AR.alloc([128, 4, D], BF16)
    P.dma(LQ, lambda e: e.dma_start(out=gstg[:, 0, 0:128], in_=k_c128), writes=["gstg"])
    P.dve(lambda e: e.tensor_copy(cdb, gstg[:, 0, 0:128]), reads=["gstg"], writes=["cdb"])
    P.dma(LQ, lambda e: e.dma_start(out=gstg[:, 0, 0:128], in_=k_s128), reads=["gstg"], writes=["gstg"])
    P.dve(lambda e: e.tensor_scalar_mul(sdb, gstg[:, 0, 0:128], -1.0), reads=["gstg"], writes=["sdb"])
    P.dma(LQ, lambda e: e.dma_start(out=gstg[:, 0:4, :], in_=w_fourier.rearrange("(g m) n -> m g n", m=128)), reads=["gstg"], writes=["gstg"])
    P.dve(lambda e: e.tensor_copy(wf_b, gstg[:, 0:4, :]), reads=["gstg"], writes=["wf_b"])
    for g in range(4):
        for ri, mat, mk in ((0, cdb, "cdb"), (1, sdb, "sdb")):
            for half in range(2):
                ps, pk = next_ps()
                P.pe(lambda e, ps=ps, mat=mat, g=g, half=half: e.matmul(ps, mat, wf_b[:, g, half * 512:(half + 1) * 512], start=True, stop=True), reads=[mk, "wf_b"], writes=[pk])
                P.act(lambda e, ps=ps, g=g, ri=ri, half=half: e.copy(wfp[:, g * 2 + ri, half * 512:(half + 1) * 512], ps), reads=[pk], writes=["wfp"])
    P.dma(LQ, lambda e: e.dma_start(out=gstg, in_=w_rnn.rearrange("(k p) n -> p k n", p=128)), reads=["gstg"], writes=["gstg"])
    P.dve(lambda e: e.tensor_copy(wr_b, gstg), reads=["gstg"], writes=["wr_b"])
    P.dma(LQ, lambda e: e.dma_start(out=gstg, in_=w_out.rearrange("(k p) n -> p k n", p=128)), reads=["gstg"], writes=["gstg"])
    for k in range(8):
        P.dve(lambda e, k=k: e.tensor_tensor(wo_b[:, k, :], gstg[:, k, :], g1_bc, ALU.mult), reads=["gstg", "g1_bc"], writes=["wo_b"])
    P.dma(LQ, lambda e: e.dma_start(out=wrt_f, in_=w_router.rearrange("(k p) n -> p k n", p=128)), writes=["wrt_f"])
    P.dma(LQ, lambda e: e.dma_start(out=brt, in_=b_router), writes=["brt"])
    P.barrier()
    AR.release()
    rtt = [AR.alloc([128, 8, 512], BF16) for _ in range(2)]
    ygt = [AR.alloc([128, 8, 512], BF16) for _ in range(2)]
    gft = [AR.alloc([128, 16, 512], BF16)] * 2
    xg_ = [AR.alloc([128, 4, D]) for _ in range(2)]
    mT = AR.alloc([128, 8, 512], BF16)
    g1t = [AR.alloc([128, 512]) for _ in range(2)]
    g2t = [AR.alloc([128, 512]) for _ in range(2)]
    h2f = AR.alloc([128, D])
    h2b = AR.alloc([128, 4, D], BF16)
    h2T = AR.alloc([128, 8, 128])
    gjunk = AR.alloc([128, D], BF16); gssq = AR.alloc([128, 4]); grstd = AR.alloc([128, 4])
    rt_tv = rt_d.rearrange("(c j) t -> j c t", j=128)
    x1_v = x1_d.rearrange("(t s p) d -> t p s d", p=128, s=4)
    h2_v = h2_d.rearrange("(t s p) d -> t p s d", p=128, s=4)

    def g_load(t):
        bi = t % 2
        sl = slice(t * 512, (t + 1) * 512)
        P.dma(LQ, lambda e: e.dma_start(out=rtt[bi], in_=rt_tv[:, :, sl]), reads=["rt_d"], writes=["rtt%d" % bi])
        P.dma(LQ, lambda e: e.dma_start(out=ygt[bi], in_=yg_v[:, :, sl]), reads=["yg_d"], writes=["ygt%d" % bi])
        P.dma(LQ, lambda e: e.dma_start(out=xg_[bi], in_=x_v[t]), writes=["xg_%d" % bi])
    def gft_load(t):
        P.dma(LQ, lambda e: e.dma_start(out=gft[0], in_=gfr_v[:, :, t * 512:(t + 1) * 512]), reads=["gfr_d"], writes=["gft0"])
    g_load(0)
    gft_load(0)
    for t in range(NT):
        bi = t % 2
        if t + 1 < NT:
            g_load(t + 1)
        x1t = xg_[bi]
        kx1 = "xg_%d" % bi
        for n in range(8):
            psF, kF = next_ps()
            psR, kR = next_ps()

            def fF(e, psF=psF, n=n, bi=bi):
                for k in range(8):
                    ins = e.matmul(psF, wfp[:, k, n * 128:(n + 1) * 128], rtt[bi][:, k, :], start=(k == 0), stop=(k == 7))
                return ins

            def fR(e, psR=psR, n=n, bi=bi):
                for k in range(8):
                    ins = e.matmul(psR, wr_b[:, k, n * 128:(n + 1) * 128], ygt[bi][:, k, :], start=(k == 0), stop=(k == 7))
                return ins
            P.pe(fF, reads=["wfp", "rtt%d" % bi], writes=[kF])
            P.pe(fR, reads=["wr_b", "ygt%d" % bi], writes=[kR])
            a_ = g1t[n % 2]; b_ = g2t[n % 2]; ka = "g1t%d" % (n % 2); kb = "g2t%d" % (n % 2)
            P.dve(lambda e, psF=psF, a_=a_, n=n, bi=bi: e.tensor_tensor(a_, psF, gft[bi][:, n, :], ALU.mult), reads=[kF, "gft0"], writes=[ka])
            P.dve(lambda e, psR=psR, b_=b_, n=n, bi=bi: e.tensor_tensor(b_, psR, gft[bi][:, 8 + n, :], ALU.mult), reads=[kR, "gft0"], writes=[kb])
            P.dve(lambda e, a_=a_, b_=b_, n=n: e.tensor_tensor(mT[:, n, :], a_, b_, ALU.add), reads=[ka, kb], writes=["mT"])
        if t + 1 < NT:
            gft_load(t + 1)
        for s_ in range(4):
            for half in range(2):
                ps, pk = next_ps()

                def fO(e, ps=ps, s_=s_, half=half):
                    for k in range(8):
                        ins = e.matmul(ps, mT[:, k, s_ * 128:(s_ + 1) * 128], wo_b[:, k, half * 512:(half + 1) * 512], start=(k == 0), stop=(k == 7))
                    return ins
                P.pe(fO, reads=["mT", "wo_b"], writes=[pk])
                P.dve(lambda e, ps=ps, s_=s_, half=half, bi=bi: e.tensor_tensor(xg_[bi][:, s_, half * 512:(half + 1) * 512], ps, xg_[bi][:, s_, half * 512:(half + 1) * 512], ALU.add), reads=[pk, kx1], writes=[kx1])
        P.dma(SQ, lambda e, t=t, x1t=x1t: e.dma_start(out=x1_v[t], in_=x1t), reads=[kx1], writes=["x1_d"])
        rms_rows(x1t, 4, gssq, grstd, gjunk, kx1, "g_")
        for s_ in range(4):
            ti = t * 4 + s_
            P.dve(lambda e, s_=s_, x1t=x1t: e.scalar_tensor_tensor(h2f, x1t[:, s_, :], grstd[:, s_:s_ + 1], gm2_bc, ALU.mult, ALU.mult), reads=[kx1, "g_rstd", "gm2_bc"], writes=["h2f"])
            P.dve(lambda e: e.tensor_tensor(h2f, h2f, sh2_bc, ALU.add), reads=["h2f", "sh2_bc"], writes=["h2f"])
            P.act(lambda e, s_=s_: e.copy(h2b[:, s_, :], h2f), reads=["h2f"], writes=["h2b"])
            for q in range(2):
                ps, pk = next_ps()

                def fT(e, ps=ps, q=q):
                    for kk in range(4):
                        kc = q * 4 + kk
                        ins = e.transpose(ps[:, kk * 128:(kk + 1) * 128], h2f[:, kc * 128:(kc + 1) * 128], ident_f)
                    return ins
                P.pe(fT, reads=["h2f", "ident_f"], writes=[pk])
                if q == 0:
                    P.act(lambda e, ps=ps, q=q: e.copy(h2T[:, q * 4:(q + 1) * 4, :], ps.rearrange("p (a b) -> p a b", a=4)), reads=[pk], writes=["h2T"])
                else:
                    P.dve(lambda e, ps=ps, q=q: e.tensor_copy(h2T[:, q * 4:(q + 1) * 4, :], ps.rearrange("p (a b) -> p a b", a=4)), reads=[pk], writes=["h2T"])
            ps, pk = next_ps()

            def fL(e, ps=ps):
                for k in range(8):
                    e.matmul(ps[:, 0:NE], h2T[:, k, :], wrt_f[:, k, :], start=(k == 0), stop=False)
                return e.matmul(ps[:, 0:NE], ones_f[0:1, :], brt[0:1, :], start=False, stop=True)
            P.pe(fL, reads=["h2T", "wrt_f", "brt", "ones_f"], writes=[pk])
            P.act(lambda e, ps=ps, ti=ti: e.copy(Lg[:, ti, :], ps[:, 0:NE]), reads=[pk], writes=["Lg"])
        P.dma(SQ, lambda e, t=t: e.dma_start(out=h2_v[t], in_=h2b), reads=["h2b"], writes=["h2_d"])
    P.barrier()
    AR.release()
    if stop_after == "G":
        return finish()

    AR.mark()
    NTI = 64
    m8 = AR.alloc([128, NTI, 8]); i8 = AR.alloc([128, NTI, 8], U32); i8f = AR.alloc([128, NTI, 8])
    iota_e = AR.alloc([128, NE]); iota_i = AR.alloc([128, NE], I32)
    oh = [AR.alloc([128, NTI, NE]) for _ in range(4)]
    msk = AR.alloc([128, NTI, NE]); ex = AR.alloc([128, NTI, NE]); den = AR.alloc([128, NTI]); nmx = AR.alloc([128, NTI])
    cntp = AR.alloc([128, NE]); cntp_b = AR.alloc([128, NE], BF16)
    base = AR.alloc([128, NE]); tot = AR.alloc([128, NE]); pad = AR.alloc([128, NE]); ends = AR.alloc([128, NE]); starts = AR.alloc([128, NE])
    pref = AR.alloc([128, NTI, NE]); dst = AR.alloc([128, NTI, NE]); tmp3 = AR.alloc([128, NTI, NE])
    dk = AR.alloc([128, NTI, 4]); ones_e = AR.alloc([128, NTI])
    bthr = AR.alloc([128, NBLK]); bthr_i = AR.alloc([128, NBLK], I32); cmp = AR.alloc([128, NBLK, NE]); ebf = AR.alloc([128, NBLK])
    P.pool(lambda e: e.iota(iota_i, pattern=[[1, NE]], base=0, channel_multiplier=0), writes=["iota_i"])
    P.dve(lambda e: e.tensor_copy(iota_e, iota_i), reads=["iota_i"], writes=["iota_e"])
    P.pool(lambda e: e.iota(bthr_i, pattern=[[BLK, NBLK]], base=0, channel_multiplier=0), writes=["bthr_i"])
    P.dve(lambda e: e.tensor_copy(bthr, bthr_i), reads=["bthr_i"], writes=["bthr"])
    P.pool(lambda e: e.memset(ones_e, 1.0), writes=["ones_e"])
    for ti in range(NTI):
        P.dve(lambda e, ti=ti: e.max(m8[:, ti, :], Lg[:, ti, :]), reads=["Lg"], writes=["m8"])
        P.dve(lambda e, ti=ti: e.max_index(i8[:, ti, :], m8[:, ti, :], Lg[:, ti, :]), reads=["Lg", "m8"], writes=["i8"])
    P.dve(lambda e: e.tensor_copy(i8f, i8), reads=["i8"], writes=["i8f"])
    for k in range(4):
        P.dve(lambda e, k=k: e.tensor_tensor(oh[k], iota_e.unsqueeze(1).to_broadcast([128, NTI, NE]), i8f[:, :, k:k + 1].to_broadcast([128, NTI, NE]), ALU.is_equal), reads=["iota_e", "i8f"], writes=["oh%d" % k])
    P.dve(lambda e: e.tensor_tensor(msk, oh[0], oh[1], ALU.add), reads=["oh0", "oh1"], writes=["msk"])
    P.dve(lambda e: e.tensor_tensor(msk, msk, oh[2], ALU.add), reads=["msk", "oh2"], writes=["msk"])
    P.dve(lambda e: e.tensor_tensor(msk, msk, oh[3], ALU.add), reads=["msk", "oh3"], writes=["msk"])
    P.dve(lambda e: e.tensor_tensor(ex, Lg, m8[:, :, 0:1].to_broadcast([128, NTI, NE]), ALU.subtract), reads=["Lg", "m8"], writes=["ex"])
    P.act(lambda e: e.activation(out=ex, in_=ex, func=AF.Exp), reads=["ex"], writes=["ex"])
    P.dve(lambda e: e.tensor_tensor(ex, ex, msk, ALU.mult), reads=["ex", "msk"], writes=["ex"])
    P.dve(lambda e: e.tensor_reduce(den, ex, AX.X, ALU.add), reads=["ex"], writes=["den"])
    P.dve(lambda e: e.reciprocal(den, den), reads=["den"], writes=["den"])
    P.dve(lambda e: e.tensor_tensor(ex, ex, den.unsqueeze(2).to_broadcast([128, NTI, NE]), ALU.mult), reads=["ex", "den"], writes=["ex"])
    P.dve(lambda e: e.tensor_reduce(cntp, msk.rearrange("p t e -> p e t"), AX.X, ALU.add), reads=["msk"], writes=["cntp"])
    P.dve(lambda e: e.tensor_copy(cntp_b, cntp), reads=["cntp"], writes=["cntp_b"])
    psb_, kb_ = next_ps()
    P.pe(lambda e: e.matmul(psb_[:, 0:NE], ltri_b, cntp_b, start=True, stop=True), reads=["ltri_b", "cntp_b"], writes=[kb_])
    P.dve(lambda e: e.tensor_copy(base, psb_[:, 0:NE]), reads=[kb_], writes=["base"])
    pst_, kt_ = next_ps()
    P.pe(lambda e: e.matmul(pst_[:, 0:NE], ones_b[:, 0:128], cntp_b, start=True, stop=True), reads=["ones_b", "cntp_b"], writes=[kt_])
    P.dve(lambda e: e.tensor_copy(tot, pst_[:, 0:NE]), reads=[kt_], writes=["tot"])
    P.dve(lambda e: e.tensor_scalar(pad, tot, float(BLK - 1), 1.0 / BLK, ALU.add, ALU.mult), reads=["tot"], writes=["pad"])
    P.dve(lambda e: e.tensor_scalar_add(pad, pad, -0.4990234375), reads=["pad"], writes=["pad"])
    P.dve(lambda e: e.tensor_scalar_add(pad, pad, 8388608.0), reads=["pad"], writes=["pad"])
    P.dve(lambda e: e.tensor_scalar_add(pad, pad, -8388608.0), reads=["pad"], writes=["pad"])
    P.dve(lambda e: e.tensor_scalar_mul(pad, pad, float(BLK)), reads=["pad"], writes=["pad"])
    P.dve(lambda e: e.tensor_tensor_scan(ends, ones_e[:, 0:NE], pad, 0.0, ALU.mult, ALU.add), reads=["pad", "ones_e"], writes=["ends"])
    P.dve(lambda e: e.tensor_tensor(starts, ends, pad, ALU.subtract), reads=["ends", "pad"], writes=["starts"])
    P.dve(lambda e: e.tensor_tensor(base, base, starts, ALU.add), reads=["base", "starts"], writes=["base"])
    for ee in range(NE):
        P.dve(lambda e, ee=ee: e.tensor_tensor_scan(pref[:, :, ee], ones_e, msk[:, :, ee], 0.0, ALU.mult, ALU.add), reads=["msk", "ones_e"], writes=["pref"])
    P.dve(lambda e: e.tensor_tensor(pref, pref, msk, ALU.subtract), reads=["pref", "msk"], writes=["pref"])
    P.dve(lambda e: e.tensor_tensor(dst, pref, base.unsqueeze(1).to_broadcast([128, NTI, NE]), ALU.add), reads=["pref", "base"], writes=["dst"])
    for k in range(4):
        P.dve(lambda e, k=k: e.tensor_tensor(tmp3, oh[k], dst, ALU.mult), reads=["oh%d" % k, "dst"], writes=["tmp3"])
        P.dve(lambda e, k=k: e.tensor_reduce(dk[:, :, k], tmp3, AX.X, ALU.add), reads=["tmp3"], writes=["dk"])
        P.dve(lambda e, k=k: e.tensor_tensor(tmp3, oh[k], ex, ALU.mult), reads=["oh%d" % k, "ex", "tmp3"], writes=["tmp3"])
        P.dve(lambda e, k=k: e.tensor_reduce(wk[:, :, k], tmp3, AX.X, ALU.add), reads=["tmp3"], writes=["wk"])
    P.dve(lambda e: e.tensor_copy(dest_i, dk), reads=["dk"], writes=["dest_i"])
    P.dve(lambda e: e.tensor_tensor(cmp, ends.unsqueeze(1).to_broadcast([128, NBLK, NE]), bthr.unsqueeze(2).to_broadcast([128, NBLK, NE]), ALU.is_le), reads=["ends", "bthr"], writes=["cmp"])
    P.dve(lambda e: e.tensor_reduce(ebf, cmp, AX.X, ALU.add), reads=["cmp"], writes=["ebf"])
    P.dve(lambda e: e.tensor_scalar_min(ebf, ebf, float(NE - 1)), reads=["ebf"], writes=["ebf"])
    P.dve(lambda e: e.tensor_copy(eb_i, ebf), reads=["ebf"], writes=["eb_i"])
    pidx_i = AR.alloc([128, 8], I32); pidx = AR.alloc([128, 8]); widx_f = AR.alloc([128, NBLK, 8])
    P.pool(lambda e: e.iota(pidx_i, pattern=[[128, 8]], base=0, channel_multiplier=1), writes=["pidx_i"])
    P.dve(lambda e: e.tensor_copy(pidx, pidx_i), reads=["pidx_i"], writes=["pidx"])
    P.dve(lambda e: e.tensor_scalar_mul(ebf, ebf, 1024.0), reads=["ebf", "eb_i"], writes=["ebf"])
    P.dve(lambda e: e.tensor_tensor(widx_f, ebf.unsqueeze(2).to_broadcast([128, NBLK, 8]), pidx.unsqueeze(1).to_broadcast([128, NBLK, 8]), ALU.add), reads=["ebf", "pidx"], writes=["widx_f"])
    P.dve(lambda e: e.tensor_copy(widx, widx_f), reads=["widx_f"], writes=["widx"])
    AR.mark()
    hrow = [AR.alloc([128, D], BF16) for _ in range(2)]
    h2_r = h2_d.rearrange("(t p) d -> t p d", p=128)
    for ti in range(NTI):
        bi = ti % 2
        P.dma(LQ, lambda e, ti=ti, bi=bi: e.dma_start(out=hrow[bi], in_=h2_r[ti]), reads=["h2_d"], writes=["hrow%d" % bi])
        for k in range(4):
            P.dma("pool", lambda e, ti=ti, bi=bi, k=k: e.indirect_dma_start(out=xg_d, out_offset=bass.IndirectOffsetOnAxis(ap=dest_i[:, ti, k:k + 1], axis=0), in_=hrow[bi], in_offset=None), reads=["hrow%d" % bi, "dest_i"], writes=["xg_d"])
    P.barrier()
    AR.release()
    AR.release()
    if stop_after == "H":
        return finish()

    AR.release()
    AR.mark()
    NBIG = 3
    NSML = 4
    stgB = [AR.alloc([128, 2048]) for _ in range(NBIG)]
    stgS = [AR.alloc([128, 1024]) for _ in range(NSML)]
    wgu_b = [AR.alloc([128, 8, 2 * D], BF16) for _ in range(2)]
    wdn_b = AR.alloc([128, 8, D], BF16)
    bgb = [AR.alloc([1, 3 * D], BF16) for _ in range(2)]
    xrows = AR.alloc([128, 4, D], BF16)
    xT = [AR.alloc([128, 8, BLK], BF16) for _ in range(2)]
    aT = AR.alloc([128, 8, BLK], BF16)
    eg = [AR.alloc([128, BLK]) for _ in range(2)]
    es = [AR.alloc([128, BLK]) for _ in range(2)]
    eu = [AR.alloc([128, BLK]) for _ in range(2)]
    ysb = [AR.alloc([128, D]) for _ in range(2)]
    xg_v = xg_d.rearrange("(b s p) d -> b p s d", p=128, s=4)
    ys_v = ys_d.rearrange("(b s p) d -> b s p d", p=128, s=4)
    wgu_rows = w_gu.rearrange("e k n -> (e k) n")
    wdn_rows = w_down.rearrange("e k n -> (e k) n")
    rB = [0]
    rS = [0]

    def item(kind, b, kc=0):
        pb = b % 2
        if kind in ("wgu", "bgu"):
            si = rB[0] % NBIG
            rB[0] += 1
            stg = stgB[si]; sk = "stgB%d" % si; ncol = 2048
        else:
            si = rS[0] % NSML
            rS[0] += 1
            stg = stgS[si]; sk = "stgS%d" % si; ncol = 1024
        if kind == "wgu":
            src, idx, dst, dkey, p0 = wgu_rows, widx[:, b, kc:kc + 1], wgu_b[pb][:, kc, :], "wgu%d_%d" % (pb, kc), 128
        elif kind == "wdn":
            src, idx, dst, dkey, p0 = wdn_rows, widx[:, b, kc:kc + 1], wdn_b[:, kc, :], "wdn_%d" % kc, 128
        elif kind == "bgu":
            src, idx, dst, dkey, p0 = b_gu, eb_i[:, b:b + 1], bgb[pb][0:1, 0:2048], "bgb%d" % pb, 1
        else:
            src, idx, dst, dkey, p0 = b_down, eb_i[:, b:b + 1], bgb[pb][0:1, 2048:3072], "bgb%d" % pb, 1

        def g_emit():
            P.dma("pool", lambda e: e.indirect_dma_start(out=stg[:, 0:ncol], out_offset=None, in_=src, in_offset=bass.IndirectOffsetOnAxis(ap=idx, axis=0)), reads=["widx", "eb_i"], writes=[sk])

        def c_emit():
            P.act(lambda e: e.copy(dst, stg[0:p0, 0:ncol]), reads=[sk], writes=[dkey])
        return g_emit, c_emit

    for it_ in [item("bgu", 0), item("bdn", 0)] + [item("wgu", 0, kc) for kc in range(8)]:
        it_[0]()
        it_[1]()
    P.dma(LQ, lambda e: e.dma_start(out=xrows, in_=xg_v[0]), reads=["xg_d"], writes=["xrows"])
    for b in range(NBLK):
        pb = b % 2
        gath = [[] for _ in range(12)]
        cast = [[] for _ in range(12)]
        if b + 1 < NBLK:
            for kind in ("bgu", "bdn"):
                ge, ce = item(kind, b + 1)
                gath[0].append(ge); cast[1].append(ce)
        for kc in range(8):
            ge, ce = item("wdn", b, kc)
            gath[kc // 2].append(ge); cast[kc // 2 + 1].append(ce)
        if b + 1 < NBLK:
            for kc in range(8):
                ge, ce = item("wgu", b + 1, kc)
                gath[kc].append(ge); cast[kc + 2].append(ce)
        for kc in range(8):
            ps, pk = next_ps()
            psb = ps.bitcast(BF16)

            def fx(e, psb=psb, kc=kc):
                for s_ in range(4):
                    ins = e.transpose(psb[:, s_ * 128:(s_ + 1) * 128], xrows[:, s_, kc * 128:(kc + 1) * 128], ident_b)
                return ins
            P.pe(fx, reads=["xrows", "ident_b"], writes=[pk])
            if kc % 2 == 0:
                P.act(lambda e, psb=psb, kc=kc, pb=pb: e.copy(xT[pb][:, kc, :], psb[:, 0:BLK]), reads=[pk], writes=["xT%d" % pb])
            else:
                P.dve(lambda e, psb=psb, kc=kc, pb=pb: e.tensor_copy(xT[pb][:, kc, :], psb[:, 0:BLK]), reads=[pk], writes=["xT%d" % pb])
        if b + 1 < NBLK:
            P.dma(LQ, lambda e, b=b: e.dma_start(out=xrows, in_=xg_v[b + 1]), reads=["xg_d"], writes=["xrows"])
        wkeys = ["wgu%d_%d" % (pb, kc) for kc in range(8)]
        pend = None
        for cc in range(8):
            for fn in gath[cc]:
                fn()
            for fn in cast[cc]:
                fn()
            psg, kg = next_ps()
            psu, ku = next_ps()

            def fg(e, psg=psg, cc=cc, pb=pb):
                for k in range(8):
                    e.matmul(psg, wgu_b[pb][:, k, cc * 128:(cc + 1) * 128], xT[pb][:, k, :], start=(k == 0), stop=False)
                return e.matmul(psg, bgb[pb][0:1, cc * 128:(cc + 1) * 128], ones_b[0:1, 0:BLK], start=False, stop=True)

            def fu(e, psu=psu, cc=cc, pb=pb):
                for k in range(8):
                    e.matmul(psu, wgu_b[pb][:, k, D + cc * 128:D + (cc + 1) * 128], xT[pb][:, k, :], start=(k == 0), stop=False)
                return e.matmul(psu, bgb[pb][0:1, D + cc * 128:D + (cc + 1) * 128], ones_b[0:1, 0:BLK], start=False, stop=True)
            P.pe(fg, reads=wkeys + ["xT%d" % pb, "bgb%d" % pb, "ones_b"], writes=[kg])
            P.pe(fu, reads=wkeys + ["xT%d" % pb, "bgb%d" % pb, "ones_b"], writes=[ku])
            q = cc % 2
            P.dve(lambda e, psg=psg, q=q: e.tensor_scalar_min(eg[q], psg, 7.0), reads=[kg], writes=["eg%d" % q])
            P.act(lambda e, q=q: e.activation(out=es[q], in_=eg[q], func=AF.Sigmoid, scale=1.702), reads=["eg%d" % q], writes=["es%d" % q])
            P.dve(lambda e, psu=psu, q=q: e.tensor_scalar(eu[q], psu, 7.0, -7.0, ALU.min, ALU.max), reads=[ku], writes=["eu%d" % q])

            def tail(cc=cc, q=q):
                P.dve(lambda e: e.tensor_tensor(eg[q], eg[q], es[q], ALU.mult), reads=["eg%d" % q, "es%d" % q], writes=["eg%d" % q])
                P.dve(lambda e: e.scalar_tensor_tensor(aT[:, cc, :], eu[q], 1.0, eg[q], ALU.add, ALU.mult), reads=["eu%d" % q, "eg%d" % q], writes=["aT"])
            if pend is not None:
                pend()
            pend = tail
        pend()
        dkeys = ["wdn_%d" % kc for kc in range(8)]
        for s_ in range(4):
            for fn in gath[8 + s_]:
                fn()
            for fn in cast[8 + s_]:
                fn()
            yb = ysb[s_ % 2]; ky = "ysb%d" % (s_ % 2)
            for half in range(2):
                ps, pk = next_ps()

                def fd(e, ps=ps, s_=s_, half=half, pb=pb):
                    for k in range(8):
                        e.matmul(ps, aT[:, k, s_ * 128:(s_ + 1) * 128], wdn_b[:, k, half * 512:(half + 1) * 512], start=(k == 0), stop=False)
                    c0 = 2 * D + half * 512
                    return e.matmul(ps, ones_b[0:1, 0:128], bgb[pb][0:1, c0:c0 + 512], start=False, stop=True)
                P.pe(fd, reads=["aT", "bgb%d" % pb, "ones_b"] + dkeys, writes=[pk])
                if half == 0:
                    P.act(lambda e, ps=ps, yb=yb: e.copy(yb[:, 0:512], ps), reads=[pk], writes=[ky])
                else:
                    P.dve(lambda e, ps=ps, yb=yb: e.tensor_copy(yb[:, 512:1024], ps), reads=[pk], writes=[ky])
            P.dma(LQ, lambda e, b=b, s_=s_, yb=yb: e.dma_start(out=ys_v[b, s_], in_=yb), reads=[ky], writes=["ys_d"])
    P.barrier()
    AR.release()
    if stop_after == "I":
        return finish()

    AR.mark()
    yk = [[AR.alloc([128, D]) for _ in range(4)] for _ in range(2)]
    x1r = [AR.alloc([128, D]) for _ in range(2)]
    acc = AR.alloc([128, D]); outt = [AR.alloc([128, D]) for _ in range(2)]
    jjunk = AR.alloc([128, D]); jssq = AR.alloc([128, 64]); jrstd = AR.alloc([128, 64])
    x1_r = x1_d.rearrange("(t p) d -> t p d", p=128)
    y_r = y.rearrange("(t p) d -> t p d", p=128)

    def j_load(ti):
        bi = ti % 2
        for k in range(4):
            P.dma("pool", lambda e, k=k: e.indirect_dma_start(out=yk[bi][k], out_offset=None, in_=ys_d, in_offset=bass.IndirectOffsetOnAxis(ap=dest_i[:, ti, k:k + 1], axis=0)), reads=["ys_d", "dest_i"], writes=["yk%d_%d" % (bi, k)])
        P.dma(LQ, lambda e: e.dma_start(out=x1r[bi], in_=x1_r[ti]), reads=["x1_d"], writes=["x1r%d" % bi])
    j_load(0)
    for ti in range(NTI):
        bi = ti % 2
        if ti + 1 < NTI:
            j_load(ti + 1)
        P.dve(lambda e, bi=bi, ti=ti: e.tensor_scalar_mul(acc, yk[bi][0], wk[:, ti, 0:1]), reads=["yk%d_0" % bi, "wk"], writes=["acc"])
        for k in range(1, 4):
            P.dve(lambda e, bi=bi, ti=ti, k=k: e.scalar_tensor_tensor(acc, yk[bi][k], wk[:, ti, k:k + 1], acc, ALU.mult, ALU.add), reads=["yk%d_%d" % (bi, k), "wk", "acc"], writes=["acc"])
        P.pool(lambda e: e.tensor_tensor(acc, acc, g2_bc, ALU.mult), reads=["acc", "g2_bc"], writes=["acc"])
        P.pool(lambda e, bi=bi: e.tensor_tensor(acc, acc, x1r[bi], ALU.add), reads=["acc", "x1r%d" % bi], writes=["acc"])
        P.act(lambda e, ti=ti: e.activation(out=jjunk, in_=acc, func=AF.Square, accum_out=jssq[:, ti:ti + 1]), reads=["acc"], writes=["jjunk", "jssq"])
        P.dve(lambda e, ti=ti: e.tensor_scalar(jrstd[:, ti:ti + 1], jssq[:, ti:ti + 1], 1.0 / D, EPS, ALU.mult, ALU.add), reads=["jssq"], writes=["jrstd"])
        P.act(lambda e, ti=ti: e.activation(out=jrstd[:, ti:ti + 1], in_=jrstd[:, ti:ti + 1], func=AF.Ln), reads=["jrstd"], writes=["jrstd"])
        P.act(lambda e, ti=ti: e.activation(out=jrstd[:, ti:ti + 1], in_=jrstd[:, ti:ti + 1], func=AF.Exp, scale=-0.5), reads=["jrstd"], writes=["jrstd"])
        P.dve(lambda e, bi=bi, ti=ti: e.scalar_tensor_tensor(outt[bi], acc, jrstd[:, ti:ti + 1], nf_bc, ALU.mult, ALU.mult), reads=["acc", "jrstd", "nf_bc"], writes=["outt%d" % bi])
        P.dma(LQ, lambda e, bi=bi, ti=ti: e.dma_start(out=y_r[ti], in_=outt[bi]), reads=["outt%d" % bi], writes=["y"])
    AR.release()
    return finish()


_CACHE = {}


def kernel(**inputs):
    n = 8
    if "nc" not in _CACHE:
        _CACHE["nc"] = build_program()
    nc = _CACHE["nc"]
    tabs = dft_tables()
    f = np.float32

    def a(v):
        return np.ascontiguousarray(np.asarray(v, dtype=f))
    shared = dict(
        c_ctx=a(inputs["c_ctx"]).reshape(1, D), w_ada=a(inputs["w_ada"][0]), b_ada=a(inputs["b_ada"][0]).reshape(1, -1),
        norm1=a(inputs["norm1"][0]).reshape(1, D), w_in=a(inputs["w_in"][0]), conv_w=a(inputs["conv_w"][0]),
        conv_b=a(inputs["conv_b"][0]).reshape(1, D), gate_a_w=a(inputs["gate_a_w"][0]), gate_a_b=a(inputs["gate_a_b"][0]),
        gate_x_w=a(inputs["gate_x_w"][0]), gate_x_b=a(inputs["gate_x_b"][0]), lru_lambda=a(inputs["lru_lambda"][0]),
        w_fourier=a(inputs["w_fourier"][0]), w_rnn=a(inputs["w_rnn"][0]), w_out=a(inputs["w_out"][0]),
        norm2=a(inputs["norm2"][0]).reshape(1, D), w_router=a(inputs["w_router"][0]), b_router=a(inputs["b_router"][0]).reshape(1, NE),
        w_gu=a(inputs["w_gu"][0]), b_gu=a(inputs["b_gu"][0]), w_down=a(inputs["w_down"][0]), b_down=a(inputs["b_down"][0]),
        norm_f=a(inputs["norm_f"]).reshape(1, D), **tabs)
    xs = a(inputs["x"]); cs = a(inputs["c"]); cx = a(inputs["ctx"])
    in_maps = []
    for i in range(n):
        m = dict(shared)
        m["x"] = xs[i]; m["c"] = cs[i].reshape(1, D); m["ctx"] = cx[i]
        in_maps.append(m)
    ncore = int(os.environ.get("KDBG_NCORE", "8"))
    if ncore != 8:
        res = run_bass_kernel_spmd(nc, in_maps[:ncore], core_ids=list(range(ncore)))
        return np.stack([np.asarray(r["y"], dtype=f) for r in res.results], axis=0)
    res = run_bass_kernel_spmd(nc, in_maps, core_ids=list(range(n)))
    return np.stack([np.asarray(r["y"], dtype=f) for r in res.results], axis=0)
```

```python
import contextlib
import math
import os
import numpy as np
import concourse.bass as bass
import concourse.mybir as mybir
from concourse.bass_utils import run_bass_kernel_spmd

F32 = mybir.dt.float32
BF16 = mybir.dt.bfloat16
I32 = mybir.dt.int32
U32 = mybir.dt.uint32
ALU = mybir.AluOpType
AF = mybir.ActivationFunctionType
AX = mybir.AxisListType

SELF_SYNC = True
NDSEM = 6

D = 1024
S = 8192
CTX = 256
NE = 32
BLK = 512
NBLK = 96
PSLOTS = NBLK * BLK
EPS = 1e-6


class Prog:
    ENGS = ("pe", "act", "dve", "pool", "sp")

    def __init__(self, nc):
        self.nc = nc
        self.ops = []

    def add(self, eng, fn, reads=(), writes=(), dma=False):
        self.ops.append(dict(eng=eng, fn=fn, reads=tuple(reads), writes=tuple(writes), dma=dma, bar=False))

    def pe(self, fn, reads=(), writes=()):
        self.add("pe", fn, reads, writes)

    def act(self, fn, reads=(), writes=()):
        self.add("act", fn, reads, writes)

    def dve(self, fn, reads=(), writes=()):
        self.add("dve", fn, reads, writes)

    def pool(self, fn, reads=(), writes=()):
        self.add("pool", fn, reads, writes)

    def dma(self, q, fn, reads=(), writes=()):
        self.add(q, fn, reads, writes, dma=True)

    def barrier(self):
        for e in self.ENGS:
            self.ops.append(dict(eng=e, fn=None, reads=(), writes=(), dma=False, bar=True))

    def emit(self):
        nc = self.nc
        ops = self.ops
        cnt = {e: 0 for e in self.ENGS}
        dcnt = {e: 0 for e in self.ENGS}
        latest = {}
        for op in ops:
            e = op["eng"]
            if op["bar"]:
                op["barvals"] = dict(latest)
                continue
            if op["dma"]:
                i = dcnt[e]
                dcnt[e] += 1
                op["sem"] = ("d", e, i % NDSEM)
                op["val"] = 16 * (i // NDSEM + 1)
            else:
                cnt[e] += 1
                op["sem"] = ("c", e)
                op["val"] = cnt[e]
            latest[op["sem"]] = op["val"]
        last_w = {}
        readers = {}
        for op in ops:
            if op["bar"]:
                continue
            deps = []
            for k in op["reads"]:
                if k in last_w:
                    deps.append(last_w[k])
            for k in op["writes"]:
                if k in last_w:
                    deps.append(last_w[k])
                deps.extend(readers.get(k, ()))
            op["deps"] = deps
            for k in op["writes"]:
                last_w[k] = op
                readers[k] = []
            for k in op["reads"]:
                if k not in op["writes"]:
                    readers.setdefault(k, []).append(op)
        known = {e: {} for e in self.ENGS}
        for op in ops:
            e = op["eng"]
            need = {}
            if op["bar"]:
                need = dict(op["barvals"])
                need.pop(("c", e), None)
            else:
                for d in op["deps"]:
                    if d is op:
                        continue
                    if (not d["dma"]) and d["eng"] == e and (e == "pe" or not SELF_SYNC):
                        continue
                    s, v = d["sem"], d["val"]
                    if need.get(s, 0) < v:
                        need[s] = v
                if op["dma"] and op["val"] > 16:
                    s = op["sem"]
                    if need.get(s, 0) < op["val"] - 16:
                        need[s] = op["val"] - 16
            w = []
            for s, v in need.items():
                if known[e].get(s, 0) < v:
                    known[e][s] = v
                    w.append((s, v))
            op["waits"] = w
        final = latest
        semkeys = sorted(final.keys(), key=str)
        with contextlib.ExitStack() as st:
            sems = {}
            for k in semkeys:
                sems[k] = st.enter_context(nc.semaphore("s_" + "_".join(map(str, k))))
            block = st.enter_context(nc.Block())

            def run(engname):
                def body(eng):
                    for op in ops:
                        if op["eng"] != engname:
                            continue
                        for s, v in op["waits"]:
                            eng.wait_ge(sems[s], v)
                        if op["bar"]:
                            continue
                        ins = op["fn"](eng)
                        ins.then_inc(sems[op["sem"]], 16 if op["dma"] else 1)
                    for k, v in final.items():
                        if k[0] == "d" and k[1] == engname:
                            eng.wait_ge(sems[k], v)
                return body

            block.sync(run("sp"))
            block.scalar(run("act"))
            block.vector(run("dve"))
            block.gpsimd(run("pool"))
            block.tensor(run("pe"))


class Arena:
    def __init__(self, base_ap, nbytes):
        self.base = base_ap
        self.nbytes = nbytes
        self.off = 0
        self.marks = []

    def alloc(self, shape, dt=F32):
        esz = {F32: 4, BF16: 2, I32: 4, U32: 4}[dt]
        npart = shape[0]
        fshape = list(shape[1:])
        n = 1
        for s_ in fshape:
            n *= s_
        nb = (n * esz + 31) // 32 * 32
        assert self.off + nb <= self.nbytes, ("SBUF arena overflow", self.off, nb, self.nbytes)
        a = self.base[0:npart, self.off // 4:(self.off + nb) // 4]
        self.off += nb
        if dt != F32:
            a = a.bitcast(dt)
        a = a[:, 0:n]
        if len(fshape) == 2:
            a = a.rearrange("p (a b) -> p a b", a=fshape[0])
        elif len(fshape) == 3:
            a = a.rearrange("p (a b c) -> p a b c", a=fshape[0], b=fshape[1])
        return a

    def mark(self):
        self.marks.append(self.off)

    def release(self):
        self.off = self.marks.pop()


def dft_tables():
    n = np.arange(128)
    ang = 2 * np.pi * np.outer(n, n) / 128.0
    c128 = np.cos(ang)
    s128 = np.sin(ang)
    k1 = np.arange(128)[:, None]
    n2 = np.arange(64)[None, :]
    tw = 2 * np.pi * k1 * n2 / 8192.0
    twc = np.cos(tw)
    tws = np.sin(tw)
    a64 = 2 * np.pi * np.outer(np.arange(64), np.arange(64)) / 64.0
    c2 = np.cos(a64)
    s2 = np.sin(a64)
    t3 = np.zeros((128, 128))
    t3[0:64, 0:64] = c2
    t3[64:128, 0:64] = -s2
    t3[0:64, 64:128] = s2
    t3[64:128, 64:128] = c2
    t3 = t3 / 1024.0
    f = np.float32
    return dict(k_c128=c128.astype(f), k_s128=s128.astype(f), k_twc=twc.astype(f), k_tws=tws.astype(f), k_t3=t3.astype(f))


def build_program(stop_after=None, debug=False):
    nc = bass.Bass("TRN2", target_bir_lowering=False)

    def din(name, shape, dt=F32):
        return nc.dram_tensor(name, list(shape), dt, kind="ExternalInput").ap()

    DBGSET = set(os.environ.get("KDBG_OUT", "").split(",")) if debug else set()

    def dscr(name, shape, dt=F32):
        if name in DBGSET:
            return nc.dram_tensor(name, list(shape), dt, kind="ExternalOutput").ap()
        return nc.dram_tensor(name, list(shape), dt).ap()

    x = din("x", [S, D]); c = din("c", [1, D]); ctx = din("ctx", [CTX, D]); c_ctx = din("c_ctx", [1, D])
    w_ada = din("w_ada", [D, 6 * D]); b_ada = din("b_ada", [1, 6 * D]); norm1 = din("norm1", [1, D])
    w_in = din("w_in", [D, 4608]); conv_w = din("conv_w", [4, D]); conv_b = din("conv_b", [1, D])
    gate_a_w = din("gate_a_w", [2, 8, 128, 128]); gate_a_b = din("gate_a_b", [2, D])
    gate_x_w = din("gate_x_w", [2, 8, 128, 128]); gate_x_b = din("gate_x_b", [2, D])
    lru_lambda = din("lru_lambda", [2, D])
    w_fourier = din("w_fourier", [512, D]); w_rnn = din("w_rnn", [D, D]); w_out = din("w_out", [D, D])
    norm2 = din("norm2", [1, D]); w_router = din("w_router", [D, NE]); b_router = din("b_router", [1, NE])
    w_gu = din("w_gu", [NE, D, 2 * D]); b_gu = din("b_gu", [NE, 2 * D])
    w_down = din("w_down", [NE, D, D]); b_down = din("b_down", [NE, D]); norm_f = din("norm_f", [1, D])
    k_c128 = din("k_c128", [128, 128]); k_s128 = din("k_s128", [128, 128])
    k_twc = din("k_twc", [128, 64]); k_tws = din("k_tws", [128, 64]); k_t3 = din("k_t3", [128, 128])
    y = nc.dram_tensor("y", [S, D], F32, kind="ExternalOutput").ap()
    dbg = nc.dram_tensor("dbg", [128, 2048], F32, kind="ExternalOutput").ap() if debug else None

    u_d = dscr("u_d", [S, 512], BF16)
    xc_d = dscr("xc_d", [D, S], F32)
    gg_d = dscr("gg_d", [D, S], BF16)
    gfr_d = dscr("gfr_d", [2 * D, S], BF16)
    q_d = dscr("q_d", [4, 2, 64, 128, 128], BF16)
    rt_d = dscr("rt_d", [D, S], BF16)
    yg_d = dscr("yg_d", [D, S], BF16)
    x1_d = dscr("x1_d", [S, D], F32)
    h2_d = dscr("h2_d", [S, D], BF16)
    xg_d = dscr("xg_d", [PSLOTS, D], BF16)
    ys_d = dscr("ys_d", [PSLOTS, D], F32)

    st = contextlib.ExitStack()
    SBN = 206848
    sball = st.enter_context(nc.sbuf_tensor("sball", [128, SBN // 4], F32))
    AR = Arena(sball[:], SBN)
    banks = [st.enter_context(nc.psum_tensor("psb%d" % i, [128, 512], F32)) for i in range(8)]
    P = Prog(nc)
    psn = [0]

    def next_ps():
        i = psn[0] % 8
        psn[0] += 1
        return banks[i][:], "ps%d" % i

    uid = [0]

    def K(name):
        uid[0] += 1
        return "%s#%d" % (name, uid[0])

    def finish():
        with nc.allow_low_precision(reason="bf16 matmul operands, fp32 accumulation"):
            P.emit()
        st.close()
        return nc

    LQ = "sp"
    SQ = "pool"

    ident_f = AR.alloc([128, 128]); ident_b = AR.alloc([128, 128], BF16)
    ones_b = AR.alloc([128, 512], BF16); ones_f = AR.alloc([128, 128])
    ltri_b = AR.alloc([128, 128], BF16)
    g2_bc = AR.alloc([128, D]); nf_bc = AR.alloc([128, D])
    dest_i = AR.alloc([128, 64, 4], I32); wk = AR.alloc([128, 64, 4])
    eb_i = AR.alloc([128, NBLK], I32); widx = AR.alloc([128, NBLK, 8], I32)
    AR.mark()
    g1_bc = AR.alloc([128, D]); gm2_bc = AR.alloc([128, D]); sh2_bc = AR.alloc([128, D])
    gm1T = AR.alloc([128, 8]); sh1T = AR.alloc([128, 8]); gmcT = AR.alloc([128, 8]); shcT = AR.alloc([128, 8])
    cwT = AR.alloc([128, 8, 4]); cbT = AR.alloc([128, 8])
    nbaT = AR.alloc([128, 16]); nbxT = AR.alloc([128, 16]); coefT = AR.alloc([128, 16])
    h0T = AR.alloc([128, 16])
    Lg = AR.alloc([128, 64, NE])

    P.pool(lambda e: e.memset(ident_f, 0.0), writes=["ident_f"])
    P.pool(lambda e: e.affine_select(out=ident_f, in_=ident_f, pattern=[[-1, 128]], compare_op=ALU.not_equal, fill=1.0, base=0, channel_multiplier=1), reads=["ident_f"], writes=["ident_f"])
    P.dve(lambda e: e.tensor_copy(ident_b, ident_f), reads=["ident_f"], writes=["ident_b"])
    P.pool(lambda e: e.memset(ones_b, 1.0), writes=["ones_b"])
    P.pool(lambda e: e.memset(ones_f, 1.0), writes=["ones_f"])
    P.pool(lambda e: e.memset(ltri_b, 1.0), writes=["ltri_b"])
    P.pool(lambda e: e.affine_select(out=ltri_b, in_=ltri_b, pattern=[[1, 128]], compare_op=ALU.is_gt, fill=0.0, base=0, channel_multiplier=-1), reads=["ltri_b"], writes=["ltri_b"])

    AR.mark()
    cT = AR.alloc([128, 16])
    crep = AR.alloc([128, 16, 128])
    mb = AR.alloc([128, 6 * D])
    mcb = AR.alloc([128, 2 * D])
    wa_buf = [AR.alloc([128, 8, 512]) for _ in range(2)]
    n1_bc = AR.alloc([128, D]); n2_bc = AR.alloc([128, D])
    P.dma(LQ, lambda e: e.dma_start(out=cT[:, 0:8], in_=c.rearrange("o (k p) -> p (o k)", p=128), allow_slow_non_contiguous=True), writes=["cT"])
    P.dma(LQ, lambda e: e.dma_start(out=cT[:, 8:16], in_=c_ctx.rearrange("o (k p) -> p (o k)", p=128), allow_slow_non_contiguous=True), reads=["cT"], writes=["cT"])
    P.dma(LQ, lambda e: e.dma_start(out=mb, in_=b_ada.partition_broadcast(128)), writes=["mb"])
    P.dma(LQ, lambda e: e.dma_start(out=n1_bc, in_=norm1.partition_broadcast(128)), writes=["n1_bc"])
    P.dma(LQ, lambda e: e.dma_start(out=n2_bc, in_=norm2.partition_broadcast(128)), writes=["n2_bc"])
    P.dma(LQ, lambda e: e.dma_start(out=nf_bc, in_=norm_f.partition_broadcast(128)), writes=["nf_bc"])
    ctmp = AR.alloc([128, 16])
    zt = AR.alloc([128, 4, D], BF16)
    P.pool(lambda e: e.memset(zt, 0.0), writes=["zt"])
    xgz_v = xg_d.rearrange("(b s p) d -> b p s d", p=128, s=4)
    for zb in range(NBLK):
        P.dma(SQ, lambda e, zb=zb: e.dma_start(out=xgz_v[zb], in_=zt), reads=["zt"], writes=["xg_d"])
    P.act(lambda e: e.activation(out=ctmp, in_=cT, func=AF.Exp, scale=-1.0), reads=["cT"], writes=["ctmp"])
    P.dve(lambda e: e.tensor_scalar_add(ctmp, ctmp, 1.0), reads=["ctmp"], writes=["ctmp"])
    P.dve(lambda e: e.reciprocal(ctmp, ctmp), reads=["ctmp"], writes=["ctmp"])
    P.dve(lambda e: e.tensor_tensor(cT, cT, ctmp, ALU.mult), reads=["ctmp", "cT"], writes=["cT"])
    for j in range(16):
        P.dve(lambda e, j=j: e.tensor_copy(crep[:, j, :], cT[:, j:j + 1].to_broadcast([128, 128])), reads=["cT"], writes=["crep"])
    w_ada_v = w_ada.rearrange("(k p) n -> p k n", p=128)
    for ch in range(12):
        bi = ch % 2
        P.dma(LQ, lambda e, ch=ch, bi=bi: e.dma_start(out=wa_buf[bi], in_=w_ada_v[:, :, ch * 512:(ch + 1) * 512]), writes=["wa%d" % bi])
        ps, pk = next_ps()

        def f(e, ps=ps, bi=bi):
            for k in range(8):
                ins = e.matmul(ps, crep[:, k, :], wa_buf[bi][:, k, :], start=(k == 0), stop=(k == 7))
            return ins
        P.pe(f, reads=["crep", "wa%d" % bi], writes=[pk])
        P.dve(lambda e, ps=ps, ch=ch: e.tensor_tensor(mb[:, ch * 512:(ch + 1) * 512], mb[:, ch * 512:(ch + 1) * 512], ps, ALU.add), reads=[pk, "mb"], writes=["mb"])
        if ch < 4:
            ps2, pk2 = next_ps()

            def f2(e, ps2=ps2, bi=bi):
                for k in range(8):
                    ins = e.matmul(ps2, crep[:, 8 + k, :], wa_buf[bi][:, k, :], start=(k == 0), stop=(k == 7))
                return ins
            P.pe(f2, reads=["crep", "wa%d" % bi], writes=[pk2])
            P.act(lambda e, ps2=ps2, ch=ch: e.copy(mcb[:, ch * 512:(ch + 1) * 512], ps2), reads=[pk2], writes=["mcb"])
    bada2 = AR.alloc([128, 2 * D])
    P.dma(LQ, lambda e: e.dma_start(out=bada2, in_=b_ada[:, 0:2 * D].partition_broadcast(128)), writes=["bada2"])
    P.dve(lambda e: e.tensor_tensor(mcb, mcb, bada2, ALU.add), reads=["mcb", "bada2"], writes=["mcb"])
    gm1_bc = AR.alloc([128, D]); gmc_bc = AR.alloc([128, D])
    P.dve(lambda e: e.scalar_tensor_tensor(gm1_bc, mb[:, D:2 * D], 1.0, n1_bc, ALU.add, ALU.mult), reads=["mb", "n1_bc"], writes=["gm1_bc"])
    P.dve(lambda e: e.scalar_tensor_tensor(gmc_bc, mcb[:, D:2 * D], 1.0, n1_bc, ALU.add, ALU.mult), reads=["mcb", "n1_bc"], writes=["gmc_bc"])
    P.dve(lambda e: e.scalar_tensor_tensor(gm2_bc, mb[:, 4 * D:5 * D], 1.0, n2_bc, ALU.add, ALU.mult), reads=["mb", "n2_bc"], writes=["gm2_bc"])
    P.act(lambda e: e.copy(g1_bc, mb[:, 2 * D:3 * D]), reads=["mb"], writes=["g1_bc"])
    P.act(lambda e: e.copy(sh2_bc, mb[:, 3 * D:4 * D]), reads=["mb"], writes=["sh2_bc"])
    P.act(lambda e: e.copy(g2_bc, mb[:, 5 * D:6 * D]), reads=["mb"], writes=["g2_bc"])
    for (src, sk, dst, dk) in ((gm1_bc, "gm1_bc", gm1T, "gm1T"), (mb, "mb", sh1T, "sh1T"), (gmc_bc, "gmc_bc", gmcT, "gmcT"), (mcb, "mcb", shcT, "shcT")):
        for kc in range(8):
            ps, pk = next_ps()
            P.pe(lambda e, ps=ps, src=src, kc=kc: e.transpose(ps[:, 0:128], src[:, kc * 128:(kc + 1) * 128], ident_f), reads=[sk, "ident_f"], writes=[pk])
            P.dve(lambda e, ps=ps, dst=dst, kc=kc: e.tensor_copy(dst[:, kc:kc + 1], ps[:, 0:1]), reads=[pk], writes=[dk])
    for kk_ in range(4):
        P.dma(LQ, lambda e, kk_=kk_: e.dma_start(out=cwT[:, :, kk_], in_=conv_w[kk_:kk_ + 1, :].rearrange("o (h p) -> p (o h)", p=128), allow_slow_non_contiguous=True), reads=["cwT"], writes=["cwT"])
    P.dma(LQ, lambda e: e.dma_start(out=cbT, in_=conv_b.rearrange("o (h p) -> p (o h)", p=128), allow_slow_non_contiguous=True), writes=["cbT"])
    P.dma(LQ, lambda e: e.dma_start(out=nbaT, in_=gate_a_b.rearrange("d (h p) -> p (d h)", p=128), allow_slow_non_contiguous=True), writes=["nbaT"])
    P.dma(LQ, lambda e: e.dma_start(out=nbxT, in_=gate_x_b.rearrange("d (h p) -> p (d h)", p=128), allow_slow_non_contiguous=True), writes=["nbxT"])
    P.dma(LQ, lambda e: e.dma_start(out=coefT, in_=lru_lambda.rearrange("d (h p) -> p (d h)", p=128), allow_slow_non_contiguous=True), writes=["coefT"])
    P.dve(lambda e: e.tensor_scalar_mul(nbaT, nbaT, -1.0), reads=["nbaT"], writes=["nbaT"])
    P.dve(lambda e: e.tensor_scalar_mul(nbxT, nbxT, -1.0), reads=["nbxT"], writes=["nbxT"])
    P.act(lambda e: e.activation(out=coefT, in_=coefT, func=AF.Exp, scale=-1.0), reads=["coefT"], writes=["coefT"])
    P.act(lambda e: e.activation(out=coefT, in_=coefT, func=AF.Ln, bias=1.0), reads=["coefT"], writes=["coefT"])
    P.dve(lambda e: e.tensor_scalar_mul(coefT, coefT, -8.0), reads=["coefT"], writes=["coefT"])
    P.barrier()
    AR.release()
    if stop_after == "A":
        return finish()

    AR.mark()
    gw_b = AR.alloc([128, 32, 128], BF16)
    AR.mark()
    win_b = AR.alloc([128, 8, 4608], BF16)
    AR.mark()
    stg = [AR.alloc([128, 8, 512]) for _ in range(2)]
    w_in_v = w_in.rearrange("(k p) n -> p k n", p=128)
    for ch in range(9):
        bi = ch % 2
        P.dma(LQ, lambda e, ch=ch, bi=bi: e.dma_start(out=stg[bi], in_=w_in_v[:, :, ch * 512:(ch + 1) * 512]), writes=["stg%d" % bi])
        if ch % 2 == 0:
            P.act(lambda e, ch=ch, bi=bi: e.copy(win_b[:, :, ch * 512:(ch + 1) * 512], stg[bi]), reads=["stg%d" % bi], writes=["win_b"])
        else:
            P.dve(lambda e, ch=ch, bi=bi: e.tensor_copy(win_b[:, :, ch * 512:(ch + 1) * 512], stg[bi]), reads=["stg%d" % bi], writes=["win_b"])
    for gi, gwd in enumerate((gate_a_w, gate_x_w)):
        bi = gi % 2
        P.dma(LQ, lambda e, gwd=gwd, bi=bi: e.dma_start(out=stg[bi][:, 0:4, :].rearrange("p a (b c) -> p (a b) c", c=128), in_=gwd.rearrange("d h i j -> i (d h) j")), writes=["stg%d" % bi])
        P.dve(lambda e, gi=gi, bi=bi: e.tensor_copy(gw_b[:, gi * 16:(gi + 1) * 16, :], stg[bi][:, 0:4, :].rearrange("p a (b c) -> p (a b) c", c=128)), reads=["stg%d" % bi], writes=["gw_b"])
    P.barrier()
    AR.release()
    if stop_after == "W":
        return finish()

    def rms_rows(xt, nsub, ssq, rstd, junk, kx, kpre):
        for s_ in range(nsub):
            P.act(lambda e, s_=s_: e.activation(out=junk, in_=xt[:, s_, :], func=AF.Square, accum_out=ssq[:, s_:s_ + 1]), reads=[kx], writes=[kpre + "junk", kpre + "ssq"])
        P.dve(lambda e: e.tensor_scalar(rstd[:, 0:nsub], ssq[:, 0:nsub], 1.0 / D, EPS, ALU.mult, ALU.add), reads=[kpre + "ssq"], writes=[kpre + "rstd"])
        P.act(lambda e: e.activation(out=rstd[:, 0:nsub], in_=rstd[:, 0:nsub], func=AF.Ln), reads=[kpre + "rstd"], writes=[kpre + "rstd"])
        P.act(lambda e: e.activation(out=rstd[:, 0:nsub], in_=rstd[:, 0:nsub], func=AF.Exp, scale=-0.5), reads=[kpre + "rstd"], writes=[kpre + "rstd"])

    def norm_transpose(xt, nsub, rstd, xs_b, hT, gT, sT, kx, kpre, khT):
        for s_ in range(nsub):
            P.act(lambda e, s_=s_: e.activation(out=xs_b[:, s_, :], in_=xt[:, s_, :], func=AF.Copy, scale=rstd[:, s_:s_ + 1]), reads=[kx, kpre + "rstd"], writes=[kpre + "xs"])
        for kc in range(8):
            ps, pk = next_ps()
            psb = ps.bitcast(BF16)

            def f(e, psb=psb, kc=kc):
                for s_ in range(nsub):
                    ins = e.transpose(psb[:, s_ * 128:(s_ + 1) * 128], xs_b[:, s_, kc * 128:(kc + 1) * 128], ident_b)
                return ins
            P.pe(f, reads=[kpre + "xs", "ident_b"], writes=[pk])
            n = nsub * 128
            if kc % 2 == 0:
                P.act(lambda e, psb=psb, kc=kc, n=n: e.activation(out=hT[:, kc, 0:n], in_=psb[:, 0:n], func=AF.Identity, scale=gT[:, kc:kc + 1], bias=sT[:, kc:kc + 1]), reads=[pk], writes=[khT])
            else:
                P.dve(lambda e, psb=psb, kc=kc, n=n: e.tensor_scalar(hT[:, kc, 0:n], psb[:, 0:n], gT[:, kc:kc + 1], sT[:, kc:kc + 1], ALU.mult, ALU.add), reads=[pk], writes=[khT])

    def conv_from_psum(ps, out_t, h, ntok, rowlen, kps, kout):
        nr = ntok // rowlen
        P.dve(lambda e: e.tensor_scalar(out_t[:, 0:ntok], ps[:, 0:ntok], cwT[:, h, 2:3], cbT[:, h:h + 1], ALU.mult, ALU.add), reads=[kps, "cwT", "cbT"], writes=[kout])
        o3 = out_t[:, 0:ntok].rearrange("p (r t) -> p r t", t=rowlen)
        z3 = ps[:, 0:ntok].rearrange("p (r t) -> p r t", t=rowlen)
        for (kk, sh) in ((0, -2), (1, -1), (3, 1)):
            if sh < 0:
                oo = o3[:, :, -sh:rowlen]; zz = z3[:, :, 0:rowlen + sh]
            else:
                oo = o3[:, :, 0:rowlen - sh]; zz = z3[:, :, sh:rowlen]
            P.dve(lambda e, oo=oo, zz=zz, kk=kk: e.scalar_tensor_tensor(oo, zz, cwT[:, h, kk:kk + 1], oo, ALU.mult, ALU.add), reads=[kps, kout], writes=[kout])

    def rnn_chunk(xc_f, xc_b, d, h, n, bufs, kxc, kpre):
        ia = 0 * 16 + d * 8 + h
        ix = 1 * 16 + d * 8 + h
        dh = d * 8 + h
        e1, a_, e2, s_, b_ = bufs["e1"], bufs["a"], bufs["e2"], bufs["s"], bufs["b"]
        nch = (n + 511) // 512
        psr = []
        for g_, wi in ((0, ia), (1, ix)):
            lst = []
            for j in range(nch):
                ps, pk = next_ps()
                w_ = min(512, n - j * 512)
                P.pe(lambda e, ps=ps, wi=wi, j=j, w_=w_: e.matmul(ps[:, 0:w_], gw_b[:, wi, :], xc_b[:, j * 512:j * 512 + w_], start=True, stop=True), reads=[kxc + "b", "gw_b"], writes=[pk])
                lst.append((ps, pk, j, w_))
            psr.append(lst)
        for (ps, pk, j, w_) in psr[0]:
            P.act(lambda e, ps=ps, j=j, w_=w_: e.activation(out=e1[:, j * 512:j * 512 + w_], in_=ps[:, 0:w_], func=AF.Exp, scale=-1.0, bias=nbaT[:, dh:dh + 1]), reads=[pk, "nbaT"], writes=[kpre + "e1"])
        for (ps, pk, j, w_) in psr[1]:
            P.act(lambda e, ps=ps, j=j, w_=w_: e.activation(out=e2[:, j * 512:j * 512 + w_], in_=ps[:, 0:w_], func=AF.Exp, scale=-1.0, bias=nbxT[:, dh:dh + 1]), reads=[pk, "nbxT"], writes=[kpre + "e2"])
        P.dve(lambda e: e.tensor_scalar_add(e1[:, 0:n], e1[:, 0:n], 1.0), reads=[kpre + "e1"], writes=[kpre + "e1"])
        P.dve(lambda e: e.reciprocal(e1[:, 0:n], e1[:, 0:n]), reads=[kpre + "e1"], writes=[kpre + "e1"])
        P.act(lambda e: e.activation(out=a_[:, 0:n], in_=e1[:, 0:n], func=AF.Exp, scale=coefT[:, dh:dh + 1]), reads=[kpre + "e1", "coefT"], writes=[kpre + "a"])
        P.dve(lambda e: e.tensor_scalar_add(e2[:, 0:n], e2[:, 0:n], 1.0), reads=[kpre + "e2"], writes=[kpre + "e2"])
        P.dve(lambda e: e.reciprocal(e2[:, 0:n], e2[:, 0:n]), reads=[kpre + "e2"], writes=[kpre + "e2"])
        P.dve(lambda e: e.tensor_tensor(s_[:, 0:n], a_[:, 0:n], a_[:, 0:n], ALU.mult), reads=[kpre + "a"], writes=[kpre + "s"])
        P.act(lambda e: e.activation(out=s_[:, 0:n], in_=s_[:, 0:n], func=AF.Ln, scale=-1.0, bias=1.0), reads=[kpre + "s"], writes=[kpre + "s"])
        P.act(lambda e: e.activation(out=s_[:, 0:n], in_=s_[:, 0:n], func=AF.Exp, scale=0.5), reads=[kpre + "s"], writes=[kpre + "s"])
        P.dve(lambda e: e.tensor_tensor(b_[:, 0:n], e2[:, 0:n], xc_f, ALU.mult), reads=[kpre + "e2", kxc], writes=[kpre + "b"])
        P.dve(lambda e: e.tensor_tensor(b_[:, 0:n], b_[:, 0:n], s_[:, 0:n], ALU.mult), reads=[kpre + "b", kpre + "s"], writes=[kpre + "b"])

    AR.mark()
    cx = AR.alloc([128, 2, D]); cjunk = AR.alloc([128, D]); cssq = AR.alloc([128, 4]); crstd = AR.alloc([128, 4])
    cxs = AR.alloc([128, 2, D], BF16); hcT = AR.alloc([128, 8, CTX], BF16)
    xcc = AR.alloc([128, 8, CTX]); xccb = AR.alloc([128, 8, CTX], BF16)
    cb_ = dict(e1=AR.alloc([128, CTX]), a=AR.alloc([128, CTX]), e2=AR.alloc([128, CTX]), s=AR.alloc([128, CTX]), b=AR.alloc([128, CTX]))
    chh = AR.alloc([128, CTX])
    P.dma(LQ, lambda e: e.dma_start(out=cx, in_=ctx.rearrange("(s p) d -> p s d", p=128)), writes=["cx"])
    rms_rows(cx, 2, cssq, crstd, cjunk, "cx", "c_")
    norm_transpose(cx, 2, crstd, cxs, hcT, gmcT, shcT, "cx", "c_", "hcT")
    for h in range(8):
        ps, pk = next_ps()

        def f(e, ps=ps, h=h):
            for k in range(8):
                ins = e.matmul(ps[:, 0:CTX], win_b[:, k, 512 + h * 128:512 + (h + 1) * 128], hcT[:, k, :], start=(k == 0), stop=(k == 7))
            return ins
        P.pe(f, reads=["win_b", "hcT"], writes=[pk])
        conv_from_psum(ps, xcc[:, h, :], h, CTX, CTX, pk, "xcc%d" % h)
        P.act(lambda e, h=h: e.copy(xccb[:, h, :], xcc[:, h, :]), reads=["xcc%d" % h], writes=["xcc%db" % h])
        for d in range(2):
            rnn_chunk(xcc[:, h, :], xccb[:, h, :], d, h, CTX, cb_, "xcc%d" % h, "c_")
            if d == 0:
                P.dve(lambda e: e.tensor_tensor_scan(chh, cb_["a"], cb_["b"], 0.0, ALU.mult, ALU.add), reads=["c_a", "c_b"], writes=["chh"])
                P.dve(lambda e, h=h: e.tensor_copy(h0T[:, h:h + 1], chh[:, CTX - 1:CTX]), reads=["chh"], writes=["h0T"])
            else:
                P.dve(lambda e: e.tensor_tensor_scan(chh[:, ::-1], cb_["a"][:, ::-1], cb_["b"][:, ::-1], 0.0, ALU.mult, ALU.add), reads=["c_a", "c_b"], writes=["chh"])
                P.dve(lambda e, h=h: e.tensor_copy(h0T[:, 8 + h:9 + h], chh[:, 0:1]), reads=["chh"], writes=["h0T"])
    P.barrier()
    AR.release()
    if stop_after == "C":
        return finish()

    AR.mark()
    NT = S // 512
    xt = [AR.alloc([128, 4, D]) for _ in range(2)]
    djunk = AR.alloc([128, D], BF16); dssq = AR.alloc([128, 4]); drstd = AR.alloc([128, 4])
    dxs = AR.alloc([128, 4, D], BF16)
    hxT = AR.alloc([128, 8, 512], BF16)
    u_t = AR.alloc([128, 4, 512], BF16)
    xc_t = [AR.alloc([128, 512]) for _ in range(2)]
    gg_t = AR.alloc([128, 8, 512], BF16)
    gfr_t = AR.alloc([128, 8, 512], BF16)
    tA = [AR.alloc([128, 512]) for _ in range(2)]
    tB = [AR.alloc([128, 512]) for _ in range(2)]
    x_v = x.rearrange("(t s p) d -> t p s d", p=128, s=4)
    u_v = u_d.rearrange("(t s p) n -> t p s n", p=128, s=4)
    xc_v = xc_d.rearrange("(h p) t -> p h t", p=128)
    gg_v = gg_d.rearrange("(h p) t -> p h t", p=128)
    gfr_v = gfr_d.rearrange("(h p) t -> p h t", p=128)
    P.dma(LQ, lambda e: e.dma_start(out=xt[0], in_=x_v[0]), writes=["xt0"])
    for t in range(NT):
        bi = t % 2
        if t + 1 < NT:
            P.dma(LQ, lambda e, t=t: e.dma_start(out=xt[(t + 1) % 2], in_=x_v[t + 1]), writes=["xt%d" % ((t + 1) % 2)])
        rms_rows(xt[bi], 4, dssq, drstd, djunk, "xt%d" % bi, "d_")
        norm_transpose(xt[bi], 4, drstd, dxs, hxT, gm1T, sh1T, "xt%d" % bi, "d_", "hxT")
        for s_ in range(4):
            ps, pk = next_ps()

            def f(e, ps=ps, s_=s_):
                for k in range(8):
                    ins = e.matmul(ps, hxT[:, k, s_ * 128:(s_ + 1) * 128], win_b[:, k, 0:512], start=(k == 0), stop=(k == 7))
                return ins
            P.pe(f, reads=["hxT", "win_b"], writes=[pk])
            P.act(lambda e, ps=ps, s_=s_: e.copy(u_t[:, s_, :], ps), reads=[pk], writes=["u_t"])
        P.dma(SQ, lambda e, t=t: e.dma_start(out=u_v[t], in_=u_t), reads=["u_t"], writes=["u_d"])
        for cc in range(4, 36):
            ps, pk = next_ps()

            def f(e, ps=ps, cc=cc):
                for k in range(8):
                    ins = e.matmul(ps, win_b[:, k, cc * 128:(cc + 1) * 128], hxT[:, k, :], start=(k == 0), stop=(k == 7))
                return ins
            P.pe(f, reads=["hxT", "win_b"], writes=[pk])
            if cc < 12:
                h = cc - 4
                ob = xc_t[h % 2]; ok = "xc_t%d" % (h % 2)
                conv_from_psum(ps, ob, h, 512, 64, pk, ok)
                P.dma(SQ, lambda e, ob=ob, h=h, t=t: e.dma_start(out=xc_v[:, h, t * 512:(t + 1) * 512], in_=ob), reads=[ok], writes=["xc_d"])
            elif cc < 20:
                h = cc - 12
                a_ = tA[h % 2]; b_ = tB[h % 2]; ka = "tA%d" % (h % 2); kb = "tB%d" % (h % 2)
                P.act(lambda e, ps=ps, a_=a_: e.activation(out=a_, in_=ps, func=AF.Square), reads=[pk], writes=[ka])
                P.dve(lambda e, a_=a_: e.tensor_scalar(a_, a_, 0.044715, 1.0, ALU.mult, ALU.add), reads=[ka], writes=[ka])
                P.dve(lambda e, ps=ps, a_=a_: e.tensor_tensor(a_, a_, ps, ALU.mult), reads=[ka, pk], writes=[ka])
                P.act(lambda e, a_=a_, b_=b_: e.activation(out=b_, in_=a_, func=AF.Sigmoid, scale=1.5957691216057308), reads=[ka], writes=[kb])
                P.dve(lambda e, ps=ps, b_=b_, h=h: e.tensor_tensor(gg_t[:, h, :], b_, ps, ALU.mult), reads=[kb, pk], writes=["gg_t"])
            else:
                h = cc - 20
                P.act(lambda e, ps=ps, h=h: e.activation(out=gfr_t[:, h % 8, :], in_=ps, func=AF.Sigmoid), reads=[pk], writes=["gfr_t"])
                if h % 8 == 7:
                    P.dma(SQ, lambda e, t=t, h=h: e.dma_start(out=gfr_v[:, (h // 8) * 8:(h // 8) * 8 + 8, t * 512:(t + 1) * 512], in_=gfr_t), reads=["gfr_t"], writes=["gfr_d"])
        P.dma(SQ, lambda e, t=t: e.dma_start(out=gg_v[:, :, t * 512:(t + 1) * 512], in_=gg_t), reads=["gg_t"], writes=["gg_d"])
    P.barrier()
    AR.release()
    AR.release()
    if stop_after == "D":
        return finish()

    AR.mark()
    xcf = AR.alloc([128, S]); xcb = AR.alloc([128, S], BF16); hf = AR.alloc([128, S])
    CH = 1024
    NCH = S // CH
    rb = [dict(e1=AR.alloc([128, CH]), a=AR.alloc([128, CH]), e2=AR.alloc([128, CH]), s=AR.alloc([128, CH]), b=AR.alloc([128, CH])) for _ in range(2)]
    hb = [AR.alloc([128, CH]) for _ in range(2)]
    ggc = [AR.alloc([128, CH], BF16) for _ in range(2)]
    ygc = [AR.alloc([128, CH], BF16) for _ in range(2)]
    yg_v = yg_d.rearrange("(h p) t -> p h t", p=128)
    for h in range(8):
        P.dma(LQ, lambda e, h=h: e.dma_start(out=xcf, in_=xc_v[:, h, :]), reads=["xc_d"], writes=["xcf"])
        P.act(lambda e: e.copy(xcb[:, 0:S // 2], xcf[:, 0:S // 2]), reads=["xcf"], writes=["xcfb"])
        P.dve(lambda e: e.tensor_copy(xcb[:, S // 2:S], xcf[:, S // 2:S]), reads=["xcf", "xcfb"], writes=["xcfb"])
        it = 0
        for d in range(2):
            order = list(range(NCH)) if d == 0 else list(range(NCH - 1, -1, -1))
            prev = None
            for ci in order:
                bi = it % 2
                it += 1
                sl = slice(ci * CH, (ci + 1) * CH)
                kp = "r%d_" % bi
                rnn_chunk(xcf[:, sl], xcb[:, sl], d, h, CH, rb[bi], "xcf", kp)
                dh = d * 8 + h
                if d == 0:
                    init = h0T[:, dh:dh + 1] if prev is None else hf[:, ci * CH - 1:ci * CH]
                    P.dve(lambda e, bi=bi, sl=sl, init=init: e.tensor_tensor_scan(hf[:, sl], rb[bi]["a"], rb[bi]["b"], init, ALU.mult, ALU.add), reads=[kp + "a", kp + "b", "hf", "h0T"], writes=["hf"])
                else:
                    if prev is None:
                        init = h0T[:, dh:dh + 1]; kinit = "h0T"
                    else:
                        init = hb[prev][:, 0:1]; kinit = "hb%d" % prev
                    P.dma(LQ, lambda e, bi=bi, sl=sl, h=h: e.dma_start(out=ggc[bi], in_=gg_v[:, h, sl]), reads=["gg_d"], writes=["ggc%d" % bi])
                    P.dve(lambda e, bi=bi, init=init: e.tensor_tensor_scan(hb[bi][:, ::-1], rb[bi]["a"][:, ::-1], rb[bi]["b"][:, ::-1], init, ALU.mult, ALU.add), reads=[kp + "a", kp + "b", kinit], writes=["hb%d" % bi])
                    P.dve(lambda e, bi=bi, sl=sl: e.tensor_tensor(rb[bi]["s"], hb[bi], hf[:, sl], ALU.add), reads=["hb%d" % bi, "hf", kp + "s"], writes=[kp + "s"])
                    P.dve(lambda e, bi=bi: e.tensor_tensor(ygc[bi], rb[bi]["s"], ggc[bi], ALU.mult), reads=[kp + "s", "ggc%d" % bi], writes=["ygc%d" % bi])
                    P.dma(SQ, lambda e, bi=bi, sl=sl, h=h: e.dma_start(out=yg_v[:, h, sl], in_=ygc[bi]), reads=["ygc%d" % bi], writes=["yg_d"])
                    prev = bi
                if d == 0:
                    prev = bi
    P.barrier()
    AR.release()
    AR.release()
    if stop_after == "E":
        return finish()

    AR.mark()
    c1b = AR.alloc([128, 128], BF16); s1b = AR.alloc([128, 128], BF16); t3b = AR.alloc([128, 128], BF16)
    twc = AR.alloc([128, 64]); tws = AR.alloc([128, 64])
    ftmp = AR.alloc([128, 128])
    for (src, dst, kk) in ((k_c128, c1b, "c1b"), (k_s128, s1b, "s1b"), (k_t3, t3b, "t3b")):
        P.dma(LQ, lambda e, src=src: e.dma_start(out=ftmp, in_=src), writes=["ftmp"])
        P.dve(lambda e, dst=dst: e.tensor_copy(dst, ftmp), reads=["ftmp"], writes=[kk])
    P.dma(LQ, lambda e: e.dma_start(out=twc, in_=k_twc), writes=["twc"])
    P.dma(LQ, lambda e: e.dma_start(out=tws, in_=k_tws), writes=["tws"])
    AR.mark()
    U = AR.alloc([128, 64, 512], BF16)
    qt = [AR.alloc([128, 2, 512], BF16) for _ in range(2)]
    f1 = [AR.alloc([128, 512]) for _ in range(2)]
    f2 = [AR.alloc([128, 512]) for _ in range(2)]
    u_pv = u_d.rearrange("(p n) c -> p n c", n=64)
    for uq in range(4):
        P.dma(LQ, lambda e, uq=uq: e.dma_start(out=U[:, uq * 16:(uq + 1) * 16, :], in_=u_pv[:, uq * 16:(uq + 1) * 16, :]), reads=["u_d"], writes=["U"])
    FDBG = int(os.environ.get("FDBG", "0"))
    for n2 in range(64 if FDBG != 2 else 0):
        bi = n2 % 2
        psr, kr = next_ps()
        psi, ki = next_ps()
        P.pe(lambda e, psr=psr, n2=n2: e.matmul(psr, c1b, U[:, n2, :], start=True, stop=True), reads=["U", "c1b"], writes=[kr])
        P.pe(lambda e, psi=psi, n2=n2: e.matmul(psi, s1b, U[:, n2, :], start=True, stop=True), reads=["U", "s1b"], writes=[ki])
        P.dve(lambda e, psi=psi, n2=n2, bi=bi: e.tensor_scalar_mul(f1[bi], psi, tws[:, n2:n2 + 1]), reads=[ki, "tws"], writes=["f1%d" % bi])
        P.dve(lambda e, psr=psr, n2=n2, bi=bi: e.tensor_scalar_mul(f2[bi], psr, tws[:, n2:n2 + 1]), reads=[kr, "tws"], writes=["f2%d" % bi])
        P.dve(lambda e, psr=psr, n2=n2, bi=bi: e.scalar_tensor_tensor(qt[bi][:, 0, :], psr, twc[:, n2:n2 + 1], f1[bi], ALU.mult, ALU.subtract), reads=[kr, "twc", "f1%d" % bi], writes=["qt%d" % bi])
        P.dve(lambda e, psi=psi, n2=n2, bi=bi: e.scalar_tensor_tensor(qt[bi][:, 1, :], psi, twc[:, n2:n2 + 1], f2[bi], ALU.mult, ALU.add), reads=[ki, "twc", "f2%d" % bi, "qt%d" % bi], writes=["qt%d" % bi])
        for r in range(2 if FDBG != 1 else 0):
            P.dma(SQ, lambda e, n2=n2, bi=bi, r=r: e.dma_start(out=q_d[:, r, n2, :, :].rearrange("g k c -> k g c"), in_=qt[bi][:, r, :].rearrange("k (g c) -> k g c", g=4)), reads=["qt%d" % bi], writes=["q_d"])
    P.barrier()
    AR.release()
    if stop_after == "F1":
        return finish()
    AR.mark()
    Qg = [AR.alloc([128, 128, 128], BF16) for _ in range(2)]
    RT = [AR.alloc([128, 2, S], BF16) for _ in range(2)]
    rt_v = rt_d.rearrange("(g r j) t -> g j r t", r=2, j=128)
    for g in range(4):
        bi = g % 2
        for r in range(2):
            P.dma(LQ, lambda e, g=g, r=r, bi=bi: e.dma_start(out=Qg[bi][r * 64:(r + 1) * 64, :, :], in_=q_d[g, r]), reads=["q_d"], writes=["Qg%d" % bi])
        for k0 in range(0, 128, 4):
            ps, pk = next_ps()

            def f(e, ps=ps, k0=k0, bi=bi):
                for kk in range(4):
                    ins = e.matmul(ps[:, kk * 128:(kk + 1) * 128], Qg[bi][:, k0 + kk, :], t3b, start=True, stop=True)
                return ins
            P.pe(f, reads=["Qg%d" % bi, "t3b"], writes=[pk])
            psv = ps.rearrange("j (k r n) -> j r k n", k=4, r=2)
            for r in range(2):
                ov = RT[bi][:, r, :].rearrange("j (n k) -> j k n", k=128)[:, k0:k0 + 4, :]
                if r == 0:
                    P.act(lambda e, ov=ov, psv=psv, r=r: e.copy(ov, psv[:, r, :, :]), reads=[pk], writes=["RT%d" % bi])
                else:
                    P.dve(lambda e, ov=ov, psv=psv, r=r: e.tensor_copy(ov, psv[:, r, :, :]), reads=[pk], writes=["RT%d" % bi])
        P.dma(SQ, lambda e, g=g, bi=bi: e.dma_start(out=rt_v[g], in_=RT[bi]), reads=["RT%d" % bi], writes=["rt_d"])
    P.barrier()
    AR.release()
    AR.release()
    if stop_after == "F":
        return finish()

    AR.mark()
    wfp = AR.alloc([128, 8, D], BF16)
    wr_b = AR.alloc([128, 8, D], BF16)
    wo_b = AR.alloc([128, 8, D], BF16)
    wrt_f = AR.alloc([128, 8, NE])
    brt = AR.alloc([1, NE])
    AR.mark()
    gstg = AR.alloc([128, 8, D])
    cdb = AR.alloc([128, 128], BF16); sdb = AR.alloc([128, 128], BF16)
    wf_b = AR.alloc([128, 4, D], BF16)
    P.dma(LQ, lambda e: e.dma_start(out=gstg[:, 0, 0:128], in_=k_c128), writes=["gstg"])
    P.dve(lambda e: e.tensor_copy(cdb, gstg[:, 0, 0:128]), reads=["gstg"], writes=["cdb"])
    P.dma(LQ, lambda e: e.dma_start(out=gstg[:, 0, 0:128], in_=k_s128), reads=["gstg"], writes=["gstg"])
    P.dve(lambda e: e.tensor_scalar_mul(sdb, gstg[:, 0, 0:128], -1.0), reads=["gstg"], writes=["sdb"])
    P.dma(LQ, lambda e: e.dma_start(out=gstg[:, 0:4, :], in_=w_fourier.rearrange("(g m) n -> m g n", m=128)), reads=["gstg"], writes=["gstg"])
    P.dve(lambda e: e.tensor_copy(wf_b, gstg[:, 0:4, :]), reads=["gstg"], writes=["wf_b"])
    for g in range(4):
        for ri, mat, mk in ((0, cdb, "cdb"), (1, sdb, "sdb")):
            for half in range(2):
                ps, pk = next_ps()
                P.pe(lambda e, ps=ps, mat=mat, g=g, half=half: e.matmul(ps, mat, wf_b[:, g, half * 512:(half + 1) * 512], start=True, stop=True), reads=[mk, "wf_b"], writes=[pk])
                P.act(lambda e, ps=ps, g=g, ri=ri, half=half: e.copy(wfp[:, g * 2 + ri, half * 512:(half + 1) * 512], ps), reads=[pk], writes=["wfp"])
    P.dma(LQ, lambda e: e.dma_start(out=gstg, in_=w_rnn.rearrange("(k p) n -> p k n", p=128)), reads=["gstg"], writes=["gstg"])
    P.dve(lambda e: e.tensor_copy(wr_b, gstg), reads=["gstg"], writes=["wr_b"])
    P.dma(LQ, lambda e: e.dma_start(out=gstg, in_=w_out.rearrange("(k p) n -> p k n", p=128)), reads=["gstg"], writes=["gstg"])
    for k in range(8):
        P.dve(lambda e, k=k: e.tensor_tensor(wo_b[:, k, :], gstg[:, k, :], g1_bc, ALU.mult), reads=["gstg", "g1_bc"], writes=["wo_b"])
    P.dma(LQ, lambda e: e.dma_start(out=wrt_f, in_=w_router.rearrange("(k p) n -> p k n", p=128)), writes=["wrt_f"])
    P.dma(LQ, lambda e: e.dma_start(out=brt, in_=b_router), writes=["brt"])
    P.barrier()
    AR.release()
    rtt = [AR.alloc([128, 8, 512], BF16) for _ in range(2)]
    ygt = [AR.alloc([128, 8, 512], BF16) for _ in range(2)]
    gft = [AR.alloc([128, 16, 512], BF16)] * 2
    xg_ = [AR.alloc([128, 4, D]) for _ in range(2)]
    mT = AR.alloc([128, 8, 512], BF16)
    g1t = [AR.alloc([128, 512]) for _ in range(2)]
    g2t = [AR.alloc([128, 512]) for _ in range(2)]
    h2f = AR.alloc([128, D])
    h2b = AR.alloc([128, 4, D], BF16)
    h2T = AR.alloc([128, 8, 128])
    gjunk = AR.alloc([128, D], BF16); gssq = AR.alloc([128, 4]); grstd = AR.alloc([128, 4])
    rt_tv = rt_d.rearrange("(c j) t -> j c t", j=128)
    x1_v = x1_d.rearrange("(t s p) d -> t p s d", p=128, s=4)
    h2_v = h2_d.rearrange("(t s p) d -> t p s d", p=128, s=4)

    def g_load(t):
        bi = t % 2
        sl = slice(t * 512, (t + 1) * 512)
        P.dma(LQ, lambda e: e.dma_start(out=rtt[bi], in_=rt_tv[:, :, sl]), reads=["rt_d"], writes=["rtt%d" % bi])
        P.dma(LQ, lambda e: e.dma_start(out=ygt[bi], in_=yg_v[:, :, sl]), reads=["yg_d"], writes=["ygt%d" % bi])
        P.dma(LQ, lambda e: e.dma_start(out=xg_[bi], in_=x_v[t]), writes=["xg_%d" % bi])
    def gft_load(t):
        P.dma(LQ, lambda e: e.dma_start(out=gft[0], in_=gfr_v[:, :, t * 512:(t + 1) * 512]), reads=["gfr_d"], writes=["gft0"])
    g_load(0)
    gft_load(0)
    for t in range(NT):
        bi = t % 2
        if t + 1 < NT:
            g_load(t + 1)
        x1t = xg_[bi]
        kx1 = "xg_%d" % bi
        for n in range(8):
            psF, kF = next_ps()
            psR, kR = next_ps()

            def fF(e, psF=psF, n=n, bi=bi):
                for k in range(8):
                    ins = e.matmul(psF, wfp[:, k, n * 128:(n + 1) * 128], rtt[bi][:, k, :], start=(k == 0), stop=(k == 7))
                return ins

            def fR(e, psR=psR, n=n, bi=bi):
                for k in range(8):
                    ins = e.matmul(psR, wr_b[:, k, n * 128:(n + 1) * 128], ygt[bi][:, k, :], start=(k == 0), stop=(k == 7))
                return ins
            P.pe(fF, reads=["wfp", "rtt%d" % bi], writes=[kF])
            P.pe(fR, reads=["wr_b", "ygt%d" % bi], writes=[kR])
            a_ = g1t[n % 2]; b_ = g2t[n % 2]; ka = "g1t%d" % (n % 2); kb = "g2t%d" % (n % 2)
            P.dve(lambda e, psF=psF, a_=a_, n=n, bi=bi: e.tensor_tensor(a_, psF, gft[bi][:, n, :], ALU.mult), reads=[kF, "gft0"], writes=[ka])
            P.dve(lambda e, psR=psR, b_=b_, n=n, bi=bi: e.tensor_tensor(b_, psR, gft[bi][:, 8 + n, :], ALU.mult), reads=[kR, "gft0"], writes=[kb])
            P.dve(lambda e, a_=a_, b_=b_, n=n: e.tensor_tensor(mT[:, n, :], a_, b_, ALU.add), reads=[ka, kb], writes=["mT"])
        if t + 1 < NT:
            gft_load(t + 1)
        for s_ in range(4):
            for half in range(2):
                ps, pk = next_ps()

                def fO(e, ps=ps, s_=s_, half=half):
                    for k in range(8):
                        ins = e.matmul(ps, mT[:, k, s_ * 128:(s_ + 1) * 128], wo_b[:, k, half * 512:(half + 1) * 512], start=(k == 0), stop=(k == 7))
                    return ins
                P.pe(fO, reads=["mT", "wo_b"], writes=[pk])
                P.dve(lambda e, ps=ps, s_=s_, half=half, bi=bi: e.tensor_tensor(xg_[bi][:, s_, half * 512:(half + 1) * 512], ps, xg_[bi][:, s_, half * 512:(half + 1) * 512], ALU.add), reads=[pk, kx1], writes=[kx1])
        P.dma(SQ, lambda e, t=t, x1t=x1t: e.dma_start(out=x1_v[t], in_=x1t), reads=[kx1], writes=["x1_d"])
        rms_rows(x1t, 4, gssq, grstd, gjunk, kx1, "g_")
        for s_ in range(4):
            ti = t * 4 + s_
            P.dve(lambda e, s_=s_, x1t=x1t: e.scalar_tensor_tensor(h2f, x1t[:, s_, :], grstd[:, s_:s_ + 1], gm2_bc, ALU.mult, ALU.mult), reads=[kx1, "g_rstd", "gm2_bc"], writes=["h2f"])
            P.dve(lambda e: e.tensor_tensor(h2f, h2f, sh2_bc, ALU.add), reads=["h2f", "sh2_bc"], writes=["h2f"])
            P.act(lambda e, s_=s_: e.copy(h2b[:, s_, :], h2f), reads=["h2f"], writes=["h2b"])
            for q in range(2):
                ps, pk = next_ps()

                def fT(e, ps=ps, q=q):
                    for kk in range(4):
                        kc = q * 4 + kk
                        ins = e.transpose(ps[:, kk * 128:(kk + 1) * 128], h2f[:, kc * 128:(kc + 1) * 128], ident_f)
                    return ins
                P.pe(fT, reads=["h2f", "ident_f"], writes=[pk])
                if q == 0:
                    P.act(lambda e, ps=ps, q=q: e.copy(h2T[:, q * 4:(q + 1) * 4, :], ps.rearrange("p (a b) -> p a b", a=4)), reads=[pk], writes=["h2T"])
                else:
                    P.dve(lambda e, ps=ps, q=q: e.tensor_copy(h2T[:, q * 4:(q + 1) * 4, :], ps.rearrange("p (a b) -> p a b", a=4)), reads=[pk], writes=["h2T"])
            ps, pk = next_ps()

            def fL(e, ps=ps):
                for k in range(8):
                    e.matmul(ps[:, 0:NE], h2T[:, k, :], wrt_f[:, k, :], start=(k == 0), stop=False)
                return e.matmul(ps[:, 0:NE], ones_f[0:1, :], brt[0:1, :], start=False, stop=True)
            P.pe(fL, reads=["h2T", "wrt_f", "brt", "ones_f"], writes=[pk])
            P.act(lambda e, ps=ps, ti=ti: e.copy(Lg[:, ti, :], ps[:, 0:NE]), reads=[pk], writes=["Lg"])
        P.dma(SQ, lambda e, t=t: e.dma_start(out=h2_v[t], in_=h2b), reads=["h2b"], writes=["h2_d"])
    P.barrier()
    AR.release()
    if stop_after == "G":
        return finish()

    AR.mark()
    NTI = 64
    m8 = AR.alloc([128, NTI, 8]); i8 = AR.alloc([128, NTI, 8], U32); i8f = AR.alloc([128, NTI, 8])
    iota_e = AR.alloc([128, NE]); iota_i = AR.alloc([128, NE], I32)
    oh = [AR.alloc([128, NTI, NE]) for _ in range(4)]
    msk = AR.alloc([128, NTI, NE]); ex = AR.alloc([128, NTI, NE]); den = AR.alloc([128, NTI]); nmx = AR.alloc([128, NTI])
    cntp = AR.alloc([128, NE]); cntp_b = AR.alloc([128, NE], BF16)
    base = AR.alloc([128, NE]); tot = AR.alloc([128, NE]); pad = AR.alloc([128, NE]); ends = AR.alloc([128, NE]); starts = AR.alloc([128, NE])
    pref = AR.alloc([128, NTI, NE]); dst = AR.alloc([128, NTI, NE]); tmp3 = AR.alloc([128, NTI, NE])
    dk = AR.alloc([128, NTI, 4]); ones_e = AR.alloc([128, NTI])
    bthr = AR.alloc([128, NBLK]); bthr_i = AR.alloc([128, NBLK], I32); cmp = AR.alloc([128, NBLK, NE]); ebf = AR.alloc([128, NBLK])
    P.pool(lambda e: e.iota(iota_i, pattern=[[1, NE]], base=0, channel_multiplier=0), writes=["iota_i"])
    P.dve(lambda e: e.tensor_copy(iota_e, iota_i), reads=["iota_i"], writes=["iota_e"])
    P.pool(lambda e: e.iota(bthr_i, pattern=[[BLK, NBLK]], base=0, channel_multiplier=0), writes=["bthr_i"])
    P.dve(lambda e: e.tensor_copy(bthr, bthr_i), reads=["bthr_i"], writes=["bthr"])
    P.pool(lambda e: e.memset(ones_e, 1.0), writes=["ones_e"])
    for ti in range(NTI):
        P.dve(lambda e, ti=ti: e.max(m8[:, ti, :], Lg[:, ti, :]), reads=["Lg"], writes=["m8"])
        P.dve(lambda e, ti=ti: e.max_index(i8[:, ti, :], m8[:, ti, :], Lg[:, ti, :]), reads=["Lg", "m8"], writes=["i8"])
    P.dve(lambda e: e.tensor_copy(i8f, i8), reads=["i8"], writes=["i8f"])
    for k in range(4):
        P.dve(lambda e, k=k: e.tensor_tensor(oh[k], iota_e.unsqueeze(1).to_broadcast([128, NTI, NE]), i8f[:, :, k:k + 1].to_broadcast([128, NTI, NE]), ALU.is_equal), reads=["iota_e", "i8f"], writes=["oh%d" % k])
    P.dve(lambda e: e.tensor_tensor(msk, oh[0], oh[1], ALU.add), reads=["oh0", "oh1"], writes=["msk"])
    P.dve(lambda e: e.tensor_tensor(msk, msk, oh[2], ALU.add), reads=["msk", "oh2"], writes=["msk"])
    P.dve(lambda e: e.tensor_tensor(msk, msk, oh[3], ALU.add), reads=["msk", "oh3"], writes=["msk"])
    P.dve(lambda e: e.tensor_tensor(ex, Lg, m8[:, :, 0:1].to_broadcast([128, NTI, NE]), ALU.subtract), reads=["Lg", "m8"], writes=["ex"])
    P.act(lambda e: e.activation(out=ex, in_=ex, func=AF.Exp), reads=["ex"], writes=["ex"])
    P.dve(lambda e: e.tensor_tensor(ex, ex, msk, ALU.mult), reads=["ex", "msk"], writes=["ex"])
    P.dve(lambda e: e.tensor_reduce(den, ex, AX.X, ALU.add), reads=["ex"], writes=["den"])
    P.dve(lambda e: e.reciprocal(den, den), reads=["den"], writes=["den"])
    P.dve(lambda e: e.tensor_tensor(ex, ex, den.unsqueeze(2).to_broadcast([128, NTI, NE]), ALU.mult), reads=["ex", "den"], writes=["ex"])
    P.dve(lambda e: e.tensor_reduce(cntp, msk.rearrange("p t e -> p e t"), AX.X, ALU.add), reads=["msk"], writes=["cntp"])
    P.dve(lambda e: e.tensor_copy(cntp_b, cntp), reads=["cntp"], writes=["cntp_b"])
    psb_, kb_ = next_ps()
    P.pe(lambda e: e.matmul(psb_[:, 0:NE], ltri_b, cntp_b, start=True, stop=True), reads=["ltri_b", "cntp_b"], writes=[kb_])
    P.dve(lambda e: e.tensor_copy(base, psb_[:, 0:NE]), reads=[kb_], writes=["base"])
    pst_, kt_ = next_ps()
    P.pe(lambda e: e.matmul(pst_[:, 0:NE], ones_b[:, 0:128], cntp_b, start=True, stop=True), reads=["ones_b", "cntp_b"], writes=[kt_])
    P.dve(lambda e: e.tensor_copy(tot, pst_[:, 0:NE]), reads=[kt_], writes=["tot"])
    P.dve(lambda e: e.tensor_scalar(pad, tot, float(BLK - 1), 1.0 / BLK, ALU.add, ALU.mult), reads=["tot"], writes=["pad"])
    P.dve(lambda e: e.tensor_scalar_add(pad, pad, -0.4990234375), reads=["pad"], writes=["pad"])
    P.dve(lambda e: e.tensor_scalar_add(pad, pad, 8388608.0), reads=["pad"], writes=["pad"])
    P.dve(lambda e: e.tensor_scalar_add(pad, pad, -8388608.0), reads=["pad"], writes=["pad"])
    P.dve(lambda e: e.tensor_scalar_mul(pad, pad, float(BLK)), reads=["pad"], writes=["pad"])
    P.dve(lambda e: e.tensor_tensor_scan(ends, ones_e[:, 0:NE], pad, 0.0, ALU.mult, ALU.add), reads=["pad", "ones_e"], writes=["ends"])
    P.dve(lambda e: e.tensor_tensor(starts, ends, pad, ALU.subtract), reads=["ends", "pad"], writes=["starts"])
    P.dve(lambda e: e.tensor_tensor(base, base, starts, ALU.add), reads=["base", "starts"], writes=["base"])
    for ee in range(NE):
        P.dve(lambda e, ee=ee: e.tensor_tensor_scan(pref[:, :, ee], ones_e, msk[:, :, ee], 0.0, ALU.mult, ALU.add), reads=["msk", "ones_e"], writes=["pref"])
    P.dve(lambda e: e.tensor_tensor(pref, pref, msk, ALU.subtract), reads=["pref", "msk"], writes=["pref"])
    P.dve(lambda e: e.tensor_tensor(dst, pref, base.unsqueeze(1).to_broadcast([128, NTI, NE]), ALU.add), reads=["pref", "base"], writes=["dst"])
    for k in range(4):
        P.dve(lambda e, k=k: e.tensor_tensor(tmp3, oh[k], dst, ALU.mult), reads=["oh%d" % k, "dst"], writes=["tmp3"])
        P.dve(lambda e, k=k: e.tensor_reduce(dk[:, :, k], tmp3, AX.X, ALU.add), reads=["tmp3"], writes=["dk"])
        P.dve(lambda e, k=k: e.tensor_tensor(tmp3, oh[k], ex, ALU.mult), reads=["oh%d" % k, "ex", "tmp3"], writes=["tmp3"])
        P.dve(lambda e, k=k: e.tensor_reduce(wk[:, :, k], tmp3, AX.X, ALU.add), reads=["tmp3"], writes=["wk"])
    P.dve(lambda e: e.tensor_copy(dest_i, dk), reads=["dk"], writes=["dest_i"])
    P.dve(lambda e: e.tensor_tensor(cmp, ends.unsqueeze(1).to_broadcast([128, NBLK, NE]), bthr.unsqueeze(2).to_broadcast([128, NBLK, NE]), ALU.is_le), reads=["ends", "bthr"], writes=["cmp"])
    P.dve(lambda e: e.tensor_reduce(ebf, cmp, AX.X, ALU.add), reads=["cmp"], writes=["ebf"])
    P.dve(lambda e: e.tensor_scalar_min(ebf, ebf, float(NE - 1)), reads=["ebf"], writes=["ebf"])
    P.dve(lambda e: e.tensor_copy(eb_i, ebf), reads=["ebf"], writes=["eb_i"])
    pidx_i = AR.alloc([128, 8], I32); pidx = AR.alloc([128, 8]); widx_f = AR.alloc([128, NBLK, 8])
    P.pool(lambda e: e.iota(pidx_i, pattern=[[128, 8]], base=0, channel_multiplier=1), writes=["pidx_i"])
    P.dve(lambda e: e.tensor_copy(pidx, pidx_i), reads=["pidx_i"], writes=["pidx"])
    P.dve(lambda e: e.tensor_scalar_mul(ebf, ebf, 1024.0), reads=["ebf", "eb_i"], writes=["ebf"])
    P.dve(lambda e: e.tensor_tensor(widx_f, ebf.unsqueeze(2).to_broadcast([128, NBLK, 8]), pidx.unsqueeze(1).to_broadcast([128, NBLK, 8]), ALU.add), reads=["ebf", "pidx"], writes=["widx_f"])
    P.dve(lambda e: e.tensor_copy(widx, widx_f), reads=["widx_f"], writes=["widx"])
    AR.mark()
    hrow = [AR.alloc([128, D], BF16) for _ in range(2)]
    h2_r = h2_d.rearrange("(t p) d -> t p d", p=128)
    for ti in range(NTI):
        bi = ti % 2
        P.dma(LQ, lambda e, ti=ti, bi=bi: e.dma_start(out=hrow[bi], in_=h2_r[ti]), reads=["h2_d"], writes=["hrow%d" % bi])
        for k in range(4):
            P.dma("pool", lambda e, ti=ti, bi=bi, k=k: e.indirect_dma_start(out=xg_d, out_offset=bass.IndirectOffsetOnAxis(ap=dest_i[:, ti, k:k + 1], axis=0), in_=hrow[bi], in_offset=None), reads=["hrow%d" % bi, "dest_i"], writes=["xg_d"])
    P.barrier()
    AR.release()
    AR.release()
    if stop_after == "H":
        return finish()

    AR.release()
    AR.mark()
    NBIG = 3
    NSML = 4
    stgB = [AR.alloc([128, 2048]) for _ in range(NBIG)]
    stgS = [AR.alloc([128, 1024]) for _ in range(NSML)]
    wgu_b = [AR.alloc([128, 8, 2 * D], BF16) for _ in range(2)]
    wdn_b = AR.alloc([128, 8, D], BF16)
    bgb = [AR.alloc([1, 3 * D], BF16) for _ in range(2)]
    xrows = AR.alloc([128, 4, D], BF16)
    xT = [AR.alloc([128, 8, BLK], BF16) for _ in range(2)]
    aT = AR.alloc([128, 8, BLK], BF16)
    eg = [AR.alloc([128, BLK]) for _ in range(2)]
    es = [AR.alloc([128, BLK]) for _ in range(2)]
    eu = [AR.alloc([128, BLK]) for _ in range(2)]
    ysb = [AR.alloc([128, D]) for _ in range(2)]
    xg_v = xg_d.rearrange("(b s p) d -> b p s d", p=128, s=4)
    ys_v = ys_d.rearrange("(b s p) d -> b s p d", p=128, s=4)
    wgu_rows = w_gu.rearrange("e k n -> (e k) n")
    wdn_rows = w_down.rearrange("e k n -> (e k) n")
    rB = [0]
    rS = [0]

    def item(kind, b, kc=0):
        pb = b % 2
        if kind in ("wgu", "bgu"):
            si = rB[0] % NBIG
            rB[0] += 1
            stg = stgB[si]; sk = "stgB%d" % si; ncol = 2048
        else:
            si = rS[0] % NSML
            rS[0] += 1
            stg = stgS[si]; sk = "stgS%d" % si; ncol = 1024
        if kind == "wgu":
            src, idx, dst, dkey, p0 = wgu_rows, widx[:, b, kc:kc + 1], wgu_b[pb][:, kc, :], "wgu%d_%d" % (pb, kc), 128
        elif kind == "wdn":
            src, idx, dst, dkey, p0 = wdn_rows, widx[:, b, kc:kc + 1], wdn_b[:, kc, :], "wdn_%d" % kc, 128
        elif kind == "bgu":
            src, idx, dst, dkey, p0 = b_gu, eb_i[:, b:b + 1], bgb[pb][0:1, 0:2048], "bgb%d" % pb, 1
        else:
            src, idx, dst, dkey, p0 = b_down, eb_i[:, b:b + 1], bgb[pb][0:1, 2048:3072], "bgb%d" % pb, 1

        def g_emit():
            P.dma("pool", lambda e: e.indirect_dma_start(out=stg[:, 0:ncol], out_offset=None, in_=src, in_offset=bass.IndirectOffsetOnAxis(ap=idx, axis=0)), reads=["widx", "eb_i"], writes=[sk])

        def c_emit():
            P.act(lambda e: e.copy(dst, stg[0:p0, 0:ncol]), reads=[sk], writes=[dkey])
        return g_emit, c_emit

    for it_ in [item("bgu", 0), item("bdn", 0)] + [item("wgu", 0, kc) for kc in range(8)]:
        it_[0]()
        it_[1]()
    P.dma(LQ, lambda e: e.dma_start(out=xrows, in_=xg_v[0]), reads=["xg_d"], writes=["xrows"])
    for b in range(NBLK):
        pb = b % 2
        gath = [[] for _ in range(12)]
        cast = [[] for _ in range(12)]
        if b + 1 < NBLK:
            for kind in ("bgu", "bdn"):
                ge, ce = item(kind, b + 1)
                gath[0].append(ge); cast[1].append(ce)
        for kc in range(8):
            ge, ce = item("wdn", b, kc)
            gath[kc // 2].append(ge); cast[kc // 2 + 1].append(ce)
        if b + 1 < NBLK:
            for kc in range(8):
                ge, ce = item("wgu", b + 1, kc)
                gath[kc].append(ge); cast[kc + 2].append(ce)
        for kc in range(8):
            ps, pk = next_ps()
            psb = ps.bitcast(BF16)

            def fx(e, psb=psb, kc=kc):
                for s_ in range(4):
                    ins = e.transpose(psb[:, s_ * 128:(s_ + 1) * 128], xrows[:, s_, kc * 128:(kc + 1) * 128], ident_b)
                return ins
            P.pe(fx, reads=["xrows", "ident_b"], writes=[pk])
            if kc % 2 == 0:
                P.act(lambda e, psb=psb, kc=kc, pb=pb: e.copy(xT[pb][:, kc, :], psb[:, 0:BLK]), reads=[pk], writes=["xT%d" % pb])
            else:
                P.dve(lambda e, psb=psb, kc=kc, pb=pb: e.tensor_copy(xT[pb][:, kc, :], psb[:, 0:BLK]), reads=[pk], writes=["xT%d" % pb])
        if b + 1 < NBLK:
            P.dma(LQ, lambda e, b=b: e.dma_start(out=xrows, in_=xg_v[b + 1]), reads=["xg_d"], writes=["xrows"])
        wkeys = ["wgu%d_%d" % (pb, kc) for kc in range(8)]
        pend = None
        for cc in range(8):
            for fn in gath[cc]:
                fn()
            for fn in cast[cc]:
                fn()
            psg, kg = next_ps()
            psu, ku = next_ps()

            def fg(e, psg=psg, cc=cc, pb=pb):
                for k in range(8):
                    e.matmul(psg, wgu_b[pb][:, k, cc * 128:(cc + 1) * 128], xT[pb][:, k, :], start=(k == 0), stop=False)
                return e.matmul(psg, bgb[pb][0:1, cc * 128:(cc + 1) * 128], ones_b[0:1, 0:BLK], start=False, stop=True)

            def fu(e, psu=psu, cc=cc, pb=pb):
                for k in range(8):
                    e.matmul(psu, wgu_b[pb][:, k, D + cc * 128:D + (cc + 1) * 128], xT[pb][:, k, :], start=(k == 0), stop=False)
                return e.matmul(psu, bgb[pb][0:1, D + cc * 128:D + (cc + 1) * 128], ones_b[0:1, 0:BLK], start=False, stop=True)
            P.pe(fg, reads=wkeys + ["xT%d" % pb, "bgb%d" % pb, "ones_b"], writes=[kg])
            P.pe(fu, reads=wkeys + ["xT%d" % pb, "bgb%d" % pb, "ones_b"], writes=[ku])
            q = cc % 2
            P.dve(lambda e, psg=psg, q=q: e.tensor_scalar_min(eg[q], psg, 7.0), reads=[kg], writes=["eg%d" % q])
            P.act(lambda e, q=q: e.activation(out=es[q], in_=eg[q], func=AF.Sigmoid, scale=1.702), reads=["eg%d" % q], writes=["es%d" % q])
            P.dve(lambda e, psu=psu, q=q: e.tensor_scalar(eu[q], psu, 7.0, -7.0, ALU.min, ALU.max), reads=[ku], writes=["eu%d" % q])

            def tail(cc=cc, q=q):
                P.dve(lambda e: e.tensor_tensor(eg[q], eg[q], es[q], ALU.mult), reads=["eg%d" % q, "es%d" % q], writes=["eg%d" % q])
                P.dve(lambda e: e.scalar_tensor_tensor(aT[:, cc, :], eu[q], 1.0, eg[q], ALU.add, ALU.mult), reads=["eu%d" % q, "eg%d" % q], writes=["aT"])
            if pend is not None:
                pend()
            pend = tail
        pend()
        dkeys = ["wdn_%d" % kc for kc in range(8)]
        for s_ in range(4):
            for fn in gath[8 + s_]:
                fn()
            for fn in cast[8 + s_]:
                fn()
            yb = ysb[s_ % 2]; ky = "ysb%d" % (s_ % 2)
            for half in range(2):
                ps, pk = next_ps()

                def fd(e, ps=ps, s_=s_, half=half, pb=pb):
                    for k in range(8):
                        e.matmul(ps, aT[:, k, s_ * 128:(s_ + 1) * 128], wdn_b[:, k, half * 512:(half + 1) * 512], start=(k == 0), stop=False)
                    c0 = 2 * D + half * 512
                    return e.matmul(ps, ones_b[0:1, 0:128], bgb[pb][0:1, c0:c0 + 512], start=False, stop=True)
                P.pe(fd, reads=["aT", "bgb%d" % pb, "ones_b"] + dkeys, writes=[pk])
                if half == 0:
                    P.act(lambda e, ps=ps, yb=yb: e.copy(yb[:, 0:512], ps), reads=[pk], writes=[ky])
                else:
                    P.dve(lambda e, ps=ps, yb=yb: e.tensor_copy(yb[:, 512:1024], ps), reads=[pk], writes=[ky])
            P.dma(LQ, lambda e, b=b, s_=s_, yb=yb: e.dma_start(out=ys_v[b, s_], in_=yb), reads=[ky], writes=["ys_d"])
    P.barrier()
    AR.release()
    if stop_after == "I":
        return finish()

    AR.mark()
    yk = [[AR.alloc([128, D]) for _ in range(4)] for _ in range(2)]
    x1r = [AR.alloc([128, D]) for _ in range(2)]
    acc = AR.alloc([128, D]); outt = [AR.alloc([128, D]) for _ in range(2)]
    jjunk = AR.alloc([128, D]); jssq = AR.alloc([128, 64]); jrstd = AR.alloc([128, 64])
    x1_r = x1_d.rearrange("(t p) d -> t p d", p=128)
    y_r = y.rearrange("(t p) d -> t p d", p=128)

    def j_load(ti):
        bi = ti % 2
        for k in range(4):
            P.dma("pool", lambda e, k=k: e.indirect_dma_start(out=yk[bi][k], out_offset=None, in_=ys_d, in_offset=bass.IndirectOffsetOnAxis(ap=dest_i[:, ti, k:k + 1], axis=0)), reads=["ys_d", "dest_i"], writes=["yk%d_%d" % (bi, k)])
        P.dma(LQ, lambda e: e.dma_start(out=x1r[bi], in_=x1_r[ti]), reads=["x1_d"], writes=["x1r%d" % bi])
    j_load(0)
    for ti in range(NTI):
        bi = ti % 2
        if ti + 1 < NTI:
            j_load(ti + 1)
        P.dve(lambda e, bi=bi, ti=ti: e.tensor_scalar_mul(acc, yk[bi][0], wk[:, ti, 0:1]), reads=["yk%d_0" % bi, "wk"], writes=["acc"])
        for k in range(1, 4):
            P.dve(lambda e, bi=bi, ti=ti, k=k: e.scalar_tensor_tensor(acc, yk[bi][k], wk[:, ti, k:k + 1], acc, ALU.mult, ALU.add), reads=["yk%d_%d" % (bi, k), "wk", "acc"], writes=["acc"])
        P.pool(lambda e: e.tensor_tensor(acc, acc, g2_bc, ALU.mult), reads=["acc", "g2_bc"], writes=["acc"])
        P.pool(lambda e, bi=bi: e.tensor_tensor(acc, acc, x1r[bi], ALU.add), reads=["acc", "x1r%d" % bi], writes=["acc"])
        P.act(lambda e, ti=ti: e.activation(out=jjunk, in_=acc, func=AF.Square, accum_out=jssq[:, ti:ti + 1]), reads=["acc"], writes=["jjunk", "jssq"])
        P.dve(lambda e, ti=ti: e.tensor_scalar(jrstd[:, ti:ti + 1], jssq[:, ti:ti + 1], 1.0 / D, EPS, ALU.mult, ALU.add), reads=["jssq"], writes=["jrstd"])
        P.act(lambda e, ti=ti: e.activation(out=jrstd[:, ti:ti + 1], in_=jrstd[:, ti:ti + 1], func=AF.Ln), reads=["jrstd"], writes=["jrstd"])
        P.act(lambda e, ti=ti: e.activation(out=jrstd[:, ti:ti + 1], in_=jrstd[:, ti:ti + 1], func=AF.Exp, scale=-0.5), reads=["jrstd"], writes=["jrstd"])
        P.dve(lambda e, bi=bi, ti=ti: e.scalar_tensor_tensor(outt[bi], acc, jrstd[:, ti:ti + 1], nf_bc, ALU.mult, ALU.mult), reads=["acc", "jrstd", "nf_bc"], writes=["outt%d" % bi])
        P.dma(LQ, lambda e, bi=bi, ti=ti: e.dma_start(out=y_r[ti], in_=outt[bi]), reads=["outt%d" % bi], writes=["y"])
    AR.release()
    return finish()


_CACHE = {}


def kernel(**inputs):
    n = 8
    if "nc" not in _CACHE:
        _CACHE["nc"] = build_program()
    nc = _CACHE["nc"]
    tabs = dft_tables()
    f = np.float32

    def a(v):
        return np.ascontiguousarray(np.asarray(v, dtype=f))
    shared = dict(
        c_ctx=a(inputs["c_ctx"]).reshape(1, D), w_ada=a(inputs["w_ada"][0]), b_ada=a(inputs["b_ada"][0]).reshape(1, -1),
        norm1=a(inputs["norm1"][0]).reshape(1, D), w_in=a(inputs["w_in"][0]), conv_w=a(inputs["conv_w"][0]),
        conv_b=a(inputs["conv_b"][0]).reshape(1, D), gate_a_w=a(inputs["gate_a_w"][0]), gate_a_b=a(inputs["gate_a_b"][0]),
        gate_x_w=a(inputs["gate_x_w"][0]), gate_x_b=a(inputs["gate_x_b"][0]), lru_lambda=a(inputs["lru_lambda"][0]),
        w_fourier=a(inputs["w_fourier"][0]), w_rnn=a(inputs["w_rnn"][0]), w_out=a(inputs["w_out"][0]),
        norm2=a(inputs["norm2"][0]).reshape(1, D), w_router=a(inputs["w_router"][0]), b_router=a(inputs["b_router"][0]).reshape(1, NE),
        w_gu=a(inputs["w_gu"][0]), b_gu=a(inputs["b_gu"][0]), w_down=a(inputs["w_down"][0]), b_down=a(inputs["b_down"][0]),
        norm_f=a(inputs["norm_f"]).reshape(1, D), **tabs)
    xs = a(inputs["x"]); cs = a(inputs["c"]); cx = a(inputs["ctx"])
    in_maps = []
    for i in range(n):
        m = dict(shared)
        m["x"] = xs[i]; m["c"] = cs[i].reshape(1, D); m["ctx"] = cx[i]
        in_maps.append(m)
    ncore = int(os.environ.get("KDBG_NCORE", "8"))
    if ncore != 8:
        res = run_bass_kernel_spmd(nc, in_maps[:ncore], core_ids=list(range(ncore)))
        return np.stack([np.asarray(r["y"], dtype=f) for r in res.results], axis=0)
    res = run_bass_kernel_spmd(nc, in_maps, core_ids=list(range(n)))
    return np.stack([np.asarray(r["y"], dtype=f) for r in res.results], axis=0)
```

```python
import contextlib
import math
import os
import numpy as np
import concourse.bass as bass
import concourse.mybir as mybir
from concourse.bass_utils import run_bass_kernel_spmd

F32 = mybir.dt.float32
BF16 = mybir.dt.bfloat16
I32 = mybir.dt.int32
U32 = mybir.dt.uint32
ALU = mybir.AluOpType
AF = mybir.ActivationFunctionType
AX = mybir.AxisListType

SELF_SYNC = True
NDSEM = 6

D = 1024
S = 8192
CTX = 256
NE = 32
BLK = 512
NBLK = 96
PSLOTS = NBLK * BLK
EPS = 1e-6


class Prog:
    ENGS = ("pe", "act", "dve", "pool", "sp")

    def __init__(self, nc):
        self.nc = nc
        self.ops = []

    def add(self, eng, fn, reads=(), writes=(), dma=False):
        self.ops.append(dict(eng=eng, fn=fn, reads=tuple(reads), writes=tuple(writes), dma=dma, bar=False))

    def pe(self, fn, reads=(), writes=()):
        self.add("pe", fn, reads, writes)

    def act(self, fn, reads=(), writes=()):
        self.add("act", fn, reads, writes)

    def dve(self, fn, reads=(), writes=()):
        self.add("dve", fn, reads, writes)

    def pool(self, fn, reads=(), writes=()):
        self.add("pool", fn, reads, writes)

    def dma(self, q, fn, reads=(), writes=()):
        self.add(q, fn, reads, writes, dma=True)

    def barrier(self):
        for e in self.ENGS:
            self.ops.append(dict(eng=e, fn=None, reads=(), writes=(), dma=False, bar=True))

    def emit(self):
        nc = self.nc
        ops = self.ops
        cnt = {e: 0 for e in self.ENGS}
        dcnt = {e: 0 for e in self.ENGS}
        latest = {}
        for op in ops:
            e = op["eng"]
            if op["bar"]:
                op["barvals"] = dict(latest)
                continue
            if op["dma"]:
                i = dcnt[e]
                dcnt[e] += 1
                op["sem"] = ("d", e, i % NDSEM)
                op["val"] = 16 * (i // NDSEM + 1)
            else:
                cnt[e] += 1
                op["sem"] = ("c", e)
                op["val"] = cnt[e]
            latest[op["sem"]] = op["val"]
        last_w = {}
        readers = {}
        for op in ops:
            if op["bar"]:
                continue
            deps = []
            for k in op["reads"]:
                if k in last_w:
                    deps.append(last_w[k])
            for k in op["writes"]:
                if k in last_w:
                    deps.append(last_w[k])
                deps.extend(readers.get(k, ()))
            op["deps"] = deps
            for k in op["writes"]:
                last_w[k] = op
                readers[k] = []
            for k in op["reads"]:
                if k not in op["writes"]:
                    readers.setdefault(k, []).append(op)
        known = {e: {} for e in self.ENGS}
        for op in ops:
            e = op["eng"]
            need = {}
            if op["bar"]:
                need = dict(op["barvals"])
                need.pop(("c", e), None)
            else:
                for d in op["deps"]:
                    if d is op:
                        continue
                    if (not d["dma"]) and d["eng"] == e and (e == "pe" or not SELF_SYNC):
                        continue
                    s, v = d["sem"], d["val"]
                    if need.get(s, 0) < v:
                        need[s] = v
                if op["dma"] and op["val"] > 16:
                    s = op["sem"]
                    if need.get(s, 0) < op["val"] - 16:
                        need[s] = op["val"] - 16
            w = []
            for s, v in need.items():
                if known[e].get(s, 0) < v:
                    known[e][s] = v
                    w.append((s, v))
            op["waits"] = w
        final = latest
        semkeys = sorted(final.keys(), key=str)
        with contextlib.ExitStack() as st:
            sems = {}
            for k in semkeys:
                sems[k] = st.enter_context(nc.semaphore("s_" + "_".join(map(str, k))))
            block = st.enter_context(nc.Block())

            def run(engname):
                def body(eng):
                    for op in ops:
                        if op["eng"] != engname:
                            continue
                        for s, v in op["waits"]:
                            eng.wait_ge(sems[s], v)
                        if op["bar"]:
                            continue
                        ins = op["fn"](eng)
                        ins.then_inc(sems[op["sem"]], 16 if op["dma"] else 1)
                    for k, v in final.items():
                        if k[0] == "d" and k[1] == engname:
                            eng.wait_ge(sems[k], v)
                return body

            block.sync(run("sp"))
            block.scalar(run("act"))
            block.vector(run("dve"))
            block.gpsimd(run("pool"))
            block.tensor(run("pe"))


class Arena:
    def __init__(self, base_ap, nbytes):
        self.base = base_ap
        self.nbytes = nbytes
        self.off = 0
        self.marks = []

    def alloc(self, shape, dt=F32):
        esz = {F32: 4, BF16: 2, I32: 4, U32: 4}[dt]
        npart = shape[0]
        fshape = list(shape[1:])
        n = 1
        for s_ in fshape:
            n *= s_
        nb = (n * esz + 31) // 32 * 32
        assert self.off + nb <= self.nbytes, ("SBUF arena overflow", self.off, nb, self.nbytes)
        a = self.base[0:npart, self.off // 4:(self.off + nb) // 4]
        self.off += nb
        if dt != F32:
            a = a.bitcast(dt)
        a = a[:, 0:n]
        if len(fshape) == 2:
            a = a.rearrange("p (a b) -> p a b", a=fshape[0])
        elif len(fshape) == 3:
            a = a.rearrange("p (a b c) -> p a b c", a=fshape[0], b=fshape[1])
        return a

    def mark(self):
        self.marks.append(self.off)

    def release(self):
        self.off = self.marks.pop()


def dft_tables():
    n = np.arange(128)
    ang = 2 * np.pi * np.outer(n, n) / 128.0
    c128 = np.cos(ang)
    s128 = np.sin(ang)
    k1 = np.arange(128)[:, None]
    n2 = np.arange(64)[None, :]
    tw = 2 * np.pi * k1 * n2 / 8192.0
    twc = np.cos(tw)
    tws = np.sin(tw)
    a64 = 2 * np.pi * np.outer(np.arange(64), np.arange(64)) / 64.0
    c2 = np.cos(a64)
    s2 = np.sin(a64)
    t3 = np.zeros((128, 128))
    t3[0:64, 0:64] = c2
    t3[64:128, 0:64] = -s2
    t3[0:64, 64:128] = s2
    t3[64:128, 64:128] = c2
    t3 = t3 / 1024.0
    f = np.float32
    return dict(k_c128=c128.astype(f), k_s128=s128.astype(f), k_twc=twc.astype(f), k_tws=tws.astype(f), k_t3=t3.astype(f))


def build_program(stop_after=None, debug=False):
    nc = bass.Bass("TRN2", target_bir_lowering=False)

    def din(name, shape, dt=F32):
        return nc.dram_tensor(name, list(shape), dt, kind="ExternalInput").ap()

    DBGSET = set(os.environ.get("KDBG_OUT", "").split(",")) if debug else set()

    def dscr(name, shape, dt=F32):
        if name in DBGSET:
            return nc.dram_tensor(name, list(shape), dt, kind="ExternalOutput").ap()
        return nc.dram_tensor(name, list(shape), dt).ap()

    x = din("x", [S, D]); c = din("c", [1, D]); ctx = din("ctx", [CTX, D]); c_ctx = din("c_ctx", [1, D])
    w_ada = din("w_ada", [D, 6 * D]); b_ada = din("b_ada", [1, 6 * D]); norm1 = din("norm1", [1, D])
    w_in = din("w_in", [D, 4608]); conv_w = din("conv_w", [4, D]); conv_b = din("conv_b", [1, D])
    gate_a_w = din("gate_a_w", [2, 8, 128, 128]); gate_a_b = din("gate_a_b", [2, D])
    gate_x_w = din("gate_x_w", [2, 8, 128, 128]); gate_x_b = din("gate_x_b", [2, D])
    lru_lambda = din("lru_lambda", [2, D])
    w_fourier = din("w_fourier", [512, D]); w_rnn = din("w_rnn", [D, D]); w_out = din("w_out", [D, D])
    norm2 = din("norm2", [1, D]); w_router = din("w_router", [D, NE]); b_router = din("b_router", [1, NE])
    w_gu = din("w_gu", [NE, D, 2 * D]); b_gu = din("b_gu", [NE, 2 * D])
    w_down = din("w_down", [NE, D, D]); b_down = din("b_down", [NE, D]); norm_f = din("norm_f", [1, D])
    k_c128 = din("k_c128", [128, 128]); k_s128 = din("k_s128", [128, 128])
    k_twc = din("k_twc", [128, 64]); k_tws = din("k_tws", [128, 64]); k_t3 = din("k_t3", [128, 128])
    y = nc.dram_tensor("y", [S, D], F32, kind="ExternalOutput").ap()
    dbg = nc.dram_tensor("dbg", [128, 2048], F32, kind="ExternalOutput").ap() if debug else None

    u_d = dscr("u_d", [S, 512], BF16)
    xc_d = dscr("xc_d", [D, S], F32)
    gg_d = dscr("gg_d", [D, S], BF16)
    gfr_d = dscr("gfr_d", [2 * D, S], BF16)
    q_d = dscr("q_d", [4, 2, 64, 128, 128], BF16)
    rt_d = dscr("rt_d", [D, S], BF16)
    yg_d = dscr("yg_d", [D, S], BF16)
    x1_d = dscr("x1_d", [S, D], F32)
    h2_d = dscr("h2_d", [S, D], BF16)
    xg_d = dscr("xg_d", [PSLOTS, D], BF16)
    ys_d = dscr("ys_d", [PSLOTS, D], F32)

    st = contextlib.ExitStack()
    SBN = 206848
    sball = st.enter_context(nc.sbuf_tensor("sball", [128, SBN // 4], F32))
    AR = Arena(sball[:], SBN)
    banks = [st.enter_context(nc.psum_tensor("psb%d" % i, [128, 512], F32)) for i in range(8)]
    P = Prog(nc)
    psn = [0]

    def next_ps():
        i = psn[0] % 8
        psn[0] += 1
        return banks[i][:], "ps%d" % i

    uid = [0]

    def K(name):
        uid[0] += 1
        return "%s#%d" % (name, uid[0])

    def finish():
        with nc.allow_low_precision(reason="bf16 matmul operands, fp32 accumulation"):
            P.emit()
        st.close()
        return nc

    LQ = "sp"
    SQ = "pool"

    ident_f = AR.alloc([128, 128]); ident_b = AR.alloc([128, 128], BF16)
    ones_b = AR.alloc([128, 512], BF16); ones_f = AR.alloc([128, 128])
    ltri_b = AR.alloc([128, 128], BF16)
    g2_bc = AR.alloc([128, D]); nf_bc = AR.alloc([128, D])
    dest_i = AR.alloc([128, 64, 4], I32); wk = AR.alloc([128, 64, 4])
    eb_i = AR.alloc([128, NBLK], I32); widx = AR.alloc([128, NBLK, 8], I32)
    AR.mark()
    g1_bc = AR.alloc([128, D]); gm2_bc = AR.alloc([128, D]); sh2_bc = AR.alloc([128, D])
    gm1T = AR.alloc([128, 8]); sh1T = AR.alloc([128, 8]); gmcT = AR.alloc([128, 8]); shcT = AR.alloc([128, 8])
    cwT = AR.alloc([128, 8, 4]); cbT = AR.alloc([128, 8])
    nbaT = AR.alloc([128, 16]); nbxT = AR.alloc([128, 16]); coefT = AR.alloc([128, 16])
    h0T = AR.alloc([128, 16])
    Lg = AR.alloc([128, 64, NE])

    P.pool(lambda e: e.memset(ident_f, 0.0), writes=["ident_f"])
    P.pool(lambda e: e.affine_select(out=ident_f, in_=ident_f, pattern=[[-1, 128]], compare_op=ALU.not_equal, fill=1.0, base=0, channel_multiplier=1), reads=["ident_f"], writes=["ident_f"])
    P.dve(lambda e: e.tensor_copy(ident_b, ident_f), reads=["ident_f"], writes=["ident_b"])
    P.pool(lambda e: e.memset(ones_b, 1.0), writes=["ones_b"])
    P.pool(lambda e: e.memset(ones_f, 1.0), writes=["ones_f"])
    P.pool(lambda e: e.memset(ltri_b, 1.0), writes=["ltri_b"])
    P.pool(lambda e: e.affine_select(out=ltri_b, in_=ltri_b, pattern=[[1, 128]], compare_op=ALU.is_gt, fill=0.0, base=0, channel_multiplier=-1), reads=["ltri_b"], writes=["ltri_b"])

    AR.mark()
    cT = AR.alloc([128, 16])
    crep = AR.alloc([128, 16, 128])
    mb = AR.alloc([128, 6 * D])
    mcb = AR.alloc([128, 2 * D])
    wa_buf = [AR.alloc([128, 8, 512]) for _ in range(2)]
    n1_bc = AR.alloc([128, D]); n2_bc = AR.alloc([128, D])
    P.dma(LQ, lambda e: e.dma_start(out=cT[:, 0:8], in_=c.rearrange("o (k p) -> p (o k)", p=128), allow_slow_non_contiguous=True), writes=["cT"])
    P.dma(LQ, lambda e: e.dma_start(out=cT[:, 8:16], in_=c_ctx.rearrange("o (k p) -> p (o k)", p=128), allow_slow_non_contiguous=True), reads=["cT"], writes=["cT"])
    P.dma(LQ, lambda e: e.dma_start(out=mb, in_=b_ada.partition_broadcast(128)), writes=["mb"])
    P.dma(LQ, lambda e: e.dma_start(out=n1_bc, in_=norm1.partition_broadcast(128)), writes=["n1_bc"])
    P.dma(LQ, lambda e: e.dma_start(out=n2_bc, in_=norm2.partition_broadcast(128)), writes=["n2_bc"])
    P.dma(LQ, lambda e: e.dma_start(out=nf_bc, in_=norm_f.partition_broadcast(128)), writes=["nf_bc"])
    ctmp = AR.alloc([128, 16])
    zt = AR.alloc([128, 4, D], BF16)
    P.pool(lambda e: e.memset(zt, 0.0), writes=["zt"])
    xgz_v = xg_d.rearrange("(b s p) d -> b p s d", p=128, s=4)
    for zb in range(NBLK):
        P.dma(SQ, lambda e, zb=zb: e.dma_start(out=xgz_v[zb], in_=zt), reads=["zt"], writes=["xg_d"])
    P.act(lambda e: e.activation(out=ctmp, in_=cT, func=AF.Exp, scale=-1.0), reads=["cT"], writes=["ctmp"])
    P.dve(lambda e: e.tensor_scalar_add(ctmp, ctmp, 1.0), reads=["ctmp"], writes=["ctmp"])
    P.dve(lambda e: e.reciprocal(ctmp, ctmp), reads=["ctmp"], writes=["ctmp"])
    P.dve(lambda e: e.tensor_tensor(cT, cT, ctmp, ALU.mult), reads=["ctmp", "cT"], writes=["cT"])
    for j in range(16):
        P.dve(lambda e, j=j: e.tensor_copy(crep[:, j, :], cT[:, j:j + 1].to_broadcast([128, 128])), reads=["cT"], writes=["crep"])
    w_ada_v = w_ada.rearrange("(k p) n -> p k n", p=128)
    for ch in range(12):
        bi = ch % 2
        P.dma(LQ, lambda e, ch=ch, bi=bi: e.dma_start(out=wa_buf[bi], in_=w_ada_v[:, :, ch * 512:(ch + 1) * 512]), writes=["wa%d" % bi])
        ps, pk = next_ps()

        def f(e, ps=ps, bi=bi):
            for k in range(8):
                ins = e.matmul(ps, crep[:, k, :], wa_buf[bi][:, k, :], start=(k == 0), stop=(k == 7))
            return ins
        P.pe(f, reads=["crep", "wa%d" % bi], writes=[pk])
        P.dve(lambda e, ps=ps, ch=ch: e.tensor_tensor(mb[:, ch * 512:(ch + 1) * 512], mb[:, ch * 512:(ch + 1) * 512], ps, ALU.add), reads=[pk, "mb"], writes=["mb"])
        if ch < 4:
            ps2, pk2 = next_ps()

            def f2(e, ps2=ps2, bi=bi):
                for k in range(8):
                    ins = e.matmul(ps2, crep[:, 8 + k, :], wa_buf[bi][:, k, :], start=(k == 0), stop=(k == 7))
                return ins
            P.pe(f2, reads=["crep", "wa%d" % bi], writes=[pk2])
            P.act(lambda e, ps2=ps2, ch=ch: e.copy(mcb[:, ch * 512:(ch + 1) * 512], ps2), reads=[pk2], writes=["mcb"])
    bada2 = AR.alloc([128, 2 * D])
    P.dma(LQ, lambda e: e.dma_start(out=bada2, in_=b_ada[:, 0:2 * D].partition_broadcast(128)), writes=["bada2"])
    P.dve(lambda e: e.tensor_tensor(mcb, mcb, bada2, ALU.add), reads=["mcb", "bada2"], writes=["mcb"])
    gm1_bc = AR.alloc([128, D]); gmc_bc = AR.alloc([128, D])
    P.dve(lambda e: e.scalar_tensor_tensor(gm1_bc, mb[:, D:2 * D], 1.0, n1_bc, ALU.add, ALU.mult), reads=["mb", "n1_bc"], writes=["gm1_bc"])
    P.dve(lambda e: e.scalar_tensor_tensor(gmc_bc, mcb[:, D:2 * D], 1.0, n1_bc, ALU.add, ALU.mult), reads=["mcb", "n1_bc"], writes=["gmc_bc"])
    P.dve(lambda e: e.scalar_tensor_tensor(gm2_bc, mb[:, 4 * D:5 * D], 1.0, n2_bc, ALU.add, ALU.mult), reads=["mb", "n2_bc"], writes=["gm2_bc"])
    P.act(lambda e: e.copy(g1_bc, mb[:, 2 * D:3 * D]), reads=["mb"], writes=["g1_bc"])
    P.act(lambda e: e.copy(sh2_bc, mb[:, 3 * D:4 * D]), reads=["mb"], writes=["sh2_bc"])
    P.act(lambda e: e.copy(g2_bc, mb[:, 5 * D:6 * D]), reads=["mb"], writes=["g2_bc"])
    for (src, sk, dst, dk) in ((gm1_bc, "gm1_bc", gm1T, "gm1T"), (mb, "mb", sh1T, "sh1T"), (gmc_bc, "gmc_bc", gmcT, "gmcT"), (mcb, "mcb", shcT, "shcT")):
        for kc in range(8):
            ps, pk = next_ps()
            P.pe(lambda e, ps=ps, src=src, kc=kc: e.transpose(ps[:, 0:128], src[:, kc * 128:(kc + 1) * 128], ident_f), reads=[sk, "ident_f"], writes=[pk])
            P.dve(lambda e, ps=ps, dst=dst, kc=kc: e.tensor_copy(dst[:, kc:kc + 1], ps[:, 0:1]), reads=[pk], writes=[dk])
    for kk_ in range(4):
        P.dma(LQ, lambda e, kk_=kk_: e.dma_start(out=cwT[:, :, kk_], in_=conv_w[kk_:kk_ + 1, :].rearrange("o (h p) -> p (o h)", p=128), allow_slow_non_contiguous=True), reads=["cwT"], writes=["cwT"])
    P.dma(LQ, lambda e: e.dma_start(out=cbT, in_=conv_b.rearrange("o (h p) -> p (o h)", p=128), allow_slow_non_contiguous=True), writes=["cbT"])
    P.dma(LQ, lambda e: e.dma_start(out=nbaT, in_=gate_a_b.rearrange("d (h p) -> p (d h)", p=128), allow_slow_non_contiguous=True), writes=["nbaT"])
    P.dma(LQ, lambda e: e.dma_start(out=nbxT, in_=gate_x_b.rearrange("d (h p) -> p (d h)", p=128), allow_slow_non_contiguous=True), writes=["nbxT"])
    P.dma(LQ, lambda e: e.dma_start(out=coefT, in_=lru_lambda.rearrange("d (h p) -> p (d h)", p=128), allow_slow_non_contiguous=True), writes=["coefT"])
    P.act(lambda e: e.activation(out=coefT, in_=coefT, func=AF.Exp, scale=-1.0), reads=["coefT"], writes=["coefT"])
    P.act(lambda e: e.activation(out=coefT, in_=coefT, func=AF.Ln, bias=1.0), reads=["coefT"], writes=["coefT"])
    P.dve(lambda e: e.tensor_scalar_mul(coefT, coefT, -8.0), reads=["coefT"], writes=["coefT"])
    P.barrier()
    AR.release()
    if stop_after == "A":
        return finish()

    AR.mark()
    gw_b = AR.alloc([128, 32, 128], BF16)
    AR.mark()
    win_b = AR.alloc([128, 8, 4608], BF16)
    AR.mark()
    stg = [AR.alloc([128, 8, 512]) for _ in range(2)]
    w_in_v = w_in.rearrange("(k p) n -> p k n", p=128)
    for ch in range(9):
        bi = ch % 2
        P.dma(LQ, lambda e, ch=ch, bi=bi: e.dma_start(out=stg[bi], in_=w_in_v[:, :, ch * 512:(ch + 1) * 512]), writes=["stg%d" % bi])
        if ch % 2 == 0:
            P.act(lambda e, ch=ch, bi=bi: e.copy(win_b[:, :, ch * 512:(ch + 1) * 512], stg[bi]), reads=["stg%d" % bi], writes=["win_b"])
        else:
            P.dve(lambda e, ch=ch, bi=bi: e.tensor_copy(win_b[:, :, ch * 512:(ch + 1) * 512], stg[bi]), reads=["stg%d" % bi], writes=["win_b"])
    for gi, gwd in enumerate((gate_a_w, gate_x_w)):
        bi = gi % 2
        P.dma(LQ, lambda e, gwd=gwd, bi=bi: e.dma_start(out=stg[bi][:, 0:4, :].rearrange("p a (b c) -> p (a b) c", c=128), in_=gwd.rearrange("d h i j -> i (d h) j")), writes=["stg%d" % bi])
        P.dve(lambda e, gi=gi, bi=bi: e.tensor_copy(gw_b[:, gi * 16:(gi + 1) * 16, :], stg[bi][:, 0:4, :].rearrange("p a (b c) -> p (a b) c", c=128)), reads=["stg%d" % bi], writes=["gw_b"])
    P.barrier()
    AR.release()
    if stop_after == "W":
        return finish()

    def rms_rows(xt, nsub, ssq, rstd, junk, kx, kpre):
        for s_ in range(nsub):
            P.act(lambda e, s_=s_: e.activation(out=junk, in_=xt[:, s_, :], func=AF.Square, accum_out=ssq[:, s_:s_ + 1]), reads=[kx], writes=[kpre + "junk", kpre + "ssq"])
        P.dve(lambda e: e.tensor_scalar(rstd[:, 0:nsub], ssq[:, 0:nsub], 1.0 / D, EPS, ALU.mult, ALU.add), reads=[kpre + "ssq"], writes=[kpre + "rstd"])
        P.act(lambda e: e.activation(out=rstd[:, 0:nsub], in_=rstd[:, 0:nsub], func=AF.Ln), reads=[kpre + "rstd"], writes=[kpre + "rstd"])
        P.act(lambda e: e.activation(out=rstd[:, 0:nsub], in_=rstd[:, 0:nsub], func=AF.Exp, scale=-0.5), reads=[kpre + "rstd"], writes=[kpre + "rstd"])

    def norm_transpose(xt, nsub, rstd, xs_b, hT, gT, sT, kx, kpre, khT):
        for s_ in range(nsub):
            P.act(lambda e, s_=s_: e.activation(out=xs_b[:, s_, :], in_=xt[:, s_, :], func=AF.Copy, scale=rstd[:, s_:s_ + 1]), reads=[kx, kpre + "rstd"], writes=[kpre + "xs"])
        for kc in range(8):
            ps, pk = next_ps()
            psb = ps.bitcast(BF16)

            def f(e, psb=psb, kc=kc):
                for s_ in range(nsub):
                    ins = e.transpose(psb[:, s_ * 128:(s_ + 1) * 128], xs_b[:, s_, kc * 128:(kc + 1) * 128], ident_b)
                return ins
            P.pe(f, reads=[kpre + "xs", "ident_b"], writes=[pk])
            n = nsub * 128
            if kc % 2 == 0:
                P.act(lambda e, psb=psb, kc=kc, n=n: e.activation(out=hT[:, kc, 0:n], in_=psb[:, 0:n], func=AF.Identity, scale=gT[:, kc:kc + 1], bias=sT[:, kc:kc + 1]), reads=[pk], writes=[khT])
            else:
                P.dve(lambda e, psb=psb, kc=kc, n=n: e.tensor_scalar(hT[:, kc, 0:n], psb[:, 0:n], gT[:, kc:kc + 1], sT[:, kc:kc + 1], ALU.mult, ALU.add), reads=[pk], writes=[khT])

    def conv_from_psum(ps, out_t, h, ntok, rowlen, kps, kout):
        nr = ntok // rowlen
        P.dve(lambda e: e.tensor_scalar(out_t[:, 0:ntok], ps[:, 0:ntok], cwT[:, h, 2:3], cbT[:, h:h + 1], ALU.mult, ALU.add), reads=[kps, "cwT", "cbT"], writes=[kout])
        o3 = out_t[:, 0:ntok].rearrange("p (r t) -> p r t", t=rowlen)
        z3 = ps[:, 0:ntok].rearrange("p (r t) -> p r t", t=rowlen)
        for (kk, sh) in ((0, -2), (1, -1), (3, 1)):
            if sh < 0:
                oo = o3[:, :, -sh:rowlen]; zz = z3[:, :, 0:rowlen + sh]
            else:
                oo = o3[:, :, 0:rowlen - sh]; zz = z3[:, :, sh:rowlen]
            P.dve(lambda e, oo=oo, zz=zz, kk=kk: e.scalar_tensor_tensor(oo, zz, cwT[:, h, kk:kk + 1], oo, ALU.mult, ALU.add), reads=[kps, kout], writes=[kout])

    def rnn_chunk(xc_f, xc_b, d, h, n, bufs, kxc, kpre):
        ia = 0 * 16 + d * 8 + h
        ix = 1 * 16 + d * 8 + h
        dh = d * 8 + h
        e1, a_, e2, s_, b_ = bufs["e1"], bufs["a"], bufs["e2"], bufs["s"], bufs["b"]
        nch = (n + 511) // 512
        psr = []
        for g_, wi in ((0, ia), (1, ix)):
            lst = []
            for j in range(nch):
                ps, pk = next_ps()
                w_ = min(512, n - j * 512)
                P.pe(lambda e, ps=ps, wi=wi, j=j, w_=w_: e.matmul(ps[:, 0:w_], gw_b[:, wi, :], xc_b[:, j * 512:j * 512 + w_], start=True, stop=True), reads=[kxc + "b", "gw_b"], writes=[pk])
                lst.append((ps, pk, j, w_))
            psr.append(lst)
        for (ps, pk, j, w_) in psr[0]:
            P.act(lambda e, ps=ps, j=j, w_=w_: e.activation(out=e1[:, j * 512:j * 512 + w_], in_=ps[:, 0:w_], func=AF.Identity, bias=nbaT[:, dh:dh + 1]), reads=[pk, "nbaT"], writes=[kpre + "e1"])
        for (ps, pk, j, w_) in psr[1]:
            P.act(lambda e, ps=ps, j=j, w_=w_: e.activation(out=e2[:, j * 512:j * 512 + w_], in_=ps[:, 0:w_], func=AF.Identity, bias=nbxT[:, dh:dh + 1]), reads=[pk, "nbxT"], writes=[kpre + "e2"])
        P.act(lambda e: e.activation(out=e1[:, 0:n], in_=e1[:, 0:n], func=AF.Sigmoid), reads=[kpre + "e1"], writes=[kpre + "e1"])
        P.act(lambda e: e.activation(out=e2[:, 0:n], in_=e2[:, 0:n], func=AF.Sigmoid), reads=[kpre + "e2"], writes=[kpre + "e2"])
        P.act(lambda e: e.activation(out=a_[:, 0:n], in_=e1[:, 0:n], func=AF.Exp, scale=coefT[:, dh:dh + 1]), reads=[kpre + "e1", "coefT"], writes=[kpre + "a"])
        P.dve(lambda e: e.tensor_tensor(s_[:, 0:n], a_[:, 0:n], a_[:, 0:n], ALU.mult), reads=[kpre + "a"], writes=[kpre + "s"])
        P.act(lambda e: e.activation(out=s_[:, 0:n], in_=s_[:, 0:n], func=AF.Ln, scale=-1.0, bias=1.0), reads=[kpre + "s"], writes=[kpre + "s"])
        P.act(lambda e: e.activation(out=s_[:, 0:n], in_=s_[:, 0:n], func=AF.Exp, scale=0.5), reads=[kpre + "s"], writes=[kpre + "s"])
        P.pool(lambda e: e.tensor_tensor(b_[:, 0:n], e2[:, 0:n], xc_f, ALU.mult), reads=[kpre + "e2", kxc], writes=[kpre + "b"])
        P.dve(lambda e: e.tensor_tensor(b_[:, 0:n], b_[:, 0:n], s_[:, 0:n], ALU.mult), reads=[kpre + "b", kpre + "s"], writes=[kpre + "b"])

    AR.mark()
    cx = AR.alloc([128, 2, D]); cjunk = AR.alloc([128, D]); cssq = AR.alloc([128, 4]); crstd = AR.alloc([128, 4])
    cxs = AR.alloc([128, 2, D], BF16); hcT = AR.alloc([128, 8, CTX], BF16)
    xcc = AR.alloc([128, 8, CTX]); xccb = AR.alloc([128, 8, CTX], BF16)
    cb_ = dict(e1=AR.alloc([128, CTX]), a=AR.alloc([128, CTX]), e2=AR.alloc([128, CTX]), s=AR.alloc([128, CTX]), b=AR.alloc([128, CTX]))
    chh = AR.alloc([128, CTX])
    P.dma(LQ, lambda e: e.dma_start(out=cx, in_=ctx.rearrange("(s p) d -> p s d", p=128)), writes=["cx"])
    rms_rows(cx, 2, cssq, crstd, cjunk, "cx", "c_")
    norm_transpose(cx, 2, crstd, cxs, hcT, gmcT, shcT, "cx", "c_", "hcT")
    for h in range(8):
        ps, pk = next_ps()

        def f(e, ps=ps, h=h):
            for k in range(8):
                ins = e.matmul(ps[:, 0:CTX], win_b[:, k, 512 + h * 128:512 + (h + 1) * 128], hcT[:, k, :], start=(k == 0), stop=(k == 7))
            return ins
        P.pe(f, reads=["win_b", "hcT"], writes=[pk])
        conv_from_psum(ps, xcc[:, h, :], h, CTX, CTX, pk, "xcc%d" % h)
        P.act(lambda e, h=h: e.copy(xccb[:, h, :], xcc[:, h, :]), reads=["xcc%d" % h], writes=["xcc%db" % h])
        for d in range(2):
            rnn_chunk(xcc[:, h, :], xccb[:, h, :], d, h, CTX, cb_, "xcc%d" % h, "c_")
            if d == 0:
                P.dve(lambda e: e.tensor_tensor_scan(chh, cb_["a"], cb_["b"], 0.0, ALU.mult, ALU.add), reads=["c_a", "c_b"], writes=["chh"])
                P.dve(lambda e, h=h: e.tensor_copy(h0T[:, h:h + 1], chh[:, CTX - 1:CTX]), reads=["chh"], writes=["h0T"])
            else:
                P.dve(lambda e: e.tensor_tensor_scan(chh[:, ::-1], cb_["a"][:, ::-1], cb_["b"][:, ::-1], 0.0, ALU.mult, ALU.add), reads=["c_a", "c_b"], writes=["chh"])
                P.dve(lambda e, h=h: e.tensor_copy(h0T[:, 8 + h:9 + h], chh[:, 0:1]), reads=["chh"], writes=["h0T"])
    P.barrier()
    AR.release()
    if stop_after == "C":
        return finish()

    AR.mark()
    NT = S // 512
    xt = [AR.alloc([128, 4, D]) for _ in range(2)]
    djunk = AR.alloc([128, D], BF16); dssq = AR.alloc([128, 4]); drstd = AR.alloc([128, 4])
    dxs = AR.alloc([128, 4, D], BF16)
    hxT = AR.alloc([128, 8, 512], BF16)
    u_t = AR.alloc([128, 4, 512], BF16)
    xc_t = [AR.alloc([128, 512]) for _ in range(2)]
    gg_t = AR.alloc([128, 8, 512], BF16)
    gfr_t = AR.alloc([128, 8, 512], BF16)
    tA = [AR.alloc([128, 512]) for _ in range(2)]
    tB = [AR.alloc([128, 512]) for _ in range(2)]
    x_v = x.rearrange("(t s p) d -> t p s d", p=128, s=4)
    u_v = u_d.rearrange("(t s p) n -> t p s n", p=128, s=4)
    xc_v = xc_d.rearrange("(h p) t -> p h t", p=128)
    gg_v = gg_d.rearrange("(h p) t -> p h t", p=128)
    gfr_v = gfr_d.rearrange("(h p) t -> p h t", p=128)
    P.dma(LQ, lambda e: e.dma_start(out=xt[0], in_=x_v[0]), writes=["xt0"])
    for t in range(NT):
        bi = t % 2
        if t + 1 < NT:
            P.dma(LQ, lambda e, t=t: e.dma_start(out=xt[(t + 1) % 2], in_=x_v[t + 1]), writes=["xt%d" % ((t + 1) % 2)])
        rms_rows(xt[bi], 4, dssq, drstd, djunk, "xt%d" % bi, "d_")
        norm_transpose(xt[bi], 4, drstd, dxs, hxT, gm1T, sh1T, "xt%d" % bi, "d_", "hxT")
        for s_ in range(4):
            ps, pk = next_ps()

            def f(e, ps=ps, s_=s_):
                for k in range(8):
                    ins = e.matmul(ps, hxT[:, k, s_ * 128:(s_ + 1) * 128], win_b[:, k, 0:512], start=(k == 0), stop=(k == 7))
                return ins
            P.pe(f, reads=["hxT", "win_b"], writes=[pk])
            P.act(lambda e, ps=ps, s_=s_: e.copy(u_t[:, s_, :], ps), reads=[pk], writes=["u_t"])
        P.dma(SQ, lambda e, t=t: e.dma_start(out=u_v[t], in_=u_t), reads=["u_t"], writes=["u_d"])
        for cc in range(4, 36):
            ps, pk = next_ps()

            def f(e, ps=ps, cc=cc):
                for k in range(8):
                    ins = e.matmul(ps, win_b[:, k, cc * 128:(cc + 1) * 128], hxT[:, k, :], start=(k == 0), stop=(k == 7))
                return ins
            P.pe(f, reads=["hxT", "win_b"], writes=[pk])
            if cc < 12:
                h = cc - 4
                ob = xc_t[h % 2]; ok = "xc_t%d" % (h % 2)
                conv_from_psum(ps, ob, h, 512, 64, pk, ok)
                P.dma(SQ, lambda e, ob=ob, h=h, t=t: e.dma_start(out=xc_v[:, h, t * 512:(t + 1) * 512], in_=ob), reads=[ok], writes=["xc_d"])
            elif cc < 20:
                h = cc - 12
                a_ = tA[h % 2]; b_ = tB[h % 2]; ka = "tA%d" % (h % 2); kb = "tB%d" % (h % 2)
                P.act(lambda e, ps=ps, a_=a_: e.activation(out=a_, in_=ps, func=AF.Square), reads=[pk], writes=[ka])
                P.dve(lambda e, a_=a_: e.tensor_scalar(a_, a_, 0.044715, 1.0, ALU.mult, ALU.add), reads=[ka], writes=[ka])
                P.dve(lambda e, ps=ps, a_=a_: e.tensor_tensor(a_, a_, ps, ALU.mult), reads=[ka, pk], writes=[ka])
                P.act(lambda e, a_=a_, b_=b_: e.activation(out=b_, in_=a_, func=AF.Sigmoid, scale=1.5957691216057308), reads=[ka], writes=[kb])
                P.dve(lambda e, ps=ps, b_=b_, h=h: e.tensor_tensor(gg_t[:, h, :], b_, ps, ALU.mult), reads=[kb, pk], writes=["gg_t"])
            else:
                h = cc - 20
                P.act(lambda e, ps=ps, h=h: e.activation(out=gfr_t[:, h % 8, :], in_=ps, func=AF.Sigmoid), reads=[pk], writes=["gfr_t"])
                if h % 8 == 7:
                    P.dma(SQ, lambda e, t=t, h=h: e.dma_start(out=gfr_v[:, (h // 8) * 8:(h // 8) * 8 + 8, t * 512:(t + 1) * 512], in_=gfr_t), reads=["gfr_t"], writes=["gfr_d"])
        P.dma(SQ, lambda e, t=t: e.dma_start(out=gg_v[:, :, t * 512:(t + 1) * 512], in_=gg_t), reads=["gg_t"], writes=["gg_d"])
    P.barrier()
    AR.release()
    AR.release()
    if stop_after == "D":
        return finish()

    AR.mark()
    xcf = AR.alloc([128, S]); xcb = AR.alloc([128, S], BF16); hf = AR.alloc([128, S])
    CH = 1024
    NCH = S // CH
    rb = [dict(e1=AR.alloc([128, CH]), a=AR.alloc([128, CH]), e2=AR.alloc([128, CH]), s=AR.alloc([128, CH]), b=AR.alloc([128, CH])) for _ in range(2)]
    hb = [AR.alloc([128, CH]) for _ in range(2)]
    ggc = [AR.alloc([128, CH], BF16) for _ in range(2)]
    ygc = [AR.alloc([128, CH], BF16) for _ in range(2)]
    yg_v = yg_d.rearrange("(h p) t -> p h t", p=128)
    for h in range(8):
        P.dma(LQ, lambda e, h=h: e.dma_start(out=xcf, in_=xc_v[:, h, :]), reads=["xc_d"], writes=["xcf"])
        P.act(lambda e: e.copy(xcb[:, 0:S // 2], xcf[:, 0:S // 2]), reads=["xcf"], writes=["xcfb"])
        P.dve(lambda e: e.tensor_copy(xcb[:, S // 2:S], xcf[:, S // 2:S]), reads=["xcf", "xcfb"], writes=["xcfb"])
        it = 0
        for d in range(2):
            order = list(range(NCH)) if d == 0 else list(range(NCH - 1, -1, -1))
            prev = None
            for ci in order:
                bi = it % 2
                it += 1
                sl = slice(ci * CH, (ci + 1) * CH)
                kp = "r%d_" % bi
                rnn_chunk(xcf[:, sl], xcb[:, sl], d, h, CH, rb[bi], "xcf", kp)
                dh = d * 8 + h
                if d == 0:
                    init = h0T[:, dh:dh + 1] if prev is None else hf[:, ci * CH - 1:ci * CH]
                    P.dve(lambda e, bi=bi, sl=sl, init=init: e.tensor_tensor_scan(hf[:, sl], rb[bi]["a"], rb[bi]["b"], init, ALU.mult, ALU.add), reads=[kp + "a", kp + "b", "hf", "h0T"], writes=["hf"])
                else:
                    if prev is None:
                        init = h0T[:, dh:dh + 1]; kinit = "h0T"
                    else:
                        init = hb[prev][:, 0:1]; kinit = "hb%d" % prev
                    P.dma(LQ, lambda e, bi=bi, sl=sl, h=h: e.dma_start(out=ggc[bi], in_=gg_v[:, h, sl]), reads=["gg_d"], writes=["ggc%d" % bi])
                    P.dve(lambda e, bi=bi, init=init: e.tensor_tensor_scan(hb[bi][:, ::-1], rb[bi]["a"][:, ::-1], rb[bi]["b"][:, ::-1], init, ALU.mult, ALU.add), reads=[kp + "a", kp + "b", kinit], writes=["hb%d" % bi])
                    P.dve(lambda e, bi=bi, sl=sl: e.tensor_tensor(rb[bi]["s"], hb[bi], hf[:, sl], ALU.add), reads=["hb%d" % bi, "hf", kp + "s"], writes=[kp + "s"])
                    P.dve(lambda e, bi=bi: e.tensor_tensor(ygc[bi], rb[bi]["s"], ggc[bi], ALU.mult), reads=[kp + "s", "ggc%d" % bi], writes=["ygc%d" % bi])
                    P.dma(SQ, lambda e, bi=bi, sl=sl, h=h: e.dma_start(out=yg_v[:, h, sl], in_=ygc[bi]), reads=["ygc%d" % bi], writes=["yg_d"])
                    prev = bi
                if d == 0:
                    prev = bi
    P.barrier()
    AR.release()
    AR.release()
    if stop_after == "E":
        return finish()

    AR.mark()
    c1b = AR.alloc([128, 128], BF16); s1b = AR.alloc([128, 128], BF16); t3b = AR.alloc([128, 128], BF16)
    twc = AR.alloc([128, 64]); tws = AR.alloc([128, 64])
    ftmp = AR.alloc([128, 128])
    for (src, dst, kk) in ((k_c128, c1b, "c1b"), (k_s128, s1b, "s1b"), (k_t3, t3b, "t3b")):
        P.dma(LQ, lambda e, src=src: e.dma_start(out=ftmp, in_=src), writes=["ftmp"])
        P.dve(lambda e, dst=dst: e.tensor_copy(dst, ftmp), reads=["ftmp"], writes=[kk])
    P.dma(LQ, lambda e: e.dma_start(out=twc, in_=k_twc), writes=["twc"])
    P.dma(LQ, lambda e: e.dma_start(out=tws, in_=k_tws), writes=["tws"])
    AR.mark()
    U = AR.alloc([128, 64, 512], BF16)
    qt = [AR.alloc([128, 2, 512], BF16) for _ in range(2)]
    f1 = [AR.alloc([128, 512]) for _ in range(2)]
    f2 = [AR.alloc([128, 512]) for _ in range(2)]
    u_pv = u_d.rearrange("(p n) c -> p n c", n=64)
    for uq in range(4):
        P.dma(LQ, lambda e, uq=uq: e.dma_start(out=U[:, uq * 16:(uq + 1) * 16, :], in_=u_pv[:, uq * 16:(uq + 1) * 16, :]), reads=["u_d"], writes=["U"])
    FDBG = int(os.environ.get("FDBG", "0"))
    for n2 in range(64 if FDBG != 2 else 0):
        bi = n2 % 2
        psr, kr = next_ps()
        psi, ki = next_ps()
        P.pe(lambda e, psr=psr, n2=n2: e.matmul(psr, c1b, U[:, n2, :], start=True, stop=True), reads=["U", "c1b"], writes=[kr])
        P.pe(lambda e, psi=psi, n2=n2: e.matmul(psi, s1b, U[:, n2, :], start=True, stop=True), reads=["U", "s1b"], writes=[ki])
        P.dve(lambda e, psi=psi, n2=n2, bi=bi: e.tensor_scalar_mul(f1[bi], psi, tws[:, n2:n2 + 1]), reads=[ki, "tws"], writes=["f1%d" % bi])
        P.dve(lambda e, psr=psr, n2=n2, bi=bi: e.tensor_scalar_mul(f2[bi], psr, tws[:, n2:n2 + 1]), reads=[kr, "tws"], writes=["f2%d" % bi])
        P.dve(lambda e, psr=psr, n2=n2, bi=bi: e.scalar_tensor_tensor(qt[bi][:, 0, :], psr, twc[:, n2:n2 + 1], f1[bi], ALU.mult, ALU.subtract), reads=[kr, "twc", "f1%d" % bi], writes=["qt%d" % bi])
        P.dve(lambda e, psi=psi, n2=n2, bi=bi: e.scalar_tensor_tensor(qt[bi][:, 1, :], psi, twc[:, n2:n2 + 1], f2[bi], ALU.mult, ALU.add), reads=[ki, "twc", "f2%d" % bi, "qt%d" % bi], writes=["qt%d" % bi])
        for r in range(2 if FDBG != 1 else 0):
            P.dma(SQ, lambda e, n2=n2, bi=bi, r=r: e.dma_start(out=q_d[:, r, n2, :, :].rearrange("g k c -> k g c"), in_=qt[bi][:, r, :].rearrange("k (g c) -> k g c", g=4)), reads=["qt%d" % bi], writes=["q_d"])
    P.barrier()
    AR.release()
    if stop_after == "F1":
        return finish()
    AR.mark()
    Qg = [AR.alloc([128, 128, 128], BF16) for _ in range(2)]
    RT = [AR.alloc([128, 2, S], BF16) for _ in range(2)]
    rt_v = rt_d.rearrange("(g r j) t -> g j r t", r=2, j=128)
    for g in range(4):
        bi = g % 2
        for r in range(2):
            P.dma(LQ, lambda e, g=g, r=r, bi=bi: e.dma_start(out=Qg[bi][r * 64:(r + 1) * 64, :, :], in_=q_d[g, r]), reads=["q_d"], writes=["Qg%d" % bi])
        for k0 in range(0, 128, 4):
            ps, pk = next_ps()

            def f(e, ps=ps, k0=k0, bi=bi):
                for kk in range(4):
                    ins = e.matmul(ps[:, kk * 128:(kk + 1) * 128], Qg[bi][:, k0 + kk, :], t3b, start=True, stop=True)
                return ins
            P.pe(f, reads=["Qg%d" % bi, "t3b"], writes=[pk])
            psv = ps.rearrange("j (k r n) -> j r k n", k=4, r=2)
            for r in range(2):
                ov = RT[bi][:, r, :].rearrange("j (n k) -> j k n", k=128)[:, k0:k0 + 4, :]
                if r == 0:
                    P.act(lambda e, ov=ov, psv=psv, r=r: e.copy(ov, psv[:, r, :, :]), reads=[pk], writes=["RT%d" % bi])
                else:
                    P.dve(lambda e, ov=ov, psv=psv, r=r: e.tensor_copy(ov, psv[:, r, :, :]), reads=[pk], writes=["RT%d" % bi])
        P.dma(SQ, lambda e, g=g, bi=bi: e.dma_start(out=rt_v[g], in_=RT[bi]), reads=["RT%d" % bi], writes=["rt_d"])
    P.barrier()
    AR.release()
    AR.release()
    if stop_after == "F":
        return finish()

    AR.mark()
    wfp = AR.alloc([128, 8, D], BF16)
    wr_b = AR.alloc([128, 8, D], BF16)
    wo_b = AR.alloc([128, 8, D], BF16)
    wrt_f = AR.alloc([128, 8, NE])
    brt = AR.alloc([1, NE])
    AR.mark()
    gstg = AR.alloc([128, 8, D])
    cdb = AR.alloc([128, 128], BF16); sdb = AR.alloc([128, 128], BF16)
    wf_b = AR.alloc([128, 4, D], BF16)
    P.dma(LQ, lambda e: e.dma_start(out=gstg[:, 0, 0:128], in_=k_c128), writes=["gstg"])
    P.dve(lambda e: e.tensor_copy(cdb, gstg[:, 0, 0:128]), reads=["gstg"], writes=["cdb"])
    P.dma(LQ, lambda e: e.dma_start(out=gstg[:, 0, 0:128], in_=k_s128), reads=["gstg"], writes=["gstg"])
    P.dve(lambda e: e.tensor_scalar_mul(sdb, gstg[:, 0, 0:128], -1.0), reads=["gstg"], writes=["sdb"])
    P.dma(LQ, lambda e: e.dma_start(out=gstg[:, 0:4, :], in_=w_fourier.rearrange("(g m) n -> m g n", m=128)), reads=["gstg"], writes=["gstg"])
    P.dve(lambda e: e.tensor_copy(wf_b, gstg[:, 0:4, :]), reads=["gstg"], writes=["wf_b"])
    for g in range(4):
        for ri, mat, mk in ((0, cdb, "cdb"), (1, sdb, "sdb")):
            for half in range(2):
                ps, pk = next_ps()
                P.pe(lambda e, ps=ps, mat=mat, g=g, half=half: e.matmul(ps, mat, wf_b[:, g, half * 512:(half + 1) * 512], start=True, stop=True), reads=[mk, "wf_b"], writes=[pk])
                P.act(lambda e, ps=ps, g=g, ri=ri, half=half: e.copy(wfp[:, g * 2 + ri, half * 512:(half + 1) * 512], ps), reads=[pk], writes=["wfp"])
    P.dma(LQ, lambda e: e.dma_start(out=gstg, in_=w_rnn.rearrange("(k p) n -> p k n", p=128)), reads=["gstg"], writes=["gstg"])
    P.dve(lambda e: e.tensor_copy(wr_b, gstg), reads=["gstg"], writes=["wr_b"])
    P.dma(LQ, lambda e: e.dma_start(out=gstg, in_=w_out.rearrange("(k p) n -> p k n", p=128)), reads=["gstg"], writes=["gstg"])
    for k in range(8):
        P.dve(lambda e, k=k: e.tensor_tensor(wo_b[:, k, :], gstg[:, k, :], g1_bc, ALU.mult), reads=["gstg", "g1_bc"], writes=["wo_b"])
    P.dma(LQ, lambda e: e.dma_start(out=wrt_f, in_=w_router.rearrange("(k p) n -> p k n", p=128)), writes=["wrt_f"])
    P.dma(LQ, lambda e: e.dma_start(out=brt, in_=b_router), writes=["brt"])
    P.barrier()
    AR.release()
    rtt = [AR.alloc([128, 8, 512], BF16) for _ in range(2)]
    ygt = [AR.alloc([128, 8, 512], BF16) for _ in range(2)]
    gft = [AR.alloc([128, 16, 512], BF16)] * 2
    xg_ = [AR.alloc([128, 4, D]) for _ in range(2)]
    mT = AR.alloc([128, 8, 512], BF16)
    g1t = [AR.alloc([128, 512]) for _ in range(2)]
    g2t = [AR.alloc([128, 512]) for _ in range(2)]
    h2f = AR.alloc([128, D])
    h2b = AR.alloc([128, 4, D], BF16)
    h2T = AR.alloc([128, 8, 128])
    gjunk = AR.alloc([128, D], BF16); gssq = AR.alloc([128, 4]); grstd = AR.alloc([128, 4])
    rt_tv = rt_d.rearrange("(c j) t -> j c t", j=128)
    x1_v = x1_d.rearrange("(t s p) d -> t p s d", p=128, s=4)
    h2_v = h2_d.rearrange("(t s p) d -> t p s d", p=128, s=4)

    def g_load(t):
        bi = t % 2
        sl = slice(t * 512, (t + 1) * 512)
        P.dma(LQ, lambda e: e.dma_start(out=rtt[bi], in_=rt_tv[:, :, sl]), reads=["rt_d"], writes=["rtt%d" % bi])
        P.dma(LQ, lambda e: e.dma_start(out=ygt[bi], in_=yg_v[:, :, sl]), reads=["yg_d"], writes=["ygt%d" % bi])
        P.dma(LQ, lambda e: e.dma_start(out=xg_[bi], in_=x_v[t]), writes=["xg_%d" % bi])
    def gft_load(t):
        P.dma(LQ, lambda e: e.dma_start(out=gft[0], in_=gfr_v[:, :, t * 512:(t + 1) * 512]), reads=["gfr_d"], writes=["gft0"])
    g_load(0)
    gft_load(0)
    for t in range(NT):
        bi = t % 2
        if t + 1 < NT:
            g_load(t + 1)
        x1t = xg_[bi]
        kx1 = "xg_%d" % bi
        for n in range(8):
            psF, kF = next_ps()
            psR, kR = next_ps()

            def fF(e, psF=psF, n=n, bi=bi):
                for k in range(8):
                    ins = e.matmul(psF, wfp[:, k, n * 128:(n + 1) * 128], rtt[bi][:, k, :], start=(k == 0), stop=(k == 7))
                return ins

            def fR(e, psR=psR, n=n, bi=bi):
                for k in range(8):
                    ins = e.matmul(psR, wr_b[:, k, n * 128:(n + 1) * 128], ygt[bi][:, k, :], start=(k == 0), stop=(k == 7))
                return ins
            P.pe(fF, reads=["wfp", "rtt%d" % bi], writes=[kF])
            P.pe(fR, reads=["wr_b", "ygt%d" % bi], writes=[kR])
            a_ = g1t[n % 2]; b_ = g2t[n % 2]; ka = "g1t%d" % (n % 2); kb = "g2t%d" % (n % 2)
            P.dve(lambda e, psF=psF, a_=a_, n=n, bi=bi: e.tensor_tensor(a_, psF, gft[bi][:, n, :], ALU.mult), reads=[kF, "gft0"], writes=[ka])
            P.dve(lambda e, psR=psR, b_=b_, n=n, bi=bi: e.tensor_tensor(b_, psR, gft[bi][:, 8 + n, :], ALU.mult), reads=[kR, "gft0"], writes=[kb])
            P.dve(lambda e, a_=a_, b_=b_, n=n: e.tensor_tensor(mT[:, n, :], a_, b_, ALU.add), reads=[ka, kb], writes=["mT"])
        if t + 1 < NT:
            gft_load(t + 1)
        for s_ in range(4):
            for half in range(2):
                ps, pk = next_ps()

                def fO(e, ps=ps, s_=s_, half=half):
                    for k in range(8):
                        ins = e.matmul(ps, mT[:, k, s_ * 128:(s_ + 1) * 128], wo_b[:, k, half * 512:(half + 1) * 512], start=(k == 0), stop=(k == 7))
                    return ins
                P.pe(fO, reads=["mT", "wo_b"], writes=[pk])
                P.dve(lambda e, ps=ps, s_=s_, half=half, bi=bi: e.tensor_tensor(xg_[bi][:, s_, half * 512:(half + 1) * 512], ps, xg_[bi][:, s_, half * 512:(half + 1) * 512], ALU.add), reads=[pk, kx1], writes=[kx1])
        P.dma(SQ, lambda e, t=t, x1t=x1t: e.dma_start(out=x1_v[t], in_=x1t), reads=[kx1], writes=["x1_d"])
        rms_rows(x1t, 4, gssq, grstd, gjunk, kx1, "g_")
        for s_ in range(4):
            ti = t * 4 + s_
            P.dve(lambda e, s_=s_, x1t=x1t: e.scalar_tensor_tensor(h2f, x1t[:, s_, :], grstd[:, s_:s_ + 1], gm2_bc, ALU.mult, ALU.mult), reads=[kx1, "g_rstd", "gm2_bc"], writes=["h2f"])
            P.dve(lambda e: e.tensor_tensor(h2f, h2f, sh2_bc, ALU.add), reads=["h2f", "sh2_bc"], writes=["h2f"])
            P.act(lambda e, s_=s_: e.copy(h2b[:, s_, :], h2f), reads=["h2f"], writes=["h2b"])
            for q in range(2):
                ps, pk = next_ps()

                def fT(e, ps=ps, q=q):
                    for kk in range(4):
                        kc = q * 4 + kk
                        ins = e.transpose(ps[:, kk * 128:(kk + 1) * 128], h2f[:, kc * 128:(kc + 1) * 128], ident_f)
                    return ins
                P.pe(fT, reads=["h2f", "ident_f"], writes=[pk])
                if q == 0:
                    P.act(lambda e, ps=ps, q=q: e.copy(h2T[:, q * 4:(q + 1) * 4, :], ps.rearrange("p (a b) -> p a b", a=4)), reads=[pk], writes=["h2T"])
                else:
                    P.dve(lambda e, ps=ps, q=q: e.tensor_copy(h2T[:, q * 4:(q + 1) * 4, :], ps.rearrange("p (a b) -> p a b", a=4)), reads=[pk], writes=["h2T"])
            ps, pk = next_ps()

            def fL(e, ps=ps):
                for k in range(8):
                    e.matmul(ps[:, 0:NE], h2T[:, k, :], wrt_f[:, k, :], start=(k == 0), stop=False)
                return e.matmul(ps[:, 0:NE], ones_f[0:1, :], brt[0:1, :], start=False, stop=True)
            P.pe(fL, reads=["h2T", "wrt_f", "brt", "ones_f"], writes=[pk])
            P.act(lambda e, ps=ps, ti=ti: e.copy(Lg[:, ti, :], ps[:, 0:NE]), reads=[pk], writes=["Lg"])
        P.dma(SQ, lambda e, t=t: e.dma_start(out=h2_v[t], in_=h2b), reads=["h2b"], writes=["h2_d"])
    P.barrier()
    AR.release()
    if stop_after == "G":
        return finish()

    AR.mark()
    NTI = 64
    m8 = AR.alloc([128, NTI, 8]); i8 = AR.alloc([128, NTI, 8], U32); i8f = AR.alloc([128, NTI, 8])
    iota_e = AR.alloc([128, NE]); iota_i = AR.alloc([128, NE], I32)
    oh = [AR.alloc([128, NTI, NE]) for _ in range(4)]
    msk = AR.alloc([128, NTI, NE]); ex = AR.alloc([128, NTI, NE]); den = AR.alloc([128, NTI]); nmx = AR.alloc([128, NTI])
    cntp = AR.alloc([128, NE]); cntp_b = AR.alloc([128, NE], BF16)
    base = AR.alloc([128, NE]); tot = AR.alloc([128, NE]); pad = AR.alloc([128, NE]); ends = AR.alloc([128, NE]); starts = AR.alloc([128, NE])
    pref = AR.alloc([128, NTI, NE]); dst = AR.alloc([128, NTI, NE]); tmp3 = AR.alloc([128, NTI, NE])
    dk = AR.alloc([128, NTI, 4]); ones_e = AR.alloc([128, NTI])
    bthr = AR.alloc([128, NBLK]); bthr_i = AR.alloc([128, NBLK], I32); cmp = AR.alloc([128, NBLK, NE]); ebf = AR.alloc([128, NBLK])
    P.pool(lambda e: e.iota(iota_i, pattern=[[1, NE]], base=0, channel_multiplier=0), writes=["iota_i"])
    P.dve(lambda e: e.tensor_copy(iota_e, iota_i), reads=["iota_i"], writes=["iota_e"])
    P.pool(lambda e: e.iota(bthr_i, pattern=[[BLK, NBLK]], base=0, channel_multiplier=0), writes=["bthr_i"])
    P.dve(lambda e: e.tensor_copy(bthr, bthr_i), reads=["bthr_i"], writes=["bthr"])
    P.pool(lambda e: e.memset(ones_e, 1.0), writes=["ones_e"])
    for ti in range(NTI):
        P.dve(lambda e, ti=ti: e.max(m8[:, ti, :], Lg[:, ti, :]), reads=["Lg"], writes=["m8"])
        P.dve(lambda e, ti=ti: e.max_index(i8[:, ti, :], m8[:, ti, :], Lg[:, ti, :]), reads=["Lg", "m8"], writes=["i8"])
    P.dve(lambda e: e.tensor_copy(i8f, i8), reads=["i8"], writes=["i8f"])
    for k in range(4):
        P.dve(lambda e, k=k: e.tensor_tensor(oh[k], iota_e.unsqueeze(1).to_broadcast([128, NTI, NE]), i8f[:, :, k:k + 1].to_broadcast([128, NTI, NE]), ALU.is_equal), reads=["iota_e", "i8f"], writes=["oh%d" % k])
    P.dve(lambda e: e.tensor_tensor(msk, oh[0], oh[1], ALU.add), reads=["oh0", "oh1"], writes=["msk"])
    P.dve(lambda e: e.tensor_tensor(msk, msk, oh[2], ALU.add), reads=["msk", "oh2"], writes=["msk"])
    P.dve(lambda e: e.tensor_tensor(msk, msk, oh[3], ALU.add), reads=["msk", "oh3"], writes=["msk"])
    P.dve(lambda e: e.tensor_tensor(ex, Lg, m8[:, :, 0:1].to_broadcast([128, NTI, NE]), ALU.subtract), reads=["Lg", "m8"], writes=["ex"])
    P.act(lambda e: e.activation(out=ex, in_=ex, func=AF.Exp), reads=["ex"], writes=["ex"])
    P.dve(lambda e: e.tensor_tensor(ex, ex, msk, ALU.mult), reads=["ex", "msk"], writes=["ex"])
    P.dve(lambda e: e.tensor_reduce(den, ex, AX.X, ALU.add), reads=["ex"], writes=["den"])
    P.dve(lambda e: e.reciprocal(den, den), reads=["den"], writes=["den"])
    P.dve(lambda e: e.tensor_tensor(ex, ex, den.unsqueeze(2).to_broadcast([128, NTI, NE]), ALU.mult), reads=["ex", "den"], writes=["ex"])
    P.dve(lambda e: e.tensor_reduce(cntp, msk.rearrange("p t e -> p e t"), AX.X, ALU.add), reads=["msk"], writes=["cntp"])
    P.dve(lambda e: e.tensor_copy(cntp_b, cntp), reads=["cntp"], writes=["cntp_b"])
    psb_, kb_ = next_ps()
    P.pe(lambda e: e.matmul(psb_[:, 0:NE], ltri_b, cntp_b, start=True, stop=True), reads=["ltri_b", "cntp_b"], writes=[kb_])
    P.dve(lambda e: e.tensor_copy(base, psb_[:, 0:NE]), reads=[kb_], writes=["base"])
    pst_, kt_ = next_ps()
    P.pe(lambda e: e.matmul(pst_[:, 0:NE], ones_b[:, 0:128], cntp_b, start=True, stop=True), reads=["ones_b", "cntp_b"], writes=[kt_])
    P.dve(lambda e: e.tensor_copy(tot, pst_[:, 0:NE]), reads=[kt_], writes=["tot"])
    P.dve(lambda e: e.tensor_scalar(pad, tot, float(BLK - 1), 1.0 / BLK, ALU.add, ALU.mult), reads=["tot"], writes=["pad"])
    P.dve(lambda e: e.tensor_scalar_add(pad, pad, -0.4990234375), reads=["pad"], writes=["pad"])
    P.dve(lambda e: e.tensor_scalar_add(pad, pad, 8388608.0), reads=["pad"], writes=["pad"])
    P.dve(lambda e: e.tensor_scalar_add(pad, pad, -8388608.0), reads=["pad"], writes=["pad"])
    P.dve(lambda e: e.tensor_scalar_mul(pad, pad, float(BLK)), reads=["pad"], writes=["pad"])
    P.dve(lambda e: e.tensor_tensor_scan(ends, ones_e[:, 0:NE], pad, 0.0, ALU.mult, ALU.add), reads=["pad", "ones_e"], writes=["ends"])
    P.dve(lambda e: e.tensor_tensor(starts, ends, pad, ALU.subtract), reads=["ends", "pad"], writes=["starts"])
    P.dve(lambda e: e.tensor_tensor(base, base, starts, ALU.add), reads=["base", "starts"], writes=["base"])
    for ee in range(NE):
        P.dve(lambda e, ee=ee: e.tensor_tensor_scan(pref[:, :, ee], ones_e, msk[:, :, ee], 0.0, ALU.mult, ALU.add), reads=["msk", "ones_e"], writes=["pref"])
    P.dve(lambda e: e.tensor_tensor(pref, pref, msk, ALU.subtract), reads=["pref", "msk"], writes=["pref"])
    P.dve(lambda e: e.tensor_tensor(dst, pref, base.unsqueeze(1).to_broadcast([128, NTI, NE]), ALU.add), reads=["pref", "base"], writes=["dst"])
    for k in range(4):
        P.dve(lambda e, k=k: e.tensor_tensor(tmp3, oh[k], dst, ALU.mult), reads=["oh%d" % k, "dst"], writes=["tmp3"])
        P.dve(lambda e, k=k: e.tensor_reduce(dk[:, :, k], tmp3, AX.X, ALU.add), reads=["tmp3"], writes=["dk"])
        P.dve(lambda e, k=k: e.tensor_tensor(tmp3, oh[k], ex, ALU.mult), reads=["oh%d" % k, "ex", "tmp3"], writes=["tmp3"])
        P.dve(lambda e, k=k: e.tensor_reduce(wk[:, :, k], tmp3, AX.X, ALU.add), reads=["tmp3"], writes=["wk"])
    P.dve(lambda e: e.tensor_copy(dest_i, dk), reads=["dk"], writes=["dest_i"])
    P.dve(lambda e: e.tensor_tensor(cmp, ends.unsqueeze(1).to_broadcast([128, NBLK, NE]), bthr.unsqueeze(2).to_broadcast([128, NBLK, NE]), ALU.is_le), reads=["ends", "bthr"], writes=["cmp"])
    P.dve(lambda e: e.tensor_reduce(ebf, cmp, AX.X, ALU.add), reads=["cmp"], writes=["ebf"])
    P.dve(lambda e: e.tensor_scalar_min(ebf, ebf, float(NE - 1)), reads=["ebf"], writes=["ebf"])
    P.dve(lambda e: e.tensor_copy(eb_i, ebf), reads=["ebf"], writes=["eb_i"])
    pidx_i = AR.alloc([128, 8], I32); pidx = AR.alloc([128, 8]); widx_f = AR.alloc([128, NBLK, 8])
    P.pool(lambda e: e.iota(pidx_i, pattern=[[128, 8]], base=0, channel_multiplier=1), writes=["pidx_i"])
    P.dve(lambda e: e.tensor_copy(pidx, pidx_i), reads=["pidx_i"], writes=["pidx"])
    P.dve(lambda e: e.tensor_scalar_mul(ebf, ebf, 1024.0), reads=["ebf", "eb_i"], writes=["ebf"])
    P.dve(lambda e: e.tensor_tensor(widx_f, ebf.unsqueeze(2).to_broadcast([128, NBLK, 8]), pidx.unsqueeze(1).to_broadcast([128, NBLK, 8]), ALU.add), reads=["ebf", "pidx"], writes=["widx_f"])
    P.dve(lambda e: e.tensor_copy(widx, widx_f), reads=["widx_f"], writes=["widx"])
    AR.mark()
    hrow = [AR.alloc([128, D], BF16) for _ in range(2)]
    h2_r = h2_d.rearrange("(t p) d -> t p d", p=128)
    for ti in range(NTI):
        bi = ti % 2
        P.dma(LQ, lambda e, ti=ti, bi=bi: e.dma_start(out=hrow[bi], in_=h2_r[ti]), reads=["h2_d"], writes=["hrow%d" % bi])
        for k in range(4):
            P.dma("pool", lambda e, ti=ti, bi=bi, k=k: e.indirect_dma_start(out=xg_d, out_offset=bass.IndirectOffsetOnAxis(ap=dest_i[:, ti, k:k + 1], axis=0), in_=hrow[bi], in_offset=None), reads=["hrow%d" % bi, "dest_i"], writes=["xg_d"])
    P.barrier()
    AR.release()
    AR.release()
    if stop_after == "H":
        return finish()

    AR.release()
    AR.mark()
    NBIG = 3
    NSML = 4
    stgB = [AR.alloc([128, 2048]) for _ in range(NBIG)]
    stgS = [AR.alloc([128, 1024]) for _ in range(NSML)]
    wgu_b = [AR.alloc([128, 8, 2 * D], BF16) for _ in range(2)]
    wdn_b = AR.alloc([128, 8, D], BF16)
    bgb = [AR.alloc([1, 3 * D], BF16) for _ in range(2)]
    xrows = AR.alloc([128, 4, D], BF16)
    xT = [AR.alloc([128, 8, BLK], BF16) for _ in range(2)]
    aT = AR.alloc([128, 8, BLK], BF16)
    eg = [AR.alloc([128, BLK]) for _ in range(2)]
    es = [AR.alloc([128, BLK]) for _ in range(2)]
    eu = [AR.alloc([128, BLK]) for _ in range(2)]
    ysb = [AR.alloc([128, D]) for _ in range(2)]
    xg_v = xg_d.rearrange("(b s p) d -> b p s d", p=128, s=4)
    ys_v = ys_d.rearrange("(b s p) d -> b s p d", p=128, s=4)
    wgu_rows = w_gu.rearrange("e k n -> (e k) n")
    wdn_rows = w_down.rearrange("e k n -> (e k) n")
    rB = [0]
    rS = [0]

    def item(kind, b, kc=0):
        pb = b % 2
        if kind in ("wgu", "bgu"):
            si = rB[0] % NBIG
            rB[0] += 1
            stg = stgB[si]; sk = "stgB%d" % si; ncol = 2048
        else:
            si = rS[0] % NSML
            rS[0] += 1
            stg = stgS[si]; sk = "stgS%d" % si; ncol = 1024
        if kind == "wgu":
            src, idx, dst, dkey, p0 = wgu_rows, widx[:, b, kc:kc + 1], wgu_b[pb][:, kc, :], "wgu%d_%d" % (pb, kc), 128
        elif kind == "wdn":
            src, idx, dst, dkey, p0 = wdn_rows, widx[:, b, kc:kc + 1], wdn_b[:, kc, :], "wdn_%d" % kc, 128
        elif kind == "bgu":
            src, idx, dst, dkey, p0 = b_gu, eb_i[:, b:b + 1], bgb[pb][0:1, 0:2048], "bgb%d" % pb, 1
        else:
            src, idx, dst, dkey, p0 = b_down, eb_i[:, b:b + 1], bgb[pb][0:1, 2048:3072], "bgb%d" % pb, 1

        def g_emit():
            P.dma("pool", lambda e: e.indirect_dma_start(out=stg[:, 0:ncol], out_offset=None, in_=src, in_offset=bass.IndirectOffsetOnAxis(ap=idx, axis=0)), reads=["widx", "eb_i"], writes=[sk])

        def c_emit():
            P.act(lambda e: e.copy(dst, stg[0:p0, 0:ncol]), reads=[sk], writes=[dkey])
        return g_emit, c_emit

    for it_ in [item("bgu", 0), item("bdn", 0)] + [item("wgu", 0, kc) for kc in range(8)]:
        it_[0]()
        it_[1]()
    P.dma(LQ, lambda e: e.dma_start(out=xrows, in_=xg_v[0]), reads=["xg_d"], writes=["xrows"])
    for b in range(NBLK):
        pb = b % 2
        gath = [[] for _ in range(12)]
        cast = [[] for _ in range(12)]
        if b + 1 < NBLK:
            for kind in ("bgu", "bdn"):
                ge, ce = item(kind, b + 1)
                gath[0].append(ge); cast[1].append(ce)
        for kc in range(8):
            ge, ce = item("wdn", b, kc)
            gath[kc // 2].append(ge); cast[kc // 2 + 1].append(ce)
        if b + 1 < NBLK:
            for kc in range(8):
                ge, ce = item("wgu", b + 1, kc)
                gath[kc].append(ge); cast[kc + 2].append(ce)
        for kc in range(8):
            ps, pk = next_ps()
            psb = ps.bitcast(BF16)

            def fx(e, psb=psb, kc=kc):
                for s_ in range(4):
                    ins = e.transpose(psb[:, s_ * 128:(s_ + 1) * 128], xrows[:, s_, kc * 128:(kc + 1) * 128], ident_b)
                return ins
            P.pe(fx, reads=["xrows", "ident_b"], writes=[pk])
            if kc % 2 == 0:
                P.act(lambda e, psb=psb, kc=kc, pb=pb: e.copy(xT[pb][:, kc, :], psb[:, 0:BLK]), reads=[pk], writes=["xT%d" % pb])
            else:
                P.dve(lambda e, psb=psb, kc=kc, pb=pb: e.tensor_copy(xT[pb][:, kc, :], psb[:, 0:BLK]), reads=[pk], writes=["xT%d" % pb])
        if b + 1 < NBLK:
            P.dma(LQ, lambda e, b=b: e.dma_start(out=xrows, in_=xg_v[b + 1]), reads=["xg_d"], writes=["xrows"])
        wkeys = ["wgu%d_%d" % (pb, kc) for kc in range(8)]
        pend = None
        for cc in range(8):
            for fn in gath[cc]:
                fn()
            for fn in cast[cc]:
                fn()
            psg, kg = next_ps()
            psu, ku = next_ps()

            def fg(e, psg=psg, cc=cc, pb=pb):
                for k in range(8):
                    e.matmul(psg, wgu_b[pb][:, k, cc * 128:(cc + 1) * 128], xT[pb][:, k, :], start=(k == 0), stop=False)
                return e.matmul(psg, bgb[pb][0:1, cc * 128:(cc + 1) * 128], ones_b[0:1, 0:BLK], start=False, stop=True)

            def fu(e, psu=psu, cc=cc, pb=pb):
                for k in range(8):
                    e.matmul(psu, wgu_b[pb][:, k, D + cc * 128:D + (cc + 1) * 128], xT[pb][:, k, :], start=(k == 0), stop=False)
                return e.matmul(psu, bgb[pb][0:1, D + cc * 128:D + (cc + 1) * 128], ones_b[0:1, 0:BLK], start=False, stop=True)
            P.pe(fg, reads=wkeys + ["xT%d" % pb, "bgb%d" % pb, "ones_b"], writes=[kg])
            P.pe(fu, reads=wkeys + ["xT%d" % pb, "bgb%d" % pb, "ones_b"], writes=[ku])
            q = cc % 2
            P.dve(lambda e, psg=psg, q=q: e.tensor_scalar_min(eg[q], psg, 7.0), reads=[kg], writes=["eg%d" % q])
            P.act(lambda e, q=q: e.activation(out=es[q], in_=eg[q], func=AF.Sigmoid, scale=1.702), reads=["eg%d" % q], writes=["es%d" % q])
            P.dve(lambda e, psu=psu, q=q: e.tensor_scalar(eu[q], psu, 7.0, -7.0, ALU.min, ALU.max), reads=[ku], writes=["eu%d" % q])

            def tail(cc=cc, q=q):
                P.dve(lambda e: e.tensor_tensor(eg[q], eg[q], es[q], ALU.mult), reads=["eg%d" % q, "es%d" % q], writes=["eg%d" % q])
                P.dve(lambda e: e.scalar_tensor_tensor(aT[:, cc, :], eu[q], 1.0, eg[q], ALU.add, ALU.mult), reads=["eu%d" % q, "eg%d" % q], writes=["aT"])
            if pend is not None:
                pend()
            pend = tail
        pend()
        dkeys = ["wdn_%d" % kc for kc in range(8)]
        for s_ in range(4):
            for fn in gath[8 + s_]:
                fn()
            for fn in cast[8 + s_]:
                fn()
            yb = ysb[s_ % 2]; ky = "ysb%d" % (s_ % 2)
            for half in range(2):
                ps, pk = next_ps()

                def fd(e, ps=ps, s_=s_, half=half, pb=pb):
                    for k in range(8):
                        e.matmul(ps, aT[:, k, s_ * 128:(s_ + 1) * 128], wdn_b[:, k, half * 512:(half + 1) * 512], start=(k == 0), stop=False)
                    c0 = 2 * D + half * 512
                    return e.matmul(ps, ones_b[0:1, 0:128], bgb[pb][0:1, c0:c0 + 512], start=False, stop=True)
                P.pe(fd, reads=["aT", "bgb%d" % pb, "ones_b"] + dkeys, writes=[pk])
                if half == 0:
                    P.act(lambda e, ps=ps, yb=yb: e.copy(yb[:, 0:512], ps), reads=[pk], writes=[ky])
                else:
                    P.dve(lambda e, ps=ps, yb=yb: e.tensor_copy(yb[:, 512:1024], ps), reads=[pk], writes=[ky])
            P.dma(LQ, lambda e, b=b, s_=s_, yb=yb: e.dma_start(out=ys_v[b, s_], in_=yb), reads=[ky], writes=["ys_d"])
    P.barrier()
    AR.release()
    if stop_after == "I":
        return finish()

    AR.mark()
    yk = [[AR.alloc([128, D]) for _ in range(4)] for _ in range(2)]
    x1r = [AR.alloc([128, D]) for _ in range(2)]
    acc = AR.alloc([128, D]); outt = [AR.alloc([128, D]) for _ in range(2)]
    jjunk = AR.alloc([128, D]); jssq = AR.alloc([128, 64]); jrstd = AR.alloc([128, 64])
    x1_r = x1_d.rearrange("(t p) d -> t p d", p=128)
    y_r = y.rearrange("(t p) d -> t p d", p=128)

    def j_load(ti):
        bi = ti % 2
        for k in range(4):
            P.dma("pool", lambda e, k=k: e.indirect_dma_start(out=yk[bi][k], out_offset=None, in_=ys_d, in_offset=bass.IndirectOffsetOnAxis(ap=dest_i[:, ti, k:k + 1], axis=0)), reads=["ys_d", "dest_i"], writes=["yk%d_%d" % (bi, k)])
        P.dma(LQ, lambda e: e.dma_start(out=x1r[bi], in_=x1_r[ti]), reads=["x1_d"], writes=["x1r%d" % bi])
    j_load(0)
    for ti in range(NTI):
        bi = ti % 2
        if ti + 1 < NTI:
            j_load(ti + 1)
        P.dve(lambda e, bi=bi, ti=ti: e.tensor_scalar_mul(acc, yk[bi][0], wk[:, ti, 0:1]), reads=["yk%d_0" % bi, "wk"], writes=["acc"])
        for k in range(1, 4):
            P.dve(lambda e, bi=bi, ti=ti, k=k: e.scalar_tensor_tensor(acc, yk[bi][k], wk[:, ti, k:k + 1], acc, ALU.mult, ALU.add), reads=["yk%d_%d" % (bi, k), "wk", "acc"], writes=["acc"])
        P.pool(lambda e: e.tensor_tensor(acc, acc, g2_bc, ALU.mult), reads=["acc", "g2_bc"], writes=["acc"])
        P.pool(lambda e, bi=bi: e.tensor_tensor(acc, acc, x1r[bi], ALU.add), reads=["acc", "x1r%d" % bi], writes=["acc"])
        P.act(lambda e, ti=ti: e.activation(out=jjunk, in_=acc, func=AF.Square, accum_out=jssq[:, ti:ti + 1]), reads=["acc"], writes=["jjunk", "jssq"])
        P.dve(lambda e, ti=ti: e.tensor_scalar(jrstd[:, ti:ti + 1], jssq[:, ti:ti + 1], 1.0 / D, EPS, ALU.mult, ALU.add), reads=["jssq"], writes=["jrstd"])
        P.act(lambda e, ti=ti: e.activation(out=jrstd[:, ti:ti + 1], in_=jrstd[:, ti:ti + 1], func=AF.Ln), reads=["jrstd"], writes=["jrstd"])
        P.act(lambda e, ti=ti: e.activation(out=jrstd[:, ti:ti + 1], in_=jrstd[:, ti:ti + 1], func=AF.Exp, scale=-0.5), reads=["jrstd"], writes=["jrstd"])
        P.dve(lambda e, bi=bi, ti=ti: e.scalar_tensor_tensor(outt[bi], acc, jrstd[:, ti:ti + 1], nf_bc, ALU.mult, ALU.mult), reads=["acc", "jrstd", "nf_bc"], writes=["outt%d" % bi])
        P.dma(LQ, lambda e, bi=bi, ti=ti: e.dma_start(out=y_r[ti], in_=outt[bi]), reads=["outt%d" % bi], writes=["y"])
    AR.release()
    return finish()


_CACHE = {}


def kernel(**inputs):
    n = 8
    if "nc" not in _CACHE:
        _CACHE["nc"] = build_program()
    nc = _CACHE["nc"]
    tabs = dft_tables()
    f = np.float32

    def a(v):
        return np.ascontiguousarray(np.asarray(v, dtype=f))
    shared = dict(
        c_ctx=a(inputs["c_ctx"]).reshape(1, D), w_ada=a(inputs["w_ada"][0]), b_ada=a(inputs["b_ada"][0]).reshape(1, -1),
        norm1=a(inputs["norm1"][0]).reshape(1, D), w_in=a(inputs["w_in"][0]), conv_w=a(inputs["conv_w"][0]),
        conv_b=a(inputs["conv_b"][0]).reshape(1, D), gate_a_w=a(inputs["gate_a_w"][0]), gate_a_b=a(inputs["gate_a_b"][0]),
        gate_x_w=a(inputs["gate_x_w"][0]), gate_x_b=a(inputs["gate_x_b"][0]), lru_lambda=a(inputs["lru_lambda"][0]),
        w_fourier=a(inputs["w_fourier"][0]), w_rnn=a(inputs["w_rnn"][0]), w_out=a(inputs["w_out"][0]),
        norm2=a(inputs["norm2"][0]).reshape(1, D), w_router=a(inputs["w_router"][0]), b_router=a(inputs["b_router"][0]).reshape(1, NE),
        w_gu=a(inputs["w_gu"][0]), b_gu=a(inputs["b_gu"][0]), w_down=a(inputs["w_down"][0]), b_down=a(inputs["b_down"][0]),
        norm_f=a(inputs["norm_f"]).reshape(1, D), **tabs)
    xs = a(inputs["x"]); cs = a(inputs["c"]); cx = a(inputs["ctx"])
    in_maps = []
    for i in range(n):
        m = dict(shared)
        m["x"] = xs[i]; m["c"] = cs[i].reshape(1, D); m["ctx"] = cx[i]
        in_maps.append(m)
    ncore = int(os.environ.get("KDBG_NCORE", "8"))
    if ncore != 8:
        res = run_bass_kernel_spmd(nc, in_maps[:ncore], core_ids=list(range(ncore)))
        return np.stack([np.asarray(r["y"], dtype=f) for r in res.results], axis=0)
    res = run_bass_kernel_spmd(nc, in_maps, core_ids=list(range(n)))
    return np.stack([np.asarray(r["y"], dtype=f) for r in res.results], axis=0)
```

```python
import contextlib
import math
import os
import numpy as np
import concourse.bass as bass
import concourse.mybir as mybir
from concourse.bass_utils import run_bass_kernel_spmd

F32 = mybir.dt.float32
BF16 = mybir.dt.bfloat16
I32 = mybir.dt.int32
U32 = mybir.dt.uint32
ALU = mybir.AluOpType
AF = mybir.ActivationFunctionType
AX = mybir.AxisListType

SELF_SYNC = True
NDSEM = 6

D = 1024
S = 8192
CTX = 256
NE = 32
BLK = 512
NBLK = 96
PSLOTS = NBLK * BLK
EPS = 1e-6


class Prog:
    ENGS = ("pe", "act", "dve", "pool", "sp")

    def __init__(self, nc):
        self.nc = nc
        self.ops = []

    def add(self, eng, fn, reads=(), writes=(), dma=False):
        self.ops.append(dict(eng=eng, fn=fn, reads=tuple(reads), writes=tuple(writes), dma=dma, bar=False))

    def pe(self, fn, reads=(), writes=()):
        self.add("pe", fn, reads, writes)

    def act(self, fn, reads=(), writes=()):
        self.add("act", fn, reads, writes)

    def dve(self, fn, reads=(), writes=()):
        self.add("dve", fn, reads, writes)

    def pool(self, fn, reads=(), writes=()):
        self.add("pool", fn, reads, writes)

    def dma(self, q, fn, reads=(), writes=()):
        self.add(q, fn, reads, writes, dma=True)

    def barrier(self):
        for e in self.ENGS:
            self.ops.append(dict(eng=e, fn=None, reads=(), writes=(), dma=False, bar=True))

    def emit(self):
        nc = self.nc
        ops = self.ops
        cnt = {e: 0 for e in self.ENGS}
        dcnt = {e: 0 for e in self.ENGS}
        latest = {}
        for op in ops:
            e = op["eng"]
            if op["bar"]:
                op["barvals"] = dict(latest)
                continue
            if op["dma"]:
                i = dcnt[e]
                dcnt[e] += 1
                op["sem"] = ("d", e, i % NDSEM)
                op["val"] = 16 * (i // NDSEM + 1)
            else:
                cnt[e] += 1
                op["sem"] = ("c", e)
                op["val"] = cnt[e]
            latest[op["sem"]] = op["val"]
        last_w = {}
        readers = {}
        for op in ops:
            if op["bar"]:
                continue
            deps = []
            for k in op["reads"]:
                if k in last_w:
                    deps.append(last_w[k])
            for k in op["writes"]:
                if k in last_w:
                    deps.append(last_w[k])
                deps.extend(readers.get(k, ()))
            op["deps"] = deps
            for k in op["writes"]:
                last_w[k] = op
                readers[k] = []
            for k in op["reads"]:
                if k not in op["writes"]:
                    readers.setdefault(k, []).append(op)
        known = {e: {} for e in self.ENGS}
        for op in ops:
            e = op["eng"]
            need = {}
            if op["bar"]:
                need = dict(op["barvals"])
                need.pop(("c", e), None)
            else:
                for d in op["deps"]:
                    if d is op:
                        continue
                    if (not d["dma"]) and d["eng"] == e and (e == "pe" or not SELF_SYNC):
                        continue
                    s, v = d["sem"], d["val"]
                    if need.get(s, 0) < v:
                        need[s] = v
                if op["dma"] and op["val"] > 16:
                    s = op["sem"]
                    if need.get(s, 0) < op["val"] - 16:
                        need[s] = op["val"] - 16
            w = []
            for s, v in need.items():
                if known[e].get(s, 0) < v:
                    known[e][s] = v
                    w.append((s, v))
            op["waits"] = w
        final = latest
        semkeys = sorted(final.keys(), key=str)
        with contextlib.ExitStack() as st:
            sems = {}
            for k in semkeys:
                sems[k] = st.enter_context(nc.semaphore("s_" + "_".join(map(str, k))))
            block = st.enter_context(nc.Block())

            def run(engname):
                def body(eng):
                    for op in ops:
                        if op["eng"] != engname:
                            continue
                        for s, v in op["waits"]:
                            eng.wait_ge(sems[s], v)
                        if op["bar"]:
                            continue
                        ins = op["fn"](eng)
                        ins.then_inc(sems[op["sem"]], 16 if op["dma"] else 1)
                    for k, v in final.items():
                        if k[0] == "d" and k[1] == engname:
                            eng.wait_ge(sems[k], v)
                return body

            block.sync(run("sp"))
            block.scalar(run("act"))
            block.vector(run("dve"))
            block.gpsimd(run("pool"))
            block.tensor(run("pe"))


class Arena:
    def __init__(self, base_ap, nbytes):
        self.base = base_ap
        self.nbytes = nbytes
        self.off = 0
        self.marks = []

    def alloc(self, shape, dt=F32):
        esz = {F32: 4, BF16: 2, I32: 4, U32: 4}[dt]
        npart = shape[0]
        fshape = list(shape[1:])
        n = 1
        for s_ in fshape:
            n *= s_
        nb = (n * esz + 31) // 32 * 32
        assert self.off + nb <= self.nbytes, ("SBUF arena overflow", self.off, nb, self.nbytes)
        a = self.base[0:npart, self.off // 4:(self.off + nb) // 4]
        self.off += nb
        if dt != F32:
            a = a.bitcast(dt)
        a = a[:, 0:n]
        if len(fshape) == 2:
            a = a.rearrange("p (a b) -> p a b", a=fshape[0])
        elif len(fshape) == 3:
            a = a.rearrange("p (a b c) -> p a b c", a=fshape[0], b=fshape[1])
        return a

    def mark(self):
        self.marks.append(self.off)

    def release(self):
        self.off = self.marks.pop()


def dft_tables():
    n = np.arange(128)
    ang = 2 * np.pi * np.outer(n, n) / 128.0
    c128 = np.cos(ang)
    s128 = np.sin(ang)
    k1 = np.arange(128)[:, None]
    n2 = np.arange(64)[None, :]
    tw = 2 * np.pi * k1 * n2 / 8192.0
    twc = np.cos(tw)
    tws = np.sin(tw)
    a64 = 2 * np.pi * np.outer(np.arange(64), np.arange(64)) / 64.0
    c2 = np.cos(a64)
    s2 = np.sin(a64)
    t3 = np.zeros((128, 128))
    t3[0:64, 0:64] = c2
    t3[64:128, 0:64] = -s2
    t3[0:64, 64:128] = s2
    t3[64:128, 64:128] = c2
    t3 = t3 / 1024.0
    f = np.float32
    return dict(k_c128=c128.astype(f), k_s128=s128.astype(f), k_twc=twc.astype(f), k_tws=tws.astype(f), k_t3=t3.astype(f))


def build_program(stop_after=None, debug=False):
    nc = bass.Bass("TRN2", target_bir_lowering=False)

    def din(name, shape, dt=F32):
        return nc.dram_tensor(name, list(shape), dt, kind="ExternalInput").ap()

    DBGSET = set(os.environ.get("KDBG_OUT", "").split(",")) if debug else set()

    def dscr(name, shape, dt=F32):
        if name in DBGSET:
            return nc.dram_tensor(name, list(shape), dt, kind="ExternalOutput").ap()
        return nc.dram_tensor(name, list(shape), dt).ap()

    x = din("x", [S, D]); c = din("c", [1, D]); ctx = din("ctx", [CTX, D]); c_ctx = din("c_ctx", [1, D])
    w_ada = din("w_ada", [D, 6 * D]); b_ada = din("b_ada", [1, 6 * D]); norm1 = din("norm1", [1, D])
    w_in = din("w_in", [D, 4608]); conv_w = din("conv_w", [4, D]); conv_b = din("conv_b", [1, D])
    gate_a_w = din("gate_a_w", [2, 8, 128, 128]); gate_a_b = din("gate_a_b", [2, D])
    gate_x_w = din("gate_x_w", [2, 8, 128, 128]); gate_x_b = din("gate_x_b", [2, D])
    lru_lambda = din("lru_lambda", [2, D])
    w_fourier = din("w_fourier", [512, D]); w_rnn = din("w_rnn", [D, D]); w_out = din("w_out", [D, D])
    norm2 = din("norm2", [1, D]); w_router = din("w_router", [D, NE]); b_router = din("b_router", [1, NE])
    w_gu = din("w_gu", [NE, D, 2 * D]); b_gu = din("b_gu", [NE, 2 * D])
    w_down = din("w_down", [NE, D, D]); b_down = din("b_down", [NE, D]); norm_f = din("norm_f", [1, D])
    k_c128 = din("k_c128", [128, 128]); k_s128 = din("k_s128", [128, 128])
    k_twc = din("k_twc", [128, 64]); k_tws = din("k_tws", [128, 64]); k_t3 = din("k_t3", [128, 128])
    y = nc.dram_tensor("y", [S, D], F32, kind="ExternalOutput").ap()
    dbg = nc.dram_tensor("dbg", [128, 2048], F32, kind="ExternalOutput").ap() if debug else None

    u_d = dscr("u_d", [S, 512], BF16)
    xc_d = dscr("xc_d", [D, S], F32)
    gg_d = dscr("gg_d", [D, S], BF16)
    gfr_d = dscr("gfr_d", [2 * D, S], BF16)
    q_d = dscr("q_d", [4, 2, 64, 128, 128], BF16)
    rt_d = dscr("rt_d", [D, S], BF16)
    yg_d = dscr("yg_d", [D, S], BF16)
    x1_d = dscr("x1_d", [S, D], F32)
    h2_d = dscr("h2_d", [S, D], BF16)
    xg_d = dscr("xg_d", [PSLOTS, D], BF16)
    ys_d = dscr("ys_d", [PSLOTS, D], BF16)

    st = contextlib.ExitStack()
    SBN = 206848
    sball = st.enter_context(nc.sbuf_tensor("sball", [128, SBN // 4], F32))
    AR = Arena(sball[:], SBN)
    banks = [st.enter_context(nc.psum_tensor("psb%d" % i, [128, 512], F32)) for i in range(8)]
    P = Prog(nc)
    psn = [0]

    def next_ps():
        i = psn[0] % 8
        psn[0] += 1
        return banks[i][:], "ps%d" % i

    uid = [0]

    def K(name):
        uid[0] += 1
        return "%s#%d" % (name, uid[0])

    def finish():
        with nc.allow_low_precision(reason="bf16 matmul operands, fp32 accumulation"):
            P.emit()
        st.close()
        return nc

    LQ = "sp"
    SQ = "pool"

    ident_f = AR.alloc([128, 128]); ident_b = AR.alloc([128, 128], BF16)
    ones_b = AR.alloc([128, 512], BF16); ones_f = AR.alloc([128, 128])
    ltri_b = AR.alloc([128, 128], BF16)
    g2_bc = AR.alloc([128, D]); nf_bc = AR.alloc([128, D])
    dest_i = AR.alloc([128, 64, 4], I32); wk = AR.alloc([128, 64, 4])
    eb_i = AR.alloc([128, NBLK], I32); widx = AR.alloc([128, NBLK, 8], I32)
    AR.mark()
    g1_bc = AR.alloc([128, D]); gm2_bc = AR.alloc([128, D]); sh2_bc = AR.alloc([128, D])
    gm1T = AR.alloc([128, 8]); sh1T = AR.alloc([128, 8]); gmcT = AR.alloc([128, 8]); shcT = AR.alloc([128, 8])
    cwT = AR.alloc([128, 8, 4]); cbT = AR.alloc([128, 8])
    nbaT = AR.alloc([128, 16]); nbxT = AR.alloc([128, 16]); coefT = AR.alloc([128, 16])
    h0T = AR.alloc([128, 16])
    Lg = AR.alloc([128, 64, NE])
    zt = AR.alloc([128, 1, D], BF16)
    P.pool(lambda e: e.memset(zt, 0.0), writes=["zt"])
    xgz_v = xg_d.rearrange("(b s p) d -> b p s d", p=128, s=1)

    P.pool(lambda e: e.memset(ident_f, 0.0), writes=["ident_f"])
    P.pool(lambda e: e.affine_select(out=ident_f, in_=ident_f, pattern=[[-1, 128]], compare_op=ALU.not_equal, fill=1.0, base=0, channel_multiplier=1), reads=["ident_f"], writes=["ident_f"])
    P.dve(lambda e: e.tensor_copy(ident_b, ident_f), reads=["ident_f"], writes=["ident_b"])
    P.pool(lambda e: e.memset(ones_b, 1.0), writes=["ones_b"])
    P.pool(lambda e: e.memset(ones_f, 1.0), writes=["ones_f"])
    P.pool(lambda e: e.memset(ltri_b, 1.0), writes=["ltri_b"])
    P.pool(lambda e: e.affine_select(out=ltri_b, in_=ltri_b, pattern=[[1, 128]], compare_op=ALU.is_gt, fill=0.0, base=0, channel_multiplier=-1), reads=["ltri_b"], writes=["ltri_b"])

    AR.mark()
    cT = AR.alloc([128, 16])
    crep = AR.alloc([128, 16, 128])
    mb = AR.alloc([128, 6 * D])
    mcb = AR.alloc([128, 2 * D])
    wa_buf = [AR.alloc([128, 8, 512]) for _ in range(2)]
    n1_bc = AR.alloc([128, D]); n2_bc = AR.alloc([128, D])
    P.dma(LQ, lambda e: e.dma_start(out=cT[:, 0:8], in_=c.rearrange("o (k p) -> p (o k)", p=128), allow_slow_non_contiguous=True), writes=["cT"])
    P.dma(LQ, lambda e: e.dma_start(out=cT[:, 8:16], in_=c_ctx.rearrange("o (k p) -> p (o k)", p=128), allow_slow_non_contiguous=True), reads=["cT"], writes=["cT"])
    P.dma(LQ, lambda e: e.dma_start(out=mb, in_=b_ada.partition_broadcast(128)), writes=["mb"])
    P.dma(LQ, lambda e: e.dma_start(out=n1_bc, in_=norm1.partition_broadcast(128)), writes=["n1_bc"])
    P.dma(LQ, lambda e: e.dma_start(out=n2_bc, in_=norm2.partition_broadcast(128)), writes=["n2_bc"])
    P.dma(LQ, lambda e: e.dma_start(out=nf_bc, in_=norm_f.partition_broadcast(128)), writes=["nf_bc"])
    ctmp = AR.alloc([128, 16])
    P.act(lambda e: e.activation(out=ctmp, in_=cT, func=AF.Exp, scale=-1.0), reads=["cT"], writes=["ctmp"])
    P.dve(lambda e: e.tensor_scalar_add(ctmp, ctmp, 1.0), reads=["ctmp"], writes=["ctmp"])
    P.dve(lambda e: e.reciprocal(ctmp, ctmp), reads=["ctmp"], writes=["ctmp"])
    P.dve(lambda e: e.tensor_tensor(cT, cT, ctmp, ALU.mult), reads=["ctmp", "cT"], writes=["cT"])
    for j in range(16):
        P.dve(lambda e, j=j: e.tensor_copy(crep[:, j, :], cT[:, j:j + 1].to_broadcast([128, 128])), reads=["cT"], writes=["crep"])
    w_ada_v = w_ada.rearrange("(k p) n -> p k n", p=128)
    for ch in range(12):
        bi = ch % 2
        P.dma(LQ, lambda e, ch=ch, bi=bi: e.dma_start(out=wa_buf[bi], in_=w_ada_v[:, :, ch * 512:(ch + 1) * 512]), writes=["wa%d" % bi])
        ps, pk = next_ps()

        def f(e, ps=ps, bi=bi):
            for k in range(8):
                ins = e.matmul(ps, crep[:, k, :], wa_buf[bi][:, k, :], start=(k == 0), stop=(k == 7))
            return ins
        P.pe(f, reads=["crep", "wa%d" % bi], writes=[pk])
        P.dve(lambda e, ps=ps, ch=ch: e.tensor_tensor(mb[:, ch * 512:(ch + 1) * 512], mb[:, ch * 512:(ch + 1) * 512], ps, ALU.add), reads=[pk, "mb"], writes=["mb"])
        if ch < 4:
            ps2, pk2 = next_ps()

            def f2(e, ps2=ps2, bi=bi):
                for k in range(8):
                    ins = e.matmul(ps2, crep[:, 8 + k, :], wa_buf[bi][:, k, :], start=(k == 0), stop=(k == 7))
                return ins
            P.pe(f2, reads=["crep", "wa%d" % bi], writes=[pk2])
            P.act(lambda e, ps2=ps2, ch=ch: e.copy(mcb[:, ch * 512:(ch + 1) * 512], ps2), reads=[pk2], writes=["mcb"])
    bada2 = AR.alloc([128, 2 * D])
    P.dma(LQ, lambda e: e.dma_start(out=bada2, in_=b_ada[:, 0:2 * D].partition_broadcast(128)), writes=["bada2"])
    P.dve(lambda e: e.tensor_tensor(mcb, mcb, bada2, ALU.add), reads=["mcb", "bada2"], writes=["mcb"])
    gm1_bc = AR.alloc([128, D]); gmc_bc = AR.alloc([128, D])
    P.dve(lambda e: e.scalar_tensor_tensor(gm1_bc, mb[:, D:2 * D], 1.0, n1_bc, ALU.add, ALU.mult), reads=["mb", "n1_bc"], writes=["gm1_bc"])
    P.dve(lambda e: e.scalar_tensor_tensor(gmc_bc, mcb[:, D:2 * D], 1.0, n1_bc, ALU.add, ALU.mult), reads=["mcb", "n1_bc"], writes=["gmc_bc"])
    P.dve(lambda e: e.scalar_tensor_tensor(gm2_bc, mb[:, 4 * D:5 * D], 1.0, n2_bc, ALU.add, ALU.mult), reads=["mb", "n2_bc"], writes=["gm2_bc"])
    P.act(lambda e: e.copy(g1_bc, mb[:, 2 * D:3 * D]), reads=["mb"], writes=["g1_bc"])
    P.act(lambda e: e.copy(sh2_bc, mb[:, 3 * D:4 * D]), reads=["mb"], writes=["sh2_bc"])
    P.act(lambda e: e.copy(g2_bc, mb[:, 5 * D:6 * D]), reads=["mb"], writes=["g2_bc"])
    for (src, sk, dst, dk) in ((gm1_bc, "gm1_bc", gm1T, "gm1T"), (mb, "mb", sh1T, "sh1T"), (gmc_bc, "gmc_bc", gmcT, "gmcT"), (mcb, "mcb", shcT, "shcT")):
        for kc in range(8):
            ps, pk = next_ps()
            P.pe(lambda e, ps=ps, src=src, kc=kc: e.transpose(ps[:, 0:128], src[:, kc * 128:(kc + 1) * 128], ident_f), reads=[sk, "ident_f"], writes=[pk])
            P.dve(lambda e, ps=ps, dst=dst, kc=kc: e.tensor_copy(dst[:, kc:kc + 1], ps[:, 0:1]), reads=[pk], writes=[dk])
    for kk_ in range(4):
        P.dma(LQ, lambda e, kk_=kk_: e.dma_start(out=cwT[:, :, kk_], in_=conv_w[kk_:kk_ + 1, :].rearrange("o (h p) -> p (o h)", p=128), allow_slow_non_contiguous=True), reads=["cwT"], writes=["cwT"])
    P.dma(LQ, lambda e: e.dma_start(out=cbT, in_=conv_b.rearrange("o (h p) -> p (o h)", p=128), allow_slow_non_contiguous=True), writes=["cbT"])
    P.dma(LQ, lambda e: e.dma_start(out=nbaT, in_=gate_a_b.rearrange("d (h p) -> p (d h)", p=128), allow_slow_non_contiguous=True), writes=["nbaT"])
    P.dma(LQ, lambda e: e.dma_start(out=nbxT, in_=gate_x_b.rearrange("d (h p) -> p (d h)", p=128), allow_slow_non_contiguous=True), writes=["nbxT"])
    P.dma(LQ, lambda e: e.dma_start(out=coefT, in_=lru_lambda.rearrange("d (h p) -> p (d h)", p=128), allow_slow_non_contiguous=True), writes=["coefT"])
    P.act(lambda e: e.activation(out=coefT, in_=coefT, func=AF.Exp, scale=-1.0), reads=["coefT"], writes=["coefT"])
    P.act(lambda e: e.activation(out=coefT, in_=coefT, func=AF.Ln, bias=1.0), reads=["coefT"], writes=["coefT"])
    P.dve(lambda e: e.tensor_scalar_mul(coefT, coefT, -8.0), reads=["coefT"], writes=["coefT"])
    P.barrier()
    AR.release()
    if stop_after == "A":
        return finish()

    AR.mark()
    gw_b = AR.alloc([128, 32, 128], BF16)
    AR.mark()
    win_b = AR.alloc([128, 8, 4608], BF16)
    AR.mark()
    stg = [AR.alloc([128, 8, 512]) for _ in range(2)]
    w_in_v = w_in.rearrange("(k p) n -> p k n", p=128)
    for ch in range(9):
        bi = ch % 2
        P.dma(LQ, lambda e, ch=ch, bi=bi: e.dma_start(out=stg[bi], in_=w_in_v[:, :, ch * 512:(ch + 1) * 512]), writes=["stg%d" % bi])
        if ch % 2 == 0:
            P.act(lambda e, ch=ch, bi=bi: e.copy(win_b[:, :, ch * 512:(ch + 1) * 512], stg[bi]), reads=["stg%d" % bi], writes=["win_b"])
        else:
            P.dve(lambda e, ch=ch, bi=bi: e.tensor_copy(win_b[:, :, ch * 512:(ch + 1) * 512], stg[bi]), reads=["stg%d" % bi], writes=["win_b"])
    for gi, gwd in enumerate((gate_a_w, gate_x_w)):
        bi = gi % 2
        P.dma(LQ, lambda e, gwd=gwd, bi=bi: e.dma_start(out=stg[bi][:, 0:4, :].rearrange("p a (b c) -> p (a b) c", c=128), in_=gwd.rearrange("d h i j -> i (d h) j")), writes=["stg%d" % bi])
        P.dve(lambda e, gi=gi, bi=bi: e.tensor_copy(gw_b[:, gi * 16:(gi + 1) * 16, :], stg[bi][:, 0:4, :].rearrange("p a (b c) -> p (a b) c", c=128)), reads=["stg%d" % bi], writes=["gw_b"])
    P.barrier()
    AR.release()
    if stop_after == "W":
        return finish()

    def rms_rows(xt, nsub, ssq, rstd, junk, kx, kpre):
        for s_ in range(nsub):
            P.act(lambda e, s_=s_: e.activation(out=junk, in_=xt[:, s_, :], func=AF.Square, accum_out=ssq[:, s_:s_ + 1]), reads=[kx], writes=[kpre + "junk", kpre + "ssq"])
        P.dve(lambda e: e.tensor_scalar(rstd[:, 0:nsub], ssq[:, 0:nsub], 1.0 / D, EPS, ALU.mult, ALU.add), reads=[kpre + "ssq"], writes=[kpre + "rstd"])
        P.act(lambda e: e.activation(out=rstd[:, 0:nsub], in_=rstd[:, 0:nsub], func=AF.Ln), reads=[kpre + "rstd"], writes=[kpre + "rstd"])
        P.act(lambda e: e.activation(out=rstd[:, 0:nsub], in_=rstd[:, 0:nsub], func=AF.Exp, scale=-0.5), reads=[kpre + "rstd"], writes=[kpre + "rstd"])

    def norm_transpose(xt, nsub, rstd, xs_b, hT, gT, sT, kx, kpre, khT):
        for s_ in range(nsub):
            P.act(lambda e, s_=s_: e.activation(out=xs_b[:, s_, :], in_=xt[:, s_, :], func=AF.Copy, scale=rstd[:, s_:s_ + 1]), reads=[kx, kpre + "rstd"], writes=[kpre + "xs"])
        for kc in range(8):
            ps, pk = next_ps()
            psb = ps.bitcast(BF16)

            def f(e, psb=psb, kc=kc):
                for s_ in range(nsub):
                    ins = e.transpose(psb[:, s_ * 128:(s_ + 1) * 128], xs_b[:, s_, kc * 128:(kc + 1) * 128], ident_b)
                return ins
            P.pe(f, reads=[kpre + "xs", "ident_b"], writes=[pk])
            n = nsub * 128
            if kc % 2 == 0:
                P.act(lambda e, psb=psb, kc=kc, n=n: e.activation(out=hT[:, kc, 0:n], in_=psb[:, 0:n], func=AF.Identity, scale=gT[:, kc:kc + 1], bias=sT[:, kc:kc + 1]), reads=[pk], writes=[khT])
            else:
                P.dve(lambda e, psb=psb, kc=kc, n=n: e.tensor_scalar(hT[:, kc, 0:n], psb[:, 0:n], gT[:, kc:kc + 1], sT[:, kc:kc + 1], ALU.mult, ALU.add), reads=[pk], writes=[khT])

    def conv_from_psum(ps, out_t, h, ntok, rowlen, kps, kout):
        nr = ntok // rowlen
        P.dve(lambda e: e.tensor_scalar(out_t[:, 0:ntok], ps[:, 0:ntok], cwT[:, h, 2:3], cbT[:, h:h + 1], ALU.mult, ALU.add), reads=[kps, "cwT", "cbT"], writes=[kout])
        o3 = out_t[:, 0:ntok].rearrange("p (r t) -> p r t", t=rowlen)
        z3 = ps[:, 0:ntok].rearrange("p (r t) -> p r t", t=rowlen)
        for (kk, sh) in ((0, -2), (1, -1), (3, 1)):
            if sh < 0:
                oo = o3[:, :, -sh:rowlen]; zz = z3[:, :, 0:rowlen + sh]
            else:
                oo = o3[:, :, 0:rowlen - sh]; zz = z3[:, :, sh:rowlen]
            P.dve(lambda e, oo=oo, zz=zz, kk=kk: e.scalar_tensor_tensor(oo, zz, cwT[:, h, kk:kk + 1], oo, ALU.mult, ALU.add), reads=[kps, kout], writes=[kout])

    def rnn_chunk(xc_f, xc_b, d, h, n, bufs, kxc, kpre):
        ia = 0 * 16 + d * 8 + h
        ix = 1 * 16 + d * 8 + h
        dh = d * 8 + h
        e1, a_, e2, s_, b_ = bufs["e1"], bufs["a"], bufs["e2"], bufs["s"], bufs["b"]
        nch = (n + 511) // 512
        psr = []
        for g_, wi in ((0, ia), (1, ix)):
            lst = []
            for j in range(nch):
                ps, pk = next_ps()
                w_ = min(512, n - j * 512)
                P.pe(lambda e, ps=ps, wi=wi, j=j, w_=w_: e.matmul(ps[:, 0:w_], gw_b[:, wi, :], xc_b[:, j * 512:j * 512 + w_], start=True, stop=True), reads=[kxc + "b", "gw_b"], writes=[pk])
                lst.append((ps, pk, j, w_))
            psr.append(lst)
        for (ps, pk, j, w_) in psr[0]:
            P.act(lambda e, ps=ps, j=j, w_=w_: e.activation(out=e1[:, j * 512:j * 512 + w_], in_=ps[:, 0:w_], func=AF.Identity, bias=nbaT[:, dh:dh + 1]), reads=[pk, "nbaT"], writes=[kpre + "e1"])
        for (ps, pk, j, w_) in psr[1]:
            P.act(lambda e, ps=ps, j=j, w_=w_: e.activation(out=e2[:, j * 512:j * 512 + w_], in_=ps[:, 0:w_], func=AF.Identity, bias=nbxT[:, dh:dh + 1]), reads=[pk, "nbxT"], writes=[kpre + "e2"])
        P.act(lambda e: e.activation(out=e1[:, 0:n], in_=e1[:, 0:n], func=AF.Sigmoid), reads=[kpre + "e1"], writes=[kpre + "e1"])
        P.act(lambda e: e.activation(out=e2[:, 0:n], in_=e2[:, 0:n], func=AF.Sigmoid), reads=[kpre + "e2"], writes=[kpre + "e2"])
        P.act(lambda e: e.activation(out=a_[:, 0:n], in_=e1[:, 0:n], func=AF.Exp, scale=coefT[:, dh:dh + 1]), reads=[kpre + "e1", "coefT"], writes=[kpre + "a"])
        P.dve(lambda e: e.tensor_tensor(s_[:, 0:n], a_[:, 0:n], a_[:, 0:n], ALU.mult), reads=[kpre + "a"], writes=[kpre + "s"])
        P.act(lambda e: e.activation(out=s_[:, 0:n], in_=s_[:, 0:n], func=AF.Ln, scale=-1.0, bias=1.0), reads=[kpre + "s"], writes=[kpre + "s"])
        P.act(lambda e: e.activation(out=s_[:, 0:n], in_=s_[:, 0:n], func=AF.Exp, scale=0.5), reads=[kpre + "s"], writes=[kpre + "s"])
        P.pool(lambda e: e.tensor_tensor(b_[:, 0:n], e2[:, 0:n], xc_f, ALU.mult), reads=[kpre + "e2", kxc], writes=[kpre + "b"])
        P.dve(lambda e: e.tensor_tensor(b_[:, 0:n], b_[:, 0:n], s_[:, 0:n], ALU.mult), reads=[kpre + "b", kpre + "s"], writes=[kpre + "b"])

    AR.mark()
    cx = AR.alloc([128, 2, D]); cjunk = AR.alloc([128, D]); cssq = AR.alloc([128, 4]); crstd = AR.alloc([128, 4])
    cxs = AR.alloc([128, 2, D], BF16); hcT = AR.alloc([128, 8, CTX], BF16)
    xcc = AR.alloc([128, 8, CTX]); xccb = AR.alloc([128, 8, CTX], BF16)
    cb_ = dict(e1=AR.alloc([128, CTX]), a=AR.alloc([128, CTX]), e2=AR.alloc([128, CTX]), s=AR.alloc([128, CTX]), b=AR.alloc([128, CTX]))
    chh = AR.alloc([128, CTX])
    P.dma(LQ, lambda e: e.dma_start(out=cx, in_=ctx.rearrange("(s p) d -> p s d", p=128)), writes=["cx"])
    rms_rows(cx, 2, cssq, crstd, cjunk, "cx", "c_")
    norm_transpose(cx, 2, crstd, cxs, hcT, gmcT, shcT, "cx", "c_", "hcT")
    for h in range(8):
        ps, pk = next_ps()

        def f(e, ps=ps, h=h):
            for k in range(8):
                ins = e.matmul(ps[:, 0:CTX], win_b[:, k, 512 + h * 128:512 + (h + 1) * 128], hcT[:, k, :], start=(k == 0), stop=(k == 7))
            return ins
        P.pe(f, reads=["win_b", "hcT"], writes=[pk])
        conv_from_psum(ps, xcc[:, h, :], h, CTX, CTX, pk, "xcc%d" % h)
        P.act(lambda e, h=h: e.copy(xccb[:, h, :], xcc[:, h, :]), reads=["xcc%d" % h], writes=["xcc%db" % h])
        for d in range(2):
            rnn_chunk(xcc[:, h, :], xccb[:, h, :], d, h, CTX, cb_, "xcc%d" % h, "c_")
            if d == 0:
                P.dve(lambda e: e.tensor_tensor_scan(chh, cb_["a"], cb_["b"], 0.0, ALU.mult, ALU.add), reads=["c_a", "c_b"], writes=["chh"])
                P.dve(lambda e, h=h: e.tensor_copy(h0T[:, h:h + 1], chh[:, CTX - 1:CTX]), reads=["chh"], writes=["h0T"])
            else:
                P.dve(lambda e: e.tensor_tensor_scan(chh[:, ::-1], cb_["a"][:, ::-1], cb_["b"][:, ::-1], 0.0, ALU.mult, ALU.add), reads=["c_a", "c_b"], writes=["chh"])
                P.dve(lambda e, h=h: e.tensor_copy(h0T[:, 8 + h:9 + h], chh[:, 0:1]), reads=["chh"], writes=["h0T"])
    P.barrier()
    AR.release()
    if stop_after == "C":
        return finish()

    AR.mark()
    NT = S // 512
    xt = [AR.alloc([128, 4, D]) for _ in range(2)]
    djunk = AR.alloc([128, D], BF16); dssq = AR.alloc([128, 4]); drstd = AR.alloc([128, 4])
    dxs = AR.alloc([128, 4, D], BF16)
    hxT = AR.alloc([128, 8, 512], BF16)
    u_t = AR.alloc([128, 4, 512], BF16)
    xc_t = [AR.alloc([128, 512]) for _ in range(2)]
    gg_t = AR.alloc([128, 8, 512], BF16)
    gfr_t = AR.alloc([128, 8, 512], BF16)
    tA = [AR.alloc([128, 512]) for _ in range(2)]
    tB = [AR.alloc([128, 512]) for _ in range(2)]
    x_v = x.rearrange("(t s p) d -> t p s d", p=128, s=4)
    u_v = u_d.rearrange("(t s p) n -> t p s n", p=128, s=4)
    xc_v = xc_d.rearrange("(h p) t -> p h t", p=128)
    gg_v = gg_d.rearrange("(h p) t -> p h t", p=128)
    gfr_v = gfr_d.rearrange("(h p) t -> p h t", p=128)
    P.dma(LQ, lambda e: e.dma_start(out=xt[0], in_=x_v[0]), writes=["xt0"])
    for t in range(NT):
        bi = t % 2
        if t + 1 < NT:
            P.dma(LQ, lambda e, t=t: e.dma_start(out=xt[(t + 1) % 2], in_=x_v[t + 1]), writes=["xt%d" % ((t + 1) % 2)])
        rms_rows(xt[bi], 4, dssq, drstd, djunk, "xt%d" % bi, "d_")
        norm_transpose(xt[bi], 4, drstd, dxs, hxT, gm1T, sh1T, "xt%d" % bi, "d_", "hxT")
        for s_ in range(4):
            ps, pk = next_ps()

            def f(e, ps=ps, s_=s_):
                for k in range(8):
                    ins = e.matmul(ps, hxT[:, k, s_ * 128:(s_ + 1) * 128], win_b[:, k, 0:512], start=(k == 0), stop=(k == 7))
                return ins
            P.pe(f, reads=["hxT", "win_b"], writes=[pk])
            P.act(lambda e, ps=ps, s_=s_: e.copy(u_t[:, s_, :], ps), reads=[pk], writes=["u_t"])
        P.dma(SQ, lambda e, t=t: e.dma_start(out=u_v[t], in_=u_t), reads=["u_t"], writes=["u_d"])
        for cc in range(4, 36):
            ps, pk = next_ps()

            def f(e, ps=ps, cc=cc):
                for k in range(8):
                    ins = e.matmul(ps, win_b[:, k, cc * 128:(cc + 1) * 128], hxT[:, k, :], start=(k == 0), stop=(k == 7))
                return ins
            P.pe(f, reads=["hxT", "win_b"], writes=[pk])
            if cc < 12:
                h = cc - 4
                ob = xc_t[h % 2]; ok = "xc_t%d" % (h % 2)
                conv_from_psum(ps, ob, h, 512, 64, pk, ok)
                P.dma(SQ, lambda e, ob=ob, h=h, t=t: e.dma_start(out=xc_v[:, h, t * 512:(t + 1) * 512], in_=ob), reads=[ok], writes=["xc_d"])
            elif cc < 20:
                h = cc - 12
                a_ = tA[h % 2]; b_ = tB[h % 2]; ka = "tA%d" % (h % 2); kb = "tB%d" % (h % 2)
                P.act(lambda e, ps=ps, a_=a_: e.activation(out=a_, in_=ps, func=AF.Square), reads=[pk], writes=[ka])
                P.dve(lambda e, a_=a_: e.tensor_scalar(a_, a_, 0.044715, 1.0, ALU.mult, ALU.add), reads=[ka], writes=[ka])
                P.dve(lambda e, ps=ps, a_=a_: e.tensor_tensor(a_, a_, ps, ALU.mult), reads=[ka, pk], writes=[ka])
                P.act(lambda e, a_=a_, b_=b_: e.activation(out=b_, in_=a_, func=AF.Sigmoid, scale=1.5957691216057308), reads=[ka], writes=[kb])
                P.dve(lambda e, ps=ps, b_=b_, h=h: e.tensor_tensor(gg_t[:, h, :], b_, ps, ALU.mult), reads=[kb, pk], writes=["gg_t"])
            else:
                h = cc - 20
                P.act(lambda e, ps=ps, h=h: e.activation(out=gfr_t[:, h % 8, :], in_=ps, func=AF.Sigmoid), reads=[pk], writes=["gfr_t"])
                if h % 8 == 7:
                    P.dma(SQ, lambda e, t=t, h=h: e.dma_start(out=gfr_v[:, (h // 8) * 8:(h // 8) * 8 + 8, t * 512:(t + 1) * 512], in_=gfr_t), reads=["gfr_t"], writes=["gfr_d"])
        P.dma(SQ, lambda e, t=t: e.dma_start(out=gg_v[:, :, t * 512:(t + 1) * 512], in_=gg_t), reads=["gg_t"], writes=["gg_d"])
    P.barrier()
    AR.release()
    AR.release()
    if stop_after == "D":
        return finish()

    AR.mark()
    xcf = AR.alloc([128, S]); xcb = AR.alloc([128, S], BF16); hf = AR.alloc([128, S])
    CH = 1024
    NCH = S // CH
    rb = [dict(e1=AR.alloc([128, CH]), a=AR.alloc([128, CH]), e2=AR.alloc([128, CH]), s=AR.alloc([128, CH]), b=AR.alloc([128, CH])) for _ in range(2)]
    hb = [AR.alloc([128, CH]) for _ in range(2)]
    ggc = [AR.alloc([128, CH], BF16) for _ in range(2)]
    ygc = [AR.alloc([128, CH], BF16) for _ in range(2)]
    yg_v = yg_d.rearrange("(h p) t -> p h t", p=128)
    for h in range(8):
        P.dma(LQ, lambda e, h=h: e.dma_start(out=xcf, in_=xc_v[:, h, :]), reads=["xc_d"], writes=["xcf"])
        for zb in range(h * 48, (h + 1) * 48):
            P.dma(LQ, lambda e, zb=zb: e.dma_start(out=xgz_v[zb], in_=zt), reads=["zt"], writes=["xg_d"])
        P.act(lambda e: e.copy(xcb[:, 0:S // 2], xcf[:, 0:S // 2]), reads=["xcf"], writes=["xcfb"])
        P.dve(lambda e: e.tensor_copy(xcb[:, S // 2:S], xcf[:, S // 2:S]), reads=["xcf", "xcfb"], writes=["xcfb"])
        it = 0
        for d in range(2):
            order = list(range(NCH)) if d == 0 else list(range(NCH - 1, -1, -1))
            prev = None
            for ci in order:
                bi = it % 2
                it += 1
                sl = slice(ci * CH, (ci + 1) * CH)
                kp = "r%d_" % bi
                rnn_chunk(xcf[:, sl], xcb[:, sl], d, h, CH, rb[bi], "xcf", kp)
                dh = d * 8 + h
                if d == 0:
                    init = h0T[:, dh:dh + 1] if prev is None else hf[:, ci * CH - 1:ci * CH]
                    P.dve(lambda e, bi=bi, sl=sl, init=init: e.tensor_tensor_scan(hf[:, sl], rb[bi]["a"], rb[bi]["b"], init, ALU.mult, ALU.add), reads=[kp + "a", kp + "b", "hf", "h0T"], writes=["hf"])
                else:
                    if prev is None:
                        init = h0T[:, dh:dh + 1]; kinit = "h0T"
                    else:
                        init = hb[prev][:, 0:1]; kinit = "hb%d" % prev
                    P.dma(LQ, lambda e, bi=bi, sl=sl, h=h: e.dma_start(out=ggc[bi], in_=gg_v[:, h, sl]), reads=["gg_d"], writes=["ggc%d" % bi])
                    P.dve(lambda e, bi=bi, init=init: e.tensor_tensor_scan(hb[bi][:, ::-1], rb[bi]["a"][:, ::-1], rb[bi]["b"][:, ::-1], init, ALU.mult, ALU.add), reads=[kp + "a", kp + "b", kinit], writes=["hb%d" % bi])
                    P.dve(lambda e, bi=bi, sl=sl: e.tensor_tensor(rb[bi]["s"], hb[bi], hf[:, sl], ALU.add), reads=["hb%d" % bi, "hf", kp + "s"], writes=[kp + "s"])
                    P.dve(lambda e, bi=bi: e.tensor_tensor(ygc[bi], rb[bi]["s"], ggc[bi], ALU.mult), reads=[kp + "s", "ggc%d" % bi], writes=["ygc%d" % bi])
                    P.dma(SQ, lambda e, bi=bi, sl=sl, h=h: e.dma_start(out=yg_v[:, h, sl], in_=ygc[bi]), reads=["ygc%d" % bi], writes=["yg_d"])
                    prev = bi
                if d == 0:
                    prev = bi
    P.barrier()
    AR.release()
    AR.release()
    if stop_after == "E":
        return finish()

    AR.mark()
    c1b = AR.alloc([128, 128], BF16); s1b = AR.alloc([128, 128], BF16); t3b = AR.alloc([128, 128], BF16)
    twc = AR.alloc([128, 64]); tws = AR.alloc([128, 64])
    ftmp = AR.alloc([128, 128])
    for (src, dst, kk) in ((k_c128, c1b, "c1b"), (k_s128, s1b, "s1b"), (k_t3, t3b, "t3b")):
        P.dma(LQ, lambda e, src=src: e.dma_start(out=ftmp, in_=src), writes=["ftmp"])
        P.dve(lambda e, dst=dst: e.tensor_copy(dst, ftmp), reads=["ftmp"], writes=[kk])
    P.dma(LQ, lambda e: e.dma_start(out=twc, in_=k_twc), writes=["twc"])
    P.dma(LQ, lambda e: e.dma_start(out=tws, in_=k_tws), writes=["tws"])
    AR.mark()
    U = AR.alloc([128, 64, 512], BF16)
    qt = [AR.alloc([128, 2, 512], BF16) for _ in range(2)]
    f1 = [AR.alloc([128, 512]) for _ in range(2)]
    f2 = [AR.alloc([128, 512]) for _ in range(2)]
    u_pv = u_d.rearrange("(p n) c -> p n c", n=64)
    for uq in range(4):
        P.dma(LQ, lambda e, uq=uq: e.dma_start(out=U[:, uq * 16:(uq + 1) * 16, :], in_=u_pv[:, uq * 16:(uq + 1) * 16, :]), reads=["u_d"], writes=["U"])
    FDBG = int(os.environ.get("FDBG", "0"))
    for n2 in range(64 if FDBG != 2 else 0):
        bi = n2 % 2
        psr, kr = next_ps()
        psi, ki = next_ps()
        P.pe(lambda e, psr=psr, n2=n2: e.matmul(psr, c1b, U[:, n2, :], start=True, stop=True), reads=["U", "c1b"], writes=[kr])
        P.pe(lambda e, psi=psi, n2=n2: e.matmul(psi, s1b, U[:, n2, :], start=True, stop=True), reads=["U", "s1b"], writes=[ki])
        P.dve(lambda e, psi=psi, n2=n2, bi=bi: e.tensor_scalar_mul(f1[bi], psi, tws[:, n2:n2 + 1]), reads=[ki, "tws"], writes=["f1%d" % bi])
        P.dve(lambda e, psr=psr, n2=n2, bi=bi: e.tensor_scalar_mul(f2[bi], psr, tws[:, n2:n2 + 1]), reads=[kr, "tws"], writes=["f2%d" % bi])
        P.dve(lambda e, psr=psr, n2=n2, bi=bi: e.scalar_tensor_tensor(qt[bi][:, 0, :], psr, twc[:, n2:n2 + 1], f1[bi], ALU.mult, ALU.subtract), reads=[kr, "twc", "f1%d" % bi], writes=["qt%d" % bi])
        P.dve(lambda e, psi=psi, n2=n2, bi=bi: e.scalar_tensor_tensor(qt[bi][:, 1, :], psi, twc[:, n2:n2 + 1], f2[bi], ALU.mult, ALU.add), reads=[ki, "twc", "f2%d" % bi, "qt%d" % bi], writes=["qt%d" % bi])
        for r in range(2 if FDBG != 1 else 0):
            P.dma(SQ, lambda e, n2=n2, bi=bi, r=r: e.dma_start(out=q_d[:, r, n2, :, :].rearrange("g k c -> k g c"), in_=qt[bi][:, r, :].rearrange("k (g c) -> k g c", g=4)), reads=["qt%d" % bi], writes=["q_d"])
    P.barrier()
    AR.release()
    if stop_after == "F1":
        return finish()
    AR.mark()
    Qg = [AR.alloc([128, 128, 128], BF16) for _ in range(2)]
    RT = [AR.alloc([128, 2, S], BF16) for _ in range(2)]
    rt_v = rt_d.rearrange("(g r j) t -> g j r t", r=2, j=128)
    for g in range(4):
        bi = g % 2
        for r in range(2):
            P.dma(LQ, lambda e, g=g, r=r, bi=bi: e.dma_start(out=Qg[bi][r * 64:(r + 1) * 64, :, :], in_=q_d[g, r]), reads=["q_d"], writes=["Qg%d" % bi])
        for k0 in range(0, 128, 4):
            ps, pk = next_ps()

            def f(e, ps=ps, k0=k0, bi=bi):
                for kk in range(4):
                    ins = e.matmul(ps[:, kk * 128:(kk + 1) * 128], Qg[bi][:, k0 + kk, :], t3b, start=True, stop=True)
                return ins
            P.pe(f, reads=["Qg%d" % bi, "t3b"], writes=[pk])
            psv = ps.rearrange("j (k r n) -> j r k n", k=4, r=2)
            for r in range(2):
                ov = RT[bi][:, r, :].rearrange("j (n k) -> j k n", k=128)[:, k0:k0 + 4, :]
                if r == 0:
                    P.act(lambda e, ov=ov, psv=psv, r=r: e.copy(ov, psv[:, r, :, :]), reads=[pk], writes=["RT%d" % bi])
                else:
                    P.dve(lambda e, ov=ov, psv=psv, r=r: e.tensor_copy(ov, psv[:, r, :, :]), reads=[pk], writes=["RT%d" % bi])
        P.dma(SQ, lambda e, g=g, bi=bi: e.dma_start(out=rt_v[g], in_=RT[bi]), reads=["RT%d" % bi], writes=["rt_d"])
    P.barrier()
    AR.release()
    AR.release()
    if stop_after == "F":
        return finish()

    AR.mark()
    wfp = AR.alloc([128, 8, D], BF16)
    wr_b = AR.alloc([128, 8, D], BF16)
    wo_b = AR.alloc([128, 8, D], BF16)
    wrt_f = AR.alloc([128, 8, NE])
    brt = AR.alloc([1, NE])
    AR.mark()
    gstg = AR.alloc([128, 8, D])
    cdb = AR.alloc([128, 128], BF16); sdb = AR.alloc([128, 128], BF16)
    wf_b = AR.alloc([128, 4, D], BF16)
    P.dma(LQ, lambda e: e.dma_start(out=gstg[:, 0, 0:128], in_=k_c128), writes=["gstg"])
    P.dve(lambda e: e.tensor_copy(cdb, gstg[:, 0, 0:128]), reads=["gstg"], writes=["cdb"])
    P.dma(LQ, lambda e: e.dma_start(out=gstg[:, 0, 0:128], in_=k_s128), reads=["gstg"], writes=["gstg"])
    P.dve(lambda e: e.tensor_scalar_mul(sdb, gstg[:, 0, 0:128], -1.0), reads=["gstg"], writes=["sdb"])
    P.dma(LQ, lambda e: e.dma_start(out=gstg[:, 0:4, :], in_=w_fourier.rearrange("(g m) n -> m g n", m=128)), reads=["gstg"], writes=["gstg"])
    P.dve(lambda e: e.tensor_copy(wf_b, gstg[:, 0:4, :]), reads=["gstg"], writes=["wf_b"])
    for g in range(4):
        for ri, mat, mk in ((0, cdb, "cdb"), (1, sdb, "sdb")):
            for half in range(2):
                ps, pk = next_ps()
                P.pe(lambda e, ps=ps, mat=mat, g=g, half=half: e.matmul(ps, mat, wf_b[:, g, half * 512:(half + 1) * 512], start=True, stop=True), reads=[mk, "wf_b"], writes=[pk])
                P.act(lambda e, ps=ps, g=g, ri=ri, half=half: e.copy(wfp[:, g * 2 + ri, half * 512:(half + 1) * 512], ps), reads=[pk], writes=["wfp"])
    P.dma(LQ, lambda e: e.dma_start(out=gstg, in_=w_rnn.rearrange("(k p) n -> p k n", p=128)), reads=["gstg"], writes=["gstg"])
    P.dve(lambda e: e.tensor_copy(wr_b, gstg), reads=["gstg"], writes=["wr_b"])
    P.dma(LQ, lambda e: e.dma_start(out=gstg, in_=w_out.rearrange("(k p) n -> p k n", p=128)), reads=["gstg"], writes=["gstg"])
    for k in range(8):
        P.dve(lambda e, k=k: e.tensor_tensor(wo_b[:, k, :], gstg[:, k, :], g1_bc, ALU.mult), reads=["gstg", "g1_bc"], writes=["wo_b"])
    P.dma(LQ, lambda e: e.dma_start(out=wrt_f, in_=w_router.rearrange("(k p) n -> p k n", p=128)), writes=["wrt_f"])
    P.dma(LQ, lambda e: e.dma_start(out=brt, in_=b_router), writes=["brt"])
    P.barrier()
    AR.release()
    rtt = [AR.alloc([128, 8, 512], BF16) for _ in range(2)]
    ygt = [AR.alloc([128, 8, 512], BF16) for _ in range(2)]
    gft = [AR.alloc([128, 16, 512], BF16)] * 2
    xg_ = [AR.alloc([128, 4, D]) for _ in range(2)]
    mT = AR.alloc([128, 8, 512], BF16)
    g1t = [AR.alloc([128, 512]) for _ in range(2)]
    g2t = [AR.alloc([128, 512]) for _ in range(2)]
    h2f = AR.alloc([128, D])
    h2b = AR.alloc([128, 4, D], BF16)
    h2T = AR.alloc([128, 8, 128])
    gjunk = AR.alloc([128, D], BF16); gssq = AR.alloc([128, 4]); grstd = AR.alloc([128, 4])
    rt_tv = rt_d.rearrange("(c j) t -> j c t", j=128)
    x1_v = x1_d.rearrange("(t s p) d -> t p s d", p=128, s=4)
    h2_v = h2_d.rearrange("(t s p) d -> t p s d", p=128, s=4)

    def g_load(t):
        bi = t % 2
        sl = slice(t * 512, (t + 1) * 512)
        P.dma(LQ, lambda e: e.dma_start(out=rtt[bi], in_=rt_tv[:, :, sl]), reads=["rt_d"], writes=["rtt%d" % bi])
        P.dma(LQ, lambda e: e.dma_start(out=ygt[bi], in_=yg_v[:, :, sl]), reads=["yg_d"], writes=["ygt%d" % bi])
        P.dma(LQ, lambda e: e.dma_start(out=xg_[bi], in_=x_v[t]), writes=["xg_%d" % bi])
    def gft_load(t):
        P.dma(LQ, lambda e: e.dma_start(out=gft[0], in_=gfr_v[:, :, t * 512:(t + 1) * 512]), reads=["gfr_d"], writes=["gft0"])
    g_load(0)
    gft_load(0)
    for t in range(NT):
        bi = t % 2
        if t + 1 < NT:
            g_load(t + 1)
        x1t = xg_[bi]
        kx1 = "xg_%d" % bi
        for n in range(8):
            psF, kF = next_ps()
            psR, kR = next_ps()

            def fF(e, psF=psF, n=n, bi=bi):
                for k in range(8):
                    ins = e.matmul(psF, wfp[:, k, n * 128:(n + 1) * 128], rtt[bi][:, k, :], start=(k == 0), stop=(k == 7))
                return ins

            def fR(e, psR=psR, n=n, bi=bi):
                for k in range(8):
                    ins = e.matmul(psR, wr_b[:, k, n * 128:(n + 1) * 128], ygt[bi][:, k, :], start=(k == 0), stop=(k == 7))
                return ins
            P.pe(fF, reads=["wfp", "rtt%d" % bi], writes=[kF])
            P.pe(fR, reads=["wr_b", "ygt%d" % bi], writes=[kR])
            a_ = g1t[n % 2]; b_ = g2t[n % 2]; ka = "g1t%d" % (n % 2); kb = "g2t%d" % (n % 2)
            P.dve(lambda e, psF=psF, a_=a_, n=n, bi=bi: e.tensor_tensor(a_, psF, gft[bi][:, n, :], ALU.mult), reads=[kF, "gft0"], writes=[ka])
            P.dve(lambda e, psR=psR, b_=b_, n=n, bi=bi: e.tensor_tensor(b_, psR, gft[bi][:, 8 + n, :], ALU.mult), reads=[kR, "gft0"], writes=[kb])
            P.dve(lambda e, a_=a_, b_=b_, n=n: e.tensor_tensor(mT[:, n, :], a_, b_, ALU.add), reads=[ka, kb], writes=["mT"])
        if t + 1 < NT:
            gft_load(t + 1)
        for s_ in range(4):
            for half in range(2):
                ps, pk = next_ps()

                def fO(e, ps=ps, s_=s_, half=half):
                    for k in range(8):
                        ins = e.matmul(ps, mT[:, k, s_ * 128:(s_ + 1) * 128], wo_b[:, k, half * 512:(half + 1) * 512], start=(k == 0), stop=(k == 7))
                    return ins
                P.pe(fO, reads=["mT", "wo_b"], writes=[pk])
                P.dve(lambda e, ps=ps, s_=s_, half=half, bi=bi: e.tensor_tensor(xg_[bi][:, s_, half * 512:(half + 1) * 512], ps, xg_[bi][:, s_, half * 512:(half + 1) * 512], ALU.add), reads=[pk, kx1], writes=[kx1])
        P.dma(SQ, lambda e, t=t, x1t=x1t: e.dma_start(out=x1_v[t], in_=x1t), reads=[kx1], writes=["x1_d"])
        rms_rows(x1t, 4, gssq, grstd, gjunk, kx1, "g_")
        for s_ in range(4):
            ti = t * 4 + s_
            P.dve(lambda e, s_=s_, x1t=x1t: e.scalar_tensor_tensor(h2f, x1t[:, s_, :], grstd[:, s_:s_ + 1], gm2_bc, ALU.mult, ALU.mult), reads=[kx1, "g_rstd", "gm2_bc"], writes=["h2f"])
            P.dve(lambda e: e.tensor_tensor(h2f, h2f, sh2_bc, ALU.add), reads=["h2f", "sh2_bc"], writes=["h2f"])
            P.act(lambda e, s_=s_: e.copy(h2b[:, s_, :], h2f), reads=["h2f"], writes=["h2b"])
            for q in range(2):
                ps, pk = next_ps()

                def fT(e, ps=ps, q=q):
                    for kk in range(4):
                        kc = q * 4 + kk
                        ins = e.transpose(ps[:, kk * 128:(kk + 1) * 128], h2f[:, kc * 128:(kc + 1) * 128], ident_f)
                    return ins
                P.pe(fT, reads=["h2f", "ident_f"], writes=[pk])
                if q == 0:
                    P.act(lambda e, ps=ps, q=q: e.copy(h2T[:, q * 4:(q + 1) * 4, :], ps.rearrange("p (a b) -> p a b", a=4)), reads=[pk], writes=["h2T"])
                else:
                    P.dve(lambda e, ps=ps, q=q: e.tensor_copy(h2T[:, q * 4:(q + 1) * 4, :], ps.rearrange("p (a b) -> p a b", a=4)), reads=[pk], writes=["h2T"])
            ps, pk = next_ps()

            def fL(e, ps=ps):
                for k in range(8):
                    e.matmul(ps[:, 0:NE], h2T[:, k, :], wrt_f[:, k, :], start=(k == 0), stop=False)
                return e.matmul(ps[:, 0:NE], ones_f[0:1, :], brt[0:1, :], start=False, stop=True)
            P.pe(fL, reads=["h2T", "wrt_f", "brt", "ones_f"], writes=[pk])
            P.act(lambda e, ps=ps, ti=ti: e.copy(Lg[:, ti, :], ps[:, 0:NE]), reads=[pk], writes=["Lg"])
        P.dma(SQ, lambda e, t=t: e.dma_start(out=h2_v[t], in_=h2b), reads=["h2b"], writes=["h2_d"])
    P.barrier()
    AR.release()
    if stop_after == "G":
        return finish()

    AR.mark()
    NTI = 64
    m8 = AR.alloc([128, NTI, 8]); i8 = AR.alloc([128, NTI, 8], U32); i8f = AR.alloc([128, NTI, 8])
    iota_e = AR.alloc([128, NE]); iota_i = AR.alloc([128, NE], I32)
    oh = [AR.alloc([128, NTI, NE]) for _ in range(4)]
    msk = AR.alloc([128, NTI, NE]); ex = AR.alloc([128, NTI, NE]); den = AR.alloc([128, NTI]); nmx = AR.alloc([128, NTI])
    cntp = AR.alloc([128, NE]); cntp_b = AR.alloc([128, NE], BF16)
    base = AR.alloc([128, NE]); tot = AR.alloc([128, NE]); pad = AR.alloc([128, NE]); ends = AR.alloc([128, NE]); starts = AR.alloc([128, NE])
    pref = AR.alloc([128, NTI, NE]); dst = AR.alloc([128, NTI, NE]); tmp3 = AR.alloc([128, NTI, NE])
    dk = AR.alloc([128, NTI, 4]); ones_e = AR.alloc([128, NTI])
    bthr = AR.alloc([128, NBLK]); bthr_i = AR.alloc([128, NBLK], I32); cmp = AR.alloc([128, NBLK, NE]); ebf = AR.alloc([128, NBLK])
    P.pool(lambda e: e.iota(iota_i, pattern=[[1, NE]], base=0, channel_multiplier=0), writes=["iota_i"])
    P.dve(lambda e: e.tensor_copy(iota_e, iota_i), reads=["iota_i"], writes=["iota_e"])
    P.pool(lambda e: e.iota(bthr_i, pattern=[[BLK, NBLK]], base=0, channel_multiplier=0), writes=["bthr_i"])
    P.dve(lambda e: e.tensor_copy(bthr, bthr_i), reads=["bthr_i"], writes=["bthr"])
    P.pool(lambda e: e.memset(ones_e, 1.0), writes=["ones_e"])
    for ti in range(NTI):
        P.dve(lambda e, ti=ti: e.max(m8[:, ti, :], Lg[:, ti, :]), reads=["Lg"], writes=["m8"])
        P.dve(lambda e, ti=ti: e.max_index(i8[:, ti, :], m8[:, ti, :], Lg[:, ti, :]), reads=["Lg", "m8"], writes=["i8"])
    P.dve(lambda e: e.tensor_copy(i8f, i8), reads=["i8"], writes=["i8f"])
    for k in range(4):
        P.dve(lambda e, k=k: e.tensor_tensor(oh[k], iota_e.unsqueeze(1).to_broadcast([128, NTI, NE]), i8f[:, :, k:k + 1].to_broadcast([128, NTI, NE]), ALU.is_equal), reads=["iota_e", "i8f"], writes=["oh%d" % k])
    P.dve(lambda e: e.tensor_tensor(msk, oh[0], oh[1], ALU.add), reads=["oh0", "oh1"], writes=["msk"])
    P.dve(lambda e: e.tensor_tensor(msk, msk, oh[2], ALU.add), reads=["msk", "oh2"], writes=["msk"])
    P.dve(lambda e: e.tensor_tensor(msk, msk, oh[3], ALU.add), reads=["msk", "oh3"], writes=["msk"])
    P.dve(lambda e: e.tensor_tensor(ex, Lg, m8[:, :, 0:1].to_broadcast([128, NTI, NE]), ALU.subtract), reads=["Lg", "m8"], writes=["ex"])
    P.act(lambda e: e.activation(out=ex, in_=ex, func=AF.Exp), reads=["ex"], writes=["ex"])
    P.dve(lambda e: e.tensor_tensor(ex, ex, msk, ALU.mult), reads=["ex", "msk"], writes=["ex"])
    P.dve(lambda e: e.tensor_reduce(den, ex, AX.X, ALU.add), reads=["ex"], writes=["den"])
    P.dve(lambda e: e.reciprocal(den, den), reads=["den"], writes=["den"])
    P.dve(lambda e: e.tensor_tensor(ex, ex, den.unsqueeze(2).to_broadcast([128, NTI, NE]), ALU.mult), reads=["ex", "den"], writes=["ex"])
    P.dve(lambda e: e.tensor_reduce(cntp, msk.rearrange("p t e -> p e t"), AX.X, ALU.add), reads=["msk"], writes=["cntp"])
    P.dve(lambda e: e.tensor_copy(cntp_b, cntp), reads=["cntp"], writes=["cntp_b"])
    psb_, kb_ = next_ps()
    P.pe(lambda e: e.matmul(psb_[:, 0:NE], ltri_b, cntp_b, start=True, stop=True), reads=["ltri_b", "cntp_b"], writes=[kb_])
    P.dve(lambda e: e.tensor_copy(base, psb_[:, 0:NE]), reads=[kb_], writes=["base"])
    pst_, kt_ = next_ps()
    P.pe(lambda e: e.matmul(pst_[:, 0:NE], ones_b[:, 0:128], cntp_b, start=True, stop=True), reads=["ones_b", "cntp_b"], writes=[kt_])
    P.dve(lambda e: e.tensor_copy(tot, pst_[:, 0:NE]), reads=[kt_], writes=["tot"])
    P.dve(lambda e: e.tensor_scalar(pad, tot, float(BLK - 1), 1.0 / BLK, ALU.add, ALU.mult), reads=["tot"], writes=["pad"])
    P.dve(lambda e: e.tensor_scalar_add(pad, pad, -0.4990234375), reads=["pad"], writes=["pad"])
    P.dve(lambda e: e.tensor_scalar_add(pad, pad, 8388608.0), reads=["pad"], writes=["pad"])
    P.dve(lambda e: e.tensor_scalar_add(pad, pad, -8388608.0), reads=["pad"], writes=["pad"])
    P.dve(lambda e: e.tensor_scalar_mul(pad, pad, float(BLK)), reads=["pad"], writes=["pad"])
    P.dve(lambda e: e.tensor_tensor_scan(ends, ones_e[:, 0:NE], pad, 0.0, ALU.mult, ALU.add), reads=["pad", "ones_e"], writes=["ends"])
    P.dve(lambda e: e.tensor_tensor(starts, ends, pad, ALU.subtract), reads=["ends", "pad"], writes=["starts"])
    P.dve(lambda e: e.tensor_tensor(base, base, starts, ALU.add), reads=["base", "starts"], writes=["base"])
    for ee in range(NE):
        P.dve(lambda e, ee=ee: e.tensor_tensor_scan(pref[:, :, ee], ones_e, msk[:, :, ee], 0.0, ALU.mult, ALU.add), reads=["msk", "ones_e"], writes=["pref"])
    P.dve(lambda e: e.tensor_tensor(pref, pref, msk, ALU.subtract), reads=["pref", "msk"], writes=["pref"])
    P.dve(lambda e: e.tensor_tensor(dst, pref, base.unsqueeze(1).to_broadcast([128, NTI, NE]), ALU.add), reads=["pref", "base"], writes=["dst"])
    for k in range(4):
        P.dve(lambda e, k=k: e.tensor_tensor(tmp3, oh[k], dst, ALU.mult), reads=["oh%d" % k, "dst"], writes=["tmp3"])
        P.dve(lambda e, k=k: e.tensor_reduce(dk[:, :, k], tmp3, AX.X, ALU.add), reads=["tmp3"], writes=["dk"])
        P.dve(lambda e, k=k: e.tensor_tensor(tmp3, oh[k], ex, ALU.mult), reads=["oh%d" % k, "ex", "tmp3"], writes=["tmp3"])
        P.dve(lambda e, k=k: e.tensor_reduce(wk[:, :, k], tmp3, AX.X, ALU.add), reads=["tmp3"], writes=["wk"])
    P.dve(lambda e: e.tensor_copy(dest_i, dk), reads=["dk"], writes=["dest_i"])
    P.dve(lambda e: e.tensor_tensor(cmp, ends.unsqueeze(1).to_broadcast([128, NBLK, NE]), bthr.unsqueeze(2).to_broadcast([128, NBLK, NE]), ALU.is_le), reads=["ends", "bthr"], writes=["cmp"])
    P.dve(lambda e: e.tensor_reduce(ebf, cmp, AX.X, ALU.add), reads=["cmp"], writes=["ebf"])
    P.dve(lambda e: e.tensor_scalar_min(ebf, ebf, float(NE - 1)), reads=["ebf"], writes=["ebf"])
    P.dve(lambda e: e.tensor_copy(eb_i, ebf), reads=["ebf"], writes=["eb_i"])
    pidx_i = AR.alloc([128, 8], I32); pidx = AR.alloc([128, 8]); widx_f = AR.alloc([128, NBLK, 8])
    P.pool(lambda e: e.iota(pidx_i, pattern=[[128, 8]], base=0, channel_multiplier=1), writes=["pidx_i"])
    P.dve(lambda e: e.tensor_copy(pidx, pidx_i), reads=["pidx_i"], writes=["pidx"])
    P.dve(lambda e: e.tensor_scalar_mul(ebf, ebf, 1024.0), reads=["ebf", "eb_i"], writes=["ebf"])
    P.dve(lambda e: e.tensor_tensor(widx_f, ebf.unsqueeze(2).to_broadcast([128, NBLK, 8]), pidx.unsqueeze(1).to_broadcast([128, NBLK, 8]), ALU.add), reads=["ebf", "pidx"], writes=["widx_f"])
    P.dve(lambda e: e.tensor_copy(widx, widx_f), reads=["widx_f"], writes=["widx"])
    AR.mark()
    hrow = [AR.alloc([128, D], BF16) for _ in range(2)]
    h2_r = h2_d.rearrange("(t p) d -> t p d", p=128)
    for ti in range(NTI):
        bi = ti % 2
        P.dma(LQ, lambda e, ti=ti, bi=bi: e.dma_start(out=hrow[bi], in_=h2_r[ti]), reads=["h2_d"], writes=["hrow%d" % bi])
        for k in range(4):
            P.dma("pool", lambda e, ti=ti, bi=bi, k=k: e.indirect_dma_start(out=xg_d, out_offset=bass.IndirectOffsetOnAxis(ap=dest_i[:, ti, k:k + 1], axis=0), in_=hrow[bi], in_offset=None), reads=["hrow%d" % bi, "dest_i"], writes=["xg_d"])
    P.barrier()
    AR.release()
    AR.release()
    if stop_after == "H":
        return finish()

    AR.release()
    AR.mark()
    NBIG = 3
    NSML = 4
    stgB = [AR.alloc([128, 2048]) for _ in range(NBIG)]
    stgS = [AR.alloc([128, 1024]) for _ in range(NSML)]
    wgu_b = [AR.alloc([128, 8, 2 * D], BF16) for _ in range(2)]
    wdn_b = AR.alloc([128, 8, D], BF16)
    bgb = [AR.alloc([1, 3 * D], BF16) for _ in range(2)]
    xrows = AR.alloc([128, 4, D], BF16)
    xT = [AR.alloc([128, 8, BLK], BF16) for _ in range(2)]
    aT = AR.alloc([128, 8, BLK], BF16)
    eg = [AR.alloc([128, BLK]) for _ in range(2)]
    es = [AR.alloc([128, BLK]) for _ in range(2)]
    eu = [AR.alloc([128, BLK]) for _ in range(2)]
    ysb = [AR.alloc([128, D], BF16) for _ in range(2)]
    bT = [AR.alloc([128, 16]) for _ in range(2)]
    bT1 = [AR.alloc([128, 8]) for _ in range(2)]
    xg_v = xg_d.rearrange("(b s p) d -> b p s d", p=128, s=4)
    ys_v = ys_d.rearrange("(b s p) d -> b s p d", p=128, s=4)
    wgu_rows = w_gu.rearrange("e k n -> (e k) n")
    wdn_rows = w_down.rearrange("e k n -> (e k) n")
    rB = [0]
    rS = [0]

    def item(kind, b, kc=0):
        pb = b % 2
        if kind in ("wgu", "bgu"):
            si = rB[0] % NBIG
            rB[0] += 1
            stg = stgB[si]; sk = "stgB%d" % si; ncol = 2048
        else:
            si = rS[0] % NSML
            rS[0] += 1
            stg = stgS[si]; sk = "stgS%d" % si; ncol = 1024
        if kind == "wgu":
            src, idx, dst, dkey, p0 = wgu_rows, widx[:, b, kc:kc + 1], wgu_b[pb][:, kc, :], "wgu%d_%d" % (pb, kc), 128
        elif kind == "wdn":
            src, idx, dst, dkey, p0 = wdn_rows, widx[:, b, kc:kc + 1], wdn_b[:, kc, :], "wdn_%d" % kc, 128
        elif kind == "bgu":
            src, idx, dst, dkey, p0 = b_gu, eb_i[:, b:b + 1], bgb[pb][0:1, 0:2048], "bgb%d" % pb, 1
        else:
            src, idx, dst, dkey, p0 = b_down, eb_i[:, b:b + 1], bgb[pb][0:1, 2048:3072], "bgb%d" % pb, 1

        def g_emit():
            P.dma("pool", lambda e: e.indirect_dma_start(out=stg[:, 0:ncol], out_offset=None, in_=src, in_offset=bass.IndirectOffsetOnAxis(ap=idx, axis=0)), reads=["widx", "eb_i"], writes=[sk])

        def c_emit():
            if kind == "bgu":
                ps, pk = next_ps()

                def ft(e):
                    for c_ in range(16):
                        ins = e.transpose(ps[:, c_:c_ + 1], stg[0:1, c_ * 128:(c_ + 1) * 128], ident_f[0:1, 0:1])
                    return ins
                P.pe(ft, reads=[sk, "ident_f"], writes=[pk])
                P.dve(lambda e: e.tensor_copy(bT[pb], ps[:, 0:16]), reads=[pk], writes=["bT%d" % pb])
                P.dve(lambda e: e.tensor_scalar_add(bT1[pb], bT[pb][:, 8:16], 1.0), reads=["bT%d" % pb], writes=["bT1%d" % pb])
                return
            P.act(lambda e: e.copy(dst, stg[0:p0, 0:ncol]), reads=[sk], writes=[dkey])
        return g_emit, c_emit

    for it_ in [item("bgu", 0), item("bdn", 0)] + [item("wgu", 0, kc) for kc in range(8)]:
        it_[0]()
        it_[1]()
    P.dma(LQ, lambda e: e.dma_start(out=xrows, in_=xg_v[0]), reads=["xg_d"], writes=["xrows"])
    for b in range(NBLK):
        pb = b % 2
        gath = [[] for _ in range(12)]
        cast = [[] for _ in range(12)]
        if b + 1 < NBLK:
            for kind in ("bgu", "bdn"):
                ge, ce = item(kind, b + 1)
                gath[0].append(ge); cast[1].append(ce)
        for kc in range(8):
            ge, ce = item("wdn", b, kc)
            gath[kc // 2].append(ge); cast[kc // 2 + 1].append(ce)
        if b + 1 < NBLK:
            for kc in range(8):
                ge, ce = item("wgu", b + 1, kc)
                gath[kc].append(ge); cast[kc + 2].append(ce)
        for kc in range(8):
            ps, pk = next_ps()
            psb = ps.bitcast(BF16)

            def fx(e, psb=psb, kc=kc):
                for s_ in range(4):
                    ins = e.transpose(psb[:, s_ * 128:(s_ + 1) * 128], xrows[:, s_, kc * 128:(kc + 1) * 128], ident_b)
                return ins
            P.pe(fx, reads=["xrows", "ident_b"], writes=[pk])
            if kc % 2 == 0:
                P.act(lambda e, psb=psb, kc=kc, pb=pb: e.copy(xT[pb][:, kc, :], psb[:, 0:BLK]), reads=[pk], writes=["xT%d" % pb])
            else:
                P.dve(lambda e, psb=psb, kc=kc, pb=pb: e.tensor_copy(xT[pb][:, kc, :], psb[:, 0:BLK]), reads=[pk], writes=["xT%d" % pb])
        if b + 1 < NBLK:
            P.dma(LQ, lambda e, b=b: e.dma_start(out=xrows, in_=xg_v[b + 1]), reads=["xg_d"], writes=["xrows"])
        wkeys = ["wgu%d_%d" % (pb, kc) for kc in range(8)]
        pend = None
        for cc in range(8):
            for fn in gath[cc]:
                fn()
            for fn in cast[cc]:
                fn()
            psg, kg = next_ps()
            psu, ku = next_ps()

            def fg(e, psg=psg, cc=cc, pb=pb):
                for k in range(8):
                    ins = e.matmul(psg, wgu_b[pb][:, k, cc * 128:(cc + 1) * 128], xT[pb][:, k, :], start=(k == 0), stop=(k == 7))
                return ins

            def fu(e, psu=psu, cc=cc, pb=pb):
                for k in range(8):
                    ins = e.matmul(psu, wgu_b[pb][:, k, D + cc * 128:D + (cc + 1) * 128], xT[pb][:, k, :], start=(k == 0), stop=(k == 7))
                return ins
            P.pe(fg, reads=wkeys + ["xT%d" % pb], writes=[kg])
            P.pe(fu, reads=wkeys + ["xT%d" % pb], writes=[ku])
            q = cc % 2
            P.dve(lambda e, psg=psg, q=q, cc=cc, pb=pb: e.tensor_scalar(eg[q], psg, bT[pb][:, cc:cc + 1], 7.0, ALU.add, ALU.min), reads=[kg, "bT%d" % pb], writes=["eg%d" % q])
            P.act(lambda e, q=q: e.activation(out=es[q], in_=eg[q], func=AF.Sigmoid, scale=1.702), reads=["eg%d" % q], writes=["es%d" % q])
            P.dve(lambda e, psu=psu, q=q, cc=cc, pb=pb: e.tensor_scalar(eu[q], psu, bT1[pb][:, cc:cc + 1], 8.0, ALU.add, ALU.min), reads=[ku, "bT1%d" % pb], writes=["eu%d" % q])

            def tail(cc=cc, q=q):
                P.dve(lambda e: e.tensor_tensor(eg[q], eg[q], es[q], ALU.mult), reads=["eg%d" % q, "es%d" % q], writes=["eg%d" % q])
                P.dve(lambda e: e.scalar_tensor_tensor(aT[:, cc, :], eu[q], -6.0, eg[q], ALU.max, ALU.mult), reads=["eu%d" % q, "eg%d" % q], writes=["aT"])
            if pend is not None:
                pend()
            pend = tail
        pend()
        dkeys = ["wdn_%d" % kc for kc in range(8)]
        for s_ in range(4):
            for fn in gath[8 + s_]:
                fn()
            for fn in cast[8 + s_]:
                fn()
            yb = ysb[s_ % 2]; ky = "ysb%d" % (s_ % 2)
            for half in range(2):
                ps, pk = next_ps()

                def fd(e, ps=ps, s_=s_, half=half, pb=pb):
                    for k in range(8):
                        e.matmul(ps, aT[:, k, s_ * 128:(s_ + 1) * 128], wdn_b[:, k, half * 512:(half + 1) * 512], start=(k == 0), stop=False)
                    c0 = 2 * D + half * 512
                    return e.matmul(ps, ones_b[0:1, 0:128], bgb[pb][0:1, c0:c0 + 512], start=False, stop=True)
                P.pe(fd, reads=["aT", "bgb%d" % pb, "ones_b"] + dkeys, writes=[pk])
                if half == 0:
                    P.act(lambda e, ps=ps, yb=yb: e.copy(yb[:, 0:512], ps), reads=[pk], writes=[ky])
                else:
                    P.dve(lambda e, ps=ps, yb=yb: e.tensor_copy(yb[:, 512:1024], ps), reads=[pk], writes=[ky])
            P.dma(LQ, lambda e, b=b, s_=s_, yb=yb: e.dma_start(out=ys_v[b, s_], in_=yb), reads=[ky], writes=["ys_d"])
    P.barrier()
    AR.release()
    if stop_after == "I":
        return finish()

    AR.mark()
    yk = [[AR.alloc([128, D], BF16) for _ in range(4)] for _ in range(2)]
    x1r = [AR.alloc([128, D]) for _ in range(2)]
    acc = AR.alloc([128, D]); outt = [AR.alloc([128, D]) for _ in range(2)]
    jjunk = AR.alloc([128, D]); jssq = AR.alloc([128, 64]); jrstd = AR.alloc([128, 64])
    x1_r = x1_d.rearrange("(t p) d -> t p d", p=128)
    y_r = y.rearrange("(t p) d -> t p d", p=128)

    def j_load(ti):
        bi = ti % 2
        for k in range(4):
            P.dma("pool", lambda e, k=k: e.indirect_dma_start(out=yk[bi][k], out_offset=None, in_=ys_d, in_offset=bass.IndirectOffsetOnAxis(ap=dest_i[:, ti, k:k + 1], axis=0)), reads=["ys_d", "dest_i"], writes=["yk%d_%d" % (bi, k)])
        P.dma(LQ, lambda e: e.dma_start(out=x1r[bi], in_=x1_r[ti]), reads=["x1_d"], writes=["x1r%d" % bi])
    j_load(0)
    for ti in range(NTI):
        bi = ti % 2
        if ti + 1 < NTI:
            j_load(ti + 1)
        P.dve(lambda e, bi=bi, ti=ti: e.tensor_scalar_mul(acc, yk[bi][0], wk[:, ti, 0:1]), reads=["yk%d_0" % bi, "wk"], writes=["acc"])
        for k in range(1, 4):
            P.dve(lambda e, bi=bi, ti=ti, k=k: e.scalar_tensor_tensor(acc, yk[bi][k], wk[:, ti, k:k + 1], acc, ALU.mult, ALU.add), reads=["yk%d_%d" % (bi, k), "wk", "acc"], writes=["acc"])
        P.dve(lambda e: e.tensor_tensor(acc, acc, g2_bc, ALU.mult), reads=["acc", "g2_bc"], writes=["acc"])
        P.dve(lambda e, bi=bi: e.tensor_tensor(acc, acc, x1r[bi], ALU.add), reads=["acc", "x1r%d" % bi], writes=["acc"])
        P.act(lambda e, ti=ti: e.activation(out=jjunk, in_=acc, func=AF.Square, accum_out=jssq[:, ti:ti + 1]), reads=["acc"], writes=["jjunk", "jssq"])
        P.dve(lambda e, ti=ti: e.tensor_scalar(jrstd[:, ti:ti + 1], jssq[:, ti:ti + 1], 1.0 / D, EPS, ALU.mult, ALU.add), reads=["jssq"], writes=["jrstd"])
        P.act(lambda e, ti=ti: e.activation(out=jrstd[:, ti:ti + 1], in_=jrstd[:, ti:ti + 1], func=AF.Ln), reads=["jrstd"], writes=["jrstd"])
        P.act(lambda e, ti=ti: e.activation(out=jrstd[:, ti:ti + 1], in_=jrstd[:, ti:ti + 1], func=AF.Exp, scale=-0.5), reads=["jrstd"], writes=["jrstd"])
        P.dve(lambda e, bi=bi, ti=ti: e.scalar_tensor_tensor(outt[bi], acc, jrstd[:, ti:ti + 1], nf_bc, ALU.mult, ALU.mult), reads=["acc", "jrstd", "nf_bc"], writes=["outt%d" % bi])
        P.dma(LQ, lambda e, bi=bi, ti=ti: e.dma_start(out=y_r[ti], in_=outt[bi]), reads=["outt%d" % bi], writes=["y"])
    AR.release()
    return finish()


_CACHE = {}


def kernel(**inputs):
    n = 8
    if "nc" not in _CACHE:
        _CACHE["nc"] = build_program()
    nc = _CACHE["nc"]
    tabs = dft_tables()
    f = np.float32

    def a(v):
        return np.ascontiguousarray(np.asarray(v, dtype=f))
    shared = dict(
        c_ctx=a(inputs["c_ctx"]).reshape(1, D), w_ada=a(inputs["w_ada"][0]), b_ada=a(inputs["b_ada"][0]).reshape(1, -1),
        norm1=a(inputs["norm1"][0]).reshape(1, D), w_in=a(inputs["w_in"][0]), conv_w=a(inputs["conv_w"][0]),
        conv_b=a(inputs["conv_b"][0]).reshape(1, D), gate_a_w=a(inputs["gate_a_w"][0]), gate_a_b=a(inputs["gate_a_b"][0]),
        gate_x_w=a(inputs["gate_x_w"][0]), gate_x_b=a(inputs["gate_x_b"][0]), lru_lambda=a(inputs["lru_lambda"][0]),
        w_fourier=a(inputs["w_fourier"][0]), w_rnn=a(inputs["w_rnn"][0]), w_out=a(inputs["w_out"][0]),
        norm2=a(inputs["norm2"][0]).reshape(1, D), w_router=a(inputs["w_router"][0]), b_router=a(inputs["b_router"][0]).reshape(1, NE),
        w_gu=a(inputs["w_gu"][0]), b_gu=a(inputs["b_gu"][0]), w_down=a(inputs["w_down"][0]), b_down=a(inputs["b_down"][0]),
        norm_f=a(inputs["norm_f"]).reshape(1, D), **tabs)
    xs = a(inputs["x"]); cs = a(inputs["c"]); cx = a(inputs["ctx"])
    in_maps = []
    for i in range(n):
        m = dict(shared)
        m["x"] = xs[i]; m["c"] = cs[i].reshape(1, D); m["ctx"] = cx[i]
        in_maps.append(m)
    ncore = int(os.environ.get("KDBG_NCORE", "8"))
    if ncore != 8:
        res = run_bass_kernel_spmd(nc, in_maps[:ncore], core_ids=list(range(ncore)))
        return np.stack([np.asarray(r["y"], dtype=f) for r in res.results], axis=0)
    res = run_bass_kernel_spmd(nc, in_maps, core_ids=list(range(n)))
    return np.stack([np.asarray(r["y"], dtype=f) for r in res.results], axis=0)
```

```python
import contextlib
import math
import os
import numpy as np
import concourse.bass as bass
import concourse.mybir as mybir
from concourse.bass_utils import run_bass_kernel_spmd

F32 = mybir.dt.float32
BF16 = mybir.dt.bfloat16
I32 = mybir.dt.int32
U32 = mybir.dt.uint32
ALU = mybir.AluOpType
AF = mybir.ActivationFunctionType
AX = mybir.AxisListType

SELF_SYNC = True
NDSEM = 6

D = 1024
S = 8192
CTX = 256
NE = 32
BLK = 512
NBLK = 96
PSLOTS = NBLK * BLK
EPS = 1e-6


class Prog:
    ENGS = ("pe", "act", "dve", "pool", "sp")

    def __init__(self, nc):
        self.nc = nc
        self.ops = []

    def add(self, eng, fn, reads=(), writes=(), dma=False):
        self.ops.append(dict(eng=eng, fn=fn, reads=tuple(reads), writes=tuple(writes), dma=dma, bar=False))

    def pe(self, fn, reads=(), writes=()):
        self.add("pe", fn, reads, writes)

    def act(self, fn, reads=(), writes=()):
        self.add("act", fn, reads, writes)

    def dve(self, fn, reads=(), writes=()):
        self.add("dve", fn, reads, writes)

    def pool(self, fn, reads=(), writes=()):
        self.add("pool", fn, reads, writes)

    def dma(self, q, fn, reads=(), writes=()):
        self.add(q, fn, reads, writes, dma=True)

    def barrier(self):
        for e in self.ENGS:
            self.ops.append(dict(eng=e, fn=None, reads=(), writes=(), dma=False, bar=True))

    def emit(self):
        nc = self.nc
        ops = self.ops
        cnt = {e: 0 for e in self.ENGS}
        dcnt = {e: 0 for e in self.ENGS}
        latest = {}
        for op in ops:
            e = op["eng"]
            if op["bar"]:
                op["barvals"] = dict(latest)
                continue
            if op["dma"]:
                i = dcnt[e]
                dcnt[e] += 1
                op["sem"] = ("d", e, i % NDSEM)
                op["val"] = 16 * (i // NDSEM + 1)
            else:
                cnt[e] += 1
                op["sem"] = ("c", e)
                op["val"] = cnt[e]
            latest[op["sem"]] = op["val"]
        last_w = {}
        readers = {}
        for op in ops:
            if op["bar"]:
                continue
            deps = []
            for k in op["reads"]:
                if k in last_w:
                    deps.append(last_w[k])
            for k in op["writes"]:
                if k in last_w:
                    deps.append(last_w[k])
                deps.extend(readers.get(k, ()))
            op["deps"] = deps
            for k in op["writes"]:
                last_w[k] = op
                readers[k] = []
            for k in op["reads"]:
                if k not in op["writes"]:
                    readers.setdefault(k, []).append(op)
        known = {e: {} for e in self.ENGS}
        for op in ops:
            e = op["eng"]
            need = {}
            if op["bar"]:
                need = dict(op["barvals"])
                need.pop(("c", e), None)
            else:
                for d in op["deps"]:
                    if d is op:
                        continue
                    if (not d["dma"]) and d["eng"] == e and (e == "pe" or not SELF_SYNC):
                        continue
                    s, v = d["sem"], d["val"]
                    if need.get(s, 0) < v:
                        need[s] = v
                if op["dma"] and op["val"] > 16:
                    s = op["sem"]
                    if need.get(s, 0) < op["val"] - 16:
                        need[s] = op["val"] - 16
            w = []
            for s, v in need.items():
                if known[e].get(s, 0) < v:
                    known[e][s] = v
                    w.append((s, v))
            op["waits"] = w
        final = latest
        semkeys = sorted(final.keys(), key=str)
        with contextlib.ExitStack() as st:
            sems = {}
            for k in semkeys:
                sems[k] = st.enter_context(nc.semaphore("s_" + "_".join(map(str, k))))
            block = st.enter_context(nc.Block())

            def run(engname):
                def body(eng):
                    for op in ops:
                        if op["eng"] != engname:
                            continue
                        for s, v in op["waits"]:
                            eng.wait_ge(sems[s], v)
                        if op["bar"]:
                            continue
                        ins = op["fn"](eng)
                        ins.then_inc(sems[op["sem"]], 16 if op["dma"] else 1)
                    for k, v in final.items():
                        if k[0] == "d" and k[1] == engname:
                            eng.wait_ge(sems[k], v)
                return body

            block.sync(run("sp"))
            block.scalar(run("act"))
            block.vector(run("dve"))
            block.gpsimd(run("pool"))
            block.tensor(run("pe"))


class Arena:
    def __init__(self, base_ap, nbytes):
        self.base = base_ap
        self.nbytes = nbytes
        self.off = 0
        self.marks = []

    def alloc(self, shape, dt=F32):
        esz = {F32: 4, BF16: 2, I32: 4, U32: 4}[dt]
        npart = shape[0]
        fshape = list(shape[1:])
        n = 1
        for s_ in fshape:
            n *= s_
        nb = (n * esz + 31) // 32 * 32
        assert self.off + nb <= self.nbytes, ("SBUF arena overflow", self.off, nb, self.nbytes)
        a = self.base[0:npart, self.off // 4:(self.off + nb) // 4]
        self.off += nb
        if dt != F32:
            a = a.bitcast(dt)
        a = a[:, 0:n]
        if len(fshape) == 2:
            a = a.rearrange("p (a b) -> p a b", a=fshape[0])
        elif len(fshape) == 3:
            a = a.rearrange("p (a b c) -> p a b c", a=fshape[0], b=fshape[1])
        return a

    def mark(self):
        self.marks.append(self.off)

    def release(self):
        self.off = self.marks.pop()


def dft_tables():
    n = np.arange(128)
    ang = 2 * np.pi * np.outer(n, n) / 128.0
    c128 = np.cos(ang)
    s128 = np.sin(ang)
    k1 = np.arange(128)[:, None]
    n2 = np.arange(64)[None, :]
    tw = 2 * np.pi * k1 * n2 / 8192.0
    twc = np.cos(tw)
    tws = np.sin(tw)
    a64 = 2 * np.pi * np.outer(np.arange(64), np.arange(64)) / 64.0
    c2 = np.cos(a64)
    s2 = np.sin(a64)
    t3 = np.zeros((128, 128))
    t3[0:64, 0:64] = c2
    t3[64:128, 0:64] = -s2
    t3[0:64, 64:128] = s2
    t3[64:128, 64:128] = c2
    t3 = t3 / 1024.0
    f = np.float32
    return dict(k_c128=c128.astype(f), k_s128=s128.astype(f), k_twc=twc.astype(f), k_tws=tws.astype(f), k_t3=t3.astype(f))


def build_program(stop_after=None, debug=False):
    nc = bass.Bass("TRN2", target_bir_lowering=False)

    def din(name, shape, dt=F32):
        return nc.dram_tensor(name, list(shape), dt, kind="ExternalInput").ap()

    DBGSET = set(os.environ.get("KDBG_OUT", "").split(",")) if debug else set()

    def dscr(name, shape, dt=F32):
        if name in DBGSET:
            return nc.dram_tensor(name, list(shape), dt, kind="ExternalOutput").ap()
        return nc.dram_tensor(name, list(shape), dt).ap()

    x = din("x", [S, D]); c = din("c", [1, D]); ctx = din("ctx", [CTX, D]); c_ctx = din("c_ctx", [1, D])
    w_ada = din("w_ada", [D, 6 * D]); b_ada = din("b_ada", [1, 6 * D]); norm1 = din("norm1", [1, D])
    w_in = din("w_in", [D, 4608]); conv_w = din("conv_w", [4, D]); conv_b = din("conv_b", [1, D])
    gate_a_w = din("gate_a_w", [2, 8, 128, 128]); gate_a_b = din("gate_a_b", [2, D])
    gate_x_w = din("gate_x_w", [2, 8, 128, 128]); gate_x_b = din("gate_x_b", [2, D])
    lru_lambda = din("lru_lambda", [2, D])
    w_fourier = din("w_fourier", [512, D]); w_rnn = din("w_rnn", [D, D]); w_out = din("w_out", [D, D])
    norm2 = din("norm2", [1, D]); w_router = din("w_router", [D, NE]); b_router = din("b_router", [1, NE])
    w_gu = din("w_gu", [NE, D, 2 * D]); b_gu = din("b_gu", [NE, 2 * D])
    w_down = din("w_down", [NE, D, D]); b_down = din("b_down", [NE, D]); norm_f = din("norm_f", [1, D])
    k_c128 = din("k_c128", [128, 128]); k_s128 = din("k_s128", [128, 128])
    k_twc = din("k_twc", [128, 64]); k_tws = din("k_tws", [128, 64]); k_t3 = din("k_t3", [128, 128])
    y = nc.dram_tensor("y", [S, D], F32, kind="ExternalOutput").ap()
    dbg = nc.dram_tensor("dbg", [128, 2048], F32, kind="ExternalOutput").ap() if debug else None

    u_d = dscr("u_d", [S, 512], BF16)
    xc_d = dscr("xc_d", [D, S], F32)
    gg_d = dscr("gg_d", [D, S], BF16)
    gfr_d = dscr("gfr_d", [2 * D, S], BF16)
    q_d = dscr("q_d", [4, 2, 64, 128, 128], BF16)
    rt_d = dscr("rt_d", [D, S], BF16)
    yg_d = dscr("yg_d", [D, S], BF16)
    x1_d = dscr("x1_d", [S, D], F32)
    h2_d = dscr("h2_d", [S, D], BF16)
    xg_d = dscr("xg_d", [PSLOTS, D], BF16)
    ys_d = dscr("ys_d", [PSLOTS, D], BF16)

    st = contextlib.ExitStack()
    SBN = 206848
    sball = st.enter_context(nc.sbuf_tensor("sball", [128, SBN // 4], F32))
    AR = Arena(sball[:], SBN)
    banks = [st.enter_context(nc.psum_tensor("psb%d" % i, [128, 512], F32)) for i in range(8)]
    P = Prog(nc)
    psn = [0]

    def next_ps():
        i = psn[0] % 8
        psn[0] += 1
        return banks[i][:], "ps%d" % i

    uid = [0]

    def K(name):
        uid[0] += 1
        return "%s#%d" % (name, uid[0])

    def finish():
        with nc.allow_low_precision(reason="bf16 matmul operands, fp32 accumulation"):
            P.emit()
        st.close()
        return nc

    LQ = "sp"
    SQ = "pool"

    ident_f = AR.alloc([128, 128]); ident_b = AR.alloc([128, 128], BF16)
    ones_b = AR.alloc([128, 512], BF16); ones_f = AR.alloc([128, 128])
    ltri_b = AR.alloc([128, 128], BF16)
    g2_bc = AR.alloc([128, D]); nf_bc = AR.alloc([128, D])
    dest_i = AR.alloc([128, 64, 4], I32); wk = AR.alloc([128, 64, 4])
    eb_i = AR.alloc([128, NBLK], I32); widx = AR.alloc([128, NBLK, 8], I32)
    AR.mark()
    g1_bc = AR.alloc([128, D]); gm2_bc = AR.alloc([128, D]); sh2_bc = AR.alloc([128, D])
    gm1T = AR.alloc([128, 8]); sh1T = AR.alloc([128, 8]); gmcT = AR.alloc([128, 8]); shcT = AR.alloc([128, 8])
    cwT = AR.alloc([128, 8, 4]); cbT = AR.alloc([128, 8])
    nbaT = AR.alloc([128, 16]); nbxT = AR.alloc([128, 16]); coefT = AR.alloc([128, 16])
    h0T = AR.alloc([128, 16])
    Lg = AR.alloc([128, 64, NE])
    barow = Lg[0:1, :, :].rearrange("p a b -> p (a b)").bitcast(BF16)
    zt = AR.alloc([128, 1, D], BF16)
    P.pool(lambda e: e.memset(zt, 0.0), writes=["zt"])
    xgz_v = xg_d.rearrange("(b s p) d -> b p s d", p=128, s=1)

    P.pool(lambda e: e.memset(ident_f, 0.0), writes=["ident_f"])
    P.pool(lambda e: e.affine_select(out=ident_f, in_=ident_f, pattern=[[-1, 128]], compare_op=ALU.not_equal, fill=1.0, base=0, channel_multiplier=1), reads=["ident_f"], writes=["ident_f"])
    P.dve(lambda e: e.tensor_copy(ident_b, ident_f), reads=["ident_f"], writes=["ident_b"])
    P.pool(lambda e: e.memset(ones_b, 1.0), writes=["ones_b"])
    P.pool(lambda e: e.memset(ones_f, 1.0), writes=["ones_f"])
    P.pool(lambda e: e.memset(ltri_b, 1.0), writes=["ltri_b"])
    P.pool(lambda e: e.affine_select(out=ltri_b, in_=ltri_b, pattern=[[1, 128]], compare_op=ALU.is_gt, fill=0.0, base=0, channel_multiplier=-1), reads=["ltri_b"], writes=["ltri_b"])

    AR.mark()
    cT = AR.alloc([128, 16])
    crep = AR.alloc([128, 16, 128])
    mb = AR.alloc([128, 6 * D])
    mcb = AR.alloc([128, 2 * D])
    wa_buf = [AR.alloc([128, 8, 512]) for _ in range(2)]
    n1_bc = AR.alloc([128, D]); n2_bc = AR.alloc([128, D])
    P.dma(LQ, lambda e: e.dma_start(out=cT[:, 0:8], in_=c.rearrange("o (k p) -> p (o k)", p=128), allow_slow_non_contiguous=True), writes=["cT"])
    P.dma(LQ, lambda e: e.dma_start(out=cT[:, 8:16], in_=c_ctx.rearrange("o (k p) -> p (o k)", p=128), allow_slow_non_contiguous=True), reads=["cT"], writes=["cT"])
    P.dma(LQ, lambda e: e.dma_start(out=mb, in_=b_ada.partition_broadcast(128)), writes=["mb"])
    P.dma(LQ, lambda e: e.dma_start(out=n1_bc, in_=norm1.partition_broadcast(128)), writes=["n1_bc"])
    P.dma(LQ, lambda e: e.dma_start(out=n2_bc, in_=norm2.partition_broadcast(128)), writes=["n2_bc"])
    P.dma(LQ, lambda e: e.dma_start(out=nf_bc, in_=norm_f.partition_broadcast(128)), writes=["nf_bc"])
    ctmp = AR.alloc([128, 16])
    P.act(lambda e: e.activation(out=ctmp, in_=cT, func=AF.Exp, scale=-1.0), reads=["cT"], writes=["ctmp"])
    P.dve(lambda e: e.tensor_scalar_add(ctmp, ctmp, 1.0), reads=["ctmp"], writes=["ctmp"])
    P.dve(lambda e: e.reciprocal(ctmp, ctmp), reads=["ctmp"], writes=["ctmp"])
    P.dve(lambda e: e.tensor_tensor(cT, cT, ctmp, ALU.mult), reads=["ctmp", "cT"], writes=["cT"])
    for j in range(16):
        P.dve(lambda e, j=j: e.tensor_copy(crep[:, j, :], cT[:, j:j + 1].to_broadcast([128, 128])), reads=["cT"], writes=["crep"])
    w_ada_v = w_ada.rearrange("(k p) n -> p k n", p=128)
    for ch in range(12):
        bi = ch % 2
        P.dma(LQ, lambda e, ch=ch, bi=bi: e.dma_start(out=wa_buf[bi], in_=w_ada_v[:, :, ch * 512:(ch + 1) * 512]), writes=["wa%d" % bi])
        ps, pk = next_ps()

        def f(e, ps=ps, bi=bi):
            for k in range(8):
                ins = e.matmul(ps, crep[:, k, :], wa_buf[bi][:, k, :], start=(k == 0), stop=(k == 7))
            return ins
        P.pe(f, reads=["crep", "wa%d" % bi], writes=[pk])
        P.dve(lambda e, ps=ps, ch=ch: e.tensor_tensor(mb[:, ch * 512:(ch + 1) * 512], mb[:, ch * 512:(ch + 1) * 512], ps, ALU.add), reads=[pk, "mb"], writes=["mb"])
        if ch < 4:
            ps2, pk2 = next_ps()

            def f2(e, ps2=ps2, bi=bi):
                for k in range(8):
                    ins = e.matmul(ps2, crep[:, 8 + k, :], wa_buf[bi][:, k, :], start=(k == 0), stop=(k == 7))
                return ins
            P.pe(f2, reads=["crep", "wa%d" % bi], writes=[pk2])
            P.act(lambda e, ps2=ps2, ch=ch: e.copy(mcb[:, ch * 512:(ch + 1) * 512], ps2), reads=[pk2], writes=["mcb"])
    bada2 = AR.alloc([128, 2 * D])
    P.dma(LQ, lambda e: e.dma_start(out=bada2, in_=b_ada[:, 0:2 * D].partition_broadcast(128)), writes=["bada2"])
    P.dve(lambda e: e.tensor_tensor(mcb, mcb, bada2, ALU.add), reads=["mcb", "bada2"], writes=["mcb"])
    gm1_bc = AR.alloc([128, D]); gmc_bc = AR.alloc([128, D])
    P.dve(lambda e: e.scalar_tensor_tensor(gm1_bc, mb[:, D:2 * D], 1.0, n1_bc, ALU.add, ALU.mult), reads=["mb", "n1_bc"], writes=["gm1_bc"])
    P.dve(lambda e: e.scalar_tensor_tensor(gmc_bc, mcb[:, D:2 * D], 1.0, n1_bc, ALU.add, ALU.mult), reads=["mcb", "n1_bc"], writes=["gmc_bc"])
    P.dve(lambda e: e.scalar_tensor_tensor(gm2_bc, mb[:, 4 * D:5 * D], 1.0, n2_bc, ALU.add, ALU.mult), reads=["mb", "n2_bc"], writes=["gm2_bc"])
    P.act(lambda e: e.copy(g1_bc, mb[:, 2 * D:3 * D]), reads=["mb"], writes=["g1_bc"])
    P.act(lambda e: e.copy(sh2_bc, mb[:, 3 * D:4 * D]), reads=["mb"], writes=["sh2_bc"])
    P.act(lambda e: e.copy(g2_bc, mb[:, 5 * D:6 * D]), reads=["mb"], writes=["g2_bc"])
    for (src, sk, dst, dk) in ((gm1_bc, "gm1_bc", gm1T, "gm1T"), (mb, "mb", sh1T, "sh1T"), (gmc_bc, "gmc_bc", gmcT, "gmcT"), (mcb, "mcb", shcT, "shcT")):
        for kc in range(8):
            ps, pk = next_ps()
            P.pe(lambda e, ps=ps, src=src, kc=kc: e.transpose(ps[:, 0:128], src[:, kc * 128:(kc + 1) * 128], ident_f), reads=[sk, "ident_f"], writes=[pk])
            P.dve(lambda e, ps=ps, dst=dst, kc=kc: e.tensor_copy(dst[:, kc:kc + 1], ps[:, 0:1]), reads=[pk], writes=[dk])
    for kk_ in range(4):
        P.dma(LQ, lambda e, kk_=kk_: e.dma_start(out=cwT[:, :, kk_], in_=conv_w[kk_:kk_ + 1, :].rearrange("o (h p) -> p (o h)", p=128), allow_slow_non_contiguous=True), reads=["cwT"], writes=["cwT"])
    P.dma(LQ, lambda e: e.dma_start(out=cbT, in_=conv_b.rearrange("o (h p) -> p (o h)", p=128), allow_slow_non_contiguous=True), writes=["cbT"])
    P.dma(LQ, lambda e: e.dma_start(out=nbaT, in_=gate_a_b.rearrange("d (h p) -> p (d h)", p=128), allow_slow_non_contiguous=True), writes=["nbaT"])
    P.dma(LQ, lambda e: e.dma_start(out=nbxT, in_=gate_x_b.rearrange("d (h p) -> p (d h)", p=128), allow_slow_non_contiguous=True), writes=["nbxT"])
    P.dma(LQ, lambda e: e.dma_start(out=coefT, in_=lru_lambda.rearrange("d (h p) -> p (d h)", p=128), allow_slow_non_contiguous=True), writes=["coefT"])
    P.act(lambda e: e.activation(out=coefT, in_=coefT, func=AF.Exp, scale=-1.0), reads=["coefT"], writes=["coefT"])
    P.act(lambda e: e.activation(out=coefT, in_=coefT, func=AF.Ln, bias=1.0), reads=["coefT"], writes=["coefT"])
    P.dve(lambda e: e.tensor_scalar_mul(coefT, coefT, -8.0), reads=["coefT"], writes=["coefT"])
    P.barrier()
    AR.release()
    if stop_after == "A":
        return finish()

    AR.mark()
    gw_b = AR.alloc([128, 32, 128], BF16)
    AR.mark()
    win_b = AR.alloc([128, 8, 4608], BF16)
    AR.mark()
    stg = [AR.alloc([128, 8, 512]) for _ in range(2)]
    w_in_v = w_in.rearrange("(k p) n -> p k n", p=128)
    for ch in range(9):
        bi = ch % 2
        P.dma(LQ, lambda e, ch=ch, bi=bi: e.dma_start(out=stg[bi], in_=w_in_v[:, :, ch * 512:(ch + 1) * 512]), writes=["stg%d" % bi])
        if ch % 2 == 0:
            P.act(lambda e, ch=ch, bi=bi: e.copy(win_b[:, :, ch * 512:(ch + 1) * 512], stg[bi]), reads=["stg%d" % bi], writes=["win_b"])
        else:
            P.dve(lambda e, ch=ch, bi=bi: e.tensor_copy(win_b[:, :, ch * 512:(ch + 1) * 512], stg[bi]), reads=["stg%d" % bi], writes=["win_b"])
    for gi, gwd in enumerate((gate_a_w, gate_x_w)):
        bi = gi % 2
        P.dma(LQ, lambda e, gwd=gwd, bi=bi: e.dma_start(out=stg[bi][:, 0:4, :].rearrange("p a (b c) -> p (a b) c", c=128), in_=gwd.rearrange("d h i j -> i (d h) j")), writes=["stg%d" % bi])
        P.dve(lambda e, gi=gi, bi=bi: e.tensor_copy(gw_b[:, gi * 16:(gi + 1) * 16, :], stg[bi][:, 0:4, :].rearrange("p a (b c) -> p (a b) c", c=128)), reads=["stg%d" % bi], writes=["gw_b"])
    for gi, gbd in enumerate((gate_a_b, gate_x_b)):
        bi = gi % 2
        P.dma(LQ, lambda e, gbd=gbd, bi=bi: e.dma_start(out=stg[bi][0:1, 0:4, :].rearrange("p a b -> p (a b)"), in_=gbd.rearrange("d (o n) -> o (d n)", o=1)), reads=["stg%d" % bi], writes=["stg%d" % bi])
        P.act(lambda e, gi=gi, bi=bi: e.copy(barow[0:1, gi * 2048:(gi + 1) * 2048], stg[bi][0:1, 0:4, :].rearrange("p a b -> p (a b)")), reads=["stg%d" % bi], writes=["barow"])
    P.barrier()
    AR.release()
    if stop_after == "W":
        return finish()

    def rms_rows(xt, nsub, ssq, rstd, junk, kx, kpre):
        for s_ in range(nsub):
            P.act(lambda e, s_=s_: e.activation(out=junk, in_=xt[:, s_, :], func=AF.Square, accum_out=ssq[:, s_:s_ + 1]), reads=[kx], writes=[kpre + "junk", kpre + "ssq"])
        P.dve(lambda e: e.tensor_scalar(rstd[:, 0:nsub], ssq[:, 0:nsub], 1.0 / D, EPS, ALU.mult, ALU.add), reads=[kpre + "ssq"], writes=[kpre + "rstd"])
        P.act(lambda e: e.activation(out=rstd[:, 0:nsub], in_=rstd[:, 0:nsub], func=AF.Ln), reads=[kpre + "rstd"], writes=[kpre + "rstd"])
        P.act(lambda e: e.activation(out=rstd[:, 0:nsub], in_=rstd[:, 0:nsub], func=AF.Exp, scale=-0.5), reads=[kpre + "rstd"], writes=[kpre + "rstd"])

    def norm_transpose(xt, nsub, rstd, xs_b, hT, gT, sT, kx, kpre, khT):
        for s_ in range(nsub):
            P.act(lambda e, s_=s_: e.activation(out=xs_b[:, s_, :], in_=xt[:, s_, :], func=AF.Copy, scale=rstd[:, s_:s_ + 1]), reads=[kx, kpre + "rstd"], writes=[kpre + "xs"])
        for kc in range(8):
            ps, pk = next_ps()
            psb = ps.bitcast(BF16)

            def f(e, psb=psb, kc=kc):
                for s_ in range(nsub):
                    ins = e.transpose(psb[:, s_ * 128:(s_ + 1) * 128], xs_b[:, s_, kc * 128:(kc + 1) * 128], ident_b)
                return ins
            P.pe(f, reads=[kpre + "xs", "ident_b"], writes=[pk])
            n = nsub * 128
            if kc % 2 == 0:
                P.act(lambda e, psb=psb, kc=kc, n=n: e.activation(out=hT[:, kc, 0:n], in_=psb[:, 0:n], func=AF.Identity, scale=gT[:, kc:kc + 1], bias=sT[:, kc:kc + 1]), reads=[pk], writes=[khT])
            else:
                P.dve(lambda e, psb=psb, kc=kc, n=n: e.tensor_scalar(hT[:, kc, 0:n], psb[:, 0:n], gT[:, kc:kc + 1], sT[:, kc:kc + 1], ALU.mult, ALU.add), reads=[pk], writes=[khT])

    def conv_from_psum(ps, out_t, h, ntok, rowlen, kps, kout):
        nr = ntok // rowlen
        P.dve(lambda e: e.tensor_scalar(out_t[:, 0:ntok], ps[:, 0:ntok], cwT[:, h, 2:3], cbT[:, h:h + 1], ALU.mult, ALU.add), reads=[kps, "cwT", "cbT"], writes=[kout])
        o3 = out_t[:, 0:ntok].rearrange("p (r t) -> p r t", t=rowlen)
        z3 = ps[:, 0:ntok].rearrange("p (r t) -> p r t", t=rowlen)
        for (kk, sh) in ((0, -2), (1, -1), (3, 1)):
            if sh < 0:
                oo = o3[:, :, -sh:rowlen]; zz = z3[:, :, 0:rowlen + sh]
            else:
                oo = o3[:, :, 0:rowlen - sh]; zz = z3[:, :, sh:rowlen]
            P.dve(lambda e, oo=oo, zz=zz, kk=kk: e.scalar_tensor_tensor(oo, zz, cwT[:, h, kk:kk + 1], oo, ALU.mult, ALU.add), reads=[kps, kout], writes=[kout])

    def rnn_chunk(xc_f, xc_b, d, h, n, bufs, kxc, kpre):
        ia = 0 * 16 + d * 8 + h
        ix = 1 * 16 + d * 8 + h
        dh = d * 8 + h
        e1, a_, e2, s_, b_ = bufs["e1"], bufs["a"], bufs["e2"], bufs["s"], bufs["b"]
        nch = (n + 511) // 512
        psr = []
        for g_, wi in ((0, ia), (1, ix)):
            lst = []
            for j in range(nch):
                ps, pk = next_ps()
                w_ = min(512, n - j * 512)
                boff = g_ * 2048 + d * 1024 + h * 128

                def fgm(e, ps=ps, wi=wi, j=j, w_=w_, boff=boff):
                    e.matmul(ps[:, 0:w_], gw_b[:, wi, :], xc_b[:, j * 512:j * 512 + w_], start=True, stop=False)
                    return e.matmul(ps[:, 0:w_], barow[0:1, boff:boff + 128], ones_b[0:1, 0:w_], start=False, stop=True)
                P.pe(fgm, reads=[kxc + "b", "gw_b", "barow", "ones_b"], writes=[pk])
                lst.append((ps, pk, j, w_))
            psr.append(lst)
        for (ps, pk, j, w_) in psr[0]:
            P.act(lambda e, ps=ps, j=j, w_=w_: e.activation(out=e1[:, j * 512:j * 512 + w_], in_=ps[:, 0:w_], func=AF.Sigmoid), reads=[pk], writes=[kpre + "e1"])
        for (ps, pk, j, w_) in psr[1]:
            P.act(lambda e, ps=ps, j=j, w_=w_: e.activation(out=e2[:, j * 512:j * 512 + w_], in_=ps[:, 0:w_], func=AF.Sigmoid), reads=[pk], writes=[kpre + "e2"])
        P.act(lambda e: e.activation(out=a_[:, 0:n], in_=e1[:, 0:n], func=AF.Exp, scale=coefT[:, dh:dh + 1]), reads=[kpre + "e1", "coefT"], writes=[kpre + "a"])
        P.dve(lambda e: e.tensor_tensor(s_[:, 0:n], a_[:, 0:n], a_[:, 0:n], ALU.mult), reads=[kpre + "a"], writes=[kpre + "s"])
        P.act(lambda e: e.activation(out=s_[:, 0:n], in_=s_[:, 0:n], func=AF.Ln, scale=-1.0, bias=1.0), reads=[kpre + "s"], writes=[kpre + "s"])
        P.act(lambda e: e.activation(out=s_[:, 0:n], in_=s_[:, 0:n], func=AF.Exp, scale=0.5), reads=[kpre + "s"], writes=[kpre + "s"])
        P.pool(lambda e: e.tensor_tensor(b_[:, 0:n], e2[:, 0:n], xc_f, ALU.mult), reads=[kpre + "e2", kxc], writes=[kpre + "b"])
        P.dve(lambda e: e.tensor_tensor(b_[:, 0:n], b_[:, 0:n], s_[:, 0:n], ALU.mult), reads=[kpre + "b", kpre + "s"], writes=[kpre + "b"])

    AR.mark()
    cx = AR.alloc([128, 2, D]); cjunk = AR.alloc([128, D]); cssq = AR.alloc([128, 4]); crstd = AR.alloc([128, 4])
    cxs = AR.alloc([128, 2, D], BF16); hcT = AR.alloc([128, 8, CTX], BF16)
    xcc = AR.alloc([128, 8, CTX]); xccb = AR.alloc([128, 8, CTX], BF16)
    cb_ = dict(e1=AR.alloc([128, CTX]), a=AR.alloc([128, CTX]), e2=AR.alloc([128, CTX]), s=AR.alloc([128, CTX]), b=AR.alloc([128, CTX]))
    chh = AR.alloc([128, CTX])
    P.dma(LQ, lambda e: e.dma_start(out=cx, in_=ctx.rearrange("(s p) d -> p s d", p=128)), writes=["cx"])
    rms_rows(cx, 2, cssq, crstd, cjunk, "cx", "c_")
    norm_transpose(cx, 2, crstd, cxs, hcT, gmcT, shcT, "cx", "c_", "hcT")
    for h in range(8):
        ps, pk = next_ps()

        def f(e, ps=ps, h=h):
            for k in range(8):
                ins = e.matmul(ps[:, 0:CTX], win_b[:, k, 512 + h * 128:512 + (h + 1) * 128], hcT[:, k, :], start=(k == 0), stop=(k == 7))
            return ins
        P.pe(f, reads=["win_b", "hcT"], writes=[pk])
        conv_from_psum(ps, xcc[:, h, :], h, CTX, CTX, pk, "xcc%d" % h)
        P.act(lambda e, h=h: e.copy(xccb[:, h, :], xcc[:, h, :]), reads=["xcc%d" % h], writes=["xcc%db" % h])
        for d in range(2):
            rnn_chunk(xcc[:, h, :], xccb[:, h, :], d, h, CTX, cb_, "xcc%d" % h, "c_")
            if d == 0:
                P.dve(lambda e: e.tensor_tensor_scan(chh, cb_["a"], cb_["b"], 0.0, ALU.mult, ALU.add), reads=["c_a", "c_b"], writes=["chh"])
                P.dve(lambda e, h=h: e.tensor_copy(h0T[:, h:h + 1], chh[:, CTX - 1:CTX]), reads=["chh"], writes=["h0T"])
            else:
                P.dve(lambda e: e.tensor_tensor_scan(chh[:, ::-1], cb_["a"][:, ::-1], cb_["b"][:, ::-1], 0.0, ALU.mult, ALU.add), reads=["c_a", "c_b"], writes=["chh"])
                P.dve(lambda e, h=h: e.tensor_copy(h0T[:, 8 + h:9 + h], chh[:, 0:1]), reads=["chh"], writes=["h0T"])
    P.barrier()
    AR.release()
    if stop_after == "C":
        return finish()

    AR.mark()
    NT = S // 512
    xt = [AR.alloc([128, 4, D]) for _ in range(2)]
    djunk = AR.alloc([128, D], BF16); dssq = AR.alloc([128, 4]); drstd = AR.alloc([128, 4])
    dxs = AR.alloc([128, 4, D], BF16)
    hxT = AR.alloc([128, 8, 512], BF16)
    u_t = AR.alloc([128, 4, 512], BF16)
    xc_t = [AR.alloc([128, 512]) for _ in range(2)]
    gg_t = AR.alloc([128, 8, 512], BF16)
    gfr_t = AR.alloc([128, 8, 512], BF16)
    tA = [AR.alloc([128, 512]) for _ in range(2)]
    tB = [AR.alloc([128, 512]) for _ in range(2)]
    x_v = x.rearrange("(t s p) d -> t p s d", p=128, s=4)
    u_v = u_d.rearrange("(t s p) n -> t p s n", p=128, s=4)
    xc_v = xc_d.rearrange("(h p) t -> p h t", p=128)
    gg_v = gg_d.rearrange("(h p) t -> p h t", p=128)
    gfr_v = gfr_d.rearrange("(h p) t -> p h t", p=128)
    P.dma(LQ, lambda e: e.dma_start(out=xt[0], in_=x_v[0]), writes=["xt0"])
    for t in range(NT):
        bi = t % 2
        if t + 1 < NT:
            P.dma(LQ, lambda e, t=t: e.dma_start(out=xt[(t + 1) % 2], in_=x_v[t + 1]), writes=["xt%d" % ((t + 1) % 2)])
        rms_rows(xt[bi], 4, dssq, drstd, djunk, "xt%d" % bi, "d_")
        norm_transpose(xt[bi], 4, drstd, dxs, hxT, gm1T, sh1T, "xt%d" % bi, "d_", "hxT")
        for s_ in range(4):
            ps, pk = next_ps()

            def f(e, ps=ps, s_=s_):
                for k in range(8):
                    ins = e.matmul(ps, hxT[:, k, s_ * 128:(s_ + 1) * 128], win_b[:, k, 0:512], start=(k == 0), stop=(k == 7))
                return ins
            P.pe(f, reads=["hxT", "win_b"], writes=[pk])
            P.act(lambda e, ps=ps, s_=s_: e.copy(u_t[:, s_, :], ps), reads=[pk], writes=["u_t"])
        P.dma(SQ, lambda e, t=t: e.dma_start(out=u_v[t], in_=u_t), reads=["u_t"], writes=["u_d"])
        for cc in range(4, 36):
            ps, pk = next_ps()

            def f(e, ps=ps, cc=cc):
                for k in range(8):
                    ins = e.matmul(ps, win_b[:, k, cc * 128:(cc + 1) * 128], hxT[:, k, :], start=(k == 0), stop=(k == 7))
                return ins
            P.pe(f, reads=["hxT", "win_b"], writes=[pk])
            if cc < 12:
                h = cc - 4
                ob = xc_t[h % 2]; ok = "xc_t%d" % (h % 2)
                conv_from_psum(ps, ob, h, 512, 64, pk, ok)
                P.dma(SQ, lambda e, ob=ob, h=h, t=t: e.dma_start(out=xc_v[:, h, t * 512:(t + 1) * 512], in_=ob), reads=[ok], writes=["xc_d"])
            elif cc < 20:
                h = cc - 12
                a_ = tA[h % 2]; b_ = tB[h % 2]; ka = "tA%d" % (h % 2); kb = "tB%d" % (h % 2)
                P.act(lambda e, ps=ps, a_=a_: e.activation(out=a_, in_=ps, func=AF.Square), reads=[pk], writes=[ka])
                P.dve(lambda e, a_=a_: e.tensor_scalar(a_, a_, 0.044715, 1.0, ALU.mult, ALU.add), reads=[ka], writes=[ka])
                P.dve(lambda e, ps=ps, a_=a_: e.tensor_tensor(a_, a_, ps, ALU.mult), reads=[ka, pk], writes=[ka])
                P.act(lambda e, a_=a_, b_=b_: e.activation(out=b_, in_=a_, func=AF.Sigmoid, scale=1.5957691216057308), reads=[ka], writes=[kb])
                P.dve(lambda e, ps=ps, b_=b_, h=h: e.tensor_tensor(gg_t[:, h, :], b_, ps, ALU.mult), reads=[kb, pk], writes=["gg_t"])
            else:
                h = cc - 20
                P.act(lambda e, ps=ps, h=h: e.activation(out=gfr_t[:, h % 8, :], in_=ps, func=AF.Sigmoid), reads=[pk], writes=["gfr_t"])
                if h % 8 == 7:
                    P.dma(SQ, lambda e, t=t, h=h: e.dma_start(out=gfr_v[:, (h // 8) * 8:(h // 8) * 8 + 8, t * 512:(t + 1) * 512], in_=gfr_t), reads=["gfr_t"], writes=["gfr_d"])
        P.dma(SQ, lambda e, t=t: e.dma_start(out=gg_v[:, :, t * 512:(t + 1) * 512], in_=gg_t), reads=["gg_t"], writes=["gg_d"])
    P.barrier()
    AR.release()
    AR.release()
    if stop_after == "D":
        return finish()

    AR.mark()
    xcf = AR.alloc([128, S]); xcb = AR.alloc([128, S], BF16); hf = AR.alloc([128, S])
    CH = 1024
    NCH = S // CH
    rb = [dict(e1=AR.alloc([128, CH]), a=AR.alloc([128, CH]), e2=AR.alloc([128, CH]), s=AR.alloc([128, CH]), b=AR.alloc([128, CH])) for _ in range(2)]
    hb = [AR.alloc([128, CH]) for _ in range(2)]
    ggc = [AR.alloc([128, CH], BF16) for _ in range(2)]
    ygc = [AR.alloc([128, CH], BF16) for _ in range(2)]
    yg_v = yg_d.rearrange("(h p) t -> p h t", p=128)
    for h in range(8):
        P.dma(LQ, lambda e, h=h: e.dma_start(out=xcf, in_=xc_v[:, h, :]), reads=["xc_d"], writes=["xcf"])
        for zb in range(h * 48, (h + 1) * 48):
            P.dma(LQ, lambda e, zb=zb: e.dma_start(out=xgz_v[zb], in_=zt), reads=["zt"], writes=["xg_d"])
        P.act(lambda e: e.copy(xcb[:, 0:S // 2], xcf[:, 0:S // 2]), reads=["xcf"], writes=["xcfb"])
        P.dve(lambda e: e.tensor_copy(xcb[:, S // 2:S], xcf[:, S // 2:S]), reads=["xcf", "xcfb"], writes=["xcfb"])
        it = 0
        for d in range(2):
            order = list(range(NCH)) if d == 0 else list(range(NCH - 1, -1, -1))
            prev = None
            for ci in order:
                bi = it % 2
                it += 1
                sl = slice(ci * CH, (ci + 1) * CH)
                kp = "r%d_" % bi
                rnn_chunk(xcf[:, sl], xcb[:, sl], d, h, CH, rb[bi], "xcf", kp)
                dh = d * 8 + h
                if d == 0:
                    init = h0T[:, dh:dh + 1] if prev is None else hf[:, ci * CH - 1:ci * CH]
                    P.dve(lambda e, bi=bi, sl=sl, init=init: e.tensor_tensor_scan(hf[:, sl], rb[bi]["a"], rb[bi]["b"], init, ALU.mult, ALU.add), reads=[kp + "a", kp + "b", "hf", "h0T"], writes=["hf"])
                else:
                    if prev is None:
                        init = h0T[:, dh:dh + 1]; kinit = "h0T"
                    else:
                        init = hb[prev][:, 0:1]; kinit = "hb%d" % prev
                    P.dma(LQ, lambda e, bi=bi, sl=sl, h=h: e.dma_start(out=ggc[bi], in_=gg_v[:, h, sl]), reads=["gg_d"], writes=["ggc%d" % bi])
                    P.dve(lambda e, bi=bi, init=init: e.tensor_tensor_scan(hb[bi][:, ::-1], rb[bi]["a"][:, ::-1], rb[bi]["b"][:, ::-1], init, ALU.mult, ALU.add), reads=[kp + "a", kp + "b", kinit], writes=["hb%d" % bi])
                    P.dve(lambda e, bi=bi, sl=sl: e.tensor_tensor(rb[bi]["s"], hb[bi], hf[:, sl], ALU.add), reads=["hb%d" % bi, "hf", kp + "s"], writes=[kp + "s"])
                    P.dve(lambda e, bi=bi: e.tensor_tensor(ygc[bi], rb[bi]["s"], ggc[bi], ALU.mult), reads=[kp + "s", "ggc%d" % bi], writes=["ygc%d" % bi])
                    P.dma(SQ, lambda e, bi=bi, sl=sl, h=h: e.dma_start(out=yg_v[:, h, sl], in_=ygc[bi]), reads=["ygc%d" % bi], writes=["yg_d"])
                    prev = bi
                if d == 0:
                    prev = bi
    P.barrier()
    AR.release()
    AR.release()
    if stop_after == "E":
        return finish()

    AR.mark()
    c1b = AR.alloc([128, 128], BF16); s1b = AR.alloc([128, 128], BF16); t3b = AR.alloc([128, 128], BF16)
    twc = AR.alloc([128, 64]); tws = AR.alloc([128, 64])
    ftmp = AR.alloc([128, 128])
    for (src, dst, kk) in ((k_c128, c1b, "c1b"), (k_s128, s1b, "s1b"), (k_t3, t3b, "t3b")):
        P.dma(LQ, lambda e, src=src: e.dma_start(out=ftmp, in_=src), writes=["ftmp"])
        P.dve(lambda e, dst=dst: e.tensor_copy(dst, ftmp), reads=["ftmp"], writes=[kk])
    P.dma(LQ, lambda e: e.dma_start(out=twc, in_=k_twc), writes=["twc"])
    P.dma(LQ, lambda e: e.dma_start(out=tws, in_=k_tws), writes=["tws"])
    AR.mark()
    U = AR.alloc([128, 64, 512], BF16)
    qt = [AR.alloc([128, 2, 512], BF16) for _ in range(2)]
    f1 = [AR.alloc([128, 512]) for _ in range(2)]
    f2 = [AR.alloc([128, 512]) for _ in range(2)]
    u_pv = u_d.rearrange("(p n) c -> p n c", n=64)
    for uq in range(4):
        P.dma(LQ, lambda e, uq=uq: e.dma_start(out=U[:, uq * 16:(uq + 1) * 16, :], in_=u_pv[:, uq * 16:(uq + 1) * 16, :]), reads=["u_d"], writes=["U"])
    FDBG = int(os.environ.get("FDBG", "0"))
    for n2 in range(64 if FDBG != 2 else 0):
        bi = n2 % 2
        psr, kr = next_ps()
        psi, ki = next_ps()
        P.pe(lambda e, psr=psr, n2=n2: e.matmul(psr, c1b, U[:, n2, :], start=True, stop=True), reads=["U", "c1b"], writes=[kr])
        P.pe(lambda e, psi=psi, n2=n2: e.matmul(psi, s1b, U[:, n2, :], start=True, stop=True), reads=["U", "s1b"], writes=[ki])
        P.dve(lambda e, psi=psi, n2=n2, bi=bi: e.tensor_scalar_mul(f1[bi], psi, tws[:, n2:n2 + 1]), reads=[ki, "tws"], writes=["f1%d" % bi])
        P.dve(lambda e, psr=psr, n2=n2, bi=bi: e.tensor_scalar_mul(f2[bi], psr, tws[:, n2:n2 + 1]), reads=[kr, "tws"], writes=["f2%d" % bi])
        P.dve(lambda e, psr=psr, n2=n2, bi=bi: e.scalar_tensor_tensor(qt[bi][:, 0, :], psr, twc[:, n2:n2 + 1], f1[bi], ALU.mult, ALU.subtract), reads=[kr, "twc", "f1%d" % bi], writes=["qt%d" % bi])
        P.dve(lambda e, psi=psi, n2=n2, bi=bi: e.scalar_tensor_tensor(qt[bi][:, 1, :], psi, twc[:, n2:n2 + 1], f2[bi], ALU.mult, ALU.add), reads=[ki, "twc", "f2%d" % bi, "qt%d" % bi], writes=["qt%d" % bi])
        for r in range(2 if FDBG != 1 else 0):
            P.dma(SQ, lambda e, n2=n2, bi=bi, r=r: e.dma_start(out=q_d[:, r, n2, :, :].rearrange("g k c -> k g c"), in_=qt[bi][:, r, :].rearrange("k (g c) -> k g c", g=4)), reads=["qt%d" % bi], writes=["q_d"])
    P.barrier()
    AR.release()
    if stop_after == "F1":
        return finish()
    AR.mark()
    Qg = [AR.alloc([128, 128, 128], BF16) for _ in range(2)]
    RT = [AR.alloc([128, 2, S], BF16) for _ in range(2)]
    rt_v = rt_d.rearrange("(g r j) t -> g j r t", r=2, j=128)
    for g in range(4):
        bi = g % 2
        for r in range(2):
            P.dma(LQ, lambda e, g=g, r=r, bi=bi: e.dma_start(out=Qg[bi][r * 64:(r + 1) * 64, :, :], in_=q_d[g, r]), reads=["q_d"], writes=["Qg%d" % bi])
        for k0 in range(0, 128, 4):
            ps, pk = next_ps()

            def f(e, ps=ps, k0=k0, bi=bi):
                for kk in range(4):
                    ins = e.matmul(ps[:, kk * 128:(kk + 1) * 128], Qg[bi][:, k0 + kk, :], t3b, start=True, stop=True)
                return ins
            P.pe(f, reads=["Qg%d" % bi, "t3b"], writes=[pk])
            psv = ps.rearrange("j (k r n) -> j r k n", k=4, r=2)
            for r in range(2):
                ov = RT[bi][:, r, :].rearrange("j (n k) -> j k n", k=128)[:, k0:k0 + 4, :]
                if r == 0:
                    P.act(lambda e, ov=ov, psv=psv, r=r: e.copy(ov, psv[:, r, :, :]), reads=[pk], writes=["RT%d" % bi])
                else:
                    P.dve(lambda e, ov=ov, psv=psv, r=r: e.tensor_copy(ov, psv[:, r, :, :]), reads=[pk], writes=["RT%d" % bi])
        P.dma(SQ, lambda e, g=g, bi=bi: e.dma_start(out=rt_v[g], in_=RT[bi]), reads=["RT%d" % bi], writes=["rt_d"])
    P.barrier()
    AR.release()
    AR.release()
    if stop_after == "F":
        return finish()

    AR.mark()
    wfp = AR.alloc([128, 8, D], BF16)
    wr_b = AR.alloc([128, 8, D], BF16)
    wo_b = AR.alloc([128, 8, D], BF16)
    wrt_f = AR.alloc([128, 8, NE])
    brt = AR.alloc([1, NE])
    AR.mark()
    gstg = AR.alloc([128, 8, D])
    cdb = AR.alloc([128, 128], BF16); sdb = AR.alloc([128, 128], BF16)
    wf_b = AR.alloc([128, 4, D], BF16)
    P.dma(LQ, lambda e: e.dma_start(out=gstg[:, 0, 0:128], in_=k_c128), writes=["gstg"])
    P.dve(lambda e: e.tensor_copy(cdb, gstg[:, 0, 0:128]), reads=["gstg"], writes=["cdb"])
    P.dma(LQ, lambda e: e.dma_start(out=gstg[:, 0, 0:128], in_=k_s128), reads=["gstg"], writes=["gstg"])
    P.dve(lambda e: e.tensor_scalar_mul(sdb, gstg[:, 0, 0:128], -1.0), reads=["gstg"], writes=["sdb"])
    P.dma(LQ, lambda e: e.dma_start(out=gstg[:, 0:4, :], in_=w_fourier.rearrange("(g m) n -> m g n", m=128)), reads=["gstg"], writes=["gstg"])
    P.dve(lambda e: e.tensor_copy(wf_b, gstg[:, 0:4, :]), reads=["gstg"], writes=["wf_b"])
    for g in range(4):
        for ri, mat, mk in ((0, cdb, "cdb"), (1, sdb, "sdb")):
            for half in range(2):
                ps, pk = next_ps()
                P.pe(lambda e, ps=ps, mat=mat, g=g, half=half: e.matmul(ps, mat, wf_b[:, g, half * 512:(half + 1) * 512], start=True, stop=True), reads=[mk, "wf_b"], writes=[pk])
                P.act(lambda e, ps=ps, g=g, ri=ri, half=half: e.copy(wfp[:, g * 2 + ri, half * 512:(half + 1) * 512], ps), reads=[pk], writes=["wfp"])
    P.dma(LQ, lambda e: e.dma_start(out=gstg, in_=w_rnn.rearrange("(k p) n -> p k n", p=128)), reads=["gstg"], writes=["gstg"])
    P.dve(lambda e: e.tensor_copy(wr_b, gstg), reads=["gstg"], writes=["wr_b"])
    P.dma(LQ, lambda e: e.dma_start(out=gstg, in_=w_out.rearrange("(k p) n -> p k n", p=128)), reads=["gstg"], writes=["gstg"])
    for k in range(8):
        P.dve(lambda e, k=k: e.tensor_tensor(wo_b[:, k, :], gstg[:, k, :], g1_bc, ALU.mult), reads=["gstg", "g1_bc"], writes=["wo_b"])
    P.dma(LQ, lambda e: e.dma_start(out=wrt_f, in_=w_router.rearrange("(k p) n -> p k n", p=128)), writes=["wrt_f"])
    P.dma(LQ, lambda e: e.dma_start(out=brt, in_=b_router), writes=["brt"])
    P.barrier()
    AR.release()
    rtt = [AR.alloc([128, 8, 512], BF16) for _ in range(2)]
    ygt = [AR.alloc([128, 8, 512], BF16) for _ in range(2)]
    gft = [AR.alloc([128, 16, 512], BF16)] * 2
    xg_ = [AR.alloc([128, 4, D]) for _ in range(2)]
    mT = AR.alloc([128, 8, 512], BF16)
    g1t = [AR.alloc([128, 512]) for _ in range(2)]
    g2t = [AR.alloc([128, 512]) for _ in range(2)]
    h2f = AR.alloc([128, D])
    h2b = AR.alloc([128, 4, D], BF16)
    h2T = AR.alloc([128, 8, 128])
    gjunk = AR.alloc([128, D], BF16); gssq = AR.alloc([128, 4]); grstd = AR.alloc([128, 4])
    rt_tv = rt_d.rearrange("(c j) t -> j c t", j=128)
    x1_v = x1_d.rearrange("(t s p) d -> t p s d", p=128, s=4)
    h2_v = h2_d.rearrange("(t s p) d -> t p s d", p=128, s=4)

    def g_load(t):
        bi = t % 2
        sl = slice(t * 512, (t + 1) * 512)
        P.dma(LQ, lambda e: e.dma_start(out=rtt[bi], in_=rt_tv[:, :, sl]), reads=["rt_d"], writes=["rtt%d" % bi])
        P.dma(LQ, lambda e: e.dma_start(out=ygt[bi], in_=yg_v[:, :, sl]), reads=["yg_d"], writes=["ygt%d" % bi])
        P.dma(LQ, lambda e: e.dma_start(out=xg_[bi], in_=x_v[t]), writes=["xg_%d" % bi])
    def gft_load(t):
        P.dma(LQ, lambda e: e.dma_start(out=gft[0], in_=gfr_v[:, :, t * 512:(t + 1) * 512]), reads=["gfr_d"], writes=["gft0"])
    g_load(0)
    gft_load(0)
    for t in range(NT):
        bi = t % 2
        if t + 1 < NT:
            g_load(t + 1)
        x1t = xg_[bi]
        kx1 = "xg_%d" % bi
        for n in range(8):
            psF, kF = next_ps()
            psR, kR = next_ps()

            def fF(e, psF=psF, n=n, bi=bi):
                for k in range(8):
                    ins = e.matmul(psF, wfp[:, k, n * 128:(n + 1) * 128], rtt[bi][:, k, :], start=(k == 0), stop=(k == 7))
                return ins

            def fR(e, psR=psR, n=n, bi=bi):
                for k in range(8):
                    ins = e.matmul(psR, wr_b[:, k, n * 128:(n + 1) * 128], ygt[bi][:, k, :], start=(k == 0), stop=(k == 7))
                return ins
            P.pe(fF, reads=["wfp", "rtt%d" % bi], writes=[kF])
            P.pe(fR, reads=["wr_b", "ygt%d" % bi], writes=[kR])
            a_ = g1t[n % 2]; b_ = g2t[n % 2]; ka = "g1t%d" % (n % 2); kb = "g2t%d" % (n % 2)
            P.dve(lambda e, psF=psF, a_=a_, n=n, bi=bi: e.tensor_tensor(a_, psF, gft[bi][:, n, :], ALU.mult), reads=[kF, "gft0"], writes=[ka])
            P.dve(lambda e, psR=psR, b_=b_, n=n, bi=bi: e.tensor_tensor(b_, psR, gft[bi][:, 8 + n, :], ALU.mult), reads=[kR, "gft0"], writes=[kb])
            P.dve(lambda e, a_=a_, b_=b_, n=n: e.tensor_tensor(mT[:, n, :], a_, b_, ALU.add), reads=[ka, kb], writes=["mT"])
        if t + 1 < NT:
            gft_load(t + 1)
        for s_ in range(4):
            for half in range(2):
                ps, pk = next_ps()

                def fO(e, ps=ps, s_=s_, half=half):
                    for k in range(8):
                        ins = e.matmul(ps, mT[:, k, s_ * 128:(s_ + 1) * 128], wo_b[:, k, half * 512:(half + 1) * 512], start=(k == 0), stop=(k == 7))
                    return ins
                P.pe(fO, reads=["mT", "wo_b"], writes=[pk])
                P.dve(lambda e, ps=ps, s_=s_, half=half, bi=bi: e.tensor_tensor(xg_[bi][:, s_, half * 512:(half + 1) * 512], ps, xg_[bi][:, s_, half * 512:(half + 1) * 512], ALU.add), reads=[pk, kx1], writes=[kx1])
        P.dma(SQ, lambda e, t=t, x1t=x1t: e.dma_start(out=x1_v[t], in_=x1t), reads=[kx1], writes=["x1_d"])
        rms_rows(x1t, 4, gssq, grstd, gjunk, kx1, "g_")
        for s_ in range(4):
            ti = t * 4 + s_
            P.dve(lambda e, s_=s_, x1t=x1t: e.scalar_tensor_tensor(h2f, x1t[:, s_, :], grstd[:, s_:s_ + 1], gm2_bc, ALU.mult, ALU.mult), reads=[kx1, "g_rstd", "gm2_bc"], writes=["h2f"])
            P.dve(lambda e: e.tensor_tensor(h2f, h2f, sh2_bc, ALU.add), reads=["h2f", "sh2_bc"], writes=["h2f"])
            P.act(lambda e, s_=s_: e.copy(h2b[:, s_, :], h2f), reads=["h2f"], writes=["h2b"])
            for q in range(2):
                ps, pk = next_ps()

                def fT(e, ps=ps, q=q):
                    for kk in range(4):
                        kc = q * 4 + kk
                        ins = e.transpose(ps[:, kk * 128:(kk + 1) * 128], h2f[:, kc * 128:(kc + 1) * 128], ident_f)
                    return ins
                P.pe(fT, reads=["h2f", "ident_f"], writes=[pk])
                if q == 0:
                    P.act(lambda e, ps=ps, q=q: e.copy(h2T[:, q * 4:(q + 1) * 4, :], ps.rearrange("p (a b) -> p a b", a=4)), reads=[pk], writes=["h2T"])
                else:
                    P.dve(lambda e, ps=ps, q=q: e.tensor_copy(h2T[:, q * 4:(q + 1) * 4, :], ps.rearrange("p (a b) -> p a b", a=4)), reads=[pk], writes=["h2T"])
            ps, pk = next_ps()

            def fL(e, ps=ps):
                for k in range(8):
                    e.matmul(ps[:, 0:NE], h2T[:, k, :], wrt_f[:, k, :], start=(k == 0), stop=False)
                return e.matmul(ps[:, 0:NE], ones_f[0:1, :], brt[0:1, :], start=False, stop=True)
            P.pe(fL, reads=["h2T", "wrt_f", "brt", "ones_f"], writes=[pk])
            P.act(lambda e, ps=ps, ti=ti: e.copy(Lg[:, ti, :], ps[:, 0:NE]), reads=[pk], writes=["Lg"])
        P.dma(SQ, lambda e, t=t: e.dma_start(out=h2_v[t], in_=h2b), reads=["h2b"], writes=["h2_d"])
    P.barrier()
    AR.release()
    if stop_after == "G":
        return finish()

    AR.mark()
    NTI = 64
    m8 = AR.alloc([128, NTI, 8]); i8 = AR.alloc([128, NTI, 8], U32); i8f = AR.alloc([128, NTI, 8])
    iota_e = AR.alloc([128, NE]); iota_i = AR.alloc([128, NE], I32)
    oh = [AR.alloc([128, NTI, NE]) for _ in range(4)]
    msk = AR.alloc([128, NTI, NE]); ex = AR.alloc([128, NTI, NE]); den = AR.alloc([128, NTI]); nmx = AR.alloc([128, NTI])
    cntp = AR.alloc([128, NE]); cntp_b = AR.alloc([128, NE], BF16)
    base = AR.alloc([128, NE]); tot = AR.alloc([128, NE]); pad = AR.alloc([128, NE]); ends = AR.alloc([128, NE]); starts = AR.alloc([128, NE])
    pref = AR.alloc([128, NTI, NE]); dst = AR.alloc([128, NTI, NE]); tmp3 = AR.alloc([128, NTI, NE])
    dk = AR.alloc([128, NTI, 4]); ones_e = AR.alloc([128, NTI])
    bthr = AR.alloc([128, NBLK]); bthr_i = AR.alloc([128, NBLK], I32); cmp = AR.alloc([128, NBLK, NE]); ebf = AR.alloc([128, NBLK])
    P.pool(lambda e: e.iota(iota_i, pattern=[[1, NE]], base=0, channel_multiplier=0), writes=["iota_i"])
    P.dve(lambda e: e.tensor_copy(iota_e, iota_i), reads=["iota_i"], writes=["iota_e"])
    P.pool(lambda e: e.iota(bthr_i, pattern=[[BLK, NBLK]], base=0, channel_multiplier=0), writes=["bthr_i"])
    P.dve(lambda e: e.tensor_copy(bthr, bthr_i), reads=["bthr_i"], writes=["bthr"])
    P.pool(lambda e: e.memset(ones_e, 1.0), writes=["ones_e"])
    for ti in range(NTI):
        P.dve(lambda e, ti=ti: e.max(m8[:, ti, :], Lg[:, ti, :]), reads=["Lg"], writes=["m8"])
        P.dve(lambda e, ti=ti: e.max_index(i8[:, ti, :], m8[:, ti, :], Lg[:, ti, :]), reads=["Lg", "m8"], writes=["i8"])
    P.dve(lambda e: e.tensor_copy(i8f, i8), reads=["i8"], writes=["i8f"])
    for k in range(4):
        P.dve(lambda e, k=k: e.tensor_tensor(oh[k], iota_e.unsqueeze(1).to_broadcast([128, NTI, NE]), i8f[:, :, k:k + 1].to_broadcast([128, NTI, NE]), ALU.is_equal), reads=["iota_e", "i8f"], writes=["oh%d" % k])
    P.dve(lambda e: e.tensor_tensor(msk, oh[0], oh[1], ALU.add), reads=["oh0", "oh1"], writes=["msk"])
    P.dve(lambda e: e.tensor_tensor(msk, msk, oh[2], ALU.add), reads=["msk", "oh2"], writes=["msk"])
    P.dve(lambda e: e.tensor_tensor(msk, msk, oh[3], ALU.add), reads=["msk", "oh3"], writes=["msk"])
    P.dve(lambda e: e.tensor_tensor(ex, Lg, m8[:, :, 0:1].to_broadcast([128, NTI, NE]), ALU.subtract), reads=["Lg", "m8"], writes=["ex"])
    P.act(lambda e: e.activation(out=ex, in_=ex, func=AF.Exp), reads=["ex"], writes=["ex"])
    P.dve(lambda e: e.tensor_tensor(ex, ex, msk, ALU.mult), reads=["ex", "msk"], writes=["ex"])
    P.dve(lambda e: e.tensor_reduce(den, ex, AX.X, ALU.add), reads=["ex"], writes=["den"])
    P.dve(lambda e: e.reciprocal(den, den), reads=["den"], writes=["den"])
    P.dve(lambda e: e.tensor_tensor(ex, ex, den.unsqueeze(2).to_broadcast([128, NTI, NE]), ALU.mult), reads=["ex", "den"], writes=["ex"])
    P.dve(lambda e: e.tensor_reduce(cntp, msk.rearrange("p t e -> p e t"), AX.X, ALU.add), reads=["msk"], writes=["cntp"])
    P.dve(lambda e: e.tensor_copy(cntp_b, cntp), reads=["cntp"], writes=["cntp_b"])
    psb_, kb_ = next_ps()
    P.pe(lambda e: e.matmul(psb_[:, 0:NE], ltri_b, cntp_b, start=True, stop=True), reads=["ltri_b", "cntp_b"], writes=[kb_])
    P.dve(lambda e: e.tensor_copy(base, psb_[:, 0:NE]), reads=[kb_], writes=["base"])
    pst_, kt_ = next_ps()
    P.pe(lambda e: e.matmul(pst_[:, 0:NE], ones_b[:, 0:128], cntp_b, start=True, stop=True), reads=["ones_b", "cntp_b"], writes=[kt_])
    P.dve(lambda e: e.tensor_copy(tot, pst_[:, 0:NE]), reads=[kt_], writes=["tot"])
    P.dve(lambda e: e.tensor_scalar(pad, tot, float(BLK - 1), 1.0 / BLK, ALU.add, ALU.mult), reads=["tot"], writes=["pad"])
    P.dve(lambda e: e.tensor_scalar_add(pad, pad, -0.4990234375), reads=["pad"], writes=["pad"])
    P.dve(lambda e: e.tensor_scalar_add(pad, pad, 8388608.0), reads=["pad"], writes=["pad"])
    P.dve(lambda e: e.tensor_scalar_add(pad, pad, -8388608.0), reads=["pad"], writes=["pad"])
    P.dve(lambda e: e.tensor_scalar_mul(pad, pad, float(BLK)), reads=["pad"], writes=["pad"])
    P.dve(lambda e: e.tensor_tensor_scan(ends, ones_e[:, 0:NE], pad, 0.0, ALU.mult, ALU.add), reads=["pad", "ones_e"], writes=["ends"])
    P.dve(lambda e: e.tensor_tensor(starts, ends, pad, ALU.subtract), reads=["ends", "pad"], writes=["starts"])
    P.dve(lambda e: e.tensor_tensor(base, base, starts, ALU.add), reads=["base", "starts"], writes=["base"])
    for ee in range(NE):
        P.dve(lambda e, ee=ee: e.tensor_tensor_scan(pref[:, :, ee], ones_e, msk[:, :, ee], 0.0, ALU.mult, ALU.add), reads=["msk", "ones_e"], writes=["pref"])
    P.dve(lambda e: e.tensor_tensor(pref, pref, msk, ALU.subtract), reads=["pref", "msk"], writes=["pref"])
    P.dve(lambda e: e.tensor_tensor(dst, pref, base.unsqueeze(1).to_broadcast([128, NTI, NE]), ALU.add), reads=["pref", "base"], writes=["dst"])
    for k in range(4):
        P.dve(lambda e, k=k: e.tensor_tensor(tmp3, oh[k], dst, ALU.mult), reads=["oh%d" % k, "dst"], writes=["tmp3"])
        P.dve(lambda e, k=k: e.tensor_reduce(dk[:, :, k], tmp3, AX.X, ALU.add), reads=["tmp3"], writes=["dk"])
        P.dve(lambda e, k=k: e.tensor_tensor(tmp3, oh[k], ex, ALU.mult), reads=["oh%d" % k, "ex", "tmp3"], writes=["tmp3"])
        P.dve(lambda e, k=k: e.tensor_reduce(wk[:, :, k], tmp3, AX.X, ALU.add), reads=["tmp3"], writes=["wk"])
    P.dve(lambda e: e.tensor_copy(dest_i, dk), reads=["dk"], writes=["dest_i"])
    P.dve(lambda e: e.tensor_tensor(cmp, ends.unsqueeze(1).to_broadcast([128, NBLK, NE]), bthr.unsqueeze(2).to_broadcast([128, NBLK, NE]), ALU.is_le), reads=["ends", "bthr"], writes=["cmp"])
    P.dve(lambda e: e.tensor_reduce(ebf, cmp, AX.X, ALU.add), reads=["cmp"], writes=["ebf"])
    P.dve(lambda e: e.tensor_scalar_min(ebf, ebf, float(NE - 1)), reads=["ebf"], writes=["ebf"])
    P.dve(lambda e: e.tensor_copy(eb_i, ebf), reads=["ebf"], writes=["eb_i"])
    pidx_i = AR.alloc([128, 8], I32); pidx = AR.alloc([128, 8]); widx_f = AR.alloc([128, NBLK, 8])
    P.pool(lambda e: e.iota(pidx_i, pattern=[[128, 8]], base=0, channel_multiplier=1), writes=["pidx_i"])
    P.dve(lambda e: e.tensor_copy(pidx, pidx_i), reads=["pidx_i"], writes=["pidx"])
    P.dve(lambda e: e.tensor_scalar_mul(ebf, ebf, 1024.0), reads=["ebf", "eb_i"], writes=["ebf"])
    P.dve(lambda e: e.tensor_tensor(widx_f, ebf.unsqueeze(2).to_broadcast([128, NBLK, 8]), pidx.unsqueeze(1).to_broadcast([128, NBLK, 8]), ALU.add), reads=["ebf", "pidx"], writes=["widx_f"])
    P.dve(lambda e: e.tensor_copy(widx, widx_f), reads=["widx_f"], writes=["widx"])
    AR.mark()
    hrow = [AR.alloc([128, D], BF16) for _ in range(2)]
    h2_r = h2_d.rearrange("(t p) d -> t p d", p=128)
    for ti in range(NTI):
        bi = ti % 2
        P.dma(LQ, lambda e, ti=ti, bi=bi: e.dma_start(out=hrow[bi], in_=h2_r[ti]), reads=["h2_d"], writes=["hrow%d" % bi])
        for k in range(4):
            P.dma("pool", lambda e, ti=ti, bi=bi, k=k: e.indirect_dma_start(out=xg_d, out_offset=bass.IndirectOffsetOnAxis(ap=dest_i[:, ti, k:k + 1], axis=0), in_=hrow[bi], in_offset=None), reads=["hrow%d" % bi, "dest_i"], writes=["xg_d"])
    P.barrier()
    AR.release()
    AR.release()
    if stop_after == "H":
        return finish()

    AR.release()
    AR.mark()
    NBIG = 3
    NSML = 4
    stgB = [AR.alloc([128, 2048]) for _ in range(NBIG)]
    stgS = [AR.alloc([128, 1024]) for _ in range(NSML)]
    wgu_b = [AR.alloc([128, 8, 2 * D], BF16) for _ in range(2)]
    wdn_b = AR.alloc([128, 8, D], BF16)
    bgb = [AR.alloc([1, 3 * D], BF16) for _ in range(2)]
    xrows = AR.alloc([128, 4, D], BF16)
    xT = [AR.alloc([128, 8, BLK], BF16) for _ in range(2)]
    aT = AR.alloc([128, 8, BLK], BF16)
    eg = [AR.alloc([128, BLK]) for _ in range(2)]
    es = [AR.alloc([128, BLK]) for _ in range(2)]
    eu = [AR.alloc([128, BLK]) for _ in range(2)]
    ysb = [AR.alloc([128, D], BF16) for _ in range(2)]
    bT = [AR.alloc([128, 16]) for _ in range(2)]
    bT1 = [AR.alloc([128, 8]) for _ in range(2)]
    xg_v = xg_d.rearrange("(b s p) d -> b p s d", p=128, s=4)
    ys_v = ys_d.rearrange("(b s p) d -> b s p d", p=128, s=4)
    wgu_rows = w_gu.rearrange("e k n -> (e k) n")
    wdn_rows = w_down.rearrange("e k n -> (e k) n")
    rB = [0]
    rS = [0]

    def item(kind, b, kc=0):
        pb = b % 2
        if kind in ("wgu", "bgu"):
            si = rB[0] % NBIG
            rB[0] += 1
            stg = stgB[si]; sk = "stgB%d" % si; ncol = 2048
        else:
            si = rS[0] % NSML
            rS[0] += 1
            stg = stgS[si]; sk = "stgS%d" % si; ncol = 1024
        if kind == "wgu":
            src, idx, dst, dkey, p0 = wgu_rows, widx[:, b, kc:kc + 1], wgu_b[pb][:, kc, :], "wgu%d_%d" % (pb, kc), 128
        elif kind == "wdn":
            src, idx, dst, dkey, p0 = wdn_rows, widx[:, b, kc:kc + 1], wdn_b[:, kc, :], "wdn_%d" % kc, 128
        elif kind == "bgu":
            src, idx, dst, dkey, p0 = b_gu, eb_i[:, b:b + 1], bgb[pb][0:1, 0:2048], "bgb%d" % pb, 1
        else:
            src, idx, dst, dkey, p0 = b_down, eb_i[:, b:b + 1], bgb[pb][0:1, 2048:3072], "bgb%d" % pb, 1

        def g_emit():
            P.dma("pool", lambda e: e.indirect_dma_start(out=stg[:, 0:ncol], out_offset=None, in_=src, in_offset=bass.IndirectOffsetOnAxis(ap=idx, axis=0)), reads=["widx", "eb_i"], writes=[sk])

        def c_emit():
            if kind == "bgu":
                ps, pk = next_ps()

                def ft(e):
                    for c_ in range(16):
                        ins = e.transpose(ps[:, c_:c_ + 1], stg[0:1, c_ * 128:(c_ + 1) * 128], ident_f[0:1, 0:1])
                    return ins
                P.pe(ft, reads=[sk, "ident_f"], writes=[pk])
                P.dve(lambda e: e.tensor_copy(bT[pb], ps[:, 0:16]), reads=[pk], writes=["bT%d" % pb])
                P.dve(lambda e: e.tensor_scalar_add(bT1[pb], bT[pb][:, 8:16], 1.0), reads=["bT%d" % pb], writes=["bT1%d" % pb])
                return
            P.act(lambda e: e.copy(dst, stg[0:p0, 0:ncol]), reads=[sk], writes=[dkey])
        return g_emit, c_emit

    for it_ in [item("bgu", 0), item("bdn", 0)] + [item("wgu", 0, kc) for kc in range(8)]:
        it_[0]()
        it_[1]()
    P.dma(LQ, lambda e: e.dma_start(out=xrows, in_=xg_v[0]), reads=["xg_d"], writes=["xrows"])
    for b in range(NBLK):
        pb = b % 2
        gath = [[] for _ in range(12)]
        cast = [[] for _ in range(12)]
        if b + 1 < NBLK:
            for kind in ("bgu", "bdn"):
                ge, ce = item(kind, b + 1)
                gath[0].append(ge); cast[1].append(ce)
        for kc in range(8):
            ge, ce = item("wdn", b, kc)
            gath[kc // 2].append(ge); cast[kc // 2 + 1].append(ce)
        if b + 1 < NBLK:
            for kc in range(8):
                ge, ce = item("wgu", b + 1, kc)
                gath[kc].append(ge); cast[kc + 2].append(ce)
        for kc in range(8):
            ps, pk = next_ps()
            psb = ps.bitcast(BF16)

            def fx(e, psb=psb, kc=kc):
                for s_ in range(4):
                    ins = e.transpose(psb[:, s_ * 128:(s_ + 1) * 128], xrows[:, s_, kc * 128:(kc + 1) * 128], ident_b)
                return ins
            P.pe(fx, reads=["xrows", "ident_b"], writes=[pk])
            if kc % 2 == 0:
                P.act(lambda e, psb=psb, kc=kc, pb=pb: e.copy(xT[pb][:, kc, :], psb[:, 0:BLK]), reads=[pk], writes=["xT%d" % pb])
            else:
                P.dve(lambda e, psb=psb, kc=kc, pb=pb: e.tensor_copy(xT[pb][:, kc, :], psb[:, 0:BLK]), reads=[pk], writes=["xT%d" % pb])
        if b + 1 < NBLK:
            P.dma(LQ, lambda e, b=b: e.dma_start(out=xrows, in_=xg_v[b + 1]), reads=["xg_d"], writes=["xrows"])
        wkeys = ["wgu%d_%d" % (pb, kc) for kc in range(8)]
        pend = None
        for cc in range(8):
            for fn in gath[cc]:
                fn()
            for fn in cast[cc]:
                fn()
            psg, kg = next_ps()
            psu, ku = next_ps()

            def fg(e, psg=psg, cc=cc, pb=pb):
                for k in range(8):
                    ins = e.matmul(psg, wgu_b[pb][:, k, cc * 128:(cc + 1) * 128], xT[pb][:, k, :], start=(k == 0), stop=(k == 7))
                return ins

            def fu(e, psu=psu, cc=cc, pb=pb):
                for k in range(8):
                    ins = e.matmul(psu, wgu_b[pb][:, k, D + cc * 128:D + (cc + 1) * 128], xT[pb][:, k, :], start=(k == 0), stop=(k == 7))
                return ins
            P.pe(fg, reads=wkeys + ["xT%d" % pb], writes=[kg])
            P.pe(fu, reads=wkeys + ["xT%d" % pb], writes=[ku])
            q = cc % 2
            P.dve(lambda e, psg=psg, q=q, cc=cc, pb=pb: e.tensor_scalar(eg[q], psg, bT[pb][:, cc:cc + 1], 7.0, ALU.add, ALU.min), reads=[kg, "bT%d" % pb], writes=["eg%d" % q])
            P.act(lambda e, q=q: e.activation(out=es[q], in_=eg[q], func=AF.Sigmoid, scale=1.702), reads=["eg%d" % q], writes=["es%d" % q])
            P.dve(lambda e, psu=psu, q=q, cc=cc, pb=pb: e.tensor_scalar(eu[q], psu, bT1[pb][:, cc:cc + 1], 8.0, ALU.add, ALU.min), reads=[ku, "bT1%d" % pb], writes=["eu%d" % q])

            def tail(cc=cc, q=q):
                P.dve(lambda e: e.tensor_tensor(eg[q], eg[q], es[q], ALU.mult), reads=["eg%d" % q, "es%d" % q], writes=["eg%d" % q])
                P.dve(lambda e: e.scalar_tensor_tensor(aT[:, cc, :], eu[q], -6.0, eg[q], ALU.max, ALU.mult), reads=["eu%d" % q, "eg%d" % q], writes=["aT"])
            if pend is not None:
                pend()
            pend = tail
        pend()
        dkeys = ["wdn_%d" % kc for kc in range(8)]
        for s_ in range(4):
            for fn in gath[8 + s_]:
                fn()
            for fn in cast[8 + s_]:
                fn()
            yb = ysb[s_ % 2]; ky = "ysb%d" % (s_ % 2)
            for half in range(2):
                ps, pk = next_ps()

                def fd(e, ps=ps, s_=s_, half=half, pb=pb):
                    for k in range(8):
                        e.matmul(ps, aT[:, k, s_ * 128:(s_ + 1) * 128], wdn_b[:, k, half * 512:(half + 1) * 512], start=(k == 0), stop=False)
                    c0 = 2 * D + half * 512
                    return e.matmul(ps, ones_b[0:1, 0:128], bgb[pb][0:1, c0:c0 + 512], start=False, stop=True)
                P.pe(fd, reads=["aT", "bgb%d" % pb, "ones_b"] + dkeys, writes=[pk])
                if half == 0:
                    P.act(lambda e, ps=ps, yb=yb: e.copy(yb[:, 0:512], ps), reads=[pk], writes=[ky])
                else:
                    P.dve(lambda e, ps=ps, yb=yb: e.tensor_copy(yb[:, 512:1024], ps), reads=[pk], writes=[ky])
            P.dma(LQ, lambda e, b=b, s_=s_, yb=yb: e.dma_start(out=ys_v[b, s_], in_=yb), reads=[ky], writes=["ys_d"])
    P.barrier()
    AR.release()
    if stop_after == "I":
        return finish()

    AR.mark()
    yk = [[AR.alloc([128, D], BF16) for _ in range(4)] for _ in range(2)]
    x1r = [AR.alloc([128, D]) for _ in range(2)]
    acc = AR.alloc([128, D]); outt = [AR.alloc([128, D]) for _ in range(2)]
    jjunk = AR.alloc([128, D]); jssq = AR.alloc([128, 64]); jrstd = AR.alloc([128, 64])
    x1_r = x1_d.rearrange("(t p) d -> t p d", p=128)
    y_r = y.rearrange("(t p) d -> t p d", p=128)

    def j_load(ti):
        bi = ti % 2
        for k in range(4):
            P.dma("pool", lambda e, k=k: e.indirect_dma_start(out=yk[bi][k], out_offset=None, in_=ys_d, in_offset=bass.IndirectOffsetOnAxis(ap=dest_i[:, ti, k:k + 1], axis=0)), reads=["ys_d", "dest_i"], writes=["yk%d_%d" % (bi, k)])
        P.dma(LQ, lambda e: e.dma_start(out=x1r[bi], in_=x1_r[ti]), reads=["x1_d"], writes=["x1r%d" % bi])
    j_load(0)
    for ti in range(NTI):
        bi = ti % 2
        if ti + 1 < NTI:
            j_load(ti + 1)
        P.dve(lambda e, bi=bi, ti=ti: e.tensor_scalar_mul(acc, yk[bi][0], wk[:, ti, 0:1]), reads=["yk%d_0" % bi, "wk"], writes=["acc"])
        for k in range(1, 4):
            P.dve(lambda e, bi=bi, ti=ti, k=k: e.scalar_tensor_tensor(acc, yk[bi][k], wk[:, ti, k:k + 1], acc, ALU.mult, ALU.add), reads=["yk%d_%d" % (bi, k), "wk", "acc"], writes=["acc"])
        P.dve(lambda e: e.tensor_tensor(acc, acc, g2_bc, ALU.mult), reads=["acc", "g2_bc"], writes=["acc"])
        P.dve(lambda e, bi=bi: e.tensor_tensor(acc, acc, x1r[bi], ALU.add), reads=["acc", "x1r%d" % bi], writes=["acc"])
        P.act(lambda e, ti=ti: e.activation(out=jjunk, in_=acc, func=AF.Square, accum_out=jssq[:, ti:ti + 1]), reads=["acc"], writes=["jjunk", "jssq"])
        P.dve(lambda e, ti=ti: e.tensor_scalar(jrstd[:, ti:ti + 1], jssq[:, ti:ti + 1], 1.0 / D, EPS, ALU.mult, ALU.add), reads=["jssq"], writes=["jrstd"])
        P.act(lambda e, ti=ti: e.activation(out=jrstd[:, ti:ti + 1], in_=jrstd[:, ti:ti + 1], func=AF.Ln), reads=["jrstd"], writes=["jrstd"])
        P.act(lambda e, ti=ti: e.activation(out=jrstd[:, ti:ti + 1], in_=jrstd[:, ti:ti + 1], func=AF.Exp, scale=-0.5), reads=["jrstd"], writes=["jrstd"])
        P.dve(lambda e, bi=bi, ti=ti: e.scalar_tensor_tensor(outt[bi], acc, jrstd[:, ti:ti + 1], nf_bc, ALU.mult, ALU.mult), reads=["acc", "jrstd", "nf_bc"], writes=["outt%d" % bi])
        P.dma(LQ, lambda e, bi=bi, ti=ti: e.dma_start(out=y_r[ti], in_=outt[bi]), reads=["outt%d" % bi], writes=["y"])
    AR.release()
    return finish()


_CACHE = {}


def kernel(**inputs):
    n = 8
    if "nc" not in _CACHE:
        _CACHE["nc"] = build_program()
    nc = _CACHE["nc"]
    tabs = dft_tables()
    f = np.float32

    def a(v):
        return np.ascontiguousarray(np.asarray(v, dtype=f))
    shared = dict(
        c_ctx=a(inputs["c_ctx"]).reshape(1, D), w_ada=a(inputs["w_ada"][0]), b_ada=a(inputs["b_ada"][0]).reshape(1, -1),
        norm1=a(inputs["norm1"][0]).reshape(1, D), w_in=a(inputs["w_in"][0]), conv_w=a(inputs["conv_w"][0]),
        conv_b=a(inputs["conv_b"][0]).reshape(1, D), gate_a_w=a(inputs["gate_a_w"][0]), gate_a_b=a(inputs["gate_a_b"][0]),
        gate_x_w=a(inputs["gate_x_w"][0]), gate_x_b=a(inputs["gate_x_b"][0]), lru_lambda=a(inputs["lru_lambda"][0]),
        w_fourier=a(inputs["w_fourier"][0]), w_rnn=a(inputs["w_rnn"][0]), w_out=a(inputs["w_out"][0]),
        norm2=a(inputs["norm2"][0]).reshape(1, D), w_router=a(inputs["w_router"][0]), b_router=a(inputs["b_router"][0]).reshape(1, NE),
        w_gu=a(inputs["w_gu"][0]), b_gu=a(inputs["b_gu"][0]), w_down=a(inputs["w_down"][0]), b_down=a(inputs["b_down"][0]),
        norm_f=a(inputs["norm_f"]).reshape(1, D), **tabs)
    xs = a(inputs["x"]); cs = a(inputs["c"]); cx = a(inputs["ctx"])
    in_maps = []
    for i in range(n):
        m = dict(shared)
        m["x"] = xs[i]; m["c"] = cs[i].reshape(1, D); m["ctx"] = cx[i]
        in_maps.append(m)
    ncore = int(os.environ.get("KDBG_NCORE", "8"))
    if ncore != 8:
        res = run_bass_kernel_spmd(nc, in_maps[:ncore], core_ids=list(range(ncore)))
        return np.stack([np.asarray(r["y"], dtype=f) for r in res.results], axis=0)
    res = run_bass_kernel_spmd(nc, in_maps, core_ids=list(range(n)))
    return np.stack([np.asarray(r["y"], dtype=f) for r in res.results], axis=0)
```
